# Optimizing a Trainium2 kernel written in Bass

```python
import math
import jax
import jax.numpy as jnp
from jax import lax
import numpy as np

D_MODEL = 2048
BATCH = 8
SEQ = 2048
DEPTH = 2

SSD_HEADS = 32
SSD_HEAD_DIM = 64
SSD_WIDTH = SSD_HEADS * SSD_HEAD_DIM
SSD_GROUPS = 4
SSD_STATE = 128
SSD_CONV = 4
SSD_CHUNK = 128
CONV_CH = SSD_WIDTH + 2 * SSD_GROUPS * SSD_STATE
MOBA_HEADS = 16
MOBA_HEAD_DIM = 128
MOBA_WIDTH = MOBA_HEADS * MOBA_HEAD_DIM
MOBA_BLOCK = 256
MOBA_TOPK = 3
MOBA_QCHUNK = 128
ROPE_THETA = 10000.0
GLA_HEADS = 4
GLA_KEY_DIM = D_MODEL // 2
GLA_VAL_DIM = D_MODEL
GLA_DK = GLA_KEY_DIM // GLA_HEADS
GLA_DV = GLA_VAL_DIM // GLA_HEADS
GLA_GATE_RANK = 16
GLA_GATE_NORM = 16.0
GLA_CHUNK = 64
N_EXPERTS = 32
TOP_K = 4
D_FF = D_MODEL
SWIGLU_LIMIT = 7.0
SWIGLU_ALPHA = 1.702
MOE_BLOCK = 256
DN_ALPHA = (2 * DEPTH) ** 0.25
DN_BETA = (8 * DEPTH) ** -0.25
EPS = 1e-5
N_EVEN = (DEPTH + 1) // 2
N_ODD = DEPTH // 2

HYB_SPLITS = [SSD_WIDTH,
              SSD_WIDTH + CONV_CH,
              SSD_WIDTH + CONV_CH + SSD_HEADS,
              SSD_WIDTH + CONV_CH + SSD_HEADS + MOBA_WIDTH,
              SSD_WIDTH + CONV_CH + SSD_HEADS + 2 * MOBA_WIDTH]
HYB_IN = HYB_SPLITS[-1] + MOBA_WIDTH
GLA_SPLITS = [GLA_KEY_DIM, 2 * GLA_KEY_DIM, 2 * GLA_KEY_DIM + GLA_VAL_DIM,
              2 * GLA_KEY_DIM + 2 * GLA_VAL_DIM]
GLA_IN = GLA_SPLITS[-1] + GLA_GATE_RANK

kernel_name = "hybrid_ssd_moba_gla_moe_deepnorm"


def layer_norm(x, g, b):
    xf = x.astype(jnp.float32)
    mu = jnp.mean(xf, axis=-1, keepdims=True)
    var = jnp.mean(jnp.square(xf - mu), axis=-1, keepdims=True)
    return ((xf - mu) * lax.rsqrt(var + EPS) * g + b).astype(x.dtype)


def rope(t, pos):
    half = t.shape[-1] // 2
    inv = jnp.exp(-math.log(ROPE_THETA) * jnp.arange(half, dtype=jnp.float32) / half)
    ang = pos.astype(jnp.float32)[:, None] * inv[None, :]
    cos = jnp.cos(ang)[None, :, None, :]
    sin = jnp.sin(ang)[None, :, None, :]
    tf = t.astype(jnp.float32)
    t1, t2 = tf[..., :half], tf[..., half:]
    return jnp.concatenate([t1 * cos - t2 * sin, t2 * cos + t1 * sin], axis=-1).astype(t.dtype)


def ssd_chunked(x, a, b, c):
    bsz, s, g, r, p = x.shape
    n = b.shape[-1]
    l = SSD_CHUNK
    nc = s // l
    x = x.reshape(bsz, nc, l, g, r, p)
    b = b.reshape(bsz, nc, l, g, n)
    c = c.reshape(bsz, nc, l, g, n)
    a = a.reshape(bsz, nc, l, g, r).transpose(0, 1, 3, 4, 2)
    a_cum = jnp.cumsum(a, axis=-1)
    causal = jnp.tril(jnp.ones((l, l), dtype=bool))
    seg = a_cum[..., :, None] - a_cum[..., None, :]
    decay = jnp.where(causal, jnp.exp(jnp.where(causal, seg, 0.0)), 0.0)
    cb = jnp.einsum('bclgn,bcsgn->bcgls', c, b)
    y_diag = jnp.einsum('bcgrls,bcsgrp->bclgrp', cb[:, :, :, None] * decay, x)
    decay_to_end = jnp.exp(a_cum[..., -1:] - a_cum).transpose(0, 1, 4, 2, 3)
    states = jnp.einsum('bclgn,bclgrp->bcgrpn', b, x * decay_to_end[..., None])
    chunk_decay = jnp.exp(a_cum[..., -1])

    def step(h, inp):
        s_c, d_c = inp
        return h * d_c[..., None, None] + s_c, h

    h0 = jnp.zeros((bsz, g, r, p, n), states.dtype)
    _, prev = lax.scan(step, h0, (jnp.moveaxis(states, 1, 0), jnp.moveaxis(chunk_decay, 1, 0)))
    prev = jnp.moveaxis(prev, 0, 1)
    y_off = jnp.einsum('bclgn,bcgrpn->bclgrp', c, prev) * jnp.exp(a_cum).transpose(0, 1, 4, 2, 3)[..., None]
    return (y_diag + y_off).reshape(bsz, s, g * r, p)


def moba_attention(q, k, v):
    bsz, s, h, dh = q.shape
    nb = -(-s // MOBA_BLOCK)
    sp = nb * MOBA_BLOCK
    nq = sp // MOBA_QCHUNK
    k_sel = min(MOBA_TOPK, nb - 1)
    scale = dh ** -0.5

    def to_bh(t):
        t = jnp.pad(t, ((0, 0), (0, sp - s), (0, 0), (0, 0)))
        return t.transpose(0, 2, 1, 3).reshape(bsz * h, sp, dh)

    qb, kb, vb = to_bh(q), to_bh(k), to_bh(v)
    kblk = kb.reshape(bsz * h, nb, MOBA_BLOCK, dh)
    vblk = vb.reshape(bsz * h, nb, MOBA_BLOCK, dh)
    qblock = jnp.arange(sp) // MOBA_BLOCK
    n_units = bsz * h * nq
    q_units = qb.reshape(n_units, MOBA_QCHUNK, dh)
    unit = jnp.arange(n_units)
    unit_bh = unit // nq
    unit_chunk = unit % nq

    if k_sel > 0:
        kmean = jnp.mean(kblk.astype(jnp.float32), axis=2)
        gate = jnp.einsum('utd,ujd->utj', qb.astype(jnp.float32), kmean)
        past = jnp.arange(nb)[None, :] < qblock[:, None]
        gate = jnp.where(past, gate, -jnp.inf)
        _, idx = lax.top_k(gate, k_sel)
        ok = jnp.broadcast_to(jnp.arange(k_sel)[None, :] < qblock[:, None], idx.shape)
        idx = jnp.where(ok, idx, 0).reshape(n_units, MOBA_QCHUNK, k_sel)
        ok = ok.reshape(n_units, MOBA_QCHUNK, k_sel)
        xs = (q_units, unit_bh, unit_chunk, idx, ok)
    else:
        xs = (q_units, unit_bh, unit_chunk)

    def attend(args):
        q_u, bh, ch = args[0], args[1], args[2]
        kb_u = kblk[bh]
        vb_u = vblk[bh]
        own = (ch * MOBA_QCHUNK) // MOBA_BLOCK
        k_own = kb_u[own]
        v_own = vb_u[own]
        qp = ch * MOBA_QCHUNK + jnp.arange(MOBA_QCHUNK)
        kp = own * MOBA_BLOCK + jnp.arange(MOBA_BLOCK)
        s_own = (q_u @ k_own.T).astype(jnp.float32) * scale
        s_own = jnp.where(kp[None, :] <= qp[:, None], s_own, -jnp.inf)
        if k_sel > 0:
            idx_u, ok_u = args[3], args[4]
            k_g = kb_u[idx_u]
            v_g = vb_u[idx_u]
            s_sel = jnp.einsum('qd,qjkd->qjk', q_u, k_g).astype(jnp.float32) * scale
            s_sel = jnp.where(ok_u[:, :, None], s_sel, -jnp.inf).reshape(MOBA_QCHUNK, k_sel * MOBA_BLOCK)
            p = jax.nn.softmax(jnp.concatenate([s_own, s_sel], axis=-1), axis=-1).astype(v_own.dtype)
            p_own = p[:, :MOBA_BLOCK]
            p_sel = p[:, MOBA_BLOCK:].reshape(MOBA_QCHUNK, k_sel, MOBA_BLOCK)
            return p_own @ v_own + jnp.einsum('qjk,qjkd->qd', p_sel, v_g)
        p = jax.nn.softmax(s_own, axis=-1).astype(v_own.dtype)
        return p @ v_own

    out = lax.map(attend, xs)
    out = out.reshape(bsz, h, sp, dh)[:, :, :s]
    return out.transpose(0, 2, 1, 3)


def hybrid_ssd_moba(h, w_in, conv_w, conv_b, dt_bias, a_log, d_skip, ssm_norm, w_out):
    bsz, s, _ = h.shape
    f32 = jnp.float32
    proj = h @ w_in
    z, xbc, dt, q, k, v = jnp.split(proj, HYB_SPLITS, axis=-1)
    xbc = lax.conv_general_dilated(xbc, conv_w[:, None, :].astype(xbc.dtype), window_strides=(1,),
                                   padding=[(SSD_CONV - 1, 0)],
                                   dimension_numbers=('NWC', 'WIO', 'NWC'),
                                   feature_group_count=CONV_CH)
    xbc = jax.nn.silu(xbc + conv_b)
    xs, bs, cs = jnp.split(xbc, [SSD_WIDTH, SSD_WIDTH + SSD_GROUPS * SSD_STATE], axis=-1)
    r = SSD_HEADS // SSD_GROUPS
    dt = jax.nn.softplus((dt + dt_bias).astype(f32))
    a = -jnp.exp(a_log.astype(f32))
    xh = xs.astype(f32).reshape(bsz, s, SSD_GROUPS, r, SSD_HEAD_DIM)
    dth = dt.reshape(bsz, s, SSD_GROUPS, r)
    y = ssd_chunked(xh * dth[..., None], a.reshape(SSD_GROUPS, r) * dth,
                    bs.astype(f32).reshape(bsz, s, SSD_GROUPS, SSD_STATE),
                    cs.astype(f32).reshape(bsz, s, SSD_GROUPS, SSD_STATE))
    y = y + d_skip.astype(f32)[:, None] * xh.reshape(bsz, s, SSD_HEADS, SSD_HEAD_DIM)
    y = y.reshape(bsz, s, SSD_WIDTH) * jax.nn.silu(z.astype(f32))
    yg = y.reshape(bsz, s, SSD_GROUPS, SSD_WIDTH // SSD_GROUPS)
    yg = yg * lax.rsqrt(jnp.mean(jnp.square(yg), axis=-1, keepdims=True) + EPS)
    y_ssd = (yg.reshape(bsz, s, SSD_WIDTH) * ssm_norm).astype(h.dtype)
    pos = jnp.arange(s)
    q = rope(q.reshape(bsz, s, MOBA_HEADS, MOBA_HEAD_DIM), pos)
    k = rope(k.reshape(bsz, s, MOBA_HEADS, MOBA_HEAD_DIM), pos)
    v = v.reshape(bsz, s, MOBA_HEADS, MOBA_HEAD_DIM)
    y_att = moba_attention(q, k, v).reshape(bsz, s, MOBA_WIDTH).astype(h.dtype)
    return jnp.concatenate([y_ssd, y_att], axis=-1) @ w_out


def gla_chunked(q, k, v, log_a):
    bsz, h, s, dk = q.shape
    dv = v.shape[-1]
    l = GLA_CHUNK
    nc = s // l

    def chunks(t):
        return jnp.moveaxis(t.reshape(bsz, h, nc, l, t.shape[-1]), 2, 0)

    g_cum = jnp.cumsum(chunks(log_a), axis=-2)
    g_last = g_cum[..., -1:, :]
    qc, kc, vc = chunks(q), chunks(k), chunks(v)
    q_in = qc * jnp.exp(g_cum)
    k_in = kc * jnp.exp(-g_cum)
    k_end = kc * jnp.exp(g_last - g_cum)
    causal = jnp.tril(jnp.ones((l, l), dtype=bool))

    def step(state, inp):
        qi, ki, ke, vi, gl = inp
        attn = jnp.where(causal, jnp.einsum('bhid,bhjd->bhij', qi, ki), 0.0)
        o = jnp.einsum('bhij,bhjv->bhiv', attn, vi) + jnp.einsum('bhid,bhdv->bhiv', qi, state)
        state = state * jnp.exp(gl)[:, :, 0, :, None] + jnp.einsum('bhjd,bhjv->bhdv', ke, vi)
        return state, o

    state0 = jnp.zeros((bsz, h, dk, dv), jnp.float32)
    _, o = lax.scan(step, state0, (q_in, k_in, k_end, vc, g_last))
    return jnp.moveaxis(o, 0, 2).reshape(bsz, h, s, dv)


def gla_mixer(h, w_in, w_gate2, b_gate, head_norm, w_out):
    bsz, s, _ = h.shape
    f32 = jnp.float32
    proj = h @ w_in
    q, k, v, g, gl = jnp.split(proj, GLA_SPLITS, axis=-1)
    log_a = jax.nn.log_sigmoid((gl @ w_gate2 + b_gate).astype(f32)) / GLA_GATE_NORM

    def heads(t, d):
        return t.astype(f32).reshape(bsz, s, GLA_HEADS, d).transpose(0, 2, 1, 3)

    o = gla_chunked(heads(q, GLA_DK) * GLA_DK ** -0.5, heads(k, GLA_DK),
                    heads(v, GLA_DV), heads(log_a, GLA_DK))
    o = o * lax.rsqrt(jnp.mean(jnp.square(o), axis=-1, keepdims=True) + EPS) * head_norm
    o = o.transpose(0, 2, 1, 3).reshape(bsz, s, GLA_VAL_DIM) * jax.nn.silu(g.astype(f32))
    return o.astype(h.dtype) @ w_out


def moe_ffn(h, w_router, b_router, w1, b1, w2, b2):
    bsz, s, d = h.shape
    t = bsz * s
    xf = h.reshape(t, d)
    logits = (xf @ w_router).astype(jnp.float32) + b_router
    top_val, top_e = lax.top_k(logits, TOP_K)
    gate = jax.nn.softmax(top_val, axis=-1)
    e_flat = top_e.reshape(-1)
    tok_flat = jnp.repeat(jnp.arange(t, dtype=jnp.int32), TOP_K)
    g_flat = gate.reshape(-1)
    order = jnp.argsort(e_flat)
    e_s, tok_s, g_s = e_flat[order], tok_flat[order], g_flat[order]
    counts = jnp.bincount(e_flat, length=N_EXPERTS)
    padded = (counts + MOE_BLOCK - 1) // MOE_BLOCK * MOE_BLOCK
    start = jnp.cumsum(counts) - counts
    pend = jnp.cumsum(padded)
    pstart = pend - padded
    slot = pstart[e_s] + jnp.arange(t * TOP_K) - start[e_s]
    n_blocks = -(-(t * TOP_K) // MOE_BLOCK) + N_EXPERTS
    rows = n_blocks * MOE_BLOCK
    tok_pad = jnp.full((rows,), t, jnp.int32).at[slot].set(tok_s)
    g_pad = jnp.zeros((rows,), jnp.float32).at[slot].set(g_s)
    block_e = jnp.clip(jnp.searchsorted(pend, jnp.arange(n_blocks) * MOE_BLOCK, side='right'),
                       0, N_EXPERTS - 1)
    x_pad = jnp.concatenate([xf, jnp.zeros((1, d), xf.dtype)], axis=0)

    def expert_block(args):
        tok_b, e = args
        hb = x_pad[tok_b] @ w1[e] + b1[e]
        glu = jnp.minimum(hb[:, ::2], SWIGLU_LIMIT)
        lin = jnp.clip(hb[:, 1::2], -SWIGLU_LIMIT, SWIGLU_LIMIT)
        act = glu * jax.nn.sigmoid(SWIGLU_ALPHA * glu) * (lin + 1.0)
        return act @ w2[e] + b2[e]

    y = lax.map(expert_block, (tok_pad.reshape(n_blocks, MOE_BLOCK), block_e))
    y = y.reshape(rows, d) * g_pad[:, None].astype(y.dtype)
    out = jnp.zeros((t + 1, d), y.dtype).at[tok_pad].add(y)[:t]
    return out.reshape(bsz, s, d).astype(h.dtype)


def setup_inputs(seed: int = 0) -> dict:
    key = jax.random.key(seed)
    ks = jax.random.split(key, 24)
    f32 = jnp.float32

    def nrm(k, shape, scale):
        return jax.random.normal(k, shape, f32) * scale

    x = nrm(ks[0], (BATCH, SEQ, D_MODEL), 1.0)
    hyb_w_in = nrm(ks[1], (N_EVEN, D_MODEL, HYB_IN), D_MODEL ** -0.5)
    hyb_conv_w = nrm(ks[2], (N_EVEN, SSD_CONV, CONV_CH), SSD_CONV ** -0.5)
    hyb_conv_b = nrm(ks[3], (N_EVEN, CONV_CH), 0.01)
    dt0 = jnp.exp(jax.random.uniform(ks[4], (N_EVEN, SSD_HEADS), f32, math.log(1e-3), math.log(1e-1)))
    hyb_dt_bias = dt0 + jnp.log(-jnp.expm1(-dt0))
    hyb_a_log = jnp.log(jax.random.uniform(ks[5], (N_EVEN, SSD_HEADS), f32, 1.0, 16.0))
    hyb_d = 1.0 + nrm(ks[6], (N_EVEN, SSD_HEADS), 0.01)
    hyb_norm = 1.0 + nrm(ks[7], (N_EVEN, SSD_WIDTH), 0.01)
    hyb_w_out = nrm(ks[8], (N_EVEN, SSD_WIDTH + MOBA_WIDTH, D_MODEL),
                    (SSD_WIDTH + MOBA_WIDTH) ** -0.5 * DN_BETA)
    gla_w_in = nrm(ks[9], (N_ODD, D_MODEL, GLA_IN), D_MODEL ** -0.5)
    gla_w_gate2 = nrm(ks[10], (N_ODD, GLA_GATE_RANK, GLA_KEY_DIM), GLA_GATE_RANK ** -0.5)
    gla_b_gate = nrm(ks[11], (N_ODD, GLA_KEY_DIM), 0.01)
    gla_norm = 1.0 + nrm(ks[12], (N_ODD, GLA_DV), 0.01)
    gla_w_out = nrm(ks[13], (N_ODD, GLA_VAL_DIM, D_MODEL), GLA_VAL_DIM ** -0.5 * DN_BETA)
    ln1_g = 1.0 + nrm(ks[14], (DEPTH, D_MODEL), 0.01)
    ln1_b = nrm(ks[15], (DEPTH, D_MODEL), 0.01)
    ln2_g = 1.0 + nrm(ks[16], (DEPTH, D_MODEL), 0.01)
    ln2_b = nrm(ks[17], (DEPTH, D_MODEL), 0.01)
    moe_w_router = nrm(ks[18], (DEPTH, D_MODEL, N_EXPERTS), D_MODEL ** -0.5)
    moe_b_router = nrm(ks[19], (DEPTH, N_EXPERTS), 0.01)
    moe_w1 = nrm(ks[20], (DEPTH, N_EXPERTS, D_MODEL, 2 * D_FF), D_MODEL ** -0.5)
    moe_b1 = nrm(ks[21], (DEPTH, N_EXPERTS, 2 * D_FF), 0.01)
    moe_w2 = nrm(ks[22], (DEPTH, N_EXPERTS, D_FF, D_MODEL), D_FF ** -0.5 * DN_BETA)
    moe_b2 = nrm(ks[23], (DEPTH, N_EXPERTS, D_MODEL), 0.01)
    return {"x": x, "hyb_w_in": hyb_w_in, "hyb_conv_w": hyb_conv_w, "hyb_conv_b": hyb_conv_b,
            "hyb_dt_bias": hyb_dt_bias, "hyb_a_log": hyb_a_log, "hyb_d": hyb_d,
            "hyb_norm": hyb_norm, "hyb_w_out": hyb_w_out, "gla_w_in": gla_w_in,
            "gla_w_gate2": gla_w_gate2, "gla_b_gate": gla_b_gate, "gla_norm": gla_norm,
            "gla_w_out": gla_w_out, "ln1_g": ln1_g, "ln1_b": ln1_b, "ln2_g": ln2_g,
            "ln2_b": ln2_b, "moe_w_router": moe_w_router, "moe_b_router": moe_b_router,
            "moe_w1": moe_w1, "moe_b1": moe_b1, "moe_w2": moe_w2, "moe_b2": moe_b2}


def reference(x, hyb_w_in, hyb_conv_w, hyb_conv_b, hyb_dt_bias, hyb_a_log, hyb_d, hyb_norm,
              hyb_w_out, gla_w_in, gla_w_gate2, gla_b_gate, gla_norm, gla_w_out,
              ln1_g, ln1_b, ln2_g, ln2_b, moe_w_router, moe_b_router, moe_w1, moe_b1,
              moe_w2, moe_b2):
    h = x
    for layer in range(DEPTH):
        j = layer // 2
        if layer % 2 == 0:
            mix = hybrid_ssd_moba(h, hyb_w_in[j], hyb_conv_w[j], hyb_conv_b[j], hyb_dt_bias[j],
                                  hyb_a_log[j], hyb_d[j], hyb_norm[j], hyb_w_out[j])
        else:
            mix = gla_mixer(h, gla_w_in[j], gla_w_gate2[j], gla_b_gate[j], gla_norm[j], gla_w_out[j])
        h = layer_norm(DN_ALPHA * h + mix, ln1_g[layer], ln1_b[layer])
        ffn = moe_ffn(h, moe_w_router[layer], moe_b_router[layer], moe_w1[layer], moe_b1[layer],
                      moe_w2[layer], moe_b2[layer])
        h = layer_norm(DN_ALPHA * h + ffn, ln2_g[layer], ln2_b[layer])
    return h
```

```python
import contextlib
import math
import numpy as np
import ml_dtypes
import concourse.bass as bass
import concourse.mybir as mybir
from concourse.bass_utils import run_bass_kernel_spmd

F32 = mybir.dt.float32
BF16 = mybir.dt.bfloat16
AF = mybir.ActivationFunctionType
ALU = mybir.AluOpType
AX = mybir.AxisListType

NCORES = 8
T = 2048
D = 2048
NT = T // 128
KC = D // 128
NEG = -1.0e30
DN_ALPHA = 4 ** 0.25
EPS = 1e-5
HYB_IN = 11296
GLA_IN = 6160


class Prog:
    RING = 8

    def __init__(self, nc):
        self.nc = nc
        self.E = {"pe": nc.tensor, "act": nc.scalar, "dve": nc.vector,
                  "pool": nc.gpsimd, "sp": nc.sync}
        self.sem = {}
        self.cnt = {}
        for k in self.E:
            self.sem[k] = nc.alloc_semaphore("s_" + k)
            self.cnt[k] = 0
        self.ring = {}
        self.ring_cnt = {}
        self.ring_next = {}
        for q in ("sp", "pool", "act"):
            self.ring[q] = [nc.alloc_semaphore(f"d_{q}{i}") for i in range(self.RING)]
            self.ring_cnt[q] = [0] * self.RING
            self.ring_next[q] = 0
        self.cc_sem = nc.alloc_semaphore("cc_sem")
        self.cc_cnt = 0
        self.seen = {k: {} for k in self.E}
        self.tok = {}
        self.nwaits = 0
        self.nops = 0

    def _semh(self, key):
        if key[0] == "e":
            return self.sem[key[1]]
        if key[0] == "c":
            return self.cc_sem
        return self.ring[key[1]][key[2]]

    def _wait(self, eng, ev):
        key, val = ev
        if key == ("e", "pe") and eng == "pe":
            return
        if self.seen[eng].get(key, 0) >= val:
            return
        self.E[eng].wait_ge(self._semh(key), val)
        self.seen[eng][key] = val
        self.nwaits += 1

    def _deps(self, reads, writes):
        deps = {}

        def add(k, v):
            if deps.get(k, 0) < v:
                deps[k] = v
        for t in reads:
            st = self.tok.get(t)
            if st and st["w"]:
                add(*st["w"])
        for t in writes:
            st = self.tok.get(t)
            if st:
                if st["w"]:
                    add(*st["w"])
                for k, v in st["r"].items():
                    add(k, v)
        return deps

    def _commit(self, ev, reads, writes):
        k, v = ev
        for t in reads:
            st = self.tok.setdefault(t, {"w": None, "r": {}})
            if st["r"].get(k, 0) < v:
                st["r"][k] = v
        for t in writes:
            self.tok[t] = {"w": ev, "r": {}}

    def op(self, eng, fn, reads=(), writes=()):
        for k, v in self._deps(reads, writes).items():
            self._wait(eng, (k, v))
        ins = fn(self.E[eng])
        self.cnt[eng] += 1
        ins.then_inc(self.sem[eng], 1)
        self._commit((("e", eng), self.cnt[eng]), reads, writes)
        self.nops += 1
        return ins

    def mm(self, fns, reads=(), writes=()):
        for k, v in self._deps(reads, writes).items():
            self._wait("pe", (k, v))
        ins = None
        for fn in fns:
            ins = fn(self.E["pe"])
        self.cnt["pe"] += 1
        ins.then_inc(self.sem["pe"], 1)
        self._commit((("e", "pe"), self.cnt["pe"]), reads, writes)
        self.nops += len(fns)

    def dma(self, out, in_, reads=(), writes=(), q="sp", **kw):
        i = self.ring_next[q]
        self.ring_next[q] = (i + 1) % self.RING
        key = ("d", q, i)
        if self.ring_cnt[q][i] > 0:
            self._wait(q, (key, 16 * self.ring_cnt[q][i]))
        for k, v in self._deps(reads, writes).items():
            self._wait(q, (k, v))
        ins = self.E[q].dma_start(out=out, in_=in_, **kw)
        self.ring_cnt[q][i] += 1
        ins.then_inc(self.ring[q][i], 16)
        self._commit((key, 16 * self.ring_cnt[q][i]), reads, writes)
        self.nops += 1
        return ins

    def allreduce(self, in_ap, out_ap, reads=(), writes=()):
        for k, v in self._deps(reads, writes).items():
            self._wait("pool", (k, v))
        ins = self.nc.gpsimd.collective_compute(
            "AllReduce", ALU.add, replica_groups=[list(range(NCORES))],
            ins=[in_ap.opt()], outs=[out_ap.opt()])
        self.cc_cnt += 1
        ins.then_inc(self.cc_sem)
        self._commit((("c",), self.cc_cnt), reads, writes)

    def barrier(self):
        evs = []
        for k in self.E:
            if self.cnt[k]:
                evs.append((("e", k), self.cnt[k]))
        for q in self.ring:
            for i in range(self.RING):
                if self.ring_cnt[q][i]:
                    evs.append((("d", q, i), 16 * self.ring_cnt[q][i]))
        if self.cc_cnt:
            evs.append((("c",), self.cc_cnt))
        for eng in self.E:
            for ev in evs:
                if ev[0] == ("e", eng):
                    continue
                self._wait(eng, ev)
        self.tok = {}


_UID = [0]


class Scope:
    def _name(self, name):
        _UID[0] += 1
        return f"{name}_{_UID[0]}"

    def __init__(self, p):
        self.p = p
        self.nc = p.nc
        self.es = contextlib.ExitStack()
        self.n = 0

    def sb(self, name, shape, dt=F32):
        self.n += 1
        h = self.es.enter_context(self.nc.sbuf_tensor(self._name(name), list(shape), dt))
        return h.ap()

    def ps(self, name, shape, dt=F32):
        self.n += 1
        h = self.es.enter_context(self.nc.psum_tensor(self._name(name), list(shape), dt))
        return h.ap()

    def close(self):
        self.p.barrier()
        self.es.close()


def make_consts():
    c = {}
    c["ident"] = np.eye(128, dtype=np.float32)
    c["identb"] = np.eye(128).astype(ml_dtypes.bfloat16)
    r = np.arange(128)
    c["triu"] = (r[:, None] <= r[None, :]).astype(np.float32)
    c["maskneg"] = np.where(r[None, :] < r[:, None], NEG, 0.0).astype(np.float32)
    sel = np.zeros((32, 32, 128), np.float32)
    for h in range(32):
        sel[h, h, :] = 1.0
    c["sel"] = sel.reshape(32, 32 * 128)
    rt = np.zeros((128, 128), np.float32)
    for m in range(64):
        rt[m + 64, m] = -1.0
    for m in range(64, 128):
        rt[m - 64, m] = 1.0
    c["rt"] = rt
    inv = np.exp(-math.log(10000.0) * np.arange(64, dtype=np.float32) / 64).astype(np.float32)
    ang = np.arange(T, dtype=np.float32)[None, :] * inv[:, None]
    c["cosT"] = np.concatenate([np.cos(ang), np.cos(ang)], 0).astype(np.float32)
    c["sinT"] = np.concatenate([np.sin(ang), np.sin(ang)], 0).astype(np.float32)
    c["causal"] = np.where(r[None, :] <= r[:, None], 0.0, NEG).astype(np.float32)
    same = (r[:, None] // 64) == (r[None, :] // 64)
    c["btri"] = (same & (r[:, None] <= r[None, :])).astype(np.float32)
    c["bgt"] = (same & (r[:, None] > r[None, :])).astype(np.float32)
    c["ones"] = np.ones((1, 128), np.float32)
    ls = np.zeros((128, 128), np.float32)
    ls[127, :] = 1.0
    c["lastsel"] = ls
    c["trius"] = (r[:, None] < r[None, :]).astype(np.float32)
    c["ones128"] = np.ones((128, 128), np.float32)
    c["iota"] = np.tile(np.arange(256, dtype=np.float32)[None, :], (128, 1))
    return c


CONST_SHAPES = {"ident": ([128, 128], F32), "identb": ([128, 128], BF16), "triu": ([128, 128], F32),
                "maskneg": ([128, 128], F32), "sel": ([32, 4096], F32), "rt": ([128, 128], F32),
                "cosT": ([128, T], F32), "sinT": ([128, T], F32), "causal": ([128, 128], F32),
                "btri": ([128, 128], F32), "bgt": ([128, 128], F32), "ones": ([1, 128], F32), "lastsel": ([128, 128], F32), "trius": ([128, 128], F32),
                "ones128": ([128, 128], F32), "iota": ([128, 256], F32)}


class Ctx:
    pass


def load_const(cx, S, name, tokname=None):
    shape, dt = CONST_SHAPES[name]
    t = S.sb("c_" + name, shape, dt)
    cx.p.dma(t, cx.dram[name], writes=[tokname or ("c", name, id(S))])
    return t


def bcast_row(cx, S, row_ap, n, name):
    t = S.sb(name, [128, n], F32)
    cx.p.dma(t, row_ap.partition_broadcast(128), writes=[("bc", name, id(S))])
    return t, ("bc", name, id(S))


def toks(base, rng):
    return [(base, i) for i in rng]


def transpose_in(cx, S, src_tm, dstT, dst_tok, ident, ident_tok, nt=NT, t0=0):
    p = cx.p
    ld = [S.sb("tin_ld", [128, D], F32) for _ in range(2)]
    ps = [S.ps("tin_ps", [128, 512], F32) for _ in range(2)]
    src = src_tm.rearrange("(t p) d -> t p d", p=128)
    k = 0
    for t in range(nt):
        b = ld[t % 2]
        p.dma(b, src[t0 + t], writes=[("tin_ld", t % 2)])
        for g in range(4):
            pp = ps[k % 2]
            ptok = ("tin_ps", k % 2)
            p.mm([(lambda e, pp=pp, b=b, g=g, j=j: e.transpose(out=pp[:, j * 128:(j + 1) * 128],
                                                              in_=b[:, (g * 4 + j) * 128:(g * 4 + j + 1) * 128],
                                                              identity=ident)) for j in range(4)],
                 reads=[("tin_ld", t % 2), ident_tok], writes=[ptok])
            out = dstT[:, g * 4:(g + 1) * 4, t * 128:(t + 1) * 128]
            src_ps = pp.rearrange("p (a b) -> p a b", a=4)
            if k % 2 == 0:
                p.op("dve", lambda e, out=out, s=src_ps: e.tensor_copy(out=out, in_=s), reads=[ptok], writes=[(dst_tok, t, g)])
            else:
                p.op("act", lambda e, out=out, s=src_ps: e.activation(out=out, in_=s, func=AF.Copy), reads=[ptok], writes=[(dst_tok, t, g)])
            k += 1


def xtoks(xtok, tiles):
    return [(xtok, t, g) for t in tiles for g in range(4)]


def load_cols(cx, S, vec_ap2d, r, name, ident, ident_tok, ps, pstok, ncol=128):
    p = cx.p
    st = S.sb(name + "_st", [r, ncol], F32)
    out = S.sb(name, [128, r], F32)
    p.dma(st, vec_ap2d, writes=[(name, "st")])
    p.mm([lambda e: e.transpose(out=ps[0:ncol, 0:r], in_=st, identity=ident[0:r, 0:r])],
         reads=[(name, "st"), ident_tok], writes=[pstok])
    p.op("dve", lambda e: e.tensor_copy(out=out[0:ncol, :], in_=ps[0:ncol, 0:r]), reads=[pstok], writes=[(name,)])
    return out, (name,)


class WStream:
    def __init__(self, cx, S, kc, pw, name):
        self.cx, self.kc, self.pw, self.name = cx, kc, pw, name
        self.st = [S.sb(name + "_st", [128, kc, pw], F32) for _ in range(2)]
        self.bf = [S.sb(name + "_bf", [128, kc, pw], BF16) for _ in range(2)]
        self.i = 0

    def fetch(self, w2d, c0, w):
        p = self.cx.p
        b = self.i % 2
        self.i += 1
        st, bf = self.st[b], self.bf[b]
        stok, btok = (self.name, "st", b), (self.name, "bf", b)
        src = w2d.rearrange("(k p) n -> p k n", p=128)[:, :, c0:c0 + w]
        p.dma(st[:, :, 0:w], src, writes=[stok])
        p.op("pool", lambda e: e.tensor_copy(out=bf[:, :, 0:w], in_=st[:, :, 0:w]), reads=[stok], writes=[btok])
        return bf, btok


def gemm_panels(cx, ws, w2d, panels, xT, xtok, mode, ps_list, epilogue, kc=KC, nt=NT):
    p = cx.p
    nxt = ws.fetch(w2d, panels[0][0], panels[0][1])
    k = 0
    for pi, (c0, w, tag) in enumerate(panels):
        bf, btok = nxt
        if pi + 1 < len(panels):
            nxt = ws.fetch(w2d, panels[pi + 1][0], panels[pi + 1][1])
        if mode == "tm":
            for t in range(nt):
                ps, ptok = ps_list[k % len(ps_list)]
                k += 1
                p.mm([(lambda e, c=c, ps=ps, t=t: e.matmul(ps[:, 0:w], lhsT=xT[:, c, t * 128:(t + 1) * 128], rhs=bf[:, c, 0:w],
                                                         start=(c == 0), stop=(c == kc - 1))) for c in range(kc)],
                     reads=[btok] + xtoks(xtok, [t]), writes=[ptok])
                epilogue(tag, c0, w, t, ps, ptok)
        else:
            nj = max(1, w // 128)
            m = 128
            for j in range(nj):
                for tb in range(nt // 4):
                    ps, ptok = ps_list[k % len(ps_list)]
                    k += 1
                    p.mm([(lambda e, c=c, ps=ps, tb=tb, j=j: e.matmul(ps[0:m, 0:512], lhsT=bf[:, c, j * 128:j * 128 + m],
                                                                     rhs=xT[:, c, tb * 512:(tb + 1) * 512],
                                                                     start=(c == 0), stop=(c == kc - 1))) for c in range(kc)],
                         reads=[btok] + xtoks(xtok, range(tb * 4, tb * 4 + 4)), writes=[ptok])
                    epilogue(tag, c0 + j * 128, m, tb, ps, ptok)


def stage_l0_proj(cx, h_tm):
    p, nc, dr = cx.p, cx.nc, cx.dram
    S = Scope(p)
    ident = load_const(cx, S, "ident", "ident")
    rt = load_const(cx, S, "rt", "rt")
    cosT = load_const(cx, S, "cosT", "cosT")
    sinT = load_const(cx, S, "sinT", "sinT")
    xT = S.sb("xT", [128, KC, T], BF16)
    S2 = Scope(p)
    transpose_in(cx, S2, h_tm, xT, "xT", ident, "ident")
    S2.close()
    w_in = dr["hyb_w_in"]
    ws = WStream(cx, S, KC, 256, "win")
    gps = [(S.ps("gps", [128, 512], F32), ("gps", i)) for i in range(3)]
    tps = [(S.ps("tps", [128, 512], F32), ("tps", i)) for i in range(2)]
    rps, rtok = S.ps("rps", [128, 512], F32), "rps"
    mps, mtok = S.ps("mps", [128, 512], F32), "mps"
    cwst = S.sb("cwst", [4, 3072], F32)
    p.dma(cwst, dr["hyb_conv_w"], writes=["cwst"])
    cw = S.sb("cw", [128, 24, 4], F32)
    p.mm([(lambda e, c=c: e.transpose(out=mps[:, c * 4:(c + 1) * 4], in_=cwst[0:4, c * 128:(c + 1) * 128], identity=ident[0:4, 0:4]))
          for c in range(24)], reads=["cwst", "ident"], writes=[mtok])
    p.op("dve", lambda e: e.tensor_copy(out=cw.rearrange("p a b -> p (a b)"), in_=mps[:, 0:96]), reads=[mtok], writes=["cw"])
    cb, cbtok = load_cols(cx, S, dr["hyb_conv_b"].rearrange("o (c p) -> (o c) p", p=128), 24, "cb", ident, "ident", mps, mtok)
    dtb, dtbtok = bcast_row(cx, S, dr["hyb_dt_bias"], 32, "dtb")
    abc, abctok = bcast_row(cx, S, dr["hyb_a_log"], 32, "abc")
    p.op("act", lambda e: e.activation(out=abc, in_=abc, func=AF.Exp), reads=[abctok], writes=[abctok])
    p.op("dve", lambda e: e.tensor_scalar(out=abc, in0=abc, scalar1=-1.0, scalar2=None, op0=ALU.mult), reads=[abctok], writes=[abctok])
    cin = S.sb("cin", [128, 3 + T], F32)
    p.op("pool", lambda e: e.memset(cin[:, 0:3], 0.0), writes=["cin_pad"])
    wa = S.sb("wa", [128, T], F32)
    wb = S.sb("wb", [128, T], F32)
    wc = S.sb("wc", [128, T], F32)
    obf = [S.sb("obf", [128, T], BF16) for _ in range(2)]
    tmst = S.sb("tmst", [128, NT, 128], F32)
    tmstb = S.sb("tmstb", [128, NT, 128], BF16)
    zst = [S.sb("zst", [128, 256], F32) for _ in range(2)]
    vst = [S.sb("vst", [128, 256], BF16) for _ in range(2)]
    dts = S.sb("dts", [128, NT, 32], F32)
    adts = S.sb("adts", [128, NT, 32], F32)
    sp1 = S.sb("sp1", [128, 32], F32)
    sp2 = S.sb("sp2", [128, 32], F32)
    kmean = S.sb("kmean", [128, 16, 8], F32)
    gst = S.sb("gst", [128, NT, 8], F32)
    cnt = {"z": 0, "v": 0, "o": 0, "t": 0}

    def transposes_to(dst3, dtok, src, stok):
        for g in range(4):
            ps, ptok = tps[cnt["t"] % 2]
            cnt["t"] += 1
            p.mm([(lambda e, j=j, ps=ps, g=g: e.transpose(out=ps[:, j * 128:(j + 1) * 128], in_=src[:, (g * 4 + j) * 128:(g * 4 + j + 1) * 128],
                                                        identity=ident)) for j in range(4)], reads=[stok, "ident"], writes=[ptok])
            p.op("act", lambda e, ps=ps, g=g: e.activation(out=dst3[:, g * 4:(g + 1) * 4, :], in_=ps.rearrange("p (a b) -> p a b", a=4), func=AF.Copy),
                 reads=[ptok], writes=[dtok])

    def ep(tag, c0, w, idx, ps, ptok):
        if tag == "z":
            b = cnt["z"] % 2
            cnt["z"] += 1
            p.op("act", lambda e: e.activation(out=zst[b][:, 0:w], in_=ps[:, 0:w], func=AF.Silu), reads=[ptok], writes=[("zst", b)])
            p.dma(dr["sz"][idx * 128:(idx + 1) * 128, c0:c0 + w], zst[b][:, 0:w], reads=[("zst", b)])
        elif tag == "v":
            b = cnt["v"] % 2
            cnt["v"] += 1
            p.op("dve", lambda e: e.tensor_copy(out=vst[b][:, 0:w], in_=ps[:, 0:w]), reads=[ptok], writes=[("vst", b)])
            p.dma(dr["v_tm"][idx * 128:(idx + 1) * 128, c0 - 9248:c0 - 9248 + w], vst[b][:, 0:w], reads=[("vst", b)])
        elif tag == "dt":
            t = idx
            p.op("dve", lambda e: e.tensor_tensor(out=sp1, in0=ps[:, 0:32], in1=dtb, op=ALU.add), reads=[ptok, dtbtok], writes=["sp1"])
            p.op("act", lambda e: e.activation(out=sp2, in_=sp1, func=AF.Abs), reads=["sp1"], writes=["sp2"])
            p.op("act", lambda e: e.activation(out=sp2, in_=sp2, func=AF.Exp, scale=-1.0), reads=["sp2"], writes=["sp2"])
            p.op("act", lambda e: e.activation(out=sp2, in_=sp2, func=AF.Ln, bias=1.0), reads=["sp2"], writes=["sp2"])
            p.op("dve", lambda e: e.scalar_tensor_tensor(out=dts[:, t, :], in0=sp1, scalar=0.0, in1=sp2, op0=ALU.max, op1=ALU.add),
                 reads=["sp1", "sp2"], writes=["dts"])
            p.op("dve", lambda e: e.tensor_tensor(out=adts[:, t, :], in0=dts[:, t, :], in1=abc, op=ALU.mult), reads=["dts", abctok], writes=["adts"])
            if t == NT - 1:
                p.dma(dr["dt_tm"].rearrange("(t p) h -> p t h", p=128), dts, reads=["dts"])
                p.dma(dr["adt_tm"].rearrange("(t p) h -> p t h", p=128), adts, reads=["adts"])
        elif tag == "xbc":
            tb = idx
            ci = (c0 - 2048) // 128
            p.op("act", lambda e: e.activation(out=cin[:, 3 + tb * 512:3 + (tb + 1) * 512], in_=ps[:, 0:512], func=AF.Copy),
                 reads=[ptok], writes=[("cin", tb)])
            if tb < 3:
                return
            ctoks = toks("cin", range(4)) + ["cin_pad"]
            p.op("dve", lambda e: e.tensor_scalar(out=wa, in0=cin[:, 0:T], scalar1=cw[:, ci, 0:1], scalar2=None, op0=ALU.mult),
                 reads=ctoks + ["cw"], writes=["wa"])
            for j in range(1, 4):
                p.op("dve", lambda e, j=j: e.scalar_tensor_tensor(out=wa, in0=cin[:, j:j + T], scalar=cw[:, ci, j:j + 1], in1=wa,
                                                                 op0=ALU.mult, op1=ALU.add), reads=ctoks + ["cw", "wa"], writes=["wa"])
            p.op("act", lambda e: e.activation(out=wb, in_=wa, func=AF.Silu, bias=cb[:, ci:ci + 1], scale=1.0), reads=["wa", cbtok], writes=["wb"])
            if ci < 16:
                transposes_to(tmst, "tmst", wb, "wb")
                p.dma(dr["xs_tm"].rearrange("(t p) c -> p t c", p=128)[:, :, ci * 128:(ci + 1) * 128], tmst, reads=["tmst"])
            else:
                b = cnt["o"] % 2
                cnt["o"] += 1
                p.op("pool", lambda e: e.tensor_copy(out=obf[b], in_=wb), reads=["wb"], writes=[("obf", b)])
                g = (ci - 16) % 4
                dst = dr["B_fm"] if ci < 20 else dr["C_fm"]
                p.dma(dst[g * 128:(g + 1) * 128, :], obf[b], reads=[("obf", b)])
                if ci < 20:
                    transposes_to(tmstb, "tmstb", wb, "wb")
                    p.dma(dr["B_tm"].rearrange("(t p) n -> p t n", p=128)[:, :, g * 128:(g + 1) * 128], tmstb, reads=["tmstb"])
        elif tag in ("q", "k"):
            tb = idx
            base = 5152 if tag == "q" else 7200
            h = (c0 - base) // 128
            p.op("act", lambda e: e.activation(out=wa[:, tb * 512:(tb + 1) * 512], in_=ps[:, 0:512], func=AF.Copy), reads=[ptok], writes=[("waq", tb)])
            if tb < 3:
                return
            for b4 in range(4):
                sl = slice(b4 * 512, (b4 + 1) * 512)
                p.mm([lambda e, sl=sl: e.matmul(rps[:, 0:512], lhsT=rt, rhs=wa[:, sl], start=True, stop=True)], reads=[("waq", b4), "rt"], writes=[rtok])
                p.op("dve", lambda e, sl=sl: e.tensor_tensor(out=wb[:, sl], in0=rps[:, 0:512], in1=sinT[:, sl], op=ALU.mult), reads=[rtok, "sinT"], writes=[("wbq", b4)])
                p.op("pool", lambda e, sl=sl: e.tensor_tensor(out=wc[:, sl], in0=wa[:, sl], in1=cosT[:, sl], op=ALU.mult), reads=[("waq", b4), "cosT"], writes=[("wcq", b4)])
                p.op("pool", lambda e, sl=sl: e.tensor_tensor(out=wc[:, sl], in0=wc[:, sl], in1=wb[:, sl], op=ALU.add), reads=[("wcq", b4), ("wbq", b4)], writes=[("wcq", b4)])
            b = cnt["o"] % 2
            cnt["o"] += 1
            wctoks = toks("wcq", range(4))
            if tag == "k":
                p.op("act", lambda e: e.activation(out=obf[b], in_=wc, func=AF.Copy), reads=wctoks, writes=[("obf", b)])
                p.dma(dr["k_fm"][h * 128:(h + 1) * 128, :], obf[b], reads=[("obf", b)])
                p.op("dve", lambda e: e.tensor_reduce(out=kmean[:, h, :], in_=wc.rearrange("p (a b) -> p a b", a=8), axis=AX.X, op=ALU.add),
                     reads=wctoks, writes=[("kmean", h)])
                p.op("dve", lambda e: e.tensor_scalar(out=kmean[:, h, :], in0=kmean[:, h, :], scalar1=1.0 / 256.0, scalar2=None, op0=ALU.mult),
                     reads=[("kmean", h)], writes=[("kmean", h)])
            else:
                p.op("act", lambda e: e.activation(out=obf[b], in_=wc, func=AF.Copy, scale=128.0 ** -0.5), reads=wctoks, writes=[("obf", b)])
                p.dma(dr["q_fm"][h * 128:(h + 1) * 128, :], obf[b], reads=[("obf", b)])
                p.mm([(lambda e, t=t: e.matmul(mps[:, t * 8:(t + 1) * 8], lhsT=wc[:, t * 128:(t + 1) * 128], rhs=kmean[:, h, :], start=True, stop=True))
                      for t in range(NT)], reads=wctoks + [("kmean", h)], writes=[mtok])
                p.op("dve", lambda e: e.tensor_copy(out=gst.rearrange("p a b -> p (a b)"), in_=mps[:, 0:128]), reads=[mtok], writes=["gst"])
                p.dma(dr["gate_d"][h].rearrange("(t p) e -> p t e", p=128), gst, reads=["gst"])

    panels = [(2048 + i * 256, 256, "xbc") for i in range(12)]
    gemm_panels(cx, ws, w_in, panels, xT, "xT", "fm", gps, ep)
    p.barrier()
    gemm_panels(cx, ws, w_in, [(5120, 32, "dt")], xT, "xT", "tm", gps, ep)
    p.barrier()
    panels = [(7200 + i * 256, 256, "k") for i in range(8)] + [(5152 + i * 256, 256, "q") for i in range(8)]
    gemm_panels(cx, ws, w_in, panels, xT, "xT", "fm", gps, ep)
    p.barrier()
    panels = [(i * 256, 256, "z") for i in range(8)] + [(9248 + i * 256, 256, "v") for i in range(8)]
    gemm_panels(cx, ws, w_in, panels, xT, "xT", "tm", gps, ep)
    S.close()


INPUT_SHAPES = {
    "x": ([T, D], F32),
    "hyb_w_in": ([D, HYB_IN], F32), "hyb_conv_w": ([4, 3072], F32), "hyb_conv_b": ([1, 3072], F32),
    "hyb_dt_bias": ([1, 32], F32), "hyb_a_log": ([1, 32], F32), "hyb_d": ([1, 32], F32),
    "hyb_norm": ([1, 2048], F32), "hyb_w_out": ([4096, D], F32),
    "gla_w_in": ([D, GLA_IN], F32), "gla_w_gate2": ([16, 1024], F32), "gla_b_gate": ([1, 1024], F32),
    "gla_norm": ([1, 512], F32), "gla_w_out": ([D, D], F32),
    "ln1_g": ([2, D], F32), "ln1_b": ([2, D], F32), "ln2_g": ([2, D], F32), "ln2_b": ([2, D], F32),
    "moe_w_router": ([2 * D, 32], F32), "moe_b_router": ([2, 32], F32),
    "moe_w1": ([2 * 4 * D, 2 * D], F32), "moe_b1": ([2 * 4, 2 * D], F32),
    "moe_w2": ([2 * 4 * D, D], F32), "moe_b2": ([2 * 4, D], F32),
    "oh": ([128, 8], F32),
}

SCRATCH = {
    "sz": ([T, 2048], F32), "v_tm": ([T, 2048], BF16), "dt_tm": ([T, 32], F32), "adt_tm": ([T, 32], F32),
    "xs_tm": ([T, 2048], F32), "B_fm": ([512, T], BF16), "C_fm": ([512, T], BF16), "B_tm": ([T, 512], BF16),
    "q_fm": ([2048, T], BF16), "k_fm": ([2048, T], BF16), "gate_d": ([16, T, 8], F32),
    "yT": ([4096, T], BF16), "h1": ([T, D], F32), "h2": ([T, D], F32), "h3": ([T, D], F32),
    "GH": ([8 * D, T], BF16), "GH2": ([8 * D, T], BF16), "GG": ([8 * T, 32], F32), "GG2": ([8 * T, 32], F32),
    "w1b": ([4 * 16 * 128, 4096], BF16), "w2b": ([4 * 4 * 128, 8192], BF16),
    "PART": ([8 * T, D], F32), "PART2": ([8 * T, D], F32),
    "gq_fm": ([1024, T], F32), "gk_fm": ([1024, T], F32), "gk_tm": ([T, 1024], F32), "gv_tm": ([T, 2048], BF16),
    "gsg": ([T, 2048], F32), "gla_d": ([T, 1024], F32), "out": ([T, D], F32),
    "hTo": ([D, T], BF16), "gateo": ([T, 32], F32),
}


def build(stages, dbg=(), needed_inputs=None, ext_in=(), moe_layers=2, moe_experts=4):
    nc = bass.Bass("TRN2", target_bir_lowering=False)
    cx = Ctx()
    cx.nc = nc
    cx.dram = {}
    for k, (shape, dt) in INPUT_SHAPES.items():
        if needed_inputs is not None and k not in needed_inputs:
            continue
        if k in ("moe_w1", "moe_b1", "moe_w2", "moe_b2"):
            shape = [shape[0] // 4 * moe_experts, shape[1]]
            if moe_layers == 1:
                shape = [shape[0] // 2, shape[1]]
        cx.dram[k] = nc.dram_tensor(k, shape, dt, kind="ExternalInput").ap()
    for k, (shape, dt) in CONST_SHAPES.items():
        cx.dram[k] = nc.dram_tensor(k, shape, dt, kind="ExternalInput").ap()
    for k, (shape, dt) in SCRATCH.items():
        if k in ext_in:
            cx.dram[k] = nc.dram_tensor(k, shape, dt, kind="ExternalInput").ap()
        elif k in dbg:
            cx.dram[k] = nc.dram_tensor(k, shape, dt, kind="ExternalOutput").ap()
        else:
            cx.dram[k] = nc.dram_tensor(k, shape, dt).ap()
    cx.p = Prog(nc)
    for st in stages:
        st(cx)
    cx.p.barrier()
    return nc, cx


def stage_l0_ssd(cx):
    LV = 9
    p, nc, dr = cx.p, cx.nc, cx.dram
    S = Scope(p)
    ident = load_const(cx, S, "ident", "ident")
    identb = load_const(cx, S, "identb", "identb")
    triu = load_const(cx, S, "triu", "triu")
    maskneg = load_const(cx, S, "maskneg", "maskneg")
    sel = load_const(cx, S, "sel", "sel")
    lastsel = load_const(cx, S, "lastsel", "lastsel")
    if LV == -1:
        S.close()
        return
    Bf = S.sb("Bf", [128, 4, T], BF16)
    Cf = S.sb("Cf", [128, 4, T], BF16)
    Bt = S.sb("Bt", [128, NT, 512], BF16)
    dts = S.sb("dts", [128, NT, 32], F32)
    adts = S.sb("adts", [128, NT, 32], F32)
    p.dma(Bf, dr["B_fm"].rearrange("(g p) t -> p g t", p=128), writes=["Bf"])
    p.dma(Cf, dr["C_fm"].rearrange("(g p) t -> p g t", p=128), writes=["Cf"])
    p.dma(Bt, dr["B_tm"].rearrange("(t p) n -> p t n", p=128), writes=["Bt"])
    p.dma(dts, dr["dt_tm"].rearrange("(t p) h -> p t h", p=128), writes=["dts"])
    p.dma(adts, dr["adt_tm"].rearrange("(t p) h -> p t h", p=128), writes=["adts"])
    if LV == -2:
        S.close()
        return
    dbc, dbctok = bcast_row(cx, S, dr["hyb_d"], 32, "dbc")
    nw, nwtok = bcast_row(cx, S, dr["hyb_norm"], 2048, "nw")
    xs = [S.sb("xs", [128, 2048], F32) for _ in range(2)]
    szb = [S.sb("szb", [128, 2048], F32) for _ in range(2)]
    prev = S.sb("prev", [128, 4, 512], F32)
    prevb = S.sb("prevb", [128, 4, 512], BF16)
    p.op("pool", lambda e: e.memset(prev, 0.0), writes=["prev"])
    p.op("pool", lambda e: e.memset(prevb, 0.0), writes=["prevb"])
    if LV == -3:
        S.close()
        return
    adp = S.sb("adp", [128, 128], F32)
    p.op("pool", lambda e: e.memset(adp, 0.0), writes=["adp"])
    acum = S.sb("acum", [128, 32], F32)
    nacum = S.sb("nacum", [128, 32], F32)
    eac = S.sb("eac", [128, 32], F32)
    acf = S.sb("acf", [32, 128], F32)
    Dm = S.sb("Dm", [128, 8, 128], F32)
    cbt = S.sb("cbt", [128, 128], F32)
    M = S.sb("M", [128, 8, 128], BF16)
    xdt = S.sb("xdt", [128, 8, 64], BF16)
    xdte = S.sb("xdte", [128, 8, 64], BF16)
    t1 = S.sb("t1", [128, 512], F32)
    t2 = S.sb("t2", [128, 512], F32)
    ysb = S.sb("ysb", [128, 512], F32)
    junk = S.sb("junk", [128, 512], F32)
    ybf = S.sb("ybf", [128, 512], BF16)
    sm = S.sb("sm", [128, 16], F32)
    dte = S.sb("dte", [128, 32], F32)
    wde = S.sb("wde", [128, 32], F32)
    cd = S.sb("cd", [128, 32], F32)
    ptmp = S.sb("ptmp", [128, 512], F32)
    yT = S.sb("yTs", [128, 16, T], BF16)
    E = S.ps("E", [128, 1024], F32)
    aps = S.ps("aps", [128, 512], F32)
    cps = S.ps("cps", [128, 512], F32)
    Yd = S.ps("Yd", [128, 512], F32)
    Yo = S.ps("Yo", [128, 512], F32)
    Sp = S.ps("Sp", [128, 512], F32)
    tp = S.ps("tp", [128, 512], BF16)
    xsv = dr["xs_tm"].rearrange("(t p) c -> t p c", p=128)
    szv = dr["sz"].rearrange("(t p) c -> t p c", p=128)

    def load(c):
        p.dma(xs[c % 2], xsv[c], writes=[("xs", c % 2)])
        p.dma(szb[c % 2], szv[c], writes=[("szb", c % 2)])
    load(0)
    for c in range(NT if LV > 0 else 0):
        if c + 1 < NT:
            load(c + 1)
        X, Z = xs[c % 2], szb[c % 2]
        xtok, ztok = ("xs", c % 2), ("szb", c % 2)
        cs = slice(c * 128, (c + 1) * 128)
        p.mm([lambda e: e.matmul(aps[:, 0:32], lhsT=triu, rhs=adts[:, c, :], start=True, stop=True)], reads=["triu", "adts"], writes=["aps"])
        p.op("act", lambda e: e.activation(out=acum, in_=aps[:, 0:32], func=AF.Copy), reads=["aps"], writes=["acum"])
        p.op("dve", lambda e: e.tensor_scalar(out=nacum, in0=aps[:, 0:32], scalar1=-1.0, scalar2=None, op0=ALU.mult), reads=["aps"], writes=["nacum"])
        p.op("act", lambda e: e.activation(out=eac, in_=aps[:, 0:32], func=AF.Exp), reads=["aps"], writes=["eac"])
        p.op("dve", lambda e: e.tensor_copy(out=adp[:, 0:32], in_=adts[:, c, :]), reads=["adts"], writes=["adp"])
        p.mm([lambda e: e.matmul(aps[:, 128:256], lhsT=adp, rhs=triu, start=True, stop=True)], reads=["triu", "adp", "aps"], writes=["aps"])
        p.op("dve", lambda e: e.tensor_copy(out=acf, in_=aps[0:32, 128:256]), reads=["aps"], writes=["acf"])
        p.mm([lambda e: e.matmul(aps[:, 256:288], lhsT=lastsel, rhs=acum, start=True, stop=True)], reads=["lastsel", "acum", "aps"], writes=["aps"])
        p.op("dve", lambda e: e.tensor_tensor(out=dte, in0=aps[:, 256:288], in1=nacum, op=ALU.add), reads=["aps", "nacum"], writes=["dte"])
        p.op("act", lambda e: e.activation(out=dte, in_=dte, func=AF.Exp), reads=["dte"], writes=["dte"])
        p.op("act", lambda e: e.activation(out=cd, in_=aps[:, 256:288], func=AF.Exp), reads=["aps"], writes=["cd"])
        p.op("dve", lambda e: e.tensor_tensor(out=wde, in0=dte, in1=dts[:, c, :], op=ALU.mult), reads=["dte", "dts"], writes=["wde"])
        for g in range(4 if LV >= 2 else 0):
            hs = slice(g * 8, (g + 1) * 8)
            gs = slice(g * 512, (g + 1) * 512)
            fns = []
            for j in range(8):
                h = g * 8 + j
                fns.append(lambda e, j=j, h=h: e.matmul(E[:, j * 128:(j + 1) * 128], lhsT=sel[:, h * 128:(h + 1) * 128], rhs=acf, start=True, stop=False))
                fns.append(lambda e, j=j: e.matmul(E[:, j * 128:(j + 1) * 128], lhsT=ident, rhs=maskneg, start=False, stop=True))
            p.mm(fns, reads=["sel", "acf", "ident", "maskneg"], writes=["E"])
            for j in range(8):
                h = g * 8 + j
                p.op("act", lambda e, j=j, h=h: e.activation(out=Dm[:, j, :], in_=E[:, j * 128:(j + 1) * 128], func=AF.Exp, bias=nacum[:, h:h + 1], scale=1.0),
                     reads=["E", "nacum"], writes=[("Dm", j)])
            if LV < 3:
                continue
            p.mm([lambda e: e.matmul(cps[:, 0:128], lhsT=Bf[:, g, cs], rhs=Cf[:, g, cs], start=True, stop=True)], reads=["Bf", "Cf"], writes=["cps"])
            p.op("act", lambda e: e.activation(out=cbt, in_=cps[:, 0:128], func=AF.Copy), reads=["cps"], writes=["cbt"])
            p.op("dve", lambda e: e.tensor_tensor(out=M, in0=Dm, in1=cbt.unsqueeze(1).to_broadcast([128, 8, 128]), op=ALU.mult),
                 reads=toks("Dm", range(8)) + ["cbt"], writes=["M"])
            Xg = X[:, gs].rearrange("p (a b) -> p a b", a=8)
            p.op("pool", lambda e: e.tensor_tensor(out=xdt, in0=Xg, in1=dts[:, c, hs].unsqueeze(2).to_broadcast([128, 8, 64]), op=ALU.mult),
                 reads=[xtok, "dts"], writes=["xdt"])
            p.mm([(lambda e, j=j: e.matmul(Yd[:, j * 64:(j + 1) * 64], lhsT=M[:, j, :], rhs=xdt[:, j, :], start=True, stop=True)) for j in range(8)],
                 reads=["M", "xdt"], writes=["Yd"])
            p.mm([lambda e: e.matmul(Yo[:, 0:512], lhsT=Cf[:, g, cs], rhs=prevb[:, g, :], start=True, stop=True)], reads=["Cf", ("prevb", g)], writes=["Yo"])
            if LV < 4:
                continue
            p.op("dve", lambda e: e.tensor_tensor(out=t1.rearrange("p (a b) -> p a b", a=8), in0=Yo.rearrange("p (a b) -> p a b", a=8),
                                                  in1=eac[:, hs].unsqueeze(2).to_broadcast([128, 8, 64]), op=ALU.mult), reads=["Yo", "eac"], writes=["t1"])
            p.op("pool", lambda e: e.tensor_tensor(out=t2.rearrange("p (a b) -> p a b", a=8), in0=Xg,
                                                   in1=dbc[:, hs].unsqueeze(2).to_broadcast([128, 8, 64]), op=ALU.mult), reads=[xtok, dbctok], writes=["t2"])
            p.op("pool", lambda e: e.tensor_tensor(out=t2, in0=t2, in1=t1, op=ALU.add), reads=["t1", "t2"], writes=["t2"])
            p.op("dve", lambda e: e.tensor_tensor(out=ysb, in0=Yd[:, 0:512], in1=t2, op=ALU.add), reads=["Yd", "t2"], writes=["ysb"])
            p.op("pool", lambda e: e.tensor_tensor(out=ysb, in0=ysb, in1=Z[:, gs], op=ALU.mult), reads=["ysb", ztok], writes=["ysb"])
            p.op("act", lambda e: e.activation(out=junk, in_=ysb, func=AF.Square, accum_out=sm[:, 0:1]), reads=["ysb"], writes=["junk", "sm"])
            p.op("dve", lambda e: e.tensor_scalar(out=sm[:, 1:2], in0=sm[:, 0:1], scalar1=1.0 / 512.0, scalar2=EPS, op0=ALU.mult, op1=ALU.add), reads=["sm"], writes=["sm"])
            p.op("act", lambda e: e.activation(out=sm[:, 2:3], in_=sm[:, 1:2], func=AF.Sqrt), reads=["sm"], writes=["sm"])
            p.op("dve", lambda e: e.reciprocal(out=sm[:, 3:4], in_=sm[:, 2:3]), reads=["sm"], writes=["sm"])
            p.op("dve", lambda e: e.scalar_tensor_tensor(out=ybf, in0=ysb, scalar=sm[:, 3:4], in1=nw[:, gs], op0=ALU.mult, op1=ALU.mult),
                 reads=["ysb", "sm", nwtok], writes=["ybf"])
            if LV < 5:
                continue
            p.mm([(lambda e, j=j: e.transpose(out=tp[:, j * 128:(j + 1) * 128], in_=ybf[:, j * 128:(j + 1) * 128], identity=identb)) for j in range(4)],
                 reads=["ybf", "identb"], writes=["tp"])
            p.op("act", lambda e: e.activation(out=yT[:, g * 4:(g + 1) * 4, cs], in_=tp[:, 0:512].rearrange("p (a b) -> p a b", a=4), func=AF.Copy),
                 reads=["tp"], writes=[("yT", c, g)])
            if LV < 6:
                continue
            p.op("dve", lambda e: e.tensor_tensor(out=xdte, in0=Xg, in1=wde[:, hs].unsqueeze(2).to_broadcast([128, 8, 64]), op=ALU.mult),
                 reads=[xtok, "wde"], writes=["xdte"])
            p.mm([lambda e: e.matmul(Sp[:, 0:512], lhsT=Bt[:, c, g * 128:(g + 1) * 128], rhs=xdte.rearrange("p a b -> p (a b)"), start=True, stop=True)],
                 reads=["Bt", "xdte"], writes=["Sp"])
            if LV < 7:
                continue
            p.op("pool", lambda e: e.tensor_tensor(out=ptmp.rearrange("p (a b) -> p a b", a=8), in0=prev[:, g, :].rearrange("p (a b) -> p a b", a=8),
                                                   in1=cd[:, hs].unsqueeze(2).to_broadcast([128, 8, 64]), op=ALU.mult), reads=[("prev", g), "cd"], writes=["ptmp"])
            if LV < 8:
                continue
            p.op("dve", lambda e: e.tensor_tensor(out=prev[:, g, :], in0=Sp[:, 0:512], in1=ptmp, op=ALU.add), reads=["Sp", "ptmp"], writes=[("prev", g)])
            p.op("act", lambda e: e.activation(out=prevb[:, g, :], in_=prev[:, g, :], func=AF.Copy), reads=[("prev", g)], writes=[("prevb", g)])
    for k in range(16):
        p.dma(dr["yT"][k * 128:(k + 1) * 128, :], yT[:, k, :], reads=[("yT", c, k // 4) for c in range(NT)])
    S.close()


def stage_l0_att(cx):
    p, nc, dr = cx.p, cx.nc, cx.dram
    S = Scope(p)
    identb = load_const(cx, S, "identb", "identb")
    causal = load_const(cx, S, "causal", "causal")
    qT = [S.sb("qT", [128, T], BF16) for _ in range(2)]
    kT = [S.sb("kT", [128, T], BF16) for _ in range(2)]
    vt = [S.sb("vt", [128, NT, 128], BF16) for _ in range(2)]
    gt = [S.sb("gt", [128, NT, 8], F32) for _ in range(2)]
    yatt = [S.sb("yatt", [128, T], BF16) for _ in range(2)]
    Ssb = [S.sb("Ssb", [128, T], F32) for _ in range(2)]
    Pb = [S.sb("Pb", [128, T], BF16) for _ in range(2)]
    PTs = [S.sb("PTs", [128, NT, 128], BF16) for _ in range(2)]
    gsb = [S.sb("gsb", [128, 8], F32) for _ in range(2)]
    m8 = [S.sb("m8", [128, 8], F32) for _ in range(2)]
    bs = [S.sb("bs", [128, 8], F32) for _ in range(2)]
    st = [S.sb("st", [128, 4], F32) for _ in range(2)]
    Sps = [S.ps("Sps", [128, 512], F32) for _ in range(4)]
    PT = [S.ps("PT", [128, 1024], BF16) for _ in range(2)]
    OT = S.ps("OT", [128, 512], F32)

    def load(h):
        b = h % 2
        p.dma(qT[b], dr["q_fm"][h * 128:(h + 1) * 128, :], writes=[("qT", b)])
        p.dma(kT[b], dr["k_fm"][h * 128:(h + 1) * 128, :], writes=[("kT", b)])
        p.dma(vt[b], dr["v_tm"].rearrange("(t p) d -> p t d", p=128)[:, :, h * 128:(h + 1) * 128], writes=[("vt", b)])
        p.dma(gt[b], dr["gate_d"][h].rearrange("(t p) e -> p t e", p=128), writes=[("gt", b)])
    load(0)
    it = 0
    for h in range(16):
        if h + 1 < 16:
            load(h + 1)
        hb = h % 2
        for qi in range(NT):
            b = it % 2
            it += 1
            qb, half = qi // 2, qi % 2
            npast = qb * 256
            nk = npast + (half + 1) * 128
            nseg = (nk + 511) // 512
            for sg in range(nseg):
                w = min(512, nk - sg * 512)
                p.mm([lambda e, sg=sg, w=w: e.matmul(Sps[sg][:, 0:w], lhsT=qT[hb][:, qi * 128:(qi + 1) * 128], rhs=kT[hb][:, sg * 512:sg * 512 + w],
                                                     start=True, stop=True)], reads=[("qT", hb), ("kT", hb)], writes=[("Sps", sg)])
            sel = qb >= 3
            if sel:
                p.op("dve", lambda e: e.tensor_copy(out=gsb[b], in_=gt[hb][:, qi, :]), reads=[("gt", hb)], writes=[("gsb", b)])
                p.op("pool", lambda e: e.memset(gsb[b][:, qb:8], NEG), reads=[("gsb", b)], writes=[("gsb", b)])
                p.op("dve", lambda e: e.max(out=m8[b], in_=gsb[b]), reads=[("gsb", b)], writes=[("m8", b)])
                p.op("dve", lambda e: e.tensor_scalar(out=bs[b], in0=gsb[b], scalar1=m8[b][:, 2:3], scalar2=-1.0, op0=ALU.is_ge, op1=ALU.add),
                     reads=[("gsb", b), ("m8", b)], writes=[("bs", b)])
                p.op("dve", lambda e: e.tensor_scalar(out=bs[b], in0=bs[b], scalar1=1.0e30, scalar2=None, op0=ALU.mult), reads=[("bs", b)], writes=[("bs", b)])
            stok = ("Ssb", b)
            for sg in range((npast + 511) // 512):
                wp = min(512, npast - sg * 512)
                nb = wp // 256
                if sel:
                    p.op("dve", lambda e, sg=sg, wp=wp, nb=nb: e.tensor_tensor(
                        out=Ssb[b][:, sg * 512:sg * 512 + wp].rearrange("p (a c) -> p a c", a=nb),
                        in0=Sps[sg][:, 0:wp].rearrange("p (a c) -> p a c", a=nb),
                        in1=bs[b][:, sg * 2:sg * 2 + nb].unsqueeze(2).to_broadcast([128, nb, 256]), op=ALU.add),
                        reads=[("Sps", sg), ("bs", b)], writes=[stok])
                else:
                    p.op("act", lambda e, sg=sg, wp=wp: e.activation(out=Ssb[b][:, sg * 512:sg * 512 + wp], in_=Sps[sg][:, 0:wp], func=AF.Copy),
                         reads=[("Sps", sg)], writes=[stok])
            so, off = npast // 512, npast % 512
            if half == 1:
                p.op("act", lambda e: e.activation(out=Ssb[b][:, npast:npast + 128], in_=Sps[so][:, off:off + 128], func=AF.Copy),
                     reads=[("Sps", so)], writes=[stok])
            o2 = off + half * 128
            p.op("dve", lambda e: e.tensor_tensor(out=Ssb[b][:, nk - 128:nk], in0=Sps[so][:, o2:o2 + 128], in1=causal, op=ALU.add),
                 reads=[("Sps", so), "causal"], writes=[stok])
            p.op("dve", lambda e: e.reduce_max(out=st[b][:, 0:1], in_=Ssb[b][:, 0:nk], axis=AX.X, negate=True), reads=[stok], writes=[("st", b)])
            p.op("act", lambda e: e.activation(out=Ssb[b][:, 0:nk], in_=Ssb[b][:, 0:nk], func=AF.Exp, bias=st[b][:, 0:1], scale=1.0, accum_out=st[b][:, 1:2]),
                 reads=[stok, ("st", b)], writes=[stok, ("st", b)])
            p.op("dve", lambda e: e.reciprocal(out=st[b][:, 2:3], in_=st[b][:, 1:2]), reads=[("st", b)], writes=[("st", b)])
            p.op("dve", lambda e: e.tensor_scalar(out=Pb[b][:, 0:nk], in0=Ssb[b][:, 0:nk], scalar1=st[b][:, 2:3], scalar2=None, op0=ALU.mult),
                 reads=[stok, ("st", b)], writes=[("Pb", b)])
            nkb = nk // 128
            for bank in range((nkb + 7) // 8):
                n8 = min(8, nkb - bank * 8)
                p.mm([(lambda e, kb=kb, bank=bank: e.transpose(out=PT[bank][:, (kb % 8) * 128:(kb % 8 + 1) * 128], in_=Pb[b][:, kb * 128:(kb + 1) * 128],
                                                            identity=identb)) for kb in range(bank * 8, bank * 8 + n8)],
                     reads=[("Pb", b), "identb"], writes=[("PT", bank)])
                eng = "act" if bank == 0 else "dve"
                outap = PTs[b][:, bank * 8:bank * 8 + n8, :]
                inap = PT[bank][:, 0:n8 * 128].rearrange("p (a c) -> p a c", a=n8)
                if eng == "act":
                    p.op("act", lambda e, outap=outap, inap=inap: e.activation(out=outap, in_=inap, func=AF.Copy), reads=[("PT", bank)], writes=[("PTs", b, bank)])
                else:
                    p.op("dve", lambda e, outap=outap, inap=inap: e.tensor_copy(out=outap, in_=inap), reads=[("PT", bank)], writes=[("PTs", b, bank)])
            p.mm([(lambda e, kb=kb: e.matmul(OT[:, 0:128], lhsT=vt[hb][:, kb, :], rhs=PTs[b][:, kb, :], start=(kb == 0), stop=(kb == nkb - 1)))
                  for kb in range(nkb)], reads=[("vt", hb), ("PTs", b, 0), ("PTs", b, 1)], writes=["OT"])
            p.op("act", lambda e: e.activation(out=yatt[hb][:, qi * 128:(qi + 1) * 128], in_=OT[:, 0:128], func=AF.Copy), reads=["OT"], writes=[("yatt", hb)])
        p.dma(dr["yT"][2048 + h * 128:2048 + (h + 1) * 128, :], yatt[hb], reads=[("yatt", hb)])
    S.close()


def ln_tile(cx, r, rtok, junk, jtok, sm, smtok, gbc, gtok, bbc, btok):
    p = cx.p
    p.op("act", lambda e: e.activation(out=junk, in_=r, func=AF.Copy, accum_out=sm[:, 0:1]), reads=[rtok], writes=[jtok, smtok])
    p.op("act", lambda e: e.activation(out=junk, in_=r, func=AF.Square, accum_out=sm[:, 1:2]), reads=[rtok, jtok], writes=[jtok, smtok])
    p.op("dve", lambda e: e.tensor_scalar(out=sm[:, 2:3], in0=sm[:, 0:1], scalar1=1.0 / D, scalar2=None, op0=ALU.mult), reads=[smtok], writes=[smtok])
    p.op("dve", lambda e: e.scalar_tensor_tensor(out=sm[:, 3:4], in0=sm[:, 2:3], scalar=-1.0, in1=sm[:, 2:3], op0=ALU.mult, op1=ALU.mult),
         reads=[smtok], writes=[smtok])
    p.op("dve", lambda e: e.scalar_tensor_tensor(out=sm[:, 4:5], in0=sm[:, 1:2], scalar=1.0 / D, in1=sm[:, 3:4], op0=ALU.mult, op1=ALU.add),
         reads=[smtok], writes=[smtok])
    p.op("dve", lambda e: e.tensor_scalar(out=sm[:, 4:5], in0=sm[:, 4:5], scalar1=EPS, scalar2=None, op0=ALU.add), reads=[smtok], writes=[smtok])
    p.op("act", lambda e: e.activation(out=sm[:, 5:6], in_=sm[:, 4:5], func=AF.Sqrt), reads=[smtok], writes=[smtok])
    p.op("dve", lambda e: e.reciprocal(out=sm[:, 6:7], in_=sm[:, 5:6]), reads=[smtok], writes=[smtok])
    p.op("dve", lambda e: e.scalar_tensor_tensor(out=sm[:, 7:8], in0=sm[:, 2:3], scalar=-1.0, in1=sm[:, 6:7], op0=ALU.mult, op1=ALU.mult),
         reads=[smtok], writes=[smtok])
    p.op("act", lambda e: e.activation(out=r, in_=r, func=AF.Identity, scale=sm[:, 6:7], bias=sm[:, 7:8]), reads=[rtok, smtok], writes=[rtok])
    p.op("pool", lambda e: e.tensor_tensor(out=r, in0=r, in1=gbc, op=ALU.mult), reads=[rtok, gtok], writes=[rtok])
    p.op("pool", lambda e: e.tensor_tensor(out=r, in0=r, in1=bbc, op=ALU.add), reads=[rtok, btok], writes=[rtok])


def stage_outproj_ln(cx, yT_d, kc, W_d, h_in, g_row, b_row, h_out):
    p, nc, dr = cx.p, cx.nc, cx.dram
    S = Scope(p)
    pw = 128 if kc == 32 else 256
    ws = WStream(cx, S, kc, pw, "wout")
    gbc, gtok = bcast_row(cx, S, g_row, D, "gbc")
    bbc, btok = bcast_row(cx, S, b_row, D, "bbc")
    racc = S.sb("racc", [128, 4, D], F32)
    yTq = S.sb("yTq", [128, kc, 512], BF16)
    junk = S.sb("junk", [128, D], F32)
    sm = S.sb("sm", [128, 8], F32)
    gps = [(S.ps("ops", [128, 512], F32), ("ops", i)) for i in range(4)]
    yv = yT_d.rearrange("(k p) t -> p k t", p=128)
    hv = h_in.rearrange("(t p) d -> t p d", p=128)
    ov = h_out.rearrange("(t p) d -> t p d", p=128)
    panels = [(i * pw, pw) for i in range(D // pw)]
    k = 0
    for qtr in range(4):
        p.dma(yTq, yv[:, :, qtr * 512:(qtr + 1) * 512], writes=["yTq"])
        for j in range(4):
            p.dma(racc[:, j, :], hv[qtr * 4 + j], writes=[("racc", j)])
            p.op("pool", lambda e, j=j: e.tensor_scalar(out=racc[:, j, :], in0=racc[:, j, :], scalar1=DN_ALPHA, scalar2=None, op0=ALU.mult),
                 reads=[("racc", j)], writes=[("racc", j)])
        nxt = ws.fetch(W_d, panels[0][0], pw)
        for pi, (c0, w) in enumerate(panels):
            bf, btk = nxt
            if pi + 1 < len(panels):
                nxt = ws.fetch(W_d, panels[pi + 1][0], pw)
            for j in range(4):
                ps, ptok = gps[k % 4]
                k += 1
                p.mm([(lambda e, c=c, ps=ps, j=j: e.matmul(ps[:, 0:w], lhsT=yTq[:, c, j * 128:(j + 1) * 128], rhs=bf[:, c, 0:w],
                                                         start=(c == 0), stop=(c == kc - 1))) for c in range(kc)], reads=[btk, "yTq"], writes=[ptok])
                p.op("dve", lambda e, ps=ps, j=j, c0=c0: e.tensor_tensor(out=racc[:, j, c0:c0 + w], in0=ps[:, 0:w], in1=racc[:, j, c0:c0 + w], op=ALU.add),
                     reads=[ptok, ("racc", j)], writes=[("racc", j)])
        for j in range(4):
            ln_tile(cx, racc[:, j, :], ("racc", j), junk, "junk", sm, "sm", gbc, gtok, bbc, btok)
            p.dma(ov[qtr * 4 + j], racc[:, j, :], reads=[("racc", j)])
    S.close()


def stage_router(cx, L, h_tm, masked=True):
    p, nc, dr = cx.p, cx.nc, cx.dram
    S = Scope(p)
    ident = load_const(cx, S, "ident", "ident")
    oh = S.sb("oh", [128, 8], F32)
    p.dma(oh, dr["oh"], writes=["oh"])
    wr = S.sb("wr", [128, KC, 32], F32)
    p.dma(wr, dr["moe_w_router"][L * D:(L + 1) * D, :].rearrange("(k p) e -> p k e", p=128), writes=["wr"])
    brbc, brtok = bcast_row(cx, S, dr["moe_b_router"][L:L + 1, :], 32, "brbc")
    hT = S.sb("hTr", [128, KC, T], BF16)
    hTf = S.sb("hTf", [128, KC, 128], F32)
    gl = S.sb("gl", [128, NT, 32], F32)
    ld = [S.sb("rld", [128, D], F32) for _ in range(2)]
    lg = S.sb("lg", [128, 32], F32)
    ex = S.sb("ex", [128, 32], F32)
    selm = S.sb("selm", [128, 32], F32)
    m8 = S.sb("m8", [128, 8], F32)
    sm = S.sb("rsm", [128, 4], F32)
    tps = [S.ps("rtp", [128, 512], F32) for _ in range(2)]
    lps = S.ps("lps", [128, 512], F32)
    src = h_tm.rearrange("(t p) d -> t p d", p=128)
    k = 0
    for t in range(NT):
        b = ld[t % 2]
        p.dma(b, src[t], writes=[("rld", t % 2)])
        for g in range(4):
            pp, ptok = tps[k % 2], ("rtp", k % 2)
            k += 1
            p.mm([(lambda e, pp=pp, b=b, g=g, j=j: e.transpose(out=pp[:, j * 128:(j + 1) * 128], in_=b[:, (g * 4 + j) * 128:(g * 4 + j + 1) * 128],
                                                              identity=ident)) for j in range(4)], reads=[("rld", t % 2), "ident"], writes=[ptok])
            src_ps = pp.rearrange("p (a b) -> p a b", a=4)
            p.op("act", lambda e, g=g, s=src_ps: e.activation(out=hTf[:, g * 4:(g + 1) * 4, :], in_=s, func=AF.Copy), reads=[ptok], writes=[("hTf", g)])
            p.op("dve", lambda e, g=g, t=t: e.tensor_copy(out=hT[:, g * 4:(g + 1) * 4, t * 128:(t + 1) * 128], in_=hTf[:, g * 4:(g + 1) * 4, :]),
                 reads=[("hTf", g)], writes=[("hTr", t, g)])
        p.mm([(lambda e, c=c: e.matmul(lps[:, 0:32], lhsT=hTf[:, c, :], rhs=wr[:, c, :], start=(c == 0), stop=(c == KC - 1))) for c in range(KC)],
             reads=toks("hTf", range(4)) + ["wr"], writes=["lps"])
        p.op("dve", lambda e: e.tensor_tensor(out=lg, in0=lps[:, 0:32], in1=brbc, op=ALU.add), reads=["lps", brtok], writes=["lg"])
        p.op("dve", lambda e: e.max(out=m8, in_=lg), reads=["lg"], writes=["m8"])
        p.op("dve", lambda e: e.tensor_scalar(out=sm[:, 0:1], in0=m8[:, 0:1], scalar1=-1.0, scalar2=None, op0=ALU.mult), reads=["m8"], writes=["rsm"])
        p.op("act", lambda e: e.activation(out=ex, in_=lg, func=AF.Exp, bias=sm[:, 0:1], scale=1.0), reads=["lg", "rsm"], writes=["ex"])
        p.op("dve", lambda e: e.tensor_scalar(out=selm, in0=lg, scalar1=m8[:, 3:4], scalar2=0.0, op0=ALU.is_ge, op1=ALU.add), reads=["lg", "m8"], writes=["selm"])
        p.op("dve", lambda e: e.tensor_tensor(out=ex, in0=ex, in1=selm, op=ALU.mult), reads=["ex", "selm"], writes=["ex"])
        p.op("dve", lambda e: e.tensor_reduce(out=sm[:, 1:2], in_=ex, axis=AX.X, op=ALU.add), reads=["ex"], writes=["rsm"])
        p.op("dve", lambda e: e.reciprocal(out=sm[:, 2:3], in_=sm[:, 1:2]), reads=["rsm"], writes=["rsm"])
        p.op("dve", lambda e, t=t: e.tensor_scalar(out=gl[:, t, :], in0=ex, scalar1=sm[:, 2:3], scalar2=None, op0=ALU.mult), reads=["ex", "rsm"], writes=["gl"])
    if not masked:
        for k in range(KC):
            p.dma(dr["hTo"][k * 128:(k + 1) * 128, :], hT[:, k, :], reads=[("hTr", t, k // 4) for t in range(NT)])
        p.dma(dr["gateo"].rearrange("(t p) e -> p t e", p=128), gl, reads=["gl"])
        S.close()
        return
    scb = [S.sb("scb", [128, 4, T], BF16) for _ in range(2)]
    gsc = [S.sb("gsc", [128, NT, 32], F32) for _ in range(2)]
    alltok = [("hTr", t, g) for t in range(NT) for g in range(4)]
    n = 0
    for j in range(8):
        for kq in range(4):
            b = n % 2
            eng = "dve" if n % 2 == 0 else "pool"
            n += 1
            p.op(eng, lambda e, b=b, kq=kq, j=j: e.tensor_scalar(out=scb[b], in0=hT[:, kq * 4:(kq + 1) * 4, :], scalar1=oh[:, j:j + 1], scalar2=None, op0=ALU.mult),
                 reads=[("hTr", t, kq) for t in range(NT)] + ["oh"], writes=[("scb", b)])
            p.dma(dr["GH"][j * D:(j + 1) * D, :].rearrange("(k p) t -> p k t", p=128)[:, kq * 4:(kq + 1) * 4, :], scb[b], reads=[("scb", b)], writes=["GH"])
        b = j % 2
        p.op("dve", lambda e, b=b, j=j: e.tensor_scalar(out=gsc[b], in0=gl, scalar1=oh[:, j:j + 1], scalar2=None, op0=ALU.mult), reads=["gl", "oh"], writes=[("gsc", b)])
        p.dma(dr["GG"][j * T:(j + 1) * T, :].rearrange("(t p) e -> p t e", p=128), gsc[b], reads=[("gsc", b)], writes=["GG"])
    S.close()


def stage_precast(cx, L):
    p, nc, dr = cx.p, cx.nc, cx.dram
    S = Scope(p)
    st1 = [S.sb("pc1", [128, 4096], F32) for _ in range(2)]
    o1 = [S.sb("po1", [128, 16, 256], BF16) for _ in range(2)]
    st2 = [S.sb("pc2", [128, 2048], F32) for _ in range(2)]
    o2 = [S.sb("po2", [128, 2048], BF16) for _ in range(2)]
    n = 0
    for e_ in range(4):
        r0 = (L * 4 + e_) * D
        for kc in range(KC):
            b = n % 2
            n += 1
            p.dma(st1[b], dr["moe_w1"][r0 + kc * 128:r0 + (kc + 1) * 128, :], writes=[("pc1", b)])
            sv = st1[b].rearrange("p (f i two) -> p f i two", f=16, two=2)
            p.op("dve", lambda e, b=b, sv=sv: e.tensor_copy(out=o1[b][:, :, 0:128], in_=sv[:, :, :, 0]), reads=[("pc1", b)], writes=[("po1", b)])
            p.op("pool", lambda e, b=b, sv=sv: e.tensor_copy(out=o1[b][:, :, 128:256], in_=sv[:, :, :, 1]), reads=[("pc1", b)], writes=[("po1", b)])
            p.dma(dr["w1b"][e_ * 16 * 128:(e_ + 1) * 16 * 128, :].rearrange("(f p) (k c) -> p f k c", p=128, c=256)[:, :, kc, :], o1[b],
                  reads=[("po1", b)], writes=["w1b"])
            p.dma(st2[b], dr["moe_w2"][r0 + kc * 128:r0 + (kc + 1) * 128, :], writes=[("pc2", b)])
            p.op("act", lambda e, b=b: e.activation(out=o2[b], in_=st2[b], func=AF.Copy), reads=[("pc2", b)], writes=[("po2", b)])
            p.dma(dr["w2b"][e_ * 4 * 128:(e_ + 1) * 4 * 128, :].rearrange("(d p) (f c) -> p d f c", p=128, c=512)[:, :, kc, :],
                  o2[b].rearrange("p (d c) -> p d c", d=4), reads=[("po2", b)], writes=["w2b"])
    S.close()


def stage_experts(cx, L, GH2, GG2, ngroups=16):
    p, nc, dr = cx.p, cx.nc, cx.dram
    S = Scope(p)
    ident = load_const(cx, S, "ident", "ident")
    oh = S.sb("oh", [128, 8], F32)
    p.dma(oh, dr["oh"], writes=["oh"])
    mps = S.ps("emps", [128, 512], F32)
    b1st = S.sb("b1st", [64, 256], F32)
    p.dma(b1st, dr["moe_b1"][L * 4:(L + 1) * 4, :].rearrange("e (f c) -> (e f) c", c=256), writes=["b1st"])
    b1g = S.sb("b1g", [128, 64], F32)
    b1l = S.sb("b1l", [128, 64], F32)
    p.mm([lambda e: e.transpose(out=mps[:, 0:64], in_=b1st[:, 0:256:2], identity=ident[0:64, 0:64]),
          lambda e: e.transpose(out=mps[:, 64:128], in_=b1st[:, 1:256:2], identity=ident[0:64, 0:64])], reads=["b1st", "ident"], writes=["emps"])
    p.op("dve", lambda e: e.tensor_copy(out=b1g, in_=mps[:, 0:64]), reads=["emps"], writes=["b1g"])
    p.op("dve", lambda e: e.tensor_copy(out=b1l, in_=mps[:, 64:128]), reads=["emps"], writes=["b1l"])
    b2t = [S.sb("b2t", [128, 512], F32) for _ in range(2)]
    hTg = S.sb("hTg", [128, KC, 1024], BF16)
    actT = S.sb("actT", [128, KC, 1024], BF16)
    acc = S.sb("acc", [128, 8, D], F32)
    gts = S.sb("gts", [128, 8, 32], F32)
    gsel = S.sb("gsel", [128, 8, 4], F32)
    gtmp = S.sb("gtmp", [128, 8, 4], F32)
    w1p = [S.sb("w1p", [128, KC, 256], BF16) for _ in range(2)]
    w2p = [S.sb("w2p", [128, KC, 512], BF16) for _ in range(2)]
    ga = [S.sb("ga", [128, 512], F32) for _ in range(2)]
    sg = [S.sb("sg", [128, 512], F32) for _ in range(2)]
    la = [S.sb("la", [128, 512], F32) for _ in range(2)]
    yb = [S.sb("yb", [128, 512], F32) for _ in range(2)]
    Gp = [S.ps("Gp", [128, 512], F32) for _ in range(2)]
    Lp = [S.ps("Lp", [128, 512], F32) for _ in range(2)]
    Yp = [S.ps("Yp", [128, 512], F32) for _ in range(2)]
    w1v = dr["w1b"].rearrange("(ef p) x -> ef p x", p=128)
    w2v = dr["w2b"].rearrange("(ed p) x -> ed p x", p=128)
    n1 = n2 = it = 0

    def fetch1(e_, fc):
        nonlocal n1
        b = n1 % 2
        n1 += 1
        p.dma(w1p[b].rearrange("p k c -> p (k c)"), w1v[e_ * 16 + fc], reads=["w1b"], writes=[("w1p", b)])
        return b

    def fetch2(e_, dp):
        nonlocal n2
        b = n2 % 2
        n2 += 1
        p.dma(w2p[b].rearrange("p k c -> p (k c)"), w2v[e_ * 4 + dp], reads=["w2b"], writes=[("w2p", b)])
        return b
    for gi in range(ngroups):
        jb, hf = gi // 2, gi % 2
        p.dma(hTg, GH2[jb * D:(jb + 1) * D, :].rearrange("(k p) t -> p k t", p=128)[:, :, hf * 1024:(hf + 1) * 1024], reads=["GH2"], writes=["hTg"])
        p.dma(gts, GG2[jb * T + hf * 1024:jb * T + (hf + 1) * 1024, :].rearrange("(t p) e -> p t e", p=128), reads=["GG2"], writes=["gts"])
        gv = gts.rearrange("p t (j e) -> p t j e", e=4)
        p.op("dve", lambda e: e.tensor_scalar(out=gsel, in0=gv[:, :, 0, :], scalar1=oh[:, 0:1], scalar2=None, op0=ALU.mult), reads=["gts", "oh"], writes=["gsel"])
        for j in range(1, 8):
            p.op("dve", lambda e, j=j: e.scalar_tensor_tensor(out=gsel, in0=gv[:, :, j, :], scalar=oh[:, j:j + 1], in1=gsel, op0=ALU.mult, op1=ALU.add),
                 reads=["gts", "oh", "gsel"], writes=["gsel"])
        for e_ in range(4):
            nb = fetch1(e_, 0)
            for fc in range(16):
                b1 = nb
                if fc + 1 < 16:
                    nb = fetch1(e_, fc + 1)
                for tb in range(2):
                    i2 = it % 2
                    it += 1
                    tsl = slice(tb * 512, (tb + 1) * 512)
                    p.mm([(lambda e, c=c: e.matmul(Gp[i2][:, 0:512], lhsT=w1p[b1][:, c, 0:128], rhs=hTg[:, c, tsl], start=(c == 0), stop=(c == KC - 1)))
                          for c in range(KC)], reads=[("w1p", b1), "hTg"], writes=[("Gp", i2)])
                    p.mm([(lambda e, c=c: e.matmul(Lp[i2][:, 0:512], lhsT=w1p[b1][:, c, 128:256], rhs=hTg[:, c, tsl], start=(c == 0), stop=(c == KC - 1)))
                          for c in range(KC)], reads=[("w1p", b1), "hTg"], writes=[("Lp", i2)])
                    col = e_ * 16 + fc
                    p.op("dve", lambda e: e.tensor_scalar(out=ga[i2], in0=Gp[i2][:, 0:512], scalar1=b1g[:, col:col + 1], scalar2=7.0, op0=ALU.add, op1=ALU.min),
                         reads=[("Gp", i2), "b1g"], writes=[("ga", i2)])
                    p.op("act", lambda e: e.activation(out=sg[i2], in_=ga[i2], func=AF.Sigmoid, scale=1.702), reads=[("ga", i2)], writes=[("sg", i2)])
                    p.op("dve", lambda e: e.tensor_scalar(out=la[i2], in0=Lp[i2][:, 0:512], scalar1=b1l[:, col:col + 1], scalar2=7.0, op0=ALU.add, op1=ALU.min),
                         reads=[("Lp", i2), "b1l"], writes=[("la", i2)])
                    p.op("pool", lambda e: e.tensor_scalar(out=la[i2], in0=la[i2], scalar1=-7.0, scalar2=1.0, op0=ALU.max, op1=ALU.add),
                         reads=[("la", i2)], writes=[("la", i2)])
                    p.op("pool", lambda e: e.tensor_tensor(out=ga[i2], in0=ga[i2], in1=sg[i2], op=ALU.mult), reads=[("ga", i2), ("sg", i2)], writes=[("ga", i2)])
                    p.op("pool", lambda e: e.tensor_tensor(out=actT[:, fc, tsl], in0=ga[i2], in1=la[i2], op=ALU.mult), reads=[("ga", i2), ("la", i2)],
                         writes=[("actT", fc, tb)])
            nb = fetch2(e_, 0)
            for dp in range(4):
                b2 = nb
                if dp + 1 < 4:
                    nb = fetch2(e_, dp + 1)
                dsl = slice(dp * 512, (dp + 1) * 512)
                bb = n2 % 2
                p.dma(b2t[bb], dr["moe_b2"][L * 4 + e_:L * 4 + e_ + 1, dsl].partition_broadcast(128), writes=[("b2t", bb)])
                for t in range(8):
                    i2 = it % 2
                    it += 1
                    p.mm([(lambda e, c=c: e.matmul(Yp[i2][:, 0:512], lhsT=actT[:, c, t * 128:(t + 1) * 128], rhs=w2p[b2][:, c, :], start=(c == 0), stop=(c == KC - 1)))
                          for c in range(KC)], reads=[("w2p", b2)] + [("actT", c, t // 4) for c in range(KC)], writes=[("Yp", i2)])
                    p.op("dve", lambda e: e.tensor_tensor(out=yb[i2], in0=Yp[i2][:, 0:512], in1=b2t[bb], op=ALU.add), reads=[("Yp", i2), ("b2t", bb)],
                         writes=[("yb", i2)])
                    if e_ == 0:
                        p.op("pool", lambda e: e.tensor_scalar(out=acc[:, t, dsl], in0=yb[i2], scalar1=gsel[:, t, e_:e_ + 1], scalar2=None, op0=ALU.mult),
                             reads=[("yb", i2), "gsel"], writes=[("acc", t, dp)])
                    else:
                        p.op("pool", lambda e: e.tensor_scalar(out=yb[i2], in0=yb[i2], scalar1=gsel[:, t, e_:e_ + 1], scalar2=None, op0=ALU.mult),
                             reads=[("yb", i2), "gsel"], writes=[("yb", i2)])
                        p.op("pool", lambda e: e.tensor_tensor(out=acc[:, t, dsl], in0=acc[:, t, dsl], in1=yb[i2], op=ALU.add),
                             reads=[("yb", i2), ("acc", t, dp)], writes=[("acc", t, dp)])
        p.dma(dr["PART"][gi * 1024:(gi + 1) * 1024, :].rearrange("(t p) d -> p t d", p=128), acc,
              reads=[("acc", t, dp) for t in range(8) for dp in range(4)], writes=["PART"])
    S.close()


def stage_combine_ln(cx, PART2, h_in, g_row, b_row, h_out, nblk=8, use_oh=True):
    p, nc, dr = cx.p, cx.nc, cx.dram
    S = Scope(p)
    oh = S.sb("oh", [128, 8], F32)
    p.dma(oh, dr["oh"], writes=["oh"])
    gbc, gtok = bcast_row(cx, S, g_row, D, "gbc")
    bbc, btok = bcast_row(cx, S, b_row, D, "bbc")
    r = [S.sb("cr", [128, D], F32) for _ in range(2)]
    pl = [S.sb("cpl", [128, D], F32) for _ in range(3)]
    junk = S.sb("junk", [128, D], F32)
    sm = S.sb("sm", [128, 8], F32)
    hv = h_in.rearrange("(t p) d -> t p d", p=128)
    ov = h_out.rearrange("(t p) d -> t p d", p=128)
    n = 0
    for t in range(NT):
        b = t % 2
        rt = ("cr", b)
        p.dma(r[b], hv[t], writes=[rt])
        p.op("pool", lambda e, b=b: e.tensor_scalar(out=r[b], in0=r[b], scalar1=DN_ALPHA, scalar2=None, op0=ALU.mult), reads=[rt], writes=[rt])
        for j in range(nblk):
            c = n % 3
            n += 1
            p.dma(pl[c], PART2[j * T + t * 128:j * T + (t + 1) * 128, :], reads=["PART2"], writes=[("cpl", c)])
            if use_oh:
                p.op("dve", lambda e, b=b, c=c, j=j: e.scalar_tensor_tensor(out=r[b], in0=pl[c], scalar=oh[:, j:j + 1], in1=r[b], op0=ALU.mult, op1=ALU.add),
                     reads=[("cpl", c), "oh", rt], writes=[rt])
            else:
                p.op("dve" if j % 2 == 0 else "pool", lambda e, b=b, c=c: e.tensor_tensor(out=r[b], in0=r[b], in1=pl[c], op=ALU.add),
                     reads=[("cpl", c), rt], writes=[rt])
        ln_tile(cx, r[b], rt, junk, "junk", sm, "sm", gbc, gtok, bbc, btok)
        p.dma(ov[t], r[b], reads=[rt])
    S.close()


def stage_gla_proj(cx, h_tm):
    p, nc, dr = cx.p, cx.nc, cx.dram
    S = Scope(p)
    ident = load_const(cx, S, "ident", "ident")
    xT = S.sb("xT", [128, KC, T], BF16)
    S2 = Scope(p)
    transpose_in(cx, S2, h_tm, xT, "xT", ident, "ident")
    S2.close()
    w_in = dr["gla_w_in"]
    ws = WStream(cx, S, KC, 256, "gwin")
    gps = [(S.ps("gps", [128, 512], F32), ("gps", i)) for i in range(3)]
    lps = [S.ps("glps", [128, 512], F32) for _ in range(2)]
    fst = [S.sb("fst", [128, T], F32) for _ in range(2)]
    tst = [S.sb("tst", [128, 256], F32) for _ in range(2)]
    vst = [S.sb("gvst", [128, 256], BF16) for _ in range(2)]
    glT = S.sb("glT", [16, T], F32)
    cnt = {"f": 0, "t": 0, "v": 0}

    def ep(tag, c0, w, idx, ps, ptok):
        if tag in ("q", "k"):
            tb = idx
            b = cnt["f"] % 2
            sc = (1.0 / 16.0) if tag == "q" else 1.0
            p.op("act", lambda e: e.activation(out=fst[b][:, tb * 512:(tb + 1) * 512], in_=ps[:, 0:512], func=AF.Copy, scale=sc), reads=[ptok], writes=[("fst", b, tb)])
            if tb == 3:
                cnt["f"] += 1
                dst = dr["gq_fm"] if tag == "q" else dr["gk_fm"]
                r0 = c0 if tag == "q" else c0 - 1024
                p.dma(dst[r0:r0 + 128, :], fst[b], reads=[("fst", b, i) for i in range(4)])
        elif tag == "gl":
            tb = idx
            p.op("act", lambda e: e.activation(out=glT[:, tb * 512:(tb + 1) * 512], in_=ps[0:16, 0:512], func=AF.Copy), reads=[ptok], writes=[("glT", tb)])
        elif tag in ("ktm", "g"):
            b = cnt["t"] % 2
            cnt["t"] += 1
            if tag == "g":
                p.op("act", lambda e: e.activation(out=tst[b][:, 0:w], in_=ps[:, 0:w], func=AF.Silu), reads=[ptok], writes=[("tst", b)])
                p.dma(dr["gsg"][idx * 128:(idx + 1) * 128, c0 - 4096:c0 - 4096 + w], tst[b][:, 0:w], reads=[("tst", b)])
            else:
                p.op("dve", lambda e: e.tensor_copy(out=tst[b][:, 0:w], in_=ps[:, 0:w]), reads=[ptok], writes=[("tst", b)])
                p.dma(dr["gk_tm"][idx * 128:(idx + 1) * 128, c0 - 1024:c0 - 1024 + w], tst[b][:, 0:w], reads=[("tst", b)])
        elif tag == "v":
            b = cnt["v"] % 2
            cnt["v"] += 1
            p.op("dve", lambda e: e.tensor_copy(out=vst[b][:, 0:w], in_=ps[:, 0:w]), reads=[ptok], writes=[("gvst", b)])
            p.dma(dr["gv_tm"][idx * 128:(idx + 1) * 128, c0 - 2048:c0 - 2048 + w], vst[b][:, 0:w], reads=[("gvst", b)])

    panels = [(i * 256, 256, "q") for i in range(4)] + [(1024 + i * 256, 256, "k") for i in range(4)] + [(6144, 16, "gl")]
    gemm_panels(cx, ws, w_in, panels, xT, "xT", "fm", gps, ep)
    p.barrier()
    panels = [(1024 + i * 256, 256, "ktm") for i in range(4)] + [(2048 + i * 256, 256, "v") for i in range(8)] + [(4096 + i * 256, 256, "g") for i in range(8)]
    gemm_panels(cx, ws, w_in, panels, xT, "xT", "tm", gps, ep)
    p.barrier()
    wg2 = S.sb("wg2", [16, 1024], F32)
    p.dma(wg2, dr["gla_w_gate2"], writes=["wg2"])
    bg, bgtok = bcast_row(cx, S, dr["gla_b_gate"], 1024, "bgate")
    xg = [S.sb("xg", [128, 1024], F32) for _ in range(2)]
    lg = [S.sb("lgl", [128, 1024], F32) for _ in range(2)]
    for t in range(NT):
        b = t % 2
        for hf in range(2):
            p.mm([lambda e, hf=hf: e.matmul(lps[hf][:, 0:512], lhsT=glT[:, t * 128:(t + 1) * 128], rhs=wg2[:, hf * 512:(hf + 1) * 512], start=True, stop=True)],
                 reads=toks("glT", range(4)) + ["wg2"], writes=[("glps", hf)])
            p.op("dve", lambda e, hf=hf: e.tensor_tensor(out=xg[b][:, hf * 512:(hf + 1) * 512], in0=lps[hf][:, 0:512], in1=bg[:, hf * 512:(hf + 1) * 512], op=ALU.add),
                 reads=[("glps", hf), bgtok], writes=[("xg", b, hf)])
        xt2 = [("xg", b, 0), ("xg", b, 1)]
        p.op("act", lambda e: e.activation(out=lg[b], in_=xg[b], func=AF.Abs), reads=xt2, writes=[("lgl", b)])
        p.op("act", lambda e: e.activation(out=lg[b], in_=lg[b], func=AF.Exp, scale=-1.0), reads=[("lgl", b)], writes=[("lgl", b)])
        p.op("act", lambda e: e.activation(out=lg[b], in_=lg[b], func=AF.Ln, bias=1.0), reads=[("lgl", b)], writes=[("lgl", b)])
        p.op("dve", lambda e: e.tensor_scalar(out=xg[b], in0=xg[b], scalar1=0.0, scalar2=1.0 / 16.0, op0=ALU.min, op1=ALU.mult), reads=xt2, writes=xt2)
        p.op("dve", lambda e: e.scalar_tensor_tensor(out=lg[b], in0=lg[b], scalar=-1.0 / 16.0, in1=xg[b], op0=ALU.mult, op1=ALU.add),
             reads=xt2 + [("lgl", b)], writes=[("lgl", b)])
        p.dma(dr["gla_d"][t * 128:(t + 1) * 128, :], lg[b], reads=[("lgl", b)])
    S.close()


def stage_gla_core(cx):
    p, nc, dr = cx.p, cx.nc, cx.dram
    S = Scope(p)
    identb = load_const(cx, S, "identb", "identb")
    btri = load_const(cx, S, "btri", "btri")
    bgt = load_const(cx, S, "bgt", "bgt")
    gnb, gntok = bcast_row(cx, S, dr["gla_norm"], 512, "gnb")
    yT = S.sb("gyT", [128, 16, T], BF16)
    St = S.sb("St", [128, 8, 512], F32)
    Sb = S.sb("Sb", [128, 8, 512], BF16)
    S1 = S.sb("S1", [128, 2, 512], F32)
    S1b = S.sb("S1b", [128, 2, 512], BF16)
    p.op("pool", lambda e: e.memset(St, 0.0), writes=toks("St", range(8)))
    p.op("pool", lambda e: e.memset(Sb, 0.0), writes=toks("Sb", range(8)))
    la = [S.sb("la", [128, 1024], F32) for _ in range(2)]
    ktm = [S.sb("ktm", [128, 1024], F32) for _ in range(2)]
    vt = [S.sb("gvt", [128, 2048], BF16) for _ in range(2)]
    sg = [S.sb("gsg", [128, 2048], F32) for _ in range(2)]
    qf = [S.sb("gqf", [128, 8, 128], F32) for _ in range(2)]
    kf = [S.sb("gkf", [128, 8, 128], F32) for _ in range(2)]
    eg = S.sb("eg", [128, 8, 128], F32)
    eng = S.sb("eng", [128, 8, 128], F32)
    egl = S.sb("egl", [128, 1024], F32)
    qin = S.sb("qin", [128, 8, 128], BF16)
    kin = S.sb("kin", [128, 8, 128], BF16)
    qA = S.sb("qA", [128, 8, 128], BF16)
    qB = S.sb("qB", [128, 8, 128], BF16)
    p.op("pool", lambda e: e.memset(qA, 0.0), writes=["qA"])
    p.op("pool", lambda e: e.memset(qB, 0.0), writes=["qB"])
    ke = S.sb("ke", [128, 1024], BF16)
    atm = S.sb("atm", [128, 128], BF16)
    junk = S.sb("gjunk", [128, 512], F32)
    on = S.sb("on", [128, 512], F32)
    ybf = S.sb("gybf", [128, 512], BF16)
    sm = S.sb("gsm", [128, 4], F32)
    GC = [S.ps("GC", [128, 512], F32) for _ in range(2)]
    GL = [S.ps("GL", [128, 512], F32) for _ in range(2)]
    ATp = S.ps("ATp", [128, 512], F32)
    Op = S.ps("Op", [128, 512], F32)
    Dp = S.ps("Dp", [128, 512], F32)
    tp = S.ps("gtp", [128, 512], BF16)
    qv = dr["gq_fm"].rearrange("(c p) t -> p c t", p=128)
    kv = dr["gk_fm"].rearrange("(c p) t -> p c t", p=128)

    def load(t):
        b = t % 2
        rs = slice(t * 128, (t + 1) * 128)
        p.dma(la[b], dr["gla_d"][rs, :], writes=[("la", b)])
        p.dma(ktm[b], dr["gk_tm"][rs, :], writes=[("ktm", b)])
        p.dma(vt[b], dr["gv_tm"][rs, :], writes=[("gvt", b)])
        p.dma(sg[b], dr["gsg"][rs, :], writes=[("gsg", b)])
        p.dma(qf[b], qv[:, :, rs], writes=[("gqf", b)])
        p.dma(kf[b], kv[:, :, rs], writes=[("gkf", b)])
    load(0)
    for t in range(NT):
        if t + 1 < NT:
            load(t + 1)
        b = t % 2
        cs = slice(t * 128, (t + 1) * 128)
        for hf in range(2):
            p.mm([(lambda e, j=j, hf=hf: e.matmul(GC[hf][:, j * 128:(j + 1) * 128], lhsT=la[b][:, (hf * 4 + j) * 128:(hf * 4 + j + 1) * 128], rhs=btri,
                                                  start=True, stop=True)) for j in range(4)], reads=[("la", b), "btri"], writes=[("GC", hf)])
            p.op("act", lambda e, hf=hf: e.activation(out=eg[:, hf * 4:(hf + 1) * 4, :], in_=GC[hf].rearrange("p (a c) -> p a c", a=4), func=AF.Exp),
                 reads=[("GC", hf)], writes=[("eg", hf)])
            p.op("act", lambda e, hf=hf: e.activation(out=eng[:, hf * 4:(hf + 1) * 4, :], in_=GC[hf].rearrange("p (a c) -> p a c", a=4), func=AF.Exp, scale=-1.0),
                 reads=[("GC", hf)], writes=[("eng", hf)])
            p.mm([lambda e, hf=hf: e.matmul(GL[hf][:, 0:512], lhsT=bgt, rhs=la[b][:, hf * 512:(hf + 1) * 512], start=True, stop=True)],
                 reads=[("la", b), "bgt"], writes=[("GL", hf)])
            p.op("act", lambda e, hf=hf: e.activation(out=egl[:, hf * 512:(hf + 1) * 512], in_=GL[hf][:, 0:512], func=AF.Exp), reads=[("GL", hf)], writes=[("egl", hf)])
        egt = [("eg", 0), ("eg", 1)]
        p.op("dve", lambda e: e.tensor_tensor(out=qin, in0=qf[b], in1=eg, op=ALU.mult), reads=[("gqf", b)] + egt, writes=["qin"])
        p.op("pool", lambda e: e.tensor_tensor(out=kin, in0=kf[b], in1=eng, op=ALU.mult), reads=[("gkf", b), ("eng", 0), ("eng", 1)], writes=["kin"])
        p.op("pool", lambda e: e.tensor_copy(out=qA[:, :, 0:64], in_=qin[:, :, 0:64]), reads=["qin"], writes=["qA"])
        p.op("pool", lambda e: e.tensor_copy(out=qB[:, :, 64:128], in_=qin[:, :, 64:128]), reads=["qin"], writes=["qB"])
        p.op("dve", lambda e: e.tensor_tensor(out=ke, in0=ktm[b], in1=egl, op=ALU.mult), reads=[("ktm", b), ("egl", 0), ("egl", 1)], writes=["ke"])
        for hd in range(4):
            vs = slice(hd * 512, (hd + 1) * 512)
            p.mm([(lambda e, dcl=dcl: e.matmul(ATp[:, 0:128], lhsT=kin[:, 2 * hd + dcl, :], rhs=qin[:, 2 * hd + dcl, :], start=(dcl == 0), stop=(dcl == 1)))
                  for dcl in range(2)], reads=["kin", "qin"], writes=["ATp"])
            p.op("dve", lambda e: e.tensor_tensor(out=atm, in0=ATp[:, 0:128], in1=btri, op=ALU.mult), reads=["ATp", "btri"], writes=["atm"])
            for dcl in range(2):
                dc = 2 * hd + dcl
                p.mm([lambda e, dc=dc: e.matmul(Dp[:, 0:512], lhsT=ke[0:64, dc * 128:(dc + 1) * 128], rhs=vt[b][0:64, vs], start=True, stop=True)],
                     reads=["ke", ("gvt", b)], writes=["Dp"])
                p.op("dve", lambda e, dc=dc, dcl=dcl: e.scalar_tensor_tensor(out=S1[:, dcl, :], in0=St[:, dc, :], scalar=eg[:, dc, 63:64], in1=Dp[:, 0:512],
                                                                            op0=ALU.mult, op1=ALU.add), reads=[("St", dc), "Dp"] + egt, writes=[("S1", dcl)])
                p.op("act", lambda e, dcl=dcl: e.activation(out=S1b[:, dcl, :], in_=S1[:, dcl, :], func=AF.Copy), reads=[("S1", dcl)], writes=[("S1b", dcl)])
            fns = [lambda e: e.matmul(Op[:, 0:512], lhsT=atm, rhs=vt[b][:, vs], start=True, stop=False)]
            for dcl in range(2):
                dc = 2 * hd + dcl
                fns.append(lambda e, dc=dc: e.matmul(Op[:, 0:512], lhsT=qA[:, dc, :], rhs=Sb[:, dc, :], start=False, stop=False))
                fns.append(lambda e, dc=dc, dcl=dcl: e.matmul(Op[:, 0:512], lhsT=qB[:, dc, :], rhs=S1b[:, dcl, :], start=False, stop=(dcl == 1)))
            p.mm(fns, reads=["atm", ("gvt", b), "qA", "qB", ("Sb", 2 * hd), ("Sb", 2 * hd + 1), ("S1b", 0), ("S1b", 1)], writes=["Op"])
            for dcl in range(2):
                dc = 2 * hd + dcl
                p.mm([lambda e, dc=dc: e.matmul(Dp[:, 0:512], lhsT=ke[64:128, dc * 128:(dc + 1) * 128], rhs=vt[b][64:128, vs], start=True, stop=True)],
                     reads=["ke", ("gvt", b)], writes=["Dp"])
                p.op("dve", lambda e, dc=dc, dcl=dcl: e.scalar_tensor_tensor(out=St[:, dc, :], in0=S1[:, dcl, :], scalar=eg[:, dc, 127:128], in1=Dp[:, 0:512],
                                                                            op0=ALU.mult, op1=ALU.add), reads=[("S1", dcl), "Dp"] + egt, writes=[("St", dc)])
                p.op("act", lambda e, dc=dc: e.activation(out=Sb[:, dc, :], in_=St[:, dc, :], func=AF.Copy), reads=[("St", dc)], writes=[("Sb", dc)])
            p.op("act", lambda e: e.activation(out=junk, in_=Op[:, 0:512], func=AF.Square, accum_out=sm[:, 0:1]), reads=["Op"], writes=["gjunk", "gsm"])
            p.op("dve", lambda e: e.tensor_scalar(out=sm[:, 1:2], in0=sm[:, 0:1], scalar1=1.0 / 512.0, scalar2=EPS, op0=ALU.mult, op1=ALU.add), reads=["gsm"], writes=["gsm"])
            p.op("act", lambda e: e.activation(out=sm[:, 2:3], in_=sm[:, 1:2], func=AF.Sqrt), reads=["gsm"], writes=["gsm"])
            p.op("dve", lambda e: e.reciprocal(out=sm[:, 3:4], in_=sm[:, 2:3]), reads=["gsm"], writes=["gsm"])
            p.op("dve", lambda e: e.scalar_tensor_tensor(out=on, in0=Op[:, 0:512], scalar=sm[:, 3:4], in1=gnb, op0=ALU.mult, op1=ALU.mult),
                 reads=["Op", "gsm", gntok], writes=["on"])
            p.op("pool", lambda e: e.tensor_tensor(out=ybf, in0=on, in1=sg[b][:, vs], op=ALU.mult), reads=["on", ("gsg", b)], writes=["gybf"])
            p.mm([(lambda e, j=j: e.transpose(out=tp[:, j * 128:(j + 1) * 128], in_=ybf[:, j * 128:(j + 1) * 128], identity=identb)) for j in range(4)],
                 reads=["gybf", "identb"], writes=["gtp"])
            p.op("act", lambda e: e.activation(out=yT[:, hd * 4:(hd + 1) * 4, cs], in_=tp[:, 0:512].rearrange("p (a c) -> p a c", a=4), func=AF.Copy),
                 reads=["gtp"], writes=[("gyT", t, hd)])
    for k in range(16):
        p.dma(dr["yT"][k * 128:(k + 1) * 128, :], yT[:, k, :], reads=[("gyT", t, k // 4) for t in range(NT)])
    S.close()


def stage_moe_local(cx, L, h_in, g_row, b_row, h_out, NE=32):
    p, nc, dr = cx.p, cx.nc, cx.dram
    S = Scope(p)
    ident = load_const(cx, S, "ident", "ident")
    mps = S.ps("emps", [128, 512], F32)
    nrow = NE * 16
    nch = (nrow + 127) // 128
    b1g = S.sb("b1g", [128, nrow], F32)
    b1l = S.sb("b1l", [128, nrow], F32)
    b1st = S.sb("b1st", [128, 256], F32)
    b1v = dr["moe_b1"][L * NE:(L + 1) * NE, :].rearrange("e (f c) -> (e f) c", c=256)
    for ch in range(nch):
        r = min(128, nrow - ch * 128)
        p.dma(b1st[0:r, :], b1v[ch * 128:ch * 128 + r, :], writes=["b1st"])
        p.mm([lambda e, r=r: e.transpose(out=mps[:, 0:r], in_=b1st[0:r, 0:256:2], identity=ident[0:r, 0:r]),
              lambda e, r=r: e.transpose(out=mps[:, 128:128 + r], in_=b1st[0:r, 1:256:2], identity=ident[0:r, 0:r])], reads=["b1st", "ident"], writes=["emps"])
        p.op("dve", lambda e, r=r, ch=ch: e.tensor_copy(out=b1g[:, ch * 128:ch * 128 + r], in_=mps[:, 0:r]), reads=["emps"], writes=["b1g"])
        p.op("dve", lambda e, r=r, ch=ch: e.tensor_copy(out=b1l[:, ch * 128:ch * 128 + r], in_=mps[:, 128:128 + r]), reads=["emps"], writes=["b1l"])
    hTg = S.sb("hTg", [128, KC, 1024], BF16)
    actT = S.sb("actT", [128, KC, 1024], BF16)
    acc = S.sb("acc", [128, 8, D], F32)
    gts = S.sb("gts", [128, 8, 32], F32)
    wst = [S.sb("wst", [128, KC, 256], F32) for _ in range(2)]
    wbf = [S.sb("wbf", [128, KC, 256], BF16) for _ in range(2)]
    b2t = [S.sb("b2t", [128, 256], F32) for _ in range(2)]
    ga = [S.sb("ga", [128, 512], F32) for _ in range(2)]
    sg = [S.sb("sg", [128, 512], F32)] * 2
    la = [S.sb("la", [128, 512], F32)] * 2
    yb = [S.sb("yb", [128, 256], F32) for _ in range(2)]
    hld = S.sb("hld", [128, D], F32)
    sm = S.sb("sm", [128, 8], F32)
    Gp = [S.ps("Gp", [128, 512], F32) for _ in range(2)]
    Lp = [S.ps("Lp", [128, 512], F32) for _ in range(2)]
    Yp = [S.ps("Yp", [128, 512], F32) for _ in range(2)]
    st_ = {"n": 0, "it": 0}

    def fetch(w2d, r0, c0, deint):
        b = st_["n"] % 2
        st_["n"] += 1
        src = w2d[r0:r0 + D, c0:c0 + 256].rearrange("(k p) n -> p k n", p=128)
        p.dma(wst[b], src, writes=[("wst", b)])
        if deint:
            sv = wst[b].rearrange("p k (i two) -> p k i two", two=2)
            p.op("act", lambda e, b=b, sv=sv: e.activation(out=wbf[b][:, :, 0:128], in_=sv[:, :, :, 0], func=AF.Copy), reads=[("wst", b)], writes=[("wbf", b, 0)])
            p.op("pool", lambda e, b=b, sv=sv: e.tensor_copy(out=wbf[b][:, :, 128:256], in_=sv[:, :, :, 1]), reads=[("wst", b)], writes=[("wbf", b, 1)])
        else:
            p.op("act", lambda e, b=b: e.activation(out=wbf[b][:, 0:8, :], in_=wst[b][:, 0:8, :], func=AF.Copy), reads=[("wst", b)], writes=[("wbf", b, 0)])
            p.op("pool", lambda e, b=b: e.tensor_copy(out=wbf[b][:, 8:16, :], in_=wst[b][:, 8:16, :]), reads=[("wst", b)], writes=[("wbf", b, 1)])
        return b
    hv = h_in.rearrange("(t p) d -> t p d", p=128)
    ov = h_out.rearrange("(t p) d -> t p d", p=128)
    for gi in range(2):
        p.dma(hTg, dr["hTo"].rearrange("(k p) t -> p k t", p=128)[:, :, gi * 1024:(gi + 1) * 1024], writes=["hTg"])
        p.dma(gts, dr["gateo"][gi * 1024:(gi + 1) * 1024, :].rearrange("(t p) e -> p t e", p=128), writes=["gts"])
        for e_ in range(NE):
            r0 = (L * NE + e_) * D
            nb = fetch(dr["moe_w1"], r0, 0, True)
            for fc in range(16):
                b1 = nb
                if fc + 1 < 16:
                    nb = fetch(dr["moe_w1"], r0, (fc + 1) * 256, True)
                else:
                    nb = fetch(dr["moe_w2"], r0, 0, False)
                wt = [("wbf", b1, 0), ("wbf", b1, 1)]
                for tb in range(2):
                    i2 = st_["it"] % 2
                    st_["it"] += 1
                    tsl = slice(tb * 512, (tb + 1) * 512)
                    p.mm([(lambda e, c=c: e.matmul(Gp[i2][:, 0:512], lhsT=wbf[b1][:, c, 0:128], rhs=hTg[:, c, tsl], start=(c == 0), stop=(c == KC - 1)))
                          for c in range(KC)], reads=wt + ["hTg"], writes=[("Gp", i2)])
                    p.mm([(lambda e, c=c: e.matmul(Lp[i2][:, 0:512], lhsT=wbf[b1][:, c, 128:256], rhs=hTg[:, c, tsl], start=(c == 0), stop=(c == KC - 1)))
                          for c in range(KC)], reads=wt + ["hTg"], writes=[("Lp", i2)])
                    col = e_ * 16 + fc
                    p.op("dve", lambda e: e.tensor_scalar(out=ga[i2], in0=Gp[i2][:, 0:512], scalar1=b1g[:, col:col + 1], scalar2=7.0, op0=ALU.add, op1=ALU.min),
                         reads=[("Gp", i2), "b1g"], writes=[("ga", i2)])
                    p.op("act", lambda e: e.activation(out=sg[i2], in_=ga[i2], func=AF.Sigmoid, scale=1.702), reads=[("ga", i2)], writes=["sg1"])
                    p.op("dve", lambda e: e.tensor_scalar(out=la[i2], in0=Lp[i2][:, 0:512], scalar1=b1l[:, col:col + 1], scalar2=7.0, op0=ALU.add, op1=ALU.min),
                         reads=[("Lp", i2), "b1l"], writes=["la1"])
                    p.op("dve", lambda e: e.tensor_scalar(out=la[i2], in0=la[i2], scalar1=-7.0, scalar2=1.0, op0=ALU.max, op1=ALU.add),
                         reads=["la1"], writes=["la1"])
                    p.op("pool", lambda e: e.tensor_tensor(out=ga[i2], in0=ga[i2], in1=sg[i2], op=ALU.mult), reads=[("ga", i2), "sg1"], writes=[("ga", i2)])
                    p.op("pool", lambda e: e.tensor_tensor(out=actT[:, fc, tsl], in0=ga[i2], in1=la[i2], op=ALU.mult), reads=[("ga", i2), "la1"],
                         writes=[("actT", fc, tb)])
            for dp in range(8):
                b2 = nb
                if dp + 1 < 8:
                    nb = fetch(dr["moe_w2"], r0, (dp + 1) * 256, False)
                wt = [("wbf", b2, 0), ("wbf", b2, 1)]
                dsl = slice(dp * 256, (dp + 1) * 256)
                bb = dp % 2
                p.dma(b2t[bb], dr["moe_b2"][L * NE + e_:L * NE + e_ + 1, dsl].partition_broadcast(128), writes=[("b2t", bb)])
                for t in range(8):
                    i2 = st_["it"] % 2
                    st_["it"] += 1
                    p.mm([(lambda e, c=c: e.matmul(Yp[i2][:, 0:256], lhsT=actT[:, c, t * 128:(t + 1) * 128], rhs=wbf[b2][:, c, :], start=(c == 0), stop=(c == KC - 1)))
                          for c in range(KC)], reads=wt + [("actT", c, t // 4) for c in range(KC)], writes=[("Yp", i2)])
                    p.op("dve", lambda e: e.tensor_tensor(out=yb[i2], in0=Yp[i2][:, 0:256], in1=b2t[bb], op=ALU.add), reads=[("Yp", i2), ("b2t", bb)],
                         writes=[("yb", i2)])
                    if e_ == 0:
                        p.op("dve", lambda e: e.tensor_scalar(out=acc[:, t, dsl], in0=yb[i2], scalar1=gts[:, t, e_:e_ + 1], scalar2=None, op0=ALU.mult),
                             reads=[("yb", i2), "gts"], writes=[("acc", t, dp)])
                    else:
                        p.op("dve", lambda e: e.scalar_tensor_tensor(out=acc[:, t, dsl], in0=yb[i2], scalar=gts[:, t, e_:e_ + 1], in1=acc[:, t, dsl],
                                                                    op0=ALU.mult, op1=ALU.add), reads=[("yb", i2), "gts", ("acc", t, dp)], writes=[("acc", t, dp)])
        gbc = wst[0].rearrange("p k c -> p (k c)")[:, 0:D]
        bbc = wst[1].rearrange("p k c -> p (k c)")[:, 0:D]
        gtok, btok = ("wst", 0), ("wst", 1)
        p.dma(gbc, g_row.partition_broadcast(128), writes=[gtok])
        p.dma(bbc, b_row.partition_broadcast(128), writes=[btok])
        for t in range(8):
            at = [("acc", t, dp) for dp in range(8)]
            p.dma(hld, hv[gi * 8 + t], writes=["hld"])
            p.op("dve", lambda e, t=t: e.scalar_tensor_tensor(out=acc[:, t, :], in0=hld, scalar=DN_ALPHA, in1=acc[:, t, :], op0=ALU.mult, op1=ALU.add),
                 reads=["hld"] + at, writes=at)
            ln_tile(cx, acc[:, t, :], ("acc", t, 0), hld, "hld", sm, "sm", gbc, gtok, bbc, btok)
            p.dma(ov[gi * 8 + t], acc[:, t, :], reads=at)
    S.close()


CAP = 256


def stage_moe_sparse(cx, L, h_in, g_row, b_row, h_out, NE=32):
    p, nc, dr = cx.p, cx.nc, cx.dram
    S = Scope(p)
    ident = load_const(cx, S, "ident", "ident")
    identb = load_const(cx, S, "identb", "identb")
    trius = load_const(cx, S, "trius", "trius")
    ones128 = load_const(cx, S, "ones128", "ones128")
    iota = load_const(cx, S, "iota", "iota")
    GA = [S.ps("GA", [128, 512], F32) for _ in range(2)]
    Gp = S.ps("Gp", [128, 512], F32)
    Lp = S.ps("Lp", [128, 512], F32)
    Yp = S.ps("Yp", [128, 512], F32)
    SC = S.ps("SC", [128, 512], F32)
    PT = [S.ps("PT", [128, 1024], BF16) for _ in range(2)]
    nrow = NE * 16
    nch = (nrow + 127) // 128
    b1g = S.sb("b1g", [128, nrow], F32)
    b1l = S.sb("b1l", [128, nrow], F32)
    b1st = S.sb("b1st", [128, 256], F32)
    b1v = dr["moe_b1"][L * NE:(L + 1) * NE, :].rearrange("e (f c) -> (e f) c", c=256)
    for ch in range(nch):
        r = min(128, nrow - ch * 128)
        p.dma(b1st[0:r, :], b1v[ch * 128:ch * 128 + r, :], writes=["b1st"])
        p.mm([lambda e, r=r: e.transpose(out=SC[:, 0:r], in_=b1st[0:r, 0:256:2], identity=ident[0:r, 0:r]),
              lambda e, r=r: e.transpose(out=SC[:, 128:128 + r], in_=b1st[0:r, 1:256:2], identity=ident[0:r, 0:r])], reads=["b1st", "ident"], writes=["SC"])
        p.op("dve", lambda e, r=r, ch=ch: e.tensor_copy(out=b1g[:, ch * 128:ch * 128 + r], in_=SC[:, 0:r]), reads=["SC"], writes=["b1g"])
        p.op("dve", lambda e, r=r, ch=ch: e.tensor_copy(out=b1l[:, ch * 128:ch * 128 + r], in_=SC[:, 128:128 + r]), reads=["SC"], writes=["b1l"])
    hb = S.sb("hb", [128, 8, D], BF16)
    acc = S.sb("acc", [128, 8, D], F32)
    gts = S.sb("gts", [128, 8, 32], F32)
    sel = S.sb("sel", [128, 8, 32], F32)
    rank = S.sb("rank", [128, 8, 32], F32)
    Pm = [S.sb("Pm", [128, 8, CAP], BF16)] * 2
    PG = S.sb("PG", [128, 8, CAP], BF16)
    PGT = S.sb("PGT", [128, 2, 8, 128], BF16)
    xgT = S.sb("xgT", [128, KC, CAP], BF16)
    actT = S.sb("actT", [128, KC, CAP], BF16)
    Yb = S.sb("Yb", [128, 2, D], BF16)
    wst = [S.sb("wst", [128, KC, 256], F32) for _ in range(2)]
    wbf = [S.sb("wbf", [128, KC, 256], BF16) for _ in range(2)]
    b2t = [S.sb("b2t", [128, 256], F32) for _ in range(2)]
    ga = [S.sb("ga", [128, CAP], F32) for _ in range(2)]
    sg = S.sb("sg", [128, CAP], F32)
    la = S.sb("la", [128, CAP], F32)
    hld = S.sb("hld", [128, D], F32)
    sm = S.sb("sm", [128, 8], F32)
    st_ = {"n": 0, "it": 0}

    def fetch(w2d, r0, c0, deint):
        b = st_["n"] % 2
        st_["n"] += 1
        src = w2d[r0:r0 + D, c0:c0 + 256].rearrange("(k p) n -> p k n", p=128)
        p.dma(wst[b], src, writes=[("wst", b)])
        if deint:
            sv = wst[b].rearrange("p k (i two) -> p k i two", two=2)
            p.op("act", lambda e, b=b, sv=sv: e.activation(out=wbf[b][:, :, 0:128], in_=sv[:, :, :, 0], func=AF.Copy), reads=[("wst", b)], writes=[("wbf", b, 0)])
            p.op("pool", lambda e, b=b, sv=sv: e.tensor_copy(out=wbf[b][:, :, 128:256], in_=sv[:, :, :, 1]), reads=[("wst", b)], writes=[("wbf", b, 1)])
        else:
            p.op("act", lambda e, b=b: e.activation(out=wbf[b][:, 0:8, :], in_=wst[b][:, 0:8, :], func=AF.Copy), reads=[("wst", b)], writes=[("wbf", b, 0)])
            p.op("pool", lambda e, b=b: e.tensor_copy(out=wbf[b][:, 8:16, :], in_=wst[b][:, 8:16, :]), reads=[("wst", b)], writes=[("wbf", b, 1)])
        return b
    hv = h_in.rearrange("(t p) d -> t p d", p=128)
    ov = h_out.rearrange("(t p) d -> t p d", p=128)
    for gi in range(2):
        for t in range(8):
            p.dma(hld, hv[gi * 8 + t], writes=["hld"])
            p.op("pool" if t % 2 else "act", (lambda e, t=t: e.tensor_copy(out=hb[:, t, :], in_=hld)) if t % 2 else
                 (lambda e, t=t: e.activation(out=hb[:, t, :], in_=hld, func=AF.Copy)), reads=["hld"], writes=[("hb", t)])
        p.dma(gts, dr["gateo"][gi * 1024:(gi + 1) * 1024, :].rearrange("(t p) e -> p t e", p=128), writes=["gts"])
        p.op("dve", lambda e: e.tensor_scalar(out=sel, in0=gts, scalar1=0.0, scalar2=0.0, op0=ALU.is_gt, op1=ALU.add), reads=["gts"], writes=["sel"])
        for t in range(8):
            fns = [lambda e, t=t: e.matmul(GA[0][:, 0:32], lhsT=trius, rhs=sel[:, t, :], start=True, stop=(t == 0))]
            for t2 in range(t):
                fns.append(lambda e, t2=t2, t=t: e.matmul(GA[0][:, 0:32], lhsT=ones128, rhs=sel[:, t2, :], start=False, stop=(t2 == t - 1)))
            p.mm(fns, reads=["sel", "trius", "ones128"], writes=[("GA", 0)])
            p.op("dve", lambda e, t=t: e.tensor_copy(out=rank[:, t, :], in_=GA[0][:, 0:32]), reads=[("GA", 0)], writes=["rank"])
        hbt = toks("hb", range(8))
        for e_ in range(NE):
            r0 = (L * NE + e_) * D
            nb = fetch(dr["moe_w1"], r0, 0, True)
            pb = 0
            for t in range(8):
                p.op("dve", lambda e, t=t: e.tensor_scalar(out=Pm[pb][:, t, :], in0=iota, scalar1=rank[:, t, e_:e_ + 1], scalar2=sel[:, t, e_:e_ + 1],
                                                        op0=ALU.is_equal, op1=ALU.mult), reads=["iota", "rank", "sel"], writes=[("Pm", pb)])
                p.op("dve", lambda e, t=t: e.tensor_scalar(out=PG[:, t, :], in0=iota, scalar1=rank[:, t, e_:e_ + 1], scalar2=gts[:, t, e_:e_ + 1],
                                                        op0=ALU.is_equal, op1=ALU.mult), reads=["iota", "rank", "gts"], writes=["PG"])
            for st in range(2):
                p.mm([(lambda e, t=t, st=st: e.transpose(out=PT[st][:, t * 128:(t + 1) * 128], in_=PG[:, t, st * 128:(st + 1) * 128], identity=identb))
                      for t in range(8)], reads=["PG", "identb"], writes=[("PT", st)])
                p.op("act", lambda e, st=st: e.activation(out=PGT[:, st, :, :], in_=PT[st].rearrange("p (a c) -> p a c", a=8), func=AF.Copy),
                     reads=[("PT", st)], writes=[("PGT", st)])
            for k2 in range(8):
                gb = k2 % 2
                fns = []
                for j in range(2):
                    kc = k2 * 2 + j
                    for t in range(8):
                        fns.append(lambda e, kc=kc, t=t, j=j: e.matmul(GA[gb][:, j * 256:(j + 1) * 256], lhsT=hb[:, t, kc * 128:(kc + 1) * 128], rhs=Pm[pb][:, t, :],
                                                                       start=(t == 0), stop=(t == 7)))
                p.mm(fns, reads=hbt + [("Pm", pb)], writes=[("GA", gb)])
                p.op("act" if k2 % 2 else "dve",
                     (lambda e, k2=k2, gb=gb: e.activation(out=xgT[:, k2 * 2:k2 * 2 + 2, :], in_=GA[gb].rearrange("p (a c) -> p a c", a=2), func=AF.Copy)) if k2 % 2 else
                     (lambda e, k2=k2, gb=gb: e.tensor_copy(out=xgT[:, k2 * 2:k2 * 2 + 2, :], in_=GA[gb].rearrange("p (a c) -> p a c", a=2))),
                     reads=[("GA", gb)], writes=[("xgT", k2)])
            xt = toks("xgT", range(8))
            for fc in range(16):
                b1 = nb
                if fc + 1 < 16:
                    nb = fetch(dr["moe_w1"], r0, (fc + 1) * 256, True)
                else:
                    nb = fetch(dr["moe_w2"], r0, 0, False)
                wt = [("wbf", b1, 0), ("wbf", b1, 1)]
                i2 = st_["it"] % 2
                st_["it"] += 1
                p.mm([(lambda e, c=c: e.matmul(Gp[:, 0:CAP], lhsT=wbf[b1][:, c, 0:128], rhs=xgT[:, c, :], start=(c == 0), stop=(c == KC - 1)))
                      for c in range(KC)], reads=wt + xt, writes=["Gp"])
                p.mm([(lambda e, c=c: e.matmul(Lp[:, 0:CAP], lhsT=wbf[b1][:, c, 128:256], rhs=xgT[:, c, :], start=(c == 0), stop=(c == KC - 1)))
                      for c in range(KC)], reads=wt + xt, writes=["Lp"])
                col = e_ * 16 + fc
                p.op("dve", lambda e: e.tensor_scalar(out=ga[i2], in0=Gp[:, 0:CAP], scalar1=b1g[:, col:col + 1], scalar2=7.0, op0=ALU.add, op1=ALU.min),
                     reads=["Gp", "b1g"], writes=[("ga", i2)])
                p.op("act", lambda e: e.activation(out=sg, in_=ga[i2], func=AF.Sigmoid, scale=1.702), reads=[("ga", i2)], writes=["sg"])
                p.op("dve", lambda e: e.tensor_scalar(out=la, in0=Lp[:, 0:CAP], scalar1=b1l[:, col:col + 1], scalar2=7.0, op0=ALU.add, op1=ALU.min),
                     reads=["Lp", "b1l"], writes=["la"])
                p.op("dve", lambda e: e.tensor_scalar(out=la, in0=la, scalar1=-7.0, scalar2=1.0, op0=ALU.max, op1=ALU.add), reads=["la"], writes=["la"])
                p.op("pool", lambda e: e.tensor_tensor(out=ga[i2], in0=ga[i2], in1=sg, op=ALU.mult), reads=[("ga", i2), "sg"], writes=[("ga", i2)])
                p.op("pool", lambda e: e.tensor_tensor(out=actT[:, fc, :], in0=ga[i2], in1=la, op=ALU.mult), reads=[("ga", i2), "la"], writes=[("actT", fc)])
            at_ = toks("actT", range(16))
            for dp in range(8):
                b2 = nb
                if dp + 1 < 8:
                    nb = fetch(dr["moe_w2"], r0, (dp + 1) * 256, False)
                wt = [("wbf", b2, 0), ("wbf", b2, 1)]
                dsl = slice(dp * 256, (dp + 1) * 256)
                bb = dp % 2
                p.dma(b2t[bb], dr["moe_b2"][L * NE + e_:L * NE + e_ + 1, dsl].partition_broadcast(128), writes=[("b2t", bb)])
                for st in range(2):
                    p.mm([(lambda e, c=c: e.matmul(Yp[:, 0:256], lhsT=actT[:, c, st * 128:(st + 1) * 128], rhs=wbf[b2][:, c, :], start=(c == 0), stop=(c == KC - 1)))
                          for c in range(KC)], reads=wt + at_, writes=["Yp"])
                    p.op("dve", lambda e, st=st: e.tensor_tensor(out=Yb[:, st, dsl], in0=Yp[:, 0:256], in1=b2t[bb], op=ALU.add), reads=["Yp", ("b2t", bb)],
                         writes=[("Yb", st, dp)])
            yt = [("Yb", st, dp) for st in range(2) for dp in range(8)]
            for t in range(8):
                for dq in range(4):
                    qs = slice(dq * 512, (dq + 1) * 512)
                    p.mm([(lambda e, st=st: e.matmul(SC[:, 0:512], lhsT=PGT[:, st, t, :], rhs=Yb[:, st, qs], start=(st == 0), stop=(st == 1))) for st in range(2)],
                         reads=yt + [("PGT", 0), ("PGT", 1)], writes=["SC"])
                    if e_ == 0:
                        p.op("dve", lambda e: e.tensor_copy(out=acc[:, t, qs], in_=SC[:, 0:512]), reads=["SC"], writes=[("acc", t, dq)])
                    else:
                        p.op("dve", lambda e: e.tensor_tensor(out=acc[:, t, qs], in0=SC[:, 0:512], in1=acc[:, t, qs], op=ALU.add), reads=["SC", ("acc", t, dq)],
                             writes=[("acc", t, dq)])
        gbc = wst[0].rearrange("p k c -> p (k c)")[:, 0:D]
        bbc = wst[1].rearrange("p k c -> p (k c)")[:, 0:D]
        gtok, btok = ("wst", 0), ("wst", 1)
        p.dma(gbc, g_row.partition_broadcast(128), writes=[gtok])
        p.dma(bbc, b_row.partition_broadcast(128), writes=[btok])
        for t in range(8):
            at = [("acc", t, dq) for dq in range(4)]
            p.dma(hld, hv[gi * 8 + t], writes=["hld"])
            p.op("dve", lambda e, t=t: e.scalar_tensor_tensor(out=acc[:, t, :], in0=hld, scalar=DN_ALPHA, in1=acc[:, t, :], op0=ALU.mult, op1=ALU.add),
                 reads=["hld"] + at, writes=at)
            ln_tile(cx, acc[:, t, :], ("acc", t, 0), hld, "hld", sm, "sm", gbc, gtok, bbc, btok)
            p.dma(ov[gi * 8 + t], acc[:, t, :], reads=at)
    S.close()


def moe_layer(cx, L, h_in, h_out):
    dr, p = cx.dram, cx.p
    stage_router(cx, L, h_in)
    p.allreduce(dr["GH"], dr["GH2"], reads=["GH"], writes=["GH2"])
    p.allreduce(dr["GG"], dr["GG2"], reads=["GG"], writes=["GG2"])
    stage_precast(cx, L)
    p.barrier()
    stage_experts(cx, L, dr["GH2"], dr["GG2"])
    p.allreduce(dr["PART"], dr["PART2"], reads=["PART"], writes=["PART2"])
    p.barrier()
    stage_combine_ln(cx, dr["PART2"], h_in, dr["ln2_g"][L:L + 1, :], dr["ln2_b"][L:L + 1, :], h_out)


def full_forward(cx):
    dr = cx.dram
    stage_l0_proj(cx, dr["x"])
    stage_l0_ssd(cx)
    stage_l0_att(cx)
    stage_outproj_ln(cx, dr["yT"], 32, dr["hyb_w_out"], dr["x"], dr["ln1_g"][0:1, :], dr["ln1_b"][0:1, :], dr["h1"])
    moe_layer(cx, 0, dr["h1"], dr["h2"])
    stage_gla_proj(cx, dr["h2"])
    stage_gla_core(cx)
    stage_outproj_ln(cx, dr["yT"][0:2048, :], 16, dr["gla_w_out"], dr["h2"], dr["ln1_g"][1:2, :], dr["ln1_b"][1:2, :], dr["h3"])
    moe_layer(cx, 1, dr["h3"], dr["out"])


CONST_NAMES = list(CONST_SHAPES.keys())


def _run(nc, in_maps):
    res = run_bass_kernel_spmd(nc, in_maps, core_ids=list(range(NCORES)))
    return res.results


def kernel_unfused(**inputs):
    x = np.asarray(inputs["x"], dtype=np.float32)
    f = lambda k: np.ascontiguousarray(np.asarray(inputs[k], dtype=np.float32))
    consts = make_consts()
    ln = {"ln1_g": f("ln1_g"), "ln1_b": f("ln1_b"), "ln2_g": f("ln2_g"), "ln2_b": f("ln2_b")}
    rt = {"moe_w_router": f("moe_w_router").reshape(2 * D, 32), "moe_b_router": f("moe_b_router")}
    hyb = {"hyb_w_in": f("hyb_w_in")[0], "hyb_conv_w": f("hyb_conv_w")[0], "hyb_conv_b": f("hyb_conv_b").reshape(1, 3072),
           "hyb_dt_bias": f("hyb_dt_bias").reshape(1, 32), "hyb_a_log": f("hyb_a_log").reshape(1, 32), "hyb_d": f("hyb_d").reshape(1, 32),
           "hyb_norm": f("hyb_norm").reshape(1, 2048), "hyb_w_out": f("hyb_w_out")[0]}
    gla = {"gla_w_in": f("gla_w_in")[0], "gla_w_gate2": f("gla_w_gate2")[0], "gla_b_gate": f("gla_b_gate").reshape(1, 1024),
           "gla_norm": f("gla_norm").reshape(1, 512), "gla_w_out": f("gla_w_out")[0]}
    w1, b1, w2, b2 = inputs["moe_w1"], inputs["moe_b1"], inputs["moe_w2"], inputs["moe_b2"]
    ohs = []
    for c in range(NCORES):
        oh = np.zeros((128, 8), np.float32)
        oh[:, c] = 1.0
        ohs.append(oh)

    def stA(cx):
        dr = cx.dram
        stage_l0_proj(cx, dr["x"])
        stage_l0_ssd(cx)
        stage_l0_att(cx)
        stage_outproj_ln(cx, dr["yT"], 32, dr["hyb_w_out"], dr["x"], dr["ln1_g"][0:1, :], dr["ln1_b"][0:1, :], dr["h1"])
        stage_router(cx, 0, dr["h1"], masked=False)
    need = ["x"] + list(hyb) + ["ln1_g", "ln1_b", "moe_w_router", "moe_b_router", "oh"]
    ncA, _ = build([stA], dbg=("h1", "hTo", "gateo"), needed_inputs=need)
    maps = [dict(hyb, **consts, **rt, x=np.ascontiguousarray(x[c]), ln1_g=ln["ln1_g"], ln1_b=ln["ln1_b"], oh=ohs[c]) for c in range(NCORES)]
    rA = _run(ncA, maps)
    h1 = [np.asarray(rA[c]["h1"]) for c in range(NCORES)]

    def stB(cx):
        dr = cx.dram
        stage_precast(cx, 0)
        stage_experts(cx, 0, dr["GH2"], dr["GG2"])
    needB = ["moe_w1", "moe_b1", "moe_w2", "moe_b2", "oh"]
    ncB, _ = build([stB], dbg=("PART",), needed_inputs=needB, ext_in=("GH2", "GG2"), moe_layers=1)

    def experts(L, rprev):
        GH = np.concatenate([np.asarray(rprev[c]["hTo"]) for c in range(NCORES)], axis=0)
        GG = np.concatenate([np.asarray(rprev[c]["gateo"]) for c in range(NCORES)], axis=0)
        maps = []
        for c in range(NCORES):
            e0 = 4 * c
            maps.append(dict(consts, GH2=GH, GG2=GG, oh=ohs[c],
                             moe_w1=np.ascontiguousarray(np.asarray(w1[L, e0:e0 + 4], dtype=np.float32)).reshape(4 * D, 2 * D),
                             moe_b1=np.ascontiguousarray(np.asarray(b1[L, e0:e0 + 4], dtype=np.float32)).reshape(4, 2 * D),
                             moe_w2=np.ascontiguousarray(np.asarray(w2[L, e0:e0 + 4], dtype=np.float32)).reshape(4 * D, D),
                             moe_b2=np.ascontiguousarray(np.asarray(b2[L, e0:e0 + 4], dtype=np.float32)).reshape(4, D)))
        rB = _run(ncB, maps)
        return [np.concatenate([np.asarray(rB[j]["PART"])[c * T:(c + 1) * T] for j in range(NCORES)], axis=0) for c in range(NCORES)]
    parts = experts(0, rA)

    def stC(cx):
        dr = cx.dram
        stage_combine_ln(cx, dr["PART2"], dr["h1"], dr["ln2_g"][0:1, :], dr["ln2_b"][0:1, :], dr["h2"], use_oh=False)
        stage_gla_proj(cx, dr["h2"])
        stage_gla_core(cx)
        stage_outproj_ln(cx, dr["yT"][0:2048, :], 16, dr["gla_w_out"], dr["h2"], dr["ln1_g"][1:2, :], dr["ln1_b"][1:2, :], dr["h3"])
        stage_router(cx, 1, dr["h3"], masked=False)
    need = list(gla) + ["ln1_g", "ln1_b", "ln2_g", "ln2_b", "moe_w_router", "moe_b_router", "oh"]
    ncC, _ = build([stC], dbg=("h3", "hTo", "gateo"), needed_inputs=need, ext_in=("PART2", "h1"))
    maps = [dict(gla, **consts, **rt, **ln, oh=ohs[c], PART2=parts[c], h1=h1[c]) for c in range(NCORES)]
    rC = _run(ncC, maps)
    h3 = [np.asarray(rC[c]["h3"]) for c in range(NCORES)]
    parts = experts(1, rC)

    def stE(cx):
        dr = cx.dram
        stage_combine_ln(cx, dr["PART2"], dr["h3"], dr["ln2_g"][1:2, :], dr["ln2_b"][1:2, :], dr["out"], use_oh=False)
    ncE, _ = build([stE], dbg=("out",), needed_inputs=["ln2_g", "ln2_b", "oh"], ext_in=("PART2", "h3"))
    maps = [dict(consts, oh=ohs[c], ln2_g=ln["ln2_g"], ln2_b=ln["ln2_b"], PART2=parts[c], h3=h3[c]) for c in range(NCORES)]
    rE = _run(ncE, maps)
    return np.stack([np.asarray(rE[c]["out"], dtype=np.float32) for c in range(NCORES)], axis=0)


def fused_forward(cx, NE=32, moe=None):
    moe = moe or stage_moe_sparse
    dr = cx.dram
    stage_l0_proj(cx, dr["x"])
    stage_l0_ssd(cx)
    stage_l0_att(cx)
    stage_outproj_ln(cx, dr["yT"], 32, dr["hyb_w_out"], dr["x"], dr["ln1_g"][0:1, :], dr["ln1_b"][0:1, :], dr["h1"])
    stage_router(cx, 0, dr["h1"], masked=False)
    moe(cx, 0, dr["h1"], dr["ln2_g"][0:1, :], dr["ln2_b"][0:1, :], dr["h2"], NE=NE)
    stage_gla_proj(cx, dr["h2"])
    stage_gla_core(cx)
    stage_outproj_ln(cx, dr["yT"][0:2048, :], 16, dr["gla_w_out"], dr["h2"], dr["ln1_g"][1:2, :], dr["ln1_b"][1:2, :], dr["h3"])
    stage_router(cx, 1, dr["h3"], masked=False)
    moe(cx, 1, dr["h3"], dr["ln2_g"][1:2, :], dr["ln2_b"][1:2, :], dr["out"], NE=NE)


def kernel(**inputs):
    x = np.asarray(inputs["x"], dtype=np.float32)
    f = lambda k: np.ascontiguousarray(np.asarray(inputs[k], dtype=np.float32))
    shared = {
        "hyb_w_in": f("hyb_w_in")[0], "hyb_conv_w": f("hyb_conv_w")[0], "hyb_conv_b": f("hyb_conv_b").reshape(1, 3072),
        "hyb_dt_bias": f("hyb_dt_bias").reshape(1, 32), "hyb_a_log": f("hyb_a_log").reshape(1, 32), "hyb_d": f("hyb_d").reshape(1, 32),
        "hyb_norm": f("hyb_norm").reshape(1, 2048), "hyb_w_out": f("hyb_w_out")[0],
        "gla_w_in": f("gla_w_in")[0], "gla_w_gate2": f("gla_w_gate2")[0], "gla_b_gate": f("gla_b_gate").reshape(1, 1024),
        "gla_norm": f("gla_norm").reshape(1, 512), "gla_w_out": f("gla_w_out")[0],
        "ln1_g": f("ln1_g"), "ln1_b": f("ln1_b"), "ln2_g": f("ln2_g"), "ln2_b": f("ln2_b"),
        "moe_w_router": f("moe_w_router").reshape(2 * D, 32), "moe_b_router": f("moe_b_router"),
        "moe_w1": f("moe_w1").reshape(2 * 32 * D, 2 * D), "moe_b1": f("moe_b1").reshape(2 * 32, 2 * D),
        "moe_w2": f("moe_w2").reshape(2 * 32 * D, D), "moe_b2": f("moe_b2").reshape(2 * 32, D),
        "oh": np.zeros((128, 8), np.float32),
    }
    shared.update(make_consts())
    in_maps = [dict(shared, x=np.ascontiguousarray(x[c])) for c in range(NCORES)]
    nc, cx = build([fused_forward], dbg=("out",), moe_experts=32)
    res = run_bass_kernel_spmd(nc, in_maps, core_ids=list(range(NCORES)))
    return np.stack([np.asarray(res.results[c]["out"], dtype=np.float32) for c in range(NCORES)], axis=0)
```

```python
import contextlib
import math
import numpy as np
import ml_dtypes
import concourse.bass as bass
import concourse.mybir as mybir
from concourse.bass_utils import run_bass_kernel_spmd

F32 = mybir.dt.float32
BF16 = mybir.dt.bfloat16
AF = mybir.ActivationFunctionType
ALU = mybir.AluOpType
AX = mybir.AxisListType

NCORES = 8
T = 2048
D = 2048
NT = T // 128
KC = D // 128
NEG = -1.0e30
DN_ALPHA = 4 ** 0.25
EPS = 1e-5
HYB_IN = 11296
GLA_IN = 6160


class Prog:
    RING = 8

    def __init__(self, nc):
        self.nc = nc
        self.E = {"pe": nc.tensor, "act": nc.scalar, "dve": nc.vector,
                  "pool": nc.gpsimd, "sp": nc.sync}
        self.sem = {}
        self.cnt = {}
        for k in self.E:
            self.sem[k] = nc.alloc_semaphore("s_" + k)
            self.cnt[k] = 0
        self.ring = {}
        self.ring_cnt = {}
        self.ring_next = {}
        for q in ("sp", "pool", "act"):
            self.ring[q] = [nc.alloc_semaphore(f"d_{q}{i}") for i in range(self.RING)]
            self.ring_cnt[q] = [0] * self.RING
            self.ring_next[q] = 0
        self.cc_sem = nc.alloc_semaphore("cc_sem")
        self.cc_cnt = 0
        self.seen = {k: {} for k in self.E}
        self.tok = {}
        self.nwaits = 0
        self.nops = 0

    def _semh(self, key):
        if key[0] == "e":
            return self.sem[key[1]]
        if key[0] == "c":
            return self.cc_sem
        return self.ring[key[1]][key[2]]

    def _wait(self, eng, ev):
        key, val = ev
        if key == ("e", "pe") and eng == "pe":
            return
        if self.seen[eng].get(key, 0) >= val:
            return
        self.E[eng].wait_ge(self._semh(key), val)
        self.seen[eng][key] = val
        self.nwaits += 1

    def _deps(self, reads, writes):
        deps = {}

        def add(k, v):
            if deps.get(k, 0) < v:
                deps[k] = v
        for t in reads:
            st = self.tok.get(t)
            if st and st["w"]:
                add(*st["w"])
        for t in writes:
            st = self.tok.get(t)
            if st:
                if st["w"]:
                    add(*st["w"])
                for k, v in st["r"].items():
                    add(k, v)
        return deps

    def _commit(self, ev, reads, writes):
        k, v = ev
        for t in reads:
            st = self.tok.setdefault(t, {"w": None, "r": {}})
            if st["r"].get(k, 0) < v:
                st["r"][k] = v
        for t in writes:
            self.tok[t] = {"w": ev, "r": {}}

    def op(self, eng, fn, reads=(), writes=()):
        for k, v in self._deps(reads, writes).items():
            self._wait(eng, (k, v))
        ins = fn(self.E[eng])
        self.cnt[eng] += 1
        ins.then_inc(self.sem[eng], 1)
        self._commit((("e", eng), self.cnt[eng]), reads, writes)
        self.nops += 1
        return ins

    def mm(self, fns, reads=(), writes=()):
        for k, v in self._deps(reads, writes).items():
            self._wait("pe", (k, v))
        ins = None
        for fn in fns:
            ins = fn(self.E["pe"])
        self.cnt["pe"] += 1
        ins.then_inc(self.sem["pe"], 1)
        self._commit((("e", "pe"), self.cnt["pe"]), reads, writes)
        self.nops += len(fns)

    def dma(self, out, in_, reads=(), writes=(), q="sp", **kw):
        i = self.ring_next[q]
        self.ring_next[q] = (i + 1) % self.RING
        key = ("d", q, i)
        if self.ring_cnt[q][i] > 0:
            self._wait(q, (key, 16 * self.ring_cnt[q][i]))
        for k, v in self._deps(reads, writes).items():
            self._wait(q, (k, v))
        ins = self.E[q].dma_start(out=out, in_=in_, **kw)
        self.ring_cnt[q][i] += 1
        ins.then_inc(self.ring[q][i], 16)
        self._commit((key, 16 * self.ring_cnt[q][i]), reads, writes)
        self.nops += 1
        return ins

    def allreduce(self, in_ap, out_ap, reads=(), writes=()):
        for k, v in self._deps(reads, writes).items():
            self._wait("pool", (k, v))
        ins = self.nc.gpsimd.collective_compute(
            "AllReduce", ALU.add, replica_groups=[list(range(NCORES))],
            ins=[in_ap.opt()], outs=[out_ap.opt()])
        self.cc_cnt += 1
        ins.then_inc(self.cc_sem)
        self._commit((("c",), self.cc_cnt), reads, writes)

    def barrier(self):
        evs = []
        for k in self.E:
            if self.cnt[k]:
                evs.append((("e", k), self.cnt[k]))
        for q in self.ring:
            for i in range(self.RING):
                if self.ring_cnt[q][i]:
                    evs.append((("d", q, i), 16 * self.ring_cnt[q][i]))
        if self.cc_cnt:
            evs.append((("c",), self.cc_cnt))
        for eng in self.E:
            for ev in evs:
                if ev[0] == ("e", eng):
                    continue
                self._wait(eng, ev)
        self.tok = {}


_UID = [0]


class Scope:
    def _name(self, name):
        _UID[0] += 1
        return f"{name}_{_UID[0]}"

    def __init__(self, p):
        self.p = p
        self.nc = p.nc
        self.es = contextlib.ExitStack()
        self.n = 0

    def sb(self, name, shape, dt=F32):
        self.n += 1
        h = self.es.enter_context(self.nc.sbuf_tensor(self._name(name), list(shape), dt))
        return h.ap()

    def ps(self, name, shape, dt=F32):
        self.n += 1
        h = self.es.enter_context(self.nc.psum_tensor(self._name(name), list(shape), dt))
        return h.ap()

    def close(self):
        self.p.barrier()
        self.es.close()


def make_consts():
    c = {}
    c["ident"] = np.eye(128, dtype=np.float32)
    c["identb"] = np.eye(128).astype(ml_dtypes.bfloat16)
    r = np.arange(128)
    c["triu"] = (r[:, None] <= r[None, :]).astype(np.float32)
    c["maskneg"] = np.where(r[None, :] < r[:, None], NEG, 0.0).astype(np.float32)
    sel = np.zeros((32, 32, 128), np.float32)
    for h in range(32):
        sel[h, h, :] = 1.0
    c["sel"] = sel.reshape(32, 32 * 128)
    rt = np.zeros((128, 128), np.float32)
    for m in range(64):
        rt[m + 64, m] = -1.0
    for m in range(64, 128):
        rt[m - 64, m] = 1.0
    c["rt"] = rt
    inv = np.exp(-math.log(10000.0) * np.arange(64, dtype=np.float32) / 64).astype(np.float32)
    ang = np.arange(T, dtype=np.float32)[None, :] * inv[:, None]
    c["cosT"] = np.concatenate([np.cos(ang), np.cos(ang)], 0).astype(np.float32)
    c["sinT"] = np.concatenate([np.sin(ang), np.sin(ang)], 0).astype(np.float32)
    c["causal"] = np.where(r[None, :] <= r[:, None], 0.0, NEG).astype(np.float32)
    same = (r[:, None] // 64) == (r[None, :] // 64)
    c["btri"] = (same & (r[:, None] <= r[None, :])).astype(np.float32)
    c["bgt"] = (same & (r[:, None] > r[None, :])).astype(np.float32)
    c["ones"] = np.ones((1, 128), np.float32)
    ls = np.zeros((128, 128), np.float32)
    ls[127, :] = 1.0
    c["lastsel"] = ls
    c["trius"] = (r[:, None] < r[None, :]).astype(np.float32)
    c["ones128"] = np.ones((128, 128), np.float32)
    c["iota"] = np.tile(np.arange(256, dtype=np.float32)[None, :], (128, 1))
    return c


CONST_SHAPES = {"ident": ([128, 128], F32), "identb": ([128, 128], BF16), "triu": ([128, 128], F32),
                "maskneg": ([128, 128], F32), "sel": ([32, 4096], F32), "rt": ([128, 128], F32),
                "cosT": ([128, T], F32), "sinT": ([128, T], F32), "causal": ([128, 128], F32),
                "btri": ([128, 128], F32), "bgt": ([128, 128], F32), "ones": ([1, 128], F32), "lastsel": ([128, 128], F32), "trius": ([128, 128], F32),
                "ones128": ([128, 128], F32), "iota": ([128, 256], F32)}


class Ctx:
    pass


def load_const(cx, S, name, tokname=None):
    shape, dt = CONST_SHAPES[name]
    t = S.sb("c_" + name, shape, dt)
    cx.p.dma(t, cx.dram[name], writes=[tokname or ("c", name, id(S))])
    return t


def bcast_row(cx, S, row_ap, n, name):
    t = S.sb(name, [128, n], F32)
    cx.p.dma(t, row_ap.partition_broadcast(128), writes=[("bc", name, id(S))])
    return t, ("bc", name, id(S))


def toks(base, rng):
    return [(base, i) for i in rng]


def transpose_in(cx, S, src_tm, dstT, dst_tok, ident, ident_tok, nt=NT, t0=0):
    p = cx.p
    ld = [S.sb("tin_ld", [128, D], F32) for _ in range(2)]
    ps = [S.ps("tin_ps", [128, 512], F32) for _ in range(2)]
    src = src_tm.rearrange("(t p) d -> t p d", p=128)
    k = 0
    for t in range(nt):
        b = ld[t % 2]
        p.dma(b, src[t0 + t], writes=[("tin_ld", t % 2)])
        for g in range(4):
            pp = ps[k % 2]
            ptok = ("tin_ps", k % 2)
            p.mm([(lambda e, pp=pp, b=b, g=g, j=j: e.transpose(out=pp[:, j * 128:(j + 1) * 128],
                                                              in_=b[:, (g * 4 + j) * 128:(g * 4 + j + 1) * 128],
                                                              identity=ident)) for j in range(4)],
                 reads=[("tin_ld", t % 2), ident_tok], writes=[ptok])
            out = dstT[:, g * 4:(g + 1) * 4, t * 128:(t + 1) * 128]
            src_ps = pp.rearrange("p (a b) -> p a b", a=4)
            if k % 2 == 0:
                p.op("dve", lambda e, out=out, s=src_ps: e.tensor_copy(out=out, in_=s), reads=[ptok], writes=[(dst_tok, t, g)])
            else:
                p.op("act", lambda e, out=out, s=src_ps: e.activation(out=out, in_=s, func=AF.Copy), reads=[ptok], writes=[(dst_tok, t, g)])
            k += 1


def xtoks(xtok, tiles):
    return [(xtok, t, g) for t in tiles for g in range(4)]


def load_cols(cx, S, vec_ap2d, r, name, ident, ident_tok, ps, pstok, ncol=128):
    p = cx.p
    st = S.sb(name + "_st", [r, ncol], F32)
    out = S.sb(name, [128, r], F32)
    p.dma(st, vec_ap2d, writes=[(name, "st")])
    p.mm([lambda e: e.transpose(out=ps[0:ncol, 0:r], in_=st, identity=ident[0:r, 0:r])],
         reads=[(name, "st"), ident_tok], writes=[pstok])
    p.op("dve", lambda e: e.tensor_copy(out=out[0:ncol, :], in_=ps[0:ncol, 0:r]), reads=[pstok], writes=[(name,)])
    return out, (name,)


class WStream:
    def __init__(self, cx, S, kc, pw, name):
        self.cx, self.kc, self.pw, self.name = cx, kc, pw, name
        self.st = [S.sb(name + "_st", [128, kc, pw], F32) for _ in range(2)]
        self.bf = [S.sb(name + "_bf", [128, kc, pw], BF16) for _ in range(2)]
        self.i = 0

    def fetch(self, w2d, c0, w):
        p = self.cx.p
        b = self.i % 2
        self.i += 1
        st, bf = self.st[b], self.bf[b]
        stok, btok = (self.name, "st", b), (self.name, "bf", b)
        src = w2d.rearrange("(k p) n -> p k n", p=128)[:, :, c0:c0 + w]
        p.dma(st[:, :, 0:w], src, writes=[stok])
        p.op("pool", lambda e: e.tensor_copy(out=bf[:, :, 0:w], in_=st[:, :, 0:w]), reads=[stok], writes=[btok])
        return bf, btok


def gemm_panels(cx, ws, w2d, panels, xT, xtok, mode, ps_list, epilogue, kc=KC, nt=NT):
    p = cx.p
    nxt = ws.fetch(w2d, panels[0][0], panels[0][1])
    k = 0
    for pi, (c0, w, tag) in enumerate(panels):
        bf, btok = nxt
        if pi + 1 < len(panels):
            nxt = ws.fetch(w2d, panels[pi + 1][0], panels[pi + 1][1])
        if mode == "tm":
            for t in range(nt):
                ps, ptok = ps_list[k % len(ps_list)]
                k += 1
                p.mm([(lambda e, c=c, ps=ps, t=t: e.matmul(ps[:, 0:w], lhsT=xT[:, c, t * 128:(t + 1) * 128], rhs=bf[:, c, 0:w],
                                                         start=(c == 0), stop=(c == kc - 1))) for c in range(kc)],
                     reads=[btok] + xtoks(xtok, [t]), writes=[ptok])
                epilogue(tag, c0, w, t, ps, ptok)
        else:
            nj = max(1, w // 128)
            m = 128
            for j in range(nj):
                for tb in range(nt // 4):
                    ps, ptok = ps_list[k % len(ps_list)]
                    k += 1
                    p.mm([(lambda e, c=c, ps=ps, tb=tb, j=j: e.matmul(ps[0:m, 0:512], lhsT=bf[:, c, j * 128:j * 128 + m],
                                                                     rhs=xT[:, c, tb * 512:(tb + 1) * 512],
                                                                     start=(c == 0), stop=(c == kc - 1))) for c in range(kc)],
                         reads=[btok] + xtoks(xtok, range(tb * 4, tb * 4 + 4)), writes=[ptok])
                    epilogue(tag, c0 + j * 128, m, tb, ps, ptok)


def stage_l0_proj(cx, h_tm):
    p, nc, dr = cx.p, cx.nc, cx.dram
    S = Scope(p)
    ident = load_const(cx, S, "ident", "ident")
    rt = load_const(cx, S, "rt", "rt")
    cosT = load_const(cx, S, "cosT", "cosT")
    sinT = load_const(cx, S, "sinT", "sinT")
    xT = S.sb("xT", [128, KC, T], BF16)
    S2 = Scope(p)
    transpose_in(cx, S2, h_tm, xT, "xT", ident, "ident")
    S2.close()
    w_in = dr["hyb_w_in"]
    ws = WStream(cx, S, KC, 256, "win")
    gps = [(S.ps("gps", [128, 512], F32), ("gps", i)) for i in range(3)]
    tps = [(S.ps("tps", [128, 512], F32), ("tps", i)) for i in range(2)]
    rps, rtok = S.ps("rps", [128, 512], F32), "rps"
    mps, mtok = S.ps("mps", [128, 512], F32), "mps"
    cwst = S.sb("cwst", [4, 3072], F32)
    p.dma(cwst, dr["hyb_conv_w"], writes=["cwst"])
    cw = S.sb("cw", [128, 24, 4], F32)
    p.mm([(lambda e, c=c: e.transpose(out=mps[:, c * 4:(c + 1) * 4], in_=cwst[0:4, c * 128:(c + 1) * 128], identity=ident[0:4, 0:4]))
          for c in range(24)], reads=["cwst", "ident"], writes=[mtok])
    p.op("dve", lambda e: e.tensor_copy(out=cw.rearrange("p a b -> p (a b)"), in_=mps[:, 0:96]), reads=[mtok], writes=["cw"])
    cb, cbtok = load_cols(cx, S, dr["hyb_conv_b"].rearrange("o (c p) -> (o c) p", p=128), 24, "cb", ident, "ident", mps, mtok)
    dtb, dtbtok = bcast_row(cx, S, dr["hyb_dt_bias"], 32, "dtb")
    abc, abctok = bcast_row(cx, S, dr["hyb_a_log"], 32, "abc")
    p.op("act", lambda e: e.activation(out=abc, in_=abc, func=AF.Exp), reads=[abctok], writes=[abctok])
    p.op("dve", lambda e: e.tensor_scalar(out=abc, in0=abc, scalar1=-1.0, scalar2=None, op0=ALU.mult), reads=[abctok], writes=[abctok])
    cin = S.sb("cin", [128, 3 + T], F32)
    p.op("pool", lambda e: e.memset(cin[:, 0:3], 0.0), writes=["cin_pad"])
    wa = S.sb("wa", [128, T], F32)
    wb = S.sb("wb", [128, T], F32)
    wc = S.sb("wc", [128, T], F32)
    obf = [S.sb("obf", [128, T], BF16) for _ in range(2)]
    tmst = S.sb("tmst", [128, NT, 128], F32)
    tmstb = S.sb("tmstb", [128, NT, 128], BF16)
    zst = [S.sb("zst", [128, 256], F32) for _ in range(2)]
    vst = [S.sb("vst", [128, 256], BF16) for _ in range(2)]
    dts = S.sb("dts", [128, NT, 32], F32)
    adts = S.sb("adts", [128, NT, 32], F32)
    sp1 = S.sb("sp1", [128, 32], F32)
    sp2 = S.sb("sp2", [128, 32], F32)
    kmean = S.sb("kmean", [128, 16, 8], F32)
    gst = S.sb("gst", [128, NT, 8], F32)
    cnt = {"z": 0, "v": 0, "o": 0, "t": 0}

    def transposes_to(dst3, dtok, src, stok):
        for g in range(4):
            ps, ptok = tps[cnt["t"] % 2]
            cnt["t"] += 1
            p.mm([(lambda e, j=j, ps=ps, g=g: e.transpose(out=ps[:, j * 128:(j + 1) * 128], in_=src[:, (g * 4 + j) * 128:(g * 4 + j + 1) * 128],
                                                        identity=ident)) for j in range(4)], reads=[stok, "ident"], writes=[ptok])
            p.op("act", lambda e, ps=ps, g=g: e.activation(out=dst3[:, g * 4:(g + 1) * 4, :], in_=ps.rearrange("p (a b) -> p a b", a=4), func=AF.Copy),
                 reads=[ptok], writes=[dtok])

    def ep(tag, c0, w, idx, ps, ptok):
        if tag == "z":
            b = cnt["z"] % 2
            cnt["z"] += 1
            p.op("act", lambda e: e.activation(out=zst[b][:, 0:w], in_=ps[:, 0:w], func=AF.Silu), reads=[ptok], writes=[("zst", b)])
            p.dma(dr["sz"][idx * 128:(idx + 1) * 128, c0:c0 + w], zst[b][:, 0:w], reads=[("zst", b)])
        elif tag == "v":
            b = cnt["v"] % 2
            cnt["v"] += 1
            p.op("dve", lambda e: e.tensor_copy(out=vst[b][:, 0:w], in_=ps[:, 0:w]), reads=[ptok], writes=[("vst", b)])
            p.dma(dr["v_tm"][idx * 128:(idx + 1) * 128, c0 - 9248:c0 - 9248 + w], vst[b][:, 0:w], reads=[("vst", b)])
        elif tag == "dt":
            t = idx
            p.op("dve", lambda e: e.tensor_tensor(out=sp1, in0=ps[:, 0:32], in1=dtb, op=ALU.add), reads=[ptok, dtbtok], writes=["sp1"])
            p.op("act", lambda e: e.activation(out=sp2, in_=sp1, func=AF.Abs), reads=["sp1"], writes=["sp2"])
            p.op("act", lambda e: e.activation(out=sp2, in_=sp2, func=AF.Exp, scale=-1.0), reads=["sp2"], writes=["sp2"])
            p.op("act", lambda e: e.activation(out=sp2, in_=sp2, func=AF.Ln, bias=1.0), reads=["sp2"], writes=["sp2"])
            p.op("dve", lambda e: e.scalar_tensor_tensor(out=dts[:, t, :], in0=sp1, scalar=0.0, in1=sp2, op0=ALU.max, op1=ALU.add),
                 reads=["sp1", "sp2"], writes=["dts"])
            p.op("dve", lambda e: e.tensor_tensor(out=adts[:, t, :], in0=dts[:, t, :], in1=abc, op=ALU.mult), reads=["dts", abctok], writes=["adts"])
            if t == NT - 1:
                p.dma(dr["dt_tm"].rearrange("(t p) h -> p t h", p=128), dts, reads=["dts"])
                p.dma(dr["adt_tm"].rearrange("(t p) h -> p t h", p=128), adts, reads=["adts"])
        elif tag == "xbc":
            tb = idx
            ci = (c0 - 2048) // 128
            p.op("act", lambda e: e.activation(out=cin[:, 3 + tb * 512:3 + (tb + 1) * 512], in_=ps[:, 0:512], func=AF.Copy),
                 reads=[ptok], writes=[("cin", tb)])
            if tb < 3:
                return
            ctoks = toks("cin", range(4)) + ["cin_pad"]
            p.op("dve", lambda e: e.tensor_scalar(out=wa, in0=cin[:, 0:T], scalar1=cw[:, ci, 0:1], scalar2=None, op0=ALU.mult),
                 reads=ctoks + ["cw"], writes=["wa"])
            for j in range(1, 4):
                p.op("dve", lambda e, j=j: e.scalar_tensor_tensor(out=wa, in0=cin[:, j:j + T], scalar=cw[:, ci, j:j + 1], in1=wa,
                                                                 op0=ALU.mult, op1=ALU.add), reads=ctoks + ["cw", "wa"], writes=["wa"])
            p.op("act", lambda e: e.activation(out=wb, in_=wa, func=AF.Silu, bias=cb[:, ci:ci + 1], scale=1.0), reads=["wa", cbtok], writes=["wb"])
            if ci < 16:
                transposes_to(tmst, "tmst", wb, "wb")
                p.dma(dr["xs_tm"].rearrange("(t p) c -> p t c", p=128)[:, :, ci * 128:(ci + 1) * 128], tmst, reads=["tmst"])
            else:
                b = cnt["o"] % 2
                cnt["o"] += 1
                p.op("pool", lambda e: e.tensor_copy(out=obf[b], in_=wb), reads=["wb"], writes=[("obf", b)])
                g = (ci - 16) % 4
                dst = dr["B_fm"] if ci < 20 else dr["C_fm"]
                p.dma(dst[g * 128:(g + 1) * 128, :], obf[b], reads=[("obf", b)])
                if ci < 20:
                    transposes_to(tmstb, "tmstb", wb, "wb")
                    p.dma(dr["B_tm"].rearrange("(t p) n -> p t n", p=128)[:, :, g * 128:(g + 1) * 128], tmstb, reads=["tmstb"])
        elif tag in ("q", "k"):
            tb = idx
            base = 5152 if tag == "q" else 7200
            h = (c0 - base) // 128
            p.op("act", lambda e: e.activation(out=wa[:, tb * 512:(tb + 1) * 512], in_=ps[:, 0:512], func=AF.Copy), reads=[ptok], writes=[("waq", tb)])
            if tb < 3:
                return
            for b4 in range(4):
                sl = slice(b4 * 512, (b4 + 1) * 512)
                p.mm([lambda e, sl=sl: e.matmul(rps[:, 0:512], lhsT=rt, rhs=wa[:, sl], start=True, stop=True)], reads=[("waq", b4), "rt"], writes=[rtok])
                p.op("dve", lambda e, sl=sl: e.tensor_tensor(out=wb[:, sl], in0=rps[:, 0:512], in1=sinT[:, sl], op=ALU.mult), reads=[rtok, "sinT"], writes=[("wbq", b4)])
                p.op("pool", lambda e, sl=sl: e.tensor_tensor(out=wc[:, sl], in0=wa[:, sl], in1=cosT[:, sl], op=ALU.mult), reads=[("waq", b4), "cosT"], writes=[("wcq", b4)])
                p.op("pool", lambda e, sl=sl: e.tensor_tensor(out=wc[:, sl], in0=wc[:, sl], in1=wb[:, sl], op=ALU.add), reads=[("wcq", b4), ("wbq", b4)], writes=[("wcq", b4)])
            b = cnt["o"] % 2
            cnt["o"] += 1
            wctoks = toks("wcq", range(4))
            if tag == "k":
                p.op("act", lambda e: e.activation(out=obf[b], in_=wc, func=AF.Copy), reads=wctoks, writes=[("obf", b)])
                p.dma(dr["k_fm"][h * 128:(h + 1) * 128, :], obf[b], reads=[("obf", b)])
                p.op("dve", lambda e: e.tensor_reduce(out=kmean[:, h, :], in_=wc.rearrange("p (a b) -> p a b", a=8), axis=AX.X, op=ALU.add),
                     reads=wctoks, writes=[("kmean", h)])
                p.op("dve", lambda e: e.tensor_scalar(out=kmean[:, h, :], in0=kmean[:, h, :], scalar1=1.0 / 256.0, scalar2=None, op0=ALU.mult),
                     reads=[("kmean", h)], writes=[("kmean", h)])
            else:
                p.op("act", lambda e: e.activation(out=obf[b], in_=wc, func=AF.Copy, scale=128.0 ** -0.5), reads=wctoks, writes=[("obf", b)])
                p.dma(dr["q_fm"][h * 128:(h + 1) * 128, :], obf[b], reads=[("obf", b)])
                p.mm([(lambda e, t=t: e.matmul(mps[:, t * 8:(t + 1) * 8], lhsT=wc[:, t * 128:(t + 1) * 128], rhs=kmean[:, h, :], start=True, stop=True))
                      for t in range(NT)], reads=wctoks + [("kmean", h)], writes=[mtok])
                p.op("dve", lambda e: e.tensor_copy(out=gst.rearrange("p a b -> p (a b)"), in_=mps[:, 0:128]), reads=[mtok], writes=["gst"])
                p.dma(dr["gate_d"][h].rearrange("(t p) e -> p t e", p=128), gst, reads=["gst"])

    panels = [(2048 + i * 256, 256, "xbc") for i in range(12)]
    gemm_panels(cx, ws, w_in, panels, xT, "xT", "fm", gps, ep)
    p.barrier()
    gemm_panels(cx, ws, w_in, [(5120, 32, "dt")], xT, "xT", "tm", gps, ep)
    p.barrier()
    panels = [(7200 + i * 256, 256, "k") for i in range(8)] + [(5152 + i * 256, 256, "q") for i in range(8)]
    gemm_panels(cx, ws, w_in, panels, xT, "xT", "fm", gps, ep)
    p.barrier()
    panels = [(i * 256, 256, "z") for i in range(8)] + [(9248 + i * 256, 256, "v") for i in range(8)]
    gemm_panels(cx, ws, w_in, panels, xT, "xT", "tm", gps, ep)
    S.close()


INPUT_SHAPES = {
    "x": ([T, D], F32),
    "hyb_w_in": ([D, HYB_IN], F32), "hyb_conv_w": ([4, 3072], F32), "hyb_conv_b": ([1, 3072], F32),
    "hyb_dt_bias": ([1, 32], F32), "hyb_a_log": ([1, 32], F32), "hyb_d": ([1, 32], F32),
    "hyb_norm": ([1, 2048], F32), "hyb_w_out": ([4096, D], F32),
    "gla_w_in": ([D, GLA_IN], F32), "gla_w_gate2": ([16, 1024], F32), "gla_b_gate": ([1, 1024], F32),
    "gla_norm": ([1, 512], F32), "gla_w_out": ([D, D], F32),
    "ln1_g": ([2, D], F32), "ln1_b": ([2, D], F32), "ln2_g": ([2, D], F32), "ln2_b": ([2, D], F32),
    "moe_w_router": ([2 * D, 32], F32), "moe_b_router": ([2, 32], F32),
    "moe_w1": ([2 * 4 * D, 2 * D], F32), "moe_b1": ([2 * 4, 2 * D], F32),
    "moe_w2": ([2 * 4 * D, D], F32), "moe_b2": ([2 * 4, D], F32),
    "oh": ([128, 8], F32),
}

SCRATCH = {
    "sz": ([T, 2048], F32), "v_tm": ([T, 2048], BF16), "dt_tm": ([T, 32], F32), "adt_tm": ([T, 32], F32),
    "xs_tm": ([T, 2048], F32), "B_fm": ([512, T], BF16), "C_fm": ([512, T], BF16), "B_tm": ([T, 512], BF16),
    "q_fm": ([2048, T], BF16), "k_fm": ([2048, T], BF16), "gate_d": ([16, T, 8], F32),
    "yT": ([4096, T], BF16), "h1": ([T, D], F32), "h2": ([T, D], F32), "h3": ([T, D], F32),
    "GH": ([8 * D, T], BF16), "GH2": ([8 * D, T], BF16), "GG": ([8 * T, 32], F32), "GG2": ([8 * T, 32], F32),
    "w1b": ([4 * 16 * 128, 4096], BF16), "w2b": ([4 * 4 * 128, 8192], BF16),
    "PART": ([8 * T, D], F32), "PART2": ([8 * T, D], F32),
    "gq_fm": ([1024, T], F32), "gk_fm": ([1024, T], F32), "gk_tm": ([T, 1024], F32), "gv_tm": ([T, 2048], BF16),
    "gsg": ([T, 2048], F32), "gla_d": ([T, 1024], F32), "out": ([T, D], F32),
    "hTo": ([D, T], BF16), "gateo": ([T, 32], F32),
    "XG": ([32 * 128, 16 * 512], BF16), "PGTd": ([32 * 128, 4096], BF16), "YBd": ([32 * 128, 4 * 2048], BF16),
}


def build(stages, dbg=(), needed_inputs=None, ext_in=(), moe_layers=2, moe_experts=4):
    nc = bass.Bass("TRN2", target_bir_lowering=False)
    cx = Ctx()
    cx.nc = nc
    cx.dram = {}
    for k, (shape, dt) in INPUT_SHAPES.items():
        if needed_inputs is not None and k not in needed_inputs:
            continue
        if k in ("moe_w1", "moe_b1", "moe_w2", "moe_b2"):
            shape = [shape[0] // 4 * moe_experts, shape[1]]
            if moe_layers == 1:
                shape = [shape[0] // 2, shape[1]]
        cx.dram[k] = nc.dram_tensor(k, shape, dt, kind="ExternalInput").ap()
    for k, (shape, dt) in CONST_SHAPES.items():
        cx.dram[k] = nc.dram_tensor(k, shape, dt, kind="ExternalInput").ap()
    for k, (shape, dt) in SCRATCH.items():
        if k in ext_in:
            cx.dram[k] = nc.dram_tensor(k, shape, dt, kind="ExternalInput").ap()
        elif k in dbg:
            cx.dram[k] = nc.dram_tensor(k, shape, dt, kind="ExternalOutput").ap()
        else:
            cx.dram[k] = nc.dram_tensor(k, shape, dt).ap()
    cx.p = Prog(nc)
    for st in stages:
        st(cx)
    cx.p.barrier()
    return nc, cx


def stage_l0_ssd(cx):
    LV = 9
    p, nc, dr = cx.p, cx.nc, cx.dram
    S = Scope(p)
    ident = load_const(cx, S, "ident", "ident")
    identb = load_const(cx, S, "identb", "identb")
    triu = load_const(cx, S, "triu", "triu")
    maskneg = load_const(cx, S, "maskneg", "maskneg")
    sel = load_const(cx, S, "sel", "sel")
    lastsel = load_const(cx, S, "lastsel", "lastsel")
    if LV == -1:
        S.close()
        return
    Bf = S.sb("Bf", [128, 4, T], BF16)
    Cf = S.sb("Cf", [128, 4, T], BF16)
    Bt = S.sb("Bt", [128, NT, 512], BF16)
    dts = S.sb("dts", [128, NT, 32], F32)
    adts = S.sb("adts", [128, NT, 32], F32)
    p.dma(Bf, dr["B_fm"].rearrange("(g p) t -> p g t", p=128), writes=["Bf"])
    p.dma(Cf, dr["C_fm"].rearrange("(g p) t -> p g t", p=128), writes=["Cf"])
    p.dma(Bt, dr["B_tm"].rearrange("(t p) n -> p t n", p=128), writes=["Bt"])
    p.dma(dts, dr["dt_tm"].rearrange("(t p) h -> p t h", p=128), writes=["dts"])
    p.dma(adts, dr["adt_tm"].rearrange("(t p) h -> p t h", p=128), writes=["adts"])
    if LV == -2:
        S.close()
        return
    dbc, dbctok = bcast_row(cx, S, dr["hyb_d"], 32, "dbc")
    nw, nwtok = bcast_row(cx, S, dr["hyb_norm"], 2048, "nw")
    xs = [S.sb("xs", [128, 2048], F32) for _ in range(2)]
    szb = [S.sb("szb", [128, 2048], F32) for _ in range(2)]
    prev = S.sb("prev", [128, 4, 512], F32)
    prevb = S.sb("prevb", [128, 4, 512], BF16)
    p.op("pool", lambda e: e.memset(prev, 0.0), writes=["prev"])
    p.op("pool", lambda e: e.memset(prevb, 0.0), writes=["prevb"])
    if LV == -3:
        S.close()
        return
    adp = S.sb("adp", [128, 128], F32)
    p.op("pool", lambda e: e.memset(adp, 0.0), writes=["adp"])
    acum = S.sb("acum", [128, 32], F32)
    nacum = S.sb("nacum", [128, 32], F32)
    eac = S.sb("eac", [128, 32], F32)
    acf = S.sb("acf", [32, 128], F32)
    Dm = S.sb("Dm", [128, 8, 128], F32)
    cbt = S.sb("cbt", [128, 128], F32)
    M = S.sb("M", [128, 8, 128], BF16)
    xdt = S.sb("xdt", [128, 8, 64], BF16)
    xdte = S.sb("xdte", [128, 8, 64], BF16)
    t1 = S.sb("t1", [128, 512], F32)
    t2 = S.sb("t2", [128, 512], F32)
    ysb = S.sb("ysb", [128, 512], F32)
    junk = S.sb("junk", [128, 512], F32)
    ybf = S.sb("ybf", [128, 512], BF16)
    sm = S.sb("sm", [128, 16], F32)
    dte = S.sb("dte", [128, 32], F32)
    wde = S.sb("wde", [128, 32], F32)
    cd = S.sb("cd", [128, 32], F32)
    ptmp = S.sb("ptmp", [128, 512], F32)
    yT = S.sb("yTs", [128, 16, T], BF16)
    E = S.ps("E", [128, 1024], F32)
    aps = S.ps("aps", [128, 512], F32)
    cps = S.ps("cps", [128, 512], F32)
    Yd = S.ps("Yd", [128, 512], F32)
    Yo = S.ps("Yo", [128, 512], F32)
    Sp = S.ps("Sp", [128, 512], F32)
    tp = S.ps("tp", [128, 512], BF16)
    xsv = dr["xs_tm"].rearrange("(t p) c -> t p c", p=128)
    szv = dr["sz"].rearrange("(t p) c -> t p c", p=128)

    def load(c):
        p.dma(xs[c % 2], xsv[c], writes=[("xs", c % 2)])
        p.dma(szb[c % 2], szv[c], writes=[("szb", c % 2)])
    load(0)
    for c in range(NT if LV > 0 else 0):
        if c + 1 < NT:
            load(c + 1)
        X, Z = xs[c % 2], szb[c % 2]
        xtok, ztok = ("xs", c % 2), ("szb", c % 2)
        cs = slice(c * 128, (c + 1) * 128)
        p.mm([lambda e: e.matmul(aps[:, 0:32], lhsT=triu, rhs=adts[:, c, :], start=True, stop=True)], reads=["triu", "adts"], writes=["aps"])
        p.op("act", lambda e: e.activation(out=acum, in_=aps[:, 0:32], func=AF.Copy), reads=["aps"], writes=["acum"])
        p.op("dve", lambda e: e.tensor_scalar(out=nacum, in0=aps[:, 0:32], scalar1=-1.0, scalar2=None, op0=ALU.mult), reads=["aps"], writes=["nacum"])
        p.op("act", lambda e: e.activation(out=eac, in_=aps[:, 0:32], func=AF.Exp), reads=["aps"], writes=["eac"])
        p.op("dve", lambda e: e.tensor_copy(out=adp[:, 0:32], in_=adts[:, c, :]), reads=["adts"], writes=["adp"])
        p.mm([lambda e: e.matmul(aps[:, 128:256], lhsT=adp, rhs=triu, start=True, stop=True)], reads=["triu", "adp", "aps"], writes=["aps"])
        p.op("dve", lambda e: e.tensor_copy(out=acf, in_=aps[0:32, 128:256]), reads=["aps"], writes=["acf"])
        p.mm([lambda e: e.matmul(aps[:, 256:288], lhsT=lastsel, rhs=acum, start=True, stop=True)], reads=["lastsel", "acum", "aps"], writes=["aps"])
        p.op("dve", lambda e: e.tensor_tensor(out=dte, in0=aps[:, 256:288], in1=nacum, op=ALU.add), reads=["aps", "nacum"], writes=["dte"])
        p.op("act", lambda e: e.activation(out=dte, in_=dte, func=AF.Exp), reads=["dte"], writes=["dte"])
        p.op("act", lambda e: e.activation(out=cd, in_=aps[:, 256:288], func=AF.Exp), reads=["aps"], writes=["cd"])
        p.op("dve", lambda e: e.tensor_tensor(out=wde, in0=dte, in1=dts[:, c, :], op=ALU.mult), reads=["dte", "dts"], writes=["wde"])
        for g in range(4 if LV >= 2 else 0):
            hs = slice(g * 8, (g + 1) * 8)
            gs = slice(g * 512, (g + 1) * 512)
            fns = []
            for j in range(8):
                h = g * 8 + j
                fns.append(lambda e, j=j, h=h: e.matmul(E[:, j * 128:(j + 1) * 128], lhsT=sel[:, h * 128:(h + 1) * 128], rhs=acf, start=True, stop=False))
                fns.append(lambda e, j=j: e.matmul(E[:, j * 128:(j + 1) * 128], lhsT=ident, rhs=maskneg, start=False, stop=True))
            p.mm(fns, reads=["sel", "acf", "ident", "maskneg"], writes=["E"])
            for j in range(8):
                h = g * 8 + j
                p.op("act", lambda e, j=j, h=h: e.activation(out=Dm[:, j, :], in_=E[:, j * 128:(j + 1) * 128], func=AF.Exp, bias=nacum[:, h:h + 1], scale=1.0),
                     reads=["E", "nacum"], writes=[("Dm", j)])
            if LV < 3:
                continue
            p.mm([lambda e: e.matmul(cps[:, 0:128], lhsT=Bf[:, g, cs], rhs=Cf[:, g, cs], start=True, stop=True)], reads=["Bf", "Cf"], writes=["cps"])
            p.op("act", lambda e: e.activation(out=cbt, in_=cps[:, 0:128], func=AF.Copy), reads=["cps"], writes=["cbt"])
            p.op("dve", lambda e: e.tensor_tensor(out=M, in0=Dm, in1=cbt.unsqueeze(1).to_broadcast([128, 8, 128]), op=ALU.mult),
                 reads=toks("Dm", range(8)) + ["cbt"], writes=["M"])
            Xg = X[:, gs].rearrange("p (a b) -> p a b", a=8)
            p.op("pool", lambda e: e.tensor_tensor(out=xdt, in0=Xg, in1=dts[:, c, hs].unsqueeze(2).to_broadcast([128, 8, 64]), op=ALU.mult),
                 reads=[xtok, "dts"], writes=["xdt"])
            p.mm([(lambda e, j=j: e.matmul(Yd[:, j * 64:(j + 1) * 64], lhsT=M[:, j, :], rhs=xdt[:, j, :], start=True, stop=True)) for j in range(8)],
                 reads=["M", "xdt"], writes=["Yd"])
            p.mm([lambda e: e.matmul(Yo[:, 0:512], lhsT=Cf[:, g, cs], rhs=prevb[:, g, :], start=True, stop=True)], reads=["Cf", ("prevb", g)], writes=["Yo"])
            if LV < 4:
                continue
            p.op("dve", lambda e: e.tensor_tensor(out=t1.rearrange("p (a b) -> p a b", a=8), in0=Yo.rearrange("p (a b) -> p a b", a=8),
                                                  in1=eac[:, hs].unsqueeze(2).to_broadcast([128, 8, 64]), op=ALU.mult), reads=["Yo", "eac"], writes=["t1"])
            p.op("pool", lambda e: e.tensor_tensor(out=t2.rearrange("p (a b) -> p a b", a=8), in0=Xg,
                                                   in1=dbc[:, hs].unsqueeze(2).to_broadcast([128, 8, 64]), op=ALU.mult), reads=[xtok, dbctok], writes=["t2"])
            p.op("pool", lambda e: e.tensor_tensor(out=t2, in0=t2, in1=t1, op=ALU.add), reads=["t1", "t2"], writes=["t2"])
            p.op("dve", lambda e: e.tensor_tensor(out=ysb, in0=Yd[:, 0:512], in1=t2, op=ALU.add), reads=["Yd", "t2"], writes=["ysb"])
            p.op("pool", lambda e: e.tensor_tensor(out=ysb, in0=ysb, in1=Z[:, gs], op=ALU.mult), reads=["ysb", ztok], writes=["ysb"])
            p.op("act", lambda e: e.activation(out=junk, in_=ysb, func=AF.Square, accum_out=sm[:, 0:1]), reads=["ysb"], writes=["junk", "sm"])
            p.op("dve", lambda e: e.tensor_scalar(out=sm[:, 1:2], in0=sm[:, 0:1], scalar1=1.0 / 512.0, scalar2=EPS, op0=ALU.mult, op1=ALU.add), reads=["sm"], writes=["sm"])
            p.op("act", lambda e: e.activation(out=sm[:, 2:3], in_=sm[:, 1:2], func=AF.Sqrt), reads=["sm"], writes=["sm"])
            p.op("dve", lambda e: e.reciprocal(out=sm[:, 3:4], in_=sm[:, 2:3]), reads=["sm"], writes=["sm"])
            p.op("dve", lambda e: e.scalar_tensor_tensor(out=ybf, in0=ysb, scalar=sm[:, 3:4], in1=nw[:, gs], op0=ALU.mult, op1=ALU.mult),
                 reads=["ysb", "sm", nwtok], writes=["ybf"])
            if LV < 5:
                continue
            p.mm([(lambda e, j=j: e.transpose(out=tp[:, j * 128:(j + 1) * 128], in_=ybf[:, j * 128:(j + 1) * 128], identity=identb)) for j in range(4)],
                 reads=["ybf", "identb"], writes=["tp"])
            p.op("act", lambda e: e.activation(out=yT[:, g * 4:(g + 1) * 4, cs], in_=tp[:, 0:512].rearrange("p (a b) -> p a b", a=4), func=AF.Copy),
                 reads=["tp"], writes=[("yT", c, g)])
            if LV < 6:
                continue
            p.op("dve", lambda e: e.tensor_tensor(out=xdte, in0=Xg, in1=wde[:, hs].unsqueeze(2).to_broadcast([128, 8, 64]), op=ALU.mult),
                 reads=[xtok, "wde"], writes=["xdte"])
            p.mm([lambda e: e.matmul(Sp[:, 0:512], lhsT=Bt[:, c, g * 128:(g + 1) * 128], rhs=xdte.rearrange("p a b -> p (a b)"), start=True, stop=True)],
                 reads=["Bt", "xdte"], writes=["Sp"])
            if LV < 7:
                continue
            p.op("pool", lambda e: e.tensor_tensor(out=ptmp.rearrange("p (a b) -> p a b", a=8), in0=prev[:, g, :].rearrange("p (a b) -> p a b", a=8),
                                                   in1=cd[:, hs].unsqueeze(2).to_broadcast([128, 8, 64]), op=ALU.mult), reads=[("prev", g), "cd"], writes=["ptmp"])
            if LV < 8:
                continue
            p.op("dve", lambda e: e.tensor_tensor(out=prev[:, g, :], in0=Sp[:, 0:512], in1=ptmp, op=ALU.add), reads=["Sp", "ptmp"], writes=[("prev", g)])
            p.op("act", lambda e: e.activation(out=prevb[:, g, :], in_=prev[:, g, :], func=AF.Copy), reads=[("prev", g)], writes=[("prevb", g)])
    for k in range(16):
        p.dma(dr["yT"][k * 128:(k + 1) * 128, :], yT[:, k, :], reads=[("yT", c, k // 4) for c in range(NT)])
    S.close()


def stage_l0_att(cx):
    p, nc, dr = cx.p, cx.nc, cx.dram
    S = Scope(p)
    identb = load_const(cx, S, "identb", "identb")
    causal = load_const(cx, S, "causal", "causal")
    qT = [S.sb("qT", [128, T], BF16) for _ in range(2)]
    kT = [S.sb("kT", [128, T], BF16) for _ in range(2)]
    vt = [S.sb("vt", [128, NT, 128], BF16) for _ in range(2)]
    gt = [S.sb("gt", [128, NT, 8], F32) for _ in range(2)]
    yatt = [S.sb("yatt", [128, T], BF16) for _ in range(2)]
    Ssb = [S.sb("Ssb", [128, T], F32) for _ in range(2)]
    Pb = [S.sb("Pb", [128, T], BF16) for _ in range(2)]
    PTs = [S.sb("PTs", [128, NT, 128], BF16) for _ in range(2)]
    gsb = [S.sb("gsb", [128, 8], F32) for _ in range(2)]
    m8 = [S.sb("m8", [128, 8], F32) for _ in range(2)]
    bs = [S.sb("bs", [128, 8], F32) for _ in range(2)]
    st = [S.sb("st", [128, 4], F32) for _ in range(2)]
    Sps = [S.ps("Sps", [128, 512], F32) for _ in range(4)]
    PT = [S.ps("PT", [128, 1024], BF16) for _ in range(2)]
    OT = S.ps("OT", [128, 512], F32)

    def load(h):
        b = h % 2
        p.dma(qT[b], dr["q_fm"][h * 128:(h + 1) * 128, :], writes=[("qT", b)])
        p.dma(kT[b], dr["k_fm"][h * 128:(h + 1) * 128, :], writes=[("kT", b)])
        p.dma(vt[b], dr["v_tm"].rearrange("(t p) d -> p t d", p=128)[:, :, h * 128:(h + 1) * 128], writes=[("vt", b)])
        p.dma(gt[b], dr["gate_d"][h].rearrange("(t p) e -> p t e", p=128), writes=[("gt", b)])
    load(0)
    it = 0
    for h in range(16):
        if h + 1 < 16:
            load(h + 1)
        hb = h % 2
        for qi in range(NT):
            b = it % 2
            it += 1
            qb, half = qi // 2, qi % 2
            npast = qb * 256
            nk = npast + (half + 1) * 128
            nseg = (nk + 511) // 512
            for sg in range(nseg):
                w = min(512, nk - sg * 512)
                p.mm([lambda e, sg=sg, w=w: e.matmul(Sps[sg][:, 0:w], lhsT=qT[hb][:, qi * 128:(qi + 1) * 128], rhs=kT[hb][:, sg * 512:sg * 512 + w],
                                                     start=True, stop=True)], reads=[("qT", hb), ("kT", hb)], writes=[("Sps", sg)])
            sel = qb >= 3
            if sel:
                p.op("dve", lambda e: e.tensor_copy(out=gsb[b], in_=gt[hb][:, qi, :]), reads=[("gt", hb)], writes=[("gsb", b)])
                p.op("pool", lambda e: e.memset(gsb[b][:, qb:8], NEG), reads=[("gsb", b)], writes=[("gsb", b)])
                p.op("dve", lambda e: e.max(out=m8[b], in_=gsb[b]), reads=[("gsb", b)], writes=[("m8", b)])
                p.op("dve", lambda e: e.tensor_scalar(out=bs[b], in0=gsb[b], scalar1=m8[b][:, 2:3], scalar2=-1.0, op0=ALU.is_ge, op1=ALU.add),
                     reads=[("gsb", b), ("m8", b)], writes=[("bs", b)])
                p.op("dve", lambda e: e.tensor_scalar(out=bs[b], in0=bs[b], scalar1=1.0e30, scalar2=None, op0=ALU.mult), reads=[("bs", b)], writes=[("bs", b)])
            stok = ("Ssb", b)
            for sg in range((npast + 511) // 512):
                wp = min(512, npast - sg * 512)
                nb = wp // 256
                if sel:
                    p.op("dve", lambda e, sg=sg, wp=wp, nb=nb: e.tensor_tensor(
                        out=Ssb[b][:, sg * 512:sg * 512 + wp].rearrange("p (a c) -> p a c", a=nb),
                        in0=Sps[sg][:, 0:wp].rearrange("p (a c) -> p a c", a=nb),
                        in1=bs[b][:, sg * 2:sg * 2 + nb].unsqueeze(2).to_broadcast([128, nb, 256]), op=ALU.add),
                        reads=[("Sps", sg), ("bs", b)], writes=[stok])
                else:
                    p.op("act", lambda e, sg=sg, wp=wp: e.activation(out=Ssb[b][:, sg * 512:sg * 512 + wp], in_=Sps[sg][:, 0:wp], func=AF.Copy),
                         reads=[("Sps", sg)], writes=[stok])
            so, off = npast // 512, npast % 512
            if half == 1:
                p.op("act", lambda e: e.activation(out=Ssb[b][:, npast:npast + 128], in_=Sps[so][:, off:off + 128], func=AF.Copy),
                     reads=[("Sps", so)], writes=[stok])
            o2 = off + half * 128
            p.op("dve", lambda e: e.tensor_tensor(out=Ssb[b][:, nk - 128:nk], in0=Sps[so][:, o2:o2 + 128], in1=causal, op=ALU.add),
                 reads=[("Sps", so), "causal"], writes=[stok])
            p.op("dve", lambda e: e.reduce_max(out=st[b][:, 0:1], in_=Ssb[b][:, 0:nk], axis=AX.X, negate=True), reads=[stok], writes=[("st", b)])
            p.op("act", lambda e: e.activation(out=Ssb[b][:, 0:nk], in_=Ssb[b][:, 0:nk], func=AF.Exp, bias=st[b][:, 0:1], scale=1.0, accum_out=st[b][:, 1:2]),
                 reads=[stok, ("st", b)], writes=[stok, ("st", b)])
            p.op("dve", lambda e: e.reciprocal(out=st[b][:, 2:3], in_=st[b][:, 1:2]), reads=[("st", b)], writes=[("st", b)])
            p.op("dve", lambda e: e.tensor_scalar(out=Pb[b][:, 0:nk], in0=Ssb[b][:, 0:nk], scalar1=st[b][:, 2:3], scalar2=None, op0=ALU.mult),
                 reads=[stok, ("st", b)], writes=[("Pb", b)])
            nkb = nk // 128
            for bank in range((nkb + 7) // 8):
                n8 = min(8, nkb - bank * 8)
                p.mm([(lambda e, kb=kb, bank=bank: e.transpose(out=PT[bank][:, (kb % 8) * 128:(kb % 8 + 1) * 128], in_=Pb[b][:, kb * 128:(kb + 1) * 128],
                                                            identity=identb)) for kb in range(bank * 8, bank * 8 + n8)],
                     reads=[("Pb", b), "identb"], writes=[("PT", bank)])
                eng = "act" if bank == 0 else "dve"
                outap = PTs[b][:, bank * 8:bank * 8 + n8, :]
                inap = PT[bank][:, 0:n8 * 128].rearrange("p (a c) -> p a c", a=n8)
                if eng == "act":
                    p.op("act", lambda e, outap=outap, inap=inap: e.activation(out=outap, in_=inap, func=AF.Copy), reads=[("PT", bank)], writes=[("PTs", b, bank)])
                else:
                    p.op("dve", lambda e, outap=outap, inap=inap: e.tensor_copy(out=outap, in_=inap), reads=[("PT", bank)], writes=[("PTs", b, bank)])
            p.mm([(lambda e, kb=kb: e.matmul(OT[:, 0:128], lhsT=vt[hb][:, kb, :], rhs=PTs[b][:, kb, :], start=(kb == 0), stop=(kb == nkb - 1)))
                  for kb in range(nkb)], reads=[("vt", hb), ("PTs", b, 0), ("PTs", b, 1)], writes=["OT"])
            p.op("act", lambda e: e.activation(out=yatt[hb][:, qi * 128:(qi + 1) * 128], in_=OT[:, 0:128], func=AF.Copy), reads=["OT"], writes=[("yatt", hb)])
        p.dma(dr["yT"][2048 + h * 128:2048 + (h + 1) * 128, :], yatt[hb], reads=[("yatt", hb)])
    S.close()


def ln_tile(cx, r, rtok, junk, jtok, sm, smtok, gbc, gtok, bbc, btok):
    p = cx.p
    p.op("act", lambda e: e.activation(out=junk, in_=r, func=AF.Copy, accum_out=sm[:, 0:1]), reads=[rtok], writes=[jtok, smtok])
    p.op("act", lambda e: e.activation(out=junk, in_=r, func=AF.Square, accum_out=sm[:, 1:2]), reads=[rtok, jtok], writes=[jtok, smtok])
    p.op("dve", lambda e: e.tensor_scalar(out=sm[:, 2:3], in0=sm[:, 0:1], scalar1=1.0 / D, scalar2=None, op0=ALU.mult), reads=[smtok], writes=[smtok])
    p.op("dve", lambda e: e.scalar_tensor_tensor(out=sm[:, 3:4], in0=sm[:, 2:3], scalar=-1.0, in1=sm[:, 2:3], op0=ALU.mult, op1=ALU.mult),
         reads=[smtok], writes=[smtok])
    p.op("dve", lambda e: e.scalar_tensor_tensor(out=sm[:, 4:5], in0=sm[:, 1:2], scalar=1.0 / D, in1=sm[:, 3:4], op0=ALU.mult, op1=ALU.add),
         reads=[smtok], writes=[smtok])
    p.op("dve", lambda e: e.tensor_scalar(out=sm[:, 4:5], in0=sm[:, 4:5], scalar1=EPS, scalar2=None, op0=ALU.add), reads=[smtok], writes=[smtok])
    p.op("act", lambda e: e.activation(out=sm[:, 5:6], in_=sm[:, 4:5], func=AF.Sqrt), reads=[smtok], writes=[smtok])
    p.op("dve", lambda e: e.reciprocal(out=sm[:, 6:7], in_=sm[:, 5:6]), reads=[smtok], writes=[smtok])
    p.op("dve", lambda e: e.scalar_tensor_tensor(out=sm[:, 7:8], in0=sm[:, 2:3], scalar=-1.0, in1=sm[:, 6:7], op0=ALU.mult, op1=ALU.mult),
         reads=[smtok], writes=[smtok])
    p.op("act", lambda e: e.activation(out=r, in_=r, func=AF.Identity, scale=sm[:, 6:7], bias=sm[:, 7:8]), reads=[rtok, smtok], writes=[rtok])
    p.op("pool", lambda e: e.tensor_tensor(out=r, in0=r, in1=gbc, op=ALU.mult), reads=[rtok, gtok], writes=[rtok])
    p.op("pool", lambda e: e.tensor_tensor(out=r, in0=r, in1=bbc, op=ALU.add), reads=[rtok, btok], writes=[rtok])


def stage_outproj_ln(cx, yT_d, kc, W_d, h_in, g_row, b_row, h_out):
    p, nc, dr = cx.p, cx.nc, cx.dram
    S = Scope(p)
    pw = 128 if kc == 32 else 256
    ws = WStream(cx, S, kc, pw, "wout")
    gbc, gtok = bcast_row(cx, S, g_row, D, "gbc")
    bbc, btok = bcast_row(cx, S, b_row, D, "bbc")
    racc = S.sb("racc", [128, 4, D], F32)
    yTq = S.sb("yTq", [128, kc, 512], BF16)
    junk = S.sb("junk", [128, D], F32)
    sm = S.sb("sm", [128, 8], F32)
    gps = [(S.ps("ops", [128, 512], F32), ("ops", i)) for i in range(4)]
    yv = yT_d.rearrange("(k p) t -> p k t", p=128)
    hv = h_in.rearrange("(t p) d -> t p d", p=128)
    ov = h_out.rearrange("(t p) d -> t p d", p=128)
    panels = [(i * pw, pw) for i in range(D // pw)]
    k = 0
    for qtr in range(4):
        p.dma(yTq, yv[:, :, qtr * 512:(qtr + 1) * 512], writes=["yTq"])
        for j in range(4):
            p.dma(racc[:, j, :], hv[qtr * 4 + j], writes=[("racc", j)])
            p.op("pool", lambda e, j=j: e.tensor_scalar(out=racc[:, j, :], in0=racc[:, j, :], scalar1=DN_ALPHA, scalar2=None, op0=ALU.mult),
                 reads=[("racc", j)], writes=[("racc", j)])
        nxt = ws.fetch(W_d, panels[0][0], pw)
        for pi, (c0, w) in enumerate(panels):
            bf, btk = nxt
            if pi + 1 < len(panels):
                nxt = ws.fetch(W_d, panels[pi + 1][0], pw)
            for j in range(4):
                ps, ptok = gps[k % 4]
                k += 1
                p.mm([(lambda e, c=c, ps=ps, j=j: e.matmul(ps[:, 0:w], lhsT=yTq[:, c, j * 128:(j + 1) * 128], rhs=bf[:, c, 0:w],
                                                         start=(c == 0), stop=(c == kc - 1))) for c in range(kc)], reads=[btk, "yTq"], writes=[ptok])
                p.op("dve", lambda e, ps=ps, j=j, c0=c0: e.tensor_tensor(out=racc[:, j, c0:c0 + w], in0=ps[:, 0:w], in1=racc[:, j, c0:c0 + w], op=ALU.add),
                     reads=[ptok, ("racc", j)], writes=[("racc", j)])
        for j in range(4):
            ln_tile(cx, racc[:, j, :], ("racc", j), junk, "junk", sm, "sm", gbc, gtok, bbc, btok)
            p.dma(ov[qtr * 4 + j], racc[:, j, :], reads=[("racc", j)])
    S.close()


def stage_router(cx, L, h_tm, masked=True):
    p, nc, dr = cx.p, cx.nc, cx.dram
    S = Scope(p)
    ident = load_const(cx, S, "ident", "ident")
    oh = S.sb("oh", [128, 8], F32)
    p.dma(oh, dr["oh"], writes=["oh"])
    wr = S.sb("wr", [128, KC, 32], F32)
    p.dma(wr, dr["moe_w_router"][L * D:(L + 1) * D, :].rearrange("(k p) e -> p k e", p=128), writes=["wr"])
    brbc, brtok = bcast_row(cx, S, dr["moe_b_router"][L:L + 1, :], 32, "brbc")
    hT = S.sb("hTr", [128, KC, T], BF16)
    hTf = S.sb("hTf", [128, KC, 128], F32)
    gl = S.sb("gl", [128, NT, 32], F32)
    ld = [S.sb("rld", [128, D], F32) for _ in range(2)]
    lg = S.sb("lg", [128, 32], F32)
    ex = S.sb("ex", [128, 32], F32)
    selm = S.sb("selm", [128, 32], F32)
    m8 = S.sb("m8", [128, 8], F32)
    sm = S.sb("rsm", [128, 4], F32)
    tps = [S.ps("rtp", [128, 512], F32) for _ in range(2)]
    lps = S.ps("lps", [128, 512], F32)
    src = h_tm.rearrange("(t p) d -> t p d", p=128)
    k = 0
    for t in range(NT):
        b = ld[t % 2]
        p.dma(b, src[t], writes=[("rld", t % 2)])
        for g in range(4):
            pp, ptok = tps[k % 2], ("rtp", k % 2)
            k += 1
            p.mm([(lambda e, pp=pp, b=b, g=g, j=j: e.transpose(out=pp[:, j * 128:(j + 1) * 128], in_=b[:, (g * 4 + j) * 128:(g * 4 + j + 1) * 128],
                                                              identity=ident)) for j in range(4)], reads=[("rld", t % 2), "ident"], writes=[ptok])
            src_ps = pp.rearrange("p (a b) -> p a b", a=4)
            p.op("act", lambda e, g=g, s=src_ps: e.activation(out=hTf[:, g * 4:(g + 1) * 4, :], in_=s, func=AF.Copy), reads=[ptok], writes=[("hTf", g)])
            p.op("dve", lambda e, g=g, t=t: e.tensor_copy(out=hT[:, g * 4:(g + 1) * 4, t * 128:(t + 1) * 128], in_=hTf[:, g * 4:(g + 1) * 4, :]),
                 reads=[("hTf", g)], writes=[("hTr", t, g)])
        p.mm([(lambda e, c=c: e.matmul(lps[:, 0:32], lhsT=hTf[:, c, :], rhs=wr[:, c, :], start=(c == 0), stop=(c == KC - 1))) for c in range(KC)],
             reads=toks("hTf", range(4)) + ["wr"], writes=["lps"])
        p.op("dve", lambda e: e.tensor_tensor(out=lg, in0=lps[:, 0:32], in1=brbc, op=ALU.add), reads=["lps", brtok], writes=["lg"])
        p.op("dve", lambda e: e.max(out=m8, in_=lg), reads=["lg"], writes=["m8"])
        p.op("dve", lambda e: e.tensor_scalar(out=sm[:, 0:1], in0=m8[:, 0:1], scalar1=-1.0, scalar2=None, op0=ALU.mult), reads=["m8"], writes=["rsm"])
        p.op("act", lambda e: e.activation(out=ex, in_=lg, func=AF.Exp, bias=sm[:, 0:1], scale=1.0), reads=["lg", "rsm"], writes=["ex"])
        p.op("dve", lambda e: e.tensor_scalar(out=selm, in0=lg, scalar1=m8[:, 3:4], scalar2=0.0, op0=ALU.is_ge, op1=ALU.add), reads=["lg", "m8"], writes=["selm"])
        p.op("dve", lambda e: e.tensor_tensor(out=ex, in0=ex, in1=selm, op=ALU.mult), reads=["ex", "selm"], writes=["ex"])
        p.op("dve", lambda e: e.tensor_reduce(out=sm[:, 1:2], in_=ex, axis=AX.X, op=ALU.add), reads=["ex"], writes=["rsm"])
        p.op("dve", lambda e: e.reciprocal(out=sm[:, 2:3], in_=sm[:, 1:2]), reads=["rsm"], writes=["rsm"])
        p.op("dve", lambda e, t=t: e.tensor_scalar(out=gl[:, t, :], in0=ex, scalar1=sm[:, 2:3], scalar2=None, op0=ALU.mult), reads=["ex", "rsm"], writes=["gl"])
    if not masked:
        for k in range(KC):
            p.dma(dr["hTo"][k * 128:(k + 1) * 128, :], hT[:, k, :], reads=[("hTr", t, k // 4) for t in range(NT)])
        p.dma(dr["gateo"].rearrange("(t p) e -> p t e", p=128), gl, reads=["gl"])
        S.close()
        return
    scb = [S.sb("scb", [128, 4, T], BF16) for _ in range(2)]
    gsc = [S.sb("gsc", [128, NT, 32], F32) for _ in range(2)]
    alltok = [("hTr", t, g) for t in range(NT) for g in range(4)]
    n = 0
    for j in range(8):
        for kq in range(4):
            b = n % 2
            eng = "dve" if n % 2 == 0 else "pool"
            n += 1
            p.op(eng, lambda e, b=b, kq=kq, j=j: e.tensor_scalar(out=scb[b], in0=hT[:, kq * 4:(kq + 1) * 4, :], scalar1=oh[:, j:j + 1], scalar2=None, op0=ALU.mult),
                 reads=[("hTr", t, kq) for t in range(NT)] + ["oh"], writes=[("scb", b)])
            p.dma(dr["GH"][j * D:(j + 1) * D, :].rearrange("(k p) t -> p k t", p=128)[:, kq * 4:(kq + 1) * 4, :], scb[b], reads=[("scb", b)], writes=["GH"])
        b = j % 2
        p.op("dve", lambda e, b=b, j=j: e.tensor_scalar(out=gsc[b], in0=gl, scalar1=oh[:, j:j + 1], scalar2=None, op0=ALU.mult), reads=["gl", "oh"], writes=[("gsc", b)])
        p.dma(dr["GG"][j * T:(j + 1) * T, :].rearrange("(t p) e -> p t e", p=128), gsc[b], reads=[("gsc", b)], writes=["GG"])
    S.close()


def stage_precast(cx, L):
    p, nc, dr = cx.p, cx.nc, cx.dram
    S = Scope(p)
    st1 = [S.sb("pc1", [128, 4096], F32) for _ in range(2)]
    o1 = [S.sb("po1", [128, 16, 256], BF16) for _ in range(2)]
    st2 = [S.sb("pc2", [128, 2048], F32) for _ in range(2)]
    o2 = [S.sb("po2", [128, 2048], BF16) for _ in range(2)]
    n = 0
    for e_ in range(4):
        r0 = (L * 4 + e_) * D
        for kc in range(KC):
            b = n % 2
            n += 1
            p.dma(st1[b], dr["moe_w1"][r0 + kc * 128:r0 + (kc + 1) * 128, :], writes=[("pc1", b)])
            sv = st1[b].rearrange("p (f i two) -> p f i two", f=16, two=2)
            p.op("dve", lambda e, b=b, sv=sv: e.tensor_copy(out=o1[b][:, :, 0:128], in_=sv[:, :, :, 0]), reads=[("pc1", b)], writes=[("po1", b)])
            p.op("pool", lambda e, b=b, sv=sv: e.tensor_copy(out=o1[b][:, :, 128:256], in_=sv[:, :, :, 1]), reads=[("pc1", b)], writes=[("po1", b)])
            p.dma(dr["w1b"][e_ * 16 * 128:(e_ + 1) * 16 * 128, :].rearrange("(f p) (k c) -> p f k c", p=128, c=256)[:, :, kc, :], o1[b],
                  reads=[("po1", b)], writes=["w1b"])
            p.dma(st2[b], dr["moe_w2"][r0 + kc * 128:r0 + (kc + 1) * 128, :], writes=[("pc2", b)])
            p.op("act", lambda e, b=b: e.activation(out=o2[b], in_=st2[b], func=AF.Copy), reads=[("pc2", b)], writes=[("po2", b)])
            p.dma(dr["w2b"][e_ * 4 * 128:(e_ + 1) * 4 * 128, :].rearrange("(d p) (f c) -> p d f c", p=128, c=512)[:, :, kc, :],
                  o2[b].rearrange("p (d c) -> p d c", d=4), reads=[("po2", b)], writes=["w2b"])
    S.close()


def stage_experts(cx, L, GH2, GG2, ngroups=16):
    p, nc, dr = cx.p, cx.nc, cx.dram
    S = Scope(p)
    ident = load_const(cx, S, "ident", "ident")
    oh = S.sb("oh", [128, 8], F32)
    p.dma(oh, dr["oh"], writes=["oh"])
    mps = S.ps("emps", [128, 512], F32)
    b1st = S.sb("b1st", [64, 256], F32)
    p.dma(b1st, dr["moe_b1"][L * 4:(L + 1) * 4, :].rearrange("e (f c) -> (e f) c", c=256), writes=["b1st"])
    b1g = S.sb("b1g", [128, 64], F32)
    b1l = S.sb("b1l", [128, 64], F32)
    p.mm([lambda e: e.transpose(out=mps[:, 0:64], in_=b1st[:, 0:256:2], identity=ident[0:64, 0:64]),
          lambda e: e.transpose(out=mps[:, 64:128], in_=b1st[:, 1:256:2], identity=ident[0:64, 0:64])], reads=["b1st", "ident"], writes=["emps"])
    p.op("dve", lambda e: e.tensor_copy(out=b1g, in_=mps[:, 0:64]), reads=["emps"], writes=["b1g"])
    p.op("dve", lambda e: e.tensor_copy(out=b1l, in_=mps[:, 64:128]), reads=["emps"], writes=["b1l"])
    b2t = [S.sb("b2t", [128, 512], F32) for _ in range(2)]
    hTg = S.sb("hTg", [128, KC, 1024], BF16)
    actT = S.sb("actT", [128, KC, 1024], BF16)
    acc = S.sb("acc", [128, 8, D], F32)
    gts = S.sb("gts", [128, 8, 32], F32)
    gsel = S.sb("gsel", [128, 8, 4], F32)
    gtmp = S.sb("gtmp", [128, 8, 4], F32)
    w1p = [S.sb("w1p", [128, KC, 256], BF16) for _ in range(2)]
    w2p = [S.sb("w2p", [128, KC, 512], BF16) for _ in range(2)]
    ga = [S.sb("ga", [128, 512], F32) for _ in range(2)]
    sg = [S.sb("sg", [128, 512], F32) for _ in range(2)]
    la = [S.sb("la", [128, 512], F32) for _ in range(2)]
    yb = [S.sb("yb", [128, 512], F32) for _ in range(2)]
    Gp = [S.ps("Gp", [128, 512], F32) for _ in range(2)]
    Lp = [S.ps("Lp", [128, 512], F32) for _ in range(2)]
    Yp = [S.ps("Yp", [128, 512], F32) for _ in range(2)]
    w1v = dr["w1b"].rearrange("(ef p) x -> ef p x", p=128)
    w2v = dr["w2b"].rearrange("(ed p) x -> ed p x", p=128)
    n1 = n2 = it = 0

    def fetch1(e_, fc):
        nonlocal n1
        b = n1 % 2
        n1 += 1
        p.dma(w1p[b].rearrange("p k c -> p (k c)"), w1v[e_ * 16 + fc], reads=["w1b"], writes=[("w1p", b)])
        return b

    def fetch2(e_, dp):
        nonlocal n2
        b = n2 % 2
        n2 += 1
        p.dma(w2p[b].rearrange("p k c -> p (k c)"), w2v[e_ * 4 + dp], reads=["w2b"], writes=[("w2p", b)])
        return b
    for gi in range(ngroups):
        jb, hf = gi // 2, gi % 2
        p.dma(hTg, GH2[jb * D:(jb + 1) * D, :].rearrange("(k p) t -> p k t", p=128)[:, :, hf * 1024:(hf + 1) * 1024], reads=["GH2"], writes=["hTg"])
        p.dma(gts, GG2[jb * T + hf * 1024:jb * T + (hf + 1) * 1024, :].rearrange("(t p) e -> p t e", p=128), reads=["GG2"], writes=["gts"])
        gv = gts.rearrange("p t (j e) -> p t j e", e=4)
        p.op("dve", lambda e: e.tensor_scalar(out=gsel, in0=gv[:, :, 0, :], scalar1=oh[:, 0:1], scalar2=None, op0=ALU.mult), reads=["gts", "oh"], writes=["gsel"])
        for j in range(1, 8):
            p.op("dve", lambda e, j=j: e.scalar_tensor_tensor(out=gsel, in0=gv[:, :, j, :], scalar=oh[:, j:j + 1], in1=gsel, op0=ALU.mult, op1=ALU.add),
                 reads=["gts", "oh", "gsel"], writes=["gsel"])
        for e_ in range(4):
            nb = fetch1(e_, 0)
            for fc in range(16):
                b1 = nb
                if fc + 1 < 16:
                    nb = fetch1(e_, fc + 1)
                for tb in range(2):
                    i2 = it % 2
                    it += 1
                    tsl = slice(tb * 512, (tb + 1) * 512)
                    p.mm([(lambda e, c=c: e.matmul(Gp[i2][:, 0:512], lhsT=w1p[b1][:, c, 0:128], rhs=hTg[:, c, tsl], start=(c == 0), stop=(c == KC - 1)))
                          for c in range(KC)], reads=[("w1p", b1), "hTg"], writes=[("Gp", i2)])
                    p.mm([(lambda e, c=c: e.matmul(Lp[i2][:, 0:512], lhsT=w1p[b1][:, c, 128:256], rhs=hTg[:, c, tsl], start=(c == 0), stop=(c == KC - 1)))
                          for c in range(KC)], reads=[("w1p", b1), "hTg"], writes=[("Lp", i2)])
                    col = e_ * 16 + fc
                    p.op("dve", lambda e: e.tensor_scalar(out=ga[i2], in0=Gp[i2][:, 0:512], scalar1=b1g[:, col:col + 1], scalar2=7.0, op0=ALU.add, op1=ALU.min),
                         reads=[("Gp", i2), "b1g"], writes=[("ga", i2)])
                    p.op("act", lambda e: e.activation(out=sg[i2], in_=ga[i2], func=AF.Sigmoid, scale=1.702), reads=[("ga", i2)], writes=[("sg", i2)])
                    p.op("dve", lambda e: e.tensor_scalar(out=la[i2], in0=Lp[i2][:, 0:512], scalar1=b1l[:, col:col + 1], scalar2=7.0, op0=ALU.add, op1=ALU.min),
                         reads=[("Lp", i2), "b1l"], writes=[("la", i2)])
                    p.op("pool", lambda e: e.tensor_scalar(out=la[i2], in0=la[i2], scalar1=-7.0, scalar2=1.0, op0=ALU.max, op1=ALU.add),
                         reads=[("la", i2)], writes=[("la", i2)])
                    p.op("pool", lambda e: e.tensor_tensor(out=ga[i2], in0=ga[i2], in1=sg[i2], op=ALU.mult), reads=[("ga", i2), ("sg", i2)], writes=[("ga", i2)])
                    p.op("pool", lambda e: e.tensor_tensor(out=actT[:, fc, tsl], in0=ga[i2], in1=la[i2], op=ALU.mult), reads=[("ga", i2), ("la", i2)],
                         writes=[("actT", fc, tb)])
            nb = fetch2(e_, 0)
            for dp in range(4):
                b2 = nb
                if dp + 1 < 4:
                    nb = fetch2(e_, dp + 1)
                dsl = slice(dp * 512, (dp + 1) * 512)
                bb = n2 % 2
                p.dma(b2t[bb], dr["moe_b2"][L * 4 + e_:L * 4 + e_ + 1, dsl].partition_broadcast(128), writes=[("b2t", bb)])
                for t in range(8):
                    i2 = it % 2
                    it += 1
                    p.mm([(lambda e, c=c: e.matmul(Yp[i2][:, 0:512], lhsT=actT[:, c, t * 128:(t + 1) * 128], rhs=w2p[b2][:, c, :], start=(c == 0), stop=(c == KC - 1)))
                          for c in range(KC)], reads=[("w2p", b2)] + [("actT", c, t // 4) for c in range(KC)], writes=[("Yp", i2)])
                    p.op("dve", lambda e: e.tensor_tensor(out=yb[i2], in0=Yp[i2][:, 0:512], in1=b2t[bb], op=ALU.add), reads=[("Yp", i2), ("b2t", bb)],
                         writes=[("yb", i2)])
                    if e_ == 0:
                        p.op("pool", lambda e: e.tensor_scalar(out=acc[:, t, dsl], in0=yb[i2], scalar1=gsel[:, t, e_:e_ + 1], scalar2=None, op0=ALU.mult),
                             reads=[("yb", i2), "gsel"], writes=[("acc", t, dp)])
                    else:
                        p.op("pool", lambda e: e.tensor_scalar(out=yb[i2], in0=yb[i2], scalar1=gsel[:, t, e_:e_ + 1], scalar2=None, op0=ALU.mult),
                             reads=[("yb", i2), "gsel"], writes=[("yb", i2)])
                        p.op("pool", lambda e: e.tensor_tensor(out=acc[:, t, dsl], in0=acc[:, t, dsl], in1=yb[i2], op=ALU.add),
                             reads=[("yb", i2), ("acc", t, dp)], writes=[("acc", t, dp)])
        p.dma(dr["PART"][gi * 1024:(gi + 1) * 1024, :].rearrange("(t p) d -> p t d", p=128), acc,
              reads=[("acc", t, dp) for t in range(8) for dp in range(4)], writes=["PART"])
    S.close()


def stage_combine_ln(cx, PART2, h_in, g_row, b_row, h_out, nblk=8, use_oh=True):
    p, nc, dr = cx.p, cx.nc, cx.dram
    S = Scope(p)
    oh = S.sb("oh", [128, 8], F32)
    p.dma(oh, dr["oh"], writes=["oh"])
    gbc, gtok = bcast_row(cx, S, g_row, D, "gbc")
    bbc, btok = bcast_row(cx, S, b_row, D, "bbc")
    r = [S.sb("cr", [128, D], F32) for _ in range(2)]
    pl = [S.sb("cpl", [128, D], F32) for _ in range(3)]
    junk = S.sb("junk", [128, D], F32)
    sm = S.sb("sm", [128, 8], F32)
    hv = h_in.rearrange("(t p) d -> t p d", p=128)
    ov = h_out.rearrange("(t p) d -> t p d", p=128)
    n = 0
    for t in range(NT):
        b = t % 2
        rt = ("cr", b)
        p.dma(r[b], hv[t], writes=[rt])
        p.op("pool", lambda e, b=b: e.tensor_scalar(out=r[b], in0=r[b], scalar1=DN_ALPHA, scalar2=None, op0=ALU.mult), reads=[rt], writes=[rt])
        for j in range(nblk):
            c = n % 3
            n += 1
            p.dma(pl[c], PART2[j * T + t * 128:j * T + (t + 1) * 128, :], reads=["PART2"], writes=[("cpl", c)])
            if use_oh:
                p.op("dve", lambda e, b=b, c=c, j=j: e.scalar_tensor_tensor(out=r[b], in0=pl[c], scalar=oh[:, j:j + 1], in1=r[b], op0=ALU.mult, op1=ALU.add),
                     reads=[("cpl", c), "oh", rt], writes=[rt])
            else:
                p.op("dve" if j % 2 == 0 else "pool", lambda e, b=b, c=c: e.tensor_tensor(out=r[b], in0=r[b], in1=pl[c], op=ALU.add),
                     reads=[("cpl", c), rt], writes=[rt])
        ln_tile(cx, r[b], rt, junk, "junk", sm, "sm", gbc, gtok, bbc, btok)
        p.dma(ov[t], r[b], reads=[rt])
    S.close()


def stage_gla_proj(cx, h_tm):
    p, nc, dr = cx.p, cx.nc, cx.dram
    S = Scope(p)
    ident = load_const(cx, S, "ident", "ident")
    xT = S.sb("xT", [128, KC, T], BF16)
    S2 = Scope(p)
    transpose_in(cx, S2, h_tm, xT, "xT", ident, "ident")
    S2.close()
    w_in = dr["gla_w_in"]
    ws = WStream(cx, S, KC, 256, "gwin")
    gps = [(S.ps("gps", [128, 512], F32), ("gps", i)) for i in range(3)]
    lps = [S.ps("glps", [128, 512], F32) for _ in range(2)]
    fst = [S.sb("fst", [128, T], F32) for _ in range(2)]
    tst = [S.sb("tst", [128, 256], F32) for _ in range(2)]
    vst = [S.sb("gvst", [128, 256], BF16) for _ in range(2)]
    glT = S.sb("glT", [16, T], F32)
    cnt = {"f": 0, "t": 0, "v": 0}

    def ep(tag, c0, w, idx, ps, ptok):
        if tag in ("q", "k"):
            tb = idx
            b = cnt["f"] % 2
            sc = (1.0 / 16.0) if tag == "q" else 1.0
            p.op("act", lambda e: e.activation(out=fst[b][:, tb * 512:(tb + 1) * 512], in_=ps[:, 0:512], func=AF.Copy, scale=sc), reads=[ptok], writes=[("fst", b, tb)])
            if tb == 3:
                cnt["f"] += 1
                dst = dr["gq_fm"] if tag == "q" else dr["gk_fm"]
                r0 = c0 if tag == "q" else c0 - 1024
                p.dma(dst[r0:r0 + 128, :], fst[b], reads=[("fst", b, i) for i in range(4)])
        elif tag == "gl":
            tb = idx
            p.op("act", lambda e: e.activation(out=glT[:, tb * 512:(tb + 1) * 512], in_=ps[0:16, 0:512], func=AF.Copy), reads=[ptok], writes=[("glT", tb)])
        elif tag in ("ktm", "g"):
            b = cnt["t"] % 2
            cnt["t"] += 1
            if tag == "g":
                p.op("act", lambda e: e.activation(out=tst[b][:, 0:w], in_=ps[:, 0:w], func=AF.Silu), reads=[ptok], writes=[("tst", b)])
                p.dma(dr["gsg"][idx * 128:(idx + 1) * 128, c0 - 4096:c0 - 4096 + w], tst[b][:, 0:w], reads=[("tst", b)])
            else:
                p.op("dve", lambda e: e.tensor_copy(out=tst[b][:, 0:w], in_=ps[:, 0:w]), reads=[ptok], writes=[("tst", b)])
                p.dma(dr["gk_tm"][idx * 128:(idx + 1) * 128, c0 - 1024:c0 - 1024 + w], tst[b][:, 0:w], reads=[("tst", b)])
        elif tag == "v":
            b = cnt["v"] % 2
            cnt["v"] += 1
            p.op("dve", lambda e: e.tensor_copy(out=vst[b][:, 0:w], in_=ps[:, 0:w]), reads=[ptok], writes=[("gvst", b)])
            p.dma(dr["gv_tm"][idx * 128:(idx + 1) * 128, c0 - 2048:c0 - 2048 + w], vst[b][:, 0:w], reads=[("gvst", b)])

    panels = [(i * 256, 256, "q") for i in range(4)] + [(1024 + i * 256, 256, "k") for i in range(4)] + [(6144, 16, "gl")]
    gemm_panels(cx, ws, w_in, panels, xT, "xT", "fm", gps, ep)
    p.barrier()
    panels = [(1024 + i * 256, 256, "ktm") for i in range(4)] + [(2048 + i * 256, 256, "v") for i in range(8)] + [(4096 + i * 256, 256, "g") for i in range(8)]
    gemm_panels(cx, ws, w_in, panels, xT, "xT", "tm", gps, ep)
    p.barrier()
    wg2 = S.sb("wg2", [16, 1024], F32)
    p.dma(wg2, dr["gla_w_gate2"], writes=["wg2"])
    bg, bgtok = bcast_row(cx, S, dr["gla_b_gate"], 1024, "bgate")
    xg = [S.sb("xg", [128, 1024], F32) for _ in range(2)]
    lg = [S.sb("lgl", [128, 1024], F32) for _ in range(2)]
    for t in range(NT):
        b = t % 2
        for hf in range(2):
            p.mm([lambda e, hf=hf: e.matmul(lps[hf][:, 0:512], lhsT=glT[:, t * 128:(t + 1) * 128], rhs=wg2[:, hf * 512:(hf + 1) * 512], start=True, stop=True)],
                 reads=toks("glT", range(4)) + ["wg2"], writes=[("glps", hf)])
            p.op("dve", lambda e, hf=hf: e.tensor_tensor(out=xg[b][:, hf * 512:(hf + 1) * 512], in0=lps[hf][:, 0:512], in1=bg[:, hf * 512:(hf + 1) * 512], op=ALU.add),
                 reads=[("glps", hf), bgtok], writes=[("xg", b, hf)])
        xt2 = [("xg", b, 0), ("xg", b, 1)]
        p.op("act", lambda e: e.activation(out=lg[b], in_=xg[b], func=AF.Abs), reads=xt2, writes=[("lgl", b)])
        p.op("act", lambda e: e.activation(out=lg[b], in_=lg[b], func=AF.Exp, scale=-1.0), reads=[("lgl", b)], writes=[("lgl", b)])
        p.op("act", lambda e: e.activation(out=lg[b], in_=lg[b], func=AF.Ln, bias=1.0), reads=[("lgl", b)], writes=[("lgl", b)])
        p.op("dve", lambda e: e.tensor_scalar(out=xg[b], in0=xg[b], scalar1=0.0, scalar2=1.0 / 16.0, op0=ALU.min, op1=ALU.mult), reads=xt2, writes=xt2)
        p.op("dve", lambda e: e.scalar_tensor_tensor(out=lg[b], in0=lg[b], scalar=-1.0 / 16.0, in1=xg[b], op0=ALU.mult, op1=ALU.add),
             reads=xt2 + [("lgl", b)], writes=[("lgl", b)])
        p.dma(dr["gla_d"][t * 128:(t + 1) * 128, :], lg[b], reads=[("lgl", b)])
    S.close()


def stage_gla_core(cx):
    p, nc, dr = cx.p, cx.nc, cx.dram
    S = Scope(p)
    identb = load_const(cx, S, "identb", "identb")
    btri = load_const(cx, S, "btri", "btri")
    bgt = load_const(cx, S, "bgt", "bgt")
    gnb, gntok = bcast_row(cx, S, dr["gla_norm"], 512, "gnb")
    yT = S.sb("gyT", [128, 16, T], BF16)
    St = S.sb("St", [128, 8, 512], F32)
    Sb = S.sb("Sb", [128, 8, 512], BF16)
    S1 = S.sb("S1", [128, 2, 512], F32)
    S1b = S.sb("S1b", [128, 2, 512], BF16)
    p.op("pool", lambda e: e.memset(St, 0.0), writes=toks("St", range(8)))
    p.op("pool", lambda e: e.memset(Sb, 0.0), writes=toks("Sb", range(8)))
    la = [S.sb("la", [128, 1024], F32) for _ in range(2)]
    ktm = [S.sb("ktm", [128, 1024], F32) for _ in range(2)]
    vt = [S.sb("gvt", [128, 2048], BF16) for _ in range(2)]
    sg = [S.sb("gsg", [128, 2048], F32) for _ in range(2)]
    qf = [S.sb("gqf", [128, 8, 128], F32) for _ in range(2)]
    kf = [S.sb("gkf", [128, 8, 128], F32) for _ in range(2)]
    eg = S.sb("eg", [128, 8, 128], F32)
    eng = S.sb("eng", [128, 8, 128], F32)
    egl = S.sb("egl", [128, 1024], F32)
    qin = S.sb("qin", [128, 8, 128], BF16)
    kin = S.sb("kin", [128, 8, 128], BF16)
    qA = S.sb("qA", [128, 8, 128], BF16)
    qB = S.sb("qB", [128, 8, 128], BF16)
    p.op("pool", lambda e: e.memset(qA, 0.0), writes=["qA"])
    p.op("pool", lambda e: e.memset(qB, 0.0), writes=["qB"])
    ke = S.sb("ke", [128, 1024], BF16)
    atm = S.sb("atm", [128, 128], BF16)
    junk = S.sb("gjunk", [128, 512], F32)
    on = S.sb("on", [128, 512], F32)
    ybf = S.sb("gybf", [128, 512], BF16)
    sm = S.sb("gsm", [128, 4], F32)
    GC = [S.ps("GC", [128, 512], F32) for _ in range(2)]
    GL = [S.ps("GL", [128, 512], F32) for _ in range(2)]
    ATp = S.ps("ATp", [128, 512], F32)
    Op = S.ps("Op", [128, 512], F32)
    Dp = S.ps("Dp", [128, 512], F32)
    tp = S.ps("gtp", [128, 512], BF16)
    qv = dr["gq_fm"].rearrange("(c p) t -> p c t", p=128)
    kv = dr["gk_fm"].rearrange("(c p) t -> p c t", p=128)

    def load(t):
        b = t % 2
        rs = slice(t * 128, (t + 1) * 128)
        p.dma(la[b], dr["gla_d"][rs, :], writes=[("la", b)])
        p.dma(ktm[b], dr["gk_tm"][rs, :], writes=[("ktm", b)])
        p.dma(vt[b], dr["gv_tm"][rs, :], writes=[("gvt", b)])
        p.dma(sg[b], dr["gsg"][rs, :], writes=[("gsg", b)])
        p.dma(qf[b], qv[:, :, rs], writes=[("gqf", b)])
        p.dma(kf[b], kv[:, :, rs], writes=[("gkf", b)])
    load(0)
    for t in range(NT):
        if t + 1 < NT:
            load(t + 1)
        b = t % 2
        cs = slice(t * 128, (t + 1) * 128)
        for hf in range(2):
            p.mm([(lambda e, j=j, hf=hf: e.matmul(GC[hf][:, j * 128:(j + 1) * 128], lhsT=la[b][:, (hf * 4 + j) * 128:(hf * 4 + j + 1) * 128], rhs=btri,
                                                  start=True, stop=True)) for j in range(4)], reads=[("la", b), "btri"], writes=[("GC", hf)])
            p.op("act", lambda e, hf=hf: e.activation(out=eg[:, hf * 4:(hf + 1) * 4, :], in_=GC[hf].rearrange("p (a c) -> p a c", a=4), func=AF.Exp),
                 reads=[("GC", hf)], writes=[("eg", hf)])
            p.op("act", lambda e, hf=hf: e.activation(out=eng[:, hf * 4:(hf + 1) * 4, :], in_=GC[hf].rearrange("p (a c) -> p a c", a=4), func=AF.Exp, scale=-1.0),
                 reads=[("GC", hf)], writes=[("eng", hf)])
            p.mm([lambda e, hf=hf: e.matmul(GL[hf][:, 0:512], lhsT=bgt, rhs=la[b][:, hf * 512:(hf + 1) * 512], start=True, stop=True)],
                 reads=[("la", b), "bgt"], writes=[("GL", hf)])
            p.op("act", lambda e, hf=hf: e.activation(out=egl[:, hf * 512:(hf + 1) * 512], in_=GL[hf][:, 0:512], func=AF.Exp), reads=[("GL", hf)], writes=[("egl", hf)])
        egt = [("eg", 0), ("eg", 1)]
        p.op("dve", lambda e: e.tensor_tensor(out=qin, in0=qf[b], in1=eg, op=ALU.mult), reads=[("gqf", b)] + egt, writes=["qin"])
        p.op("pool", lambda e: e.tensor_tensor(out=kin, in0=kf[b], in1=eng, op=ALU.mult), reads=[("gkf", b), ("eng", 0), ("eng", 1)], writes=["kin"])
        p.op("pool", lambda e: e.tensor_copy(out=qA[:, :, 0:64], in_=qin[:, :, 0:64]), reads=["qin"], writes=["qA"])
        p.op("pool", lambda e: e.tensor_copy(out=qB[:, :, 64:128], in_=qin[:, :, 64:128]), reads=["qin"], writes=["qB"])
        p.op("dve", lambda e: e.tensor_tensor(out=ke, in0=ktm[b], in1=egl, op=ALU.mult), reads=[("ktm", b), ("egl", 0), ("egl", 1)], writes=["ke"])
        for hd in range(4):
            vs = slice(hd * 512, (hd + 1) * 512)
            p.mm([(lambda e, dcl=dcl: e.matmul(ATp[:, 0:128], lhsT=kin[:, 2 * hd + dcl, :], rhs=qin[:, 2 * hd + dcl, :], start=(dcl == 0), stop=(dcl == 1)))
                  for dcl in range(2)], reads=["kin", "qin"], writes=["ATp"])
            p.op("dve", lambda e: e.tensor_tensor(out=atm, in0=ATp[:, 0:128], in1=btri, op=ALU.mult), reads=["ATp", "btri"], writes=["atm"])
            for dcl in range(2):
                dc = 2 * hd + dcl
                p.mm([lambda e, dc=dc: e.matmul(Dp[:, 0:512], lhsT=ke[0:64, dc * 128:(dc + 1) * 128], rhs=vt[b][0:64, vs], start=True, stop=True)],
                     reads=["ke", ("gvt", b)], writes=["Dp"])
                p.op("dve", lambda e, dc=dc, dcl=dcl: e.scalar_tensor_tensor(out=S1[:, dcl, :], in0=St[:, dc, :], scalar=eg[:, dc, 63:64], in1=Dp[:, 0:512],
                                                                            op0=ALU.mult, op1=ALU.add), reads=[("St", dc), "Dp"] + egt, writes=[("S1", dcl)])
                p.op("act", lambda e, dcl=dcl: e.activation(out=S1b[:, dcl, :], in_=S1[:, dcl, :], func=AF.Copy), reads=[("S1", dcl)], writes=[("S1b", dcl)])
            fns = [lambda e: e.matmul(Op[:, 0:512], lhsT=atm, rhs=vt[b][:, vs], start=True, stop=False)]
            for dcl in range(2):
                dc = 2 * hd + dcl
                fns.append(lambda e, dc=dc: e.matmul(Op[:, 0:512], lhsT=qA[:, dc, :], rhs=Sb[:, dc, :], start=False, stop=False))
                fns.append(lambda e, dc=dc, dcl=dcl: e.matmul(Op[:, 0:512], lhsT=qB[:, dc, :], rhs=S1b[:, dcl, :], start=False, stop=(dcl == 1)))
            p.mm(fns, reads=["atm", ("gvt", b), "qA", "qB", ("Sb", 2 * hd), ("Sb", 2 * hd + 1), ("S1b", 0), ("S1b", 1)], writes=["Op"])
            for dcl in range(2):
                dc = 2 * hd + dcl
                p.mm([lambda e, dc=dc: e.matmul(Dp[:, 0:512], lhsT=ke[64:128, dc * 128:(dc + 1) * 128], rhs=vt[b][64:128, vs], start=True, stop=True)],
                     reads=["ke", ("gvt", b)], writes=["Dp"])
                p.op("dve", lambda e, dc=dc, dcl=dcl: e.scalar_tensor_tensor(out=St[:, dc, :], in0=S1[:, dcl, :], scalar=eg[:, dc, 127:128], in1=Dp[:, 0:512],
                                                                            op0=ALU.mult, op1=ALU.add), reads=[("S1", dcl), "Dp"] + egt, writes=[("St", dc)])
                p.op("act", lambda e, dc=dc: e.activation(out=Sb[:, dc, :], in_=St[:, dc, :], func=AF.Copy), reads=[("St", dc)], writes=[("Sb", dc)])
            p.op("act", lambda e: e.activation(out=junk, in_=Op[:, 0:512], func=AF.Square, accum_out=sm[:, 0:1]), reads=["Op"], writes=["gjunk", "gsm"])
            p.op("dve", lambda e: e.tensor_scalar(out=sm[:, 1:2], in0=sm[:, 0:1], scalar1=1.0 / 512.0, scalar2=EPS, op0=ALU.mult, op1=ALU.add), reads=["gsm"], writes=["gsm"])
            p.op("act", lambda e: e.activation(out=sm[:, 2:3], in_=sm[:, 1:2], func=AF.Sqrt), reads=["gsm"], writes=["gsm"])
            p.op("dve", lambda e: e.reciprocal(out=sm[:, 3:4], in_=sm[:, 2:3]), reads=["gsm"], writes=["gsm"])
            p.op("dve", lambda e: e.scalar_tensor_tensor(out=on, in0=Op[:, 0:512], scalar=sm[:, 3:4], in1=gnb, op0=ALU.mult, op1=ALU.mult),
                 reads=["Op", "gsm", gntok], writes=["on"])
            p.op("pool", lambda e: e.tensor_tensor(out=ybf, in0=on, in1=sg[b][:, vs], op=ALU.mult), reads=["on", ("gsg", b)], writes=["gybf"])
            p.mm([(lambda e, j=j: e.transpose(out=tp[:, j * 128:(j + 1) * 128], in_=ybf[:, j * 128:(j + 1) * 128], identity=identb)) for j in range(4)],
                 reads=["gybf", "identb"], writes=["gtp"])
            p.op("act", lambda e: e.activation(out=yT[:, hd * 4:(hd + 1) * 4, cs], in_=tp[:, 0:512].rearrange("p (a c) -> p a c", a=4), func=AF.Copy),
                 reads=["gtp"], writes=[("gyT", t, hd)])
    for k in range(16):
        p.dma(dr["yT"][k * 128:(k + 1) * 128, :], yT[:, k, :], reads=[("gyT", t, k // 4) for t in range(NT)])
    S.close()


def stage_moe_local(cx, L, h_in, g_row, b_row, h_out, NE=32):
    p, nc, dr = cx.p, cx.nc, cx.dram
    S = Scope(p)
    ident = load_const(cx, S, "ident", "ident")
    mps = S.ps("emps", [128, 512], F32)
    nrow = NE * 16
    nch = (nrow + 127) // 128
    b1g = S.sb("b1g", [128, nrow], F32)
    b1l = S.sb("b1l", [128, nrow], F32)
    b1st = S.sb("b1st", [128, 256], F32)
    b1v = dr["moe_b1"][L * NE:(L + 1) * NE, :].rearrange("e (f c) -> (e f) c", c=256)
    for ch in range(nch):
        r = min(128, nrow - ch * 128)
        p.dma(b1st[0:r, :], b1v[ch * 128:ch * 128 + r, :], writes=["b1st"])
        p.mm([lambda e, r=r: e.transpose(out=mps[:, 0:r], in_=b1st[0:r, 0:256:2], identity=ident[0:r, 0:r]),
              lambda e, r=r: e.transpose(out=mps[:, 128:128 + r], in_=b1st[0:r, 1:256:2], identity=ident[0:r, 0:r])], reads=["b1st", "ident"], writes=["emps"])
        p.op("dve", lambda e, r=r, ch=ch: e.tensor_copy(out=b1g[:, ch * 128:ch * 128 + r], in_=mps[:, 0:r]), reads=["emps"], writes=["b1g"])
        p.op("dve", lambda e, r=r, ch=ch: e.tensor_copy(out=b1l[:, ch * 128:ch * 128 + r], in_=mps[:, 128:128 + r]), reads=["emps"], writes=["b1l"])
    hTg = S.sb("hTg", [128, KC, 1024], BF16)
    actT = S.sb("actT", [128, KC, 1024], BF16)
    acc = S.sb("acc", [128, 8, D], F32)
    gts = S.sb("gts", [128, 8, 32], F32)
    wst = [S.sb("wst", [128, KC, 256], F32) for _ in range(2)]
    wbf = [S.sb("wbf", [128, KC, 256], BF16) for _ in range(2)]
    b2t = [S.sb("b2t", [128, 256], F32) for _ in range(2)]
    ga = [S.sb("ga", [128, 512], F32) for _ in range(2)]
    sg = [S.sb("sg", [128, 512], F32)] * 2
    la = [S.sb("la", [128, 512], F32)] * 2
    yb = [S.sb("yb", [128, 256], F32) for _ in range(2)]
    hld = S.sb("hld", [128, D], F32)
    sm = S.sb("sm", [128, 8], F32)
    Gp = [S.ps("Gp", [128, 512], F32) for _ in range(2)]
    Lp = [S.ps("Lp", [128, 512], F32) for _ in range(2)]
    Yp = [S.ps("Yp", [128, 512], F32) for _ in range(2)]
    st_ = {"n": 0, "it": 0}

    def fetch(w2d, r0, c0, deint):
        b = st_["n"] % 2
        st_["n"] += 1
        src = w2d[r0:r0 + D, c0:c0 + 256].rearrange("(k p) n -> p k n", p=128)
        p.dma(wst[b], src, writes=[("wst", b)])
        if deint:
            sv = wst[b].rearrange("p k (i two) -> p k i two", two=2)
            p.op("act", lambda e, b=b, sv=sv: e.activation(out=wbf[b][:, :, 0:128], in_=sv[:, :, :, 0], func=AF.Copy), reads=[("wst", b)], writes=[("wbf", b, 0)])
            p.op("pool", lambda e, b=b, sv=sv: e.tensor_copy(out=wbf[b][:, :, 128:256], in_=sv[:, :, :, 1]), reads=[("wst", b)], writes=[("wbf", b, 1)])
        else:
            p.op("act", lambda e, b=b: e.activation(out=wbf[b][:, 0:8, :], in_=wst[b][:, 0:8, :], func=AF.Copy), reads=[("wst", b)], writes=[("wbf", b, 0)])
            p.op("pool", lambda e, b=b: e.tensor_copy(out=wbf[b][:, 8:16, :], in_=wst[b][:, 8:16, :]), reads=[("wst", b)], writes=[("wbf", b, 1)])
        return b
    hv = h_in.rearrange("(t p) d -> t p d", p=128)
    ov = h_out.rearrange("(t p) d -> t p d", p=128)
    for gi in range(2):
        p.dma(hTg, dr["hTo"].rearrange("(k p) t -> p k t", p=128)[:, :, gi * 1024:(gi + 1) * 1024], writes=["hTg"])
        p.dma(gts, dr["gateo"][gi * 1024:(gi + 1) * 1024, :].rearrange("(t p) e -> p t e", p=128), writes=["gts"])
        for e_ in range(NE):
            r0 = (L * NE + e_) * D
            nb = fetch(dr["moe_w1"], r0, 0, True)
            for fc in range(16):
                b1 = nb
                if fc + 1 < 16:
                    nb = fetch(dr["moe_w1"], r0, (fc + 1) * 256, True)
                else:
                    nb = fetch(dr["moe_w2"], r0, 0, False)
                wt = [("wbf", b1, 0), ("wbf", b1, 1)]
                for tb in range(2):
                    i2 = st_["it"] % 2
                    st_["it"] += 1
                    tsl = slice(tb * 512, (tb + 1) * 512)
                    p.mm([(lambda e, c=c: e.matmul(Gp[i2][:, 0:512], lhsT=wbf[b1][:, c, 0:128], rhs=hTg[:, c, tsl], start=(c == 0), stop=(c == KC - 1)))
                          for c in range(KC)], reads=wt + ["hTg"], writes=[("Gp", i2)])
                    p.mm([(lambda e, c=c: e.matmul(Lp[i2][:, 0:512], lhsT=wbf[b1][:, c, 128:256], rhs=hTg[:, c, tsl], start=(c == 0), stop=(c == KC - 1)))
                          for c in range(KC)], reads=wt + ["hTg"], writes=[("Lp", i2)])
                    col = e_ * 16 + fc
                    p.op("dve", lambda e: e.tensor_scalar(out=ga[i2], in0=Gp[i2][:, 0:512], scalar1=b1g[:, col:col + 1], scalar2=7.0, op0=ALU.add, op1=ALU.min),
                         reads=[("Gp", i2), "b1g"], writes=[("ga", i2)])
                    p.op("act", lambda e: e.activation(out=sg[i2], in_=ga[i2], func=AF.Sigmoid, scale=1.702), reads=[("ga", i2)], writes=["sg1"])
                    p.op("dve", lambda e: e.tensor_scalar(out=la[i2], in0=Lp[i2][:, 0:512], scalar1=b1l[:, col:col + 1], scalar2=7.0, op0=ALU.add, op1=ALU.min),
                         reads=[("Lp", i2), "b1l"], writes=["la1"])
                    p.op("dve", lambda e: e.tensor_scalar(out=la[i2], in0=la[i2], scalar1=-7.0, scalar2=1.0, op0=ALU.max, op1=ALU.add),
                         reads=["la1"], writes=["la1"])
                    p.op("pool", lambda e: e.tensor_tensor(out=ga[i2], in0=ga[i2], in1=sg[i2], op=ALU.mult), reads=[("ga", i2), "sg1"], writes=[("ga", i2)])
                    p.op("pool", lambda e: e.tensor_tensor(out=actT[:, fc, tsl], in0=ga[i2], in1=la[i2], op=ALU.mult), reads=[("ga", i2), "la1"],
                         writes=[("actT", fc, tb)])
            for dp in range(8):
                b2 = nb
                if dp + 1 < 8:
                    nb = fetch(dr["moe_w2"], r0, (dp + 1) * 256, False)
                wt = [("wbf", b2, 0), ("wbf", b2, 1)]
                dsl = slice(dp * 256, (dp + 1) * 256)
                bb = dp % 2
                p.dma(b2t[bb], dr["moe_b2"][L * NE + e_:L * NE + e_ + 1, dsl].partition_broadcast(128), writes=[("b2t", bb)])
                for t in range(8):
                    i2 = st_["it"] % 2
                    st_["it"] += 1
                    p.mm([(lambda e, c=c: e.matmul(Yp[i2][:, 0:256], lhsT=actT[:, c, t * 128:(t + 1) * 128], rhs=wbf[b2][:, c, :], start=(c == 0), stop=(c == KC - 1)))
                          for c in range(KC)], reads=wt + [("actT", c, t // 4) for c in range(KC)], writes=[("Yp", i2)])
                    p.op("dve", lambda e: e.tensor_tensor(out=yb[i2], in0=Yp[i2][:, 0:256], in1=b2t[bb], op=ALU.add), reads=[("Yp", i2), ("b2t", bb)],
                         writes=[("yb", i2)])
                    if e_ == 0:
                        p.op("dve", lambda e: e.tensor_scalar(out=acc[:, t, dsl], in0=yb[i2], scalar1=gts[:, t, e_:e_ + 1], scalar2=None, op0=ALU.mult),
                             reads=[("yb", i2), "gts"], writes=[("acc", t, dp)])
                    else:
                        p.op("dve", lambda e: e.scalar_tensor_tensor(out=acc[:, t, dsl], in0=yb[i2], scalar=gts[:, t, e_:e_ + 1], in1=acc[:, t, dsl],
                                                                    op0=ALU.mult, op1=ALU.add), reads=[("yb", i2), "gts", ("acc", t, dp)], writes=[("acc", t, dp)])
        gbc = wst[0].rearrange("p k c -> p (k c)")[:, 0:D]
        bbc = wst[1].rearrange("p k c -> p (k c)")[:, 0:D]
        gtok, btok = ("wst", 0), ("wst", 1)
        p.dma(gbc, g_row.partition_broadcast(128), writes=[gtok])
        p.dma(bbc, b_row.partition_broadcast(128), writes=[btok])
        for t in range(8):
            at = [("acc", t, dp) for dp in range(8)]
            p.dma(hld, hv[gi * 8 + t], writes=["hld"])
            p.op("dve", lambda e, t=t: e.scalar_tensor_tensor(out=acc[:, t, :], in0=hld, scalar=DN_ALPHA, in1=acc[:, t, :], op0=ALU.mult, op1=ALU.add),
                 reads=["hld"] + at, writes=at)
            ln_tile(cx, acc[:, t, :], ("acc", t, 0), hld, "hld", sm, "sm", gbc, gtok, bbc, btok)
            p.dma(ov[gi * 8 + t], acc[:, t, :], reads=at)
    S.close()


CAP = 256


def stage_moe_sparse(cx, L, h_in, g_row, b_row, h_out, NE=32):
    p, nc, dr = cx.p, cx.nc, cx.dram
    S = Scope(p)
    ident = load_const(cx, S, "ident", "ident")
    identb = load_const(cx, S, "identb", "identb")
    trius = load_const(cx, S, "trius", "trius")
    ones128 = load_const(cx, S, "ones128", "ones128")
    iota = load_const(cx, S, "iota", "iota")
    GA = [S.ps("GA", [128, 512], F32) for _ in range(2)]
    Gp = S.ps("Gp", [128, 512], F32)
    Lp = S.ps("Lp", [128, 512], F32)
    Yp = S.ps("Yp", [128, 512], F32)
    SC = S.ps("SC", [128, 512], F32)
    PT = [S.ps("PT", [128, 1024], BF16) for _ in range(2)]
    nrow = NE * 16
    nch = (nrow + 127) // 128
    b1g = S.sb("b1g", [128, nrow], F32)
    b1l = S.sb("b1l", [128, nrow], F32)
    b1st = S.sb("b1st", [128, 256], F32)
    b1v = dr["moe_b1"][L * NE:(L + 1) * NE, :].rearrange("e (f c) -> (e f) c", c=256)
    for ch in range(nch):
        r = min(128, nrow - ch * 128)
        p.dma(b1st[0:r, :], b1v[ch * 128:ch * 128 + r, :], writes=["b1st"])
        p.mm([lambda e, r=r: e.transpose(out=SC[:, 0:r], in_=b1st[0:r, 0:256:2], identity=ident[0:r, 0:r]),
              lambda e, r=r: e.transpose(out=SC[:, 128:128 + r], in_=b1st[0:r, 1:256:2], identity=ident[0:r, 0:r])], reads=["b1st", "ident"], writes=["SC"])
        p.op("dve", lambda e, r=r, ch=ch: e.tensor_copy(out=b1g[:, ch * 128:ch * 128 + r], in_=SC[:, 0:r]), reads=["SC"], writes=["b1g"])
        p.op("dve", lambda e, r=r, ch=ch: e.tensor_copy(out=b1l[:, ch * 128:ch * 128 + r], in_=SC[:, 128:128 + r]), reads=["SC"], writes=["b1l"])
    hb = S.sb("hb", [128, 8, D], BF16)
    acc = S.sb("acc", [128, 8, D], F32)
    gts = S.sb("gts", [128, 8, 32], F32)
    sel = S.sb("sel", [128, 8, 32], F32)
    rank = S.sb("rank", [128, 8, 32], F32)
    Pm = [S.sb("Pm", [128, 8, CAP], BF16)] * 2
    PG = S.sb("PG", [128, 8, CAP], BF16)
    PGT = S.sb("PGT", [128, 2, 8, 128], BF16)
    xgT = S.sb("xgT", [128, KC, CAP], BF16)
    actT = S.sb("actT", [128, KC, CAP], BF16)
    Yb = S.sb("Yb", [128, 2, D], BF16)
    wst = [S.sb("wst", [128, KC, 256], F32) for _ in range(2)]
    wbf = [S.sb("wbf", [128, KC, 256], BF16) for _ in range(2)]
    b2t = [S.sb("b2t", [128, 256], F32) for _ in range(2)]
    ga = [S.sb("ga", [128, CAP], F32) for _ in range(2)]
    sg = S.sb("sg", [128, CAP], F32)
    la = S.sb("la", [128, CAP], F32)
    hld = S.sb("hld", [128, D], F32)
    sm = S.sb("sm", [128, 8], F32)
    st_ = {"n": 0, "it": 0}

    def fetch(w2d, r0, c0, deint):
        b = st_["n"] % 2
        st_["n"] += 1
        src = w2d[r0:r0 + D, c0:c0 + 256].rearrange("(k p) n -> p k n", p=128)
        p.dma(wst[b], src, writes=[("wst", b)])
        if deint:
            sv = wst[b].rearrange("p k (i two) -> p k i two", two=2)
            p.op("act", lambda e, b=b, sv=sv: e.activation(out=wbf[b][:, :, 0:128], in_=sv[:, :, :, 0], func=AF.Copy), reads=[("wst", b)], writes=[("wbf", b, 0)])
            p.op("pool", lambda e, b=b, sv=sv: e.tensor_copy(out=wbf[b][:, :, 128:256], in_=sv[:, :, :, 1]), reads=[("wst", b)], writes=[("wbf", b, 1)])
        else:
            p.op("act", lambda e, b=b: e.activation(out=wbf[b][:, 0:8, :], in_=wst[b][:, 0:8, :], func=AF.Copy), reads=[("wst", b)], writes=[("wbf", b, 0)])
            p.op("pool", lambda e, b=b: e.tensor_copy(out=wbf[b][:, 8:16, :], in_=wst[b][:, 8:16, :]), reads=[("wst", b)], writes=[("wbf", b, 1)])
        return b
    hv = h_in.rearrange("(t p) d -> t p d", p=128)
    ov = h_out.rearrange("(t p) d -> t p d", p=128)
    for gi in range(2):
        for t in range(8):
            p.dma(hld, hv[gi * 8 + t], writes=["hld"])
            p.op("pool" if t % 2 else "act", (lambda e, t=t: e.tensor_copy(out=hb[:, t, :], in_=hld)) if t % 2 else
                 (lambda e, t=t: e.activation(out=hb[:, t, :], in_=hld, func=AF.Copy)), reads=["hld"], writes=[("hb", t)])
        p.dma(gts, dr["gateo"][gi * 1024:(gi + 1) * 1024, :].rearrange("(t p) e -> p t e", p=128), writes=["gts"])
        p.op("dve", lambda e: e.tensor_scalar(out=sel, in0=gts, scalar1=0.0, scalar2=0.0, op0=ALU.is_gt, op1=ALU.add), reads=["gts"], writes=["sel"])
        for t in range(8):
            fns = [lambda e, t=t: e.matmul(GA[0][:, 0:32], lhsT=trius, rhs=sel[:, t, :], start=True, stop=(t == 0))]
            for t2 in range(t):
                fns.append(lambda e, t2=t2, t=t: e.matmul(GA[0][:, 0:32], lhsT=ones128, rhs=sel[:, t2, :], start=False, stop=(t2 == t - 1)))
            p.mm(fns, reads=["sel", "trius", "ones128"], writes=[("GA", 0)])
            p.op("dve", lambda e, t=t: e.tensor_copy(out=rank[:, t, :], in_=GA[0][:, 0:32]), reads=[("GA", 0)], writes=["rank"])
        hbt = toks("hb", range(8))
        for e_ in range(NE):
            r0 = (L * NE + e_) * D
            nb = fetch(dr["moe_w1"], r0, 0, True)
            pb = 0
            for t in range(8):
                p.op("dve", lambda e, t=t: e.tensor_scalar(out=Pm[pb][:, t, :], in0=iota, scalar1=rank[:, t, e_:e_ + 1], scalar2=sel[:, t, e_:e_ + 1],
                                                        op0=ALU.is_equal, op1=ALU.mult), reads=["iota", "rank", "sel"], writes=[("Pm", pb)])
                p.op("dve", lambda e, t=t: e.tensor_scalar(out=PG[:, t, :], in0=iota, scalar1=rank[:, t, e_:e_ + 1], scalar2=gts[:, t, e_:e_ + 1],
                                                        op0=ALU.is_equal, op1=ALU.mult), reads=["iota", "rank", "gts"], writes=["PG"])
            for st in range(2):
                p.mm([(lambda e, t=t, st=st: e.transpose(out=PT[st][:, t * 128:(t + 1) * 128], in_=PG[:, t, st * 128:(st + 1) * 128], identity=identb))
                      for t in range(8)], reads=["PG", "identb"], writes=[("PT", st)])
                p.op("act", lambda e, st=st: e.activation(out=PGT[:, st, :, :], in_=PT[st].rearrange("p (a c) -> p a c", a=8), func=AF.Copy),
                     reads=[("PT", st)], writes=[("PGT", st)])
            for k2 in range(8):
                gb = k2 % 2
                fns = []
                for j in range(2):
                    kc = k2 * 2 + j
                    for t in range(8):
                        fns.append(lambda e, kc=kc, t=t, j=j: e.matmul(GA[gb][:, j * 256:(j + 1) * 256], lhsT=hb[:, t, kc * 128:(kc + 1) * 128], rhs=Pm[pb][:, t, :],
                                                                       start=(t == 0), stop=(t == 7)))
                p.mm(fns, reads=hbt + [("Pm", pb)], writes=[("GA", gb)])
                p.op("act" if k2 % 2 else "dve",
                     (lambda e, k2=k2, gb=gb: e.activation(out=xgT[:, k2 * 2:k2 * 2 + 2, :], in_=GA[gb].rearrange("p (a c) -> p a c", a=2), func=AF.Copy)) if k2 % 2 else
                     (lambda e, k2=k2, gb=gb: e.tensor_copy(out=xgT[:, k2 * 2:k2 * 2 + 2, :], in_=GA[gb].rearrange("p (a c) -> p a c", a=2))),
                     reads=[("GA", gb)], writes=[("xgT", k2)])
            xt = toks("xgT", range(8))
            for fc in range(16):
                b1 = nb
                if fc + 1 < 16:
                    nb = fetch(dr["moe_w1"], r0, (fc + 1) * 256, True)
                else:
                    nb = fetch(dr["moe_w2"], r0, 0, False)
                wt = [("wbf", b1, 0), ("wbf", b1, 1)]
                i2 = st_["it"] % 2
                st_["it"] += 1
                p.mm([(lambda e, c=c: e.matmul(Gp[:, 0:CAP], lhsT=wbf[b1][:, c, 0:128], rhs=xgT[:, c, :], start=(c == 0), stop=(c == KC - 1)))
                      for c in range(KC)], reads=wt + xt, writes=["Gp"])
                p.mm([(lambda e, c=c: e.matmul(Lp[:, 0:CAP], lhsT=wbf[b1][:, c, 128:256], rhs=xgT[:, c, :], start=(c == 0), stop=(c == KC - 1)))
                      for c in range(KC)], reads=wt + xt, writes=["Lp"])
                col = e_ * 16 + fc
                p.op("dve", lambda e: e.tensor_scalar(out=ga[i2], in0=Gp[:, 0:CAP], scalar1=b1g[:, col:col + 1], scalar2=7.0, op0=ALU.add, op1=ALU.min),
                     reads=["Gp", "b1g"], writes=[("ga", i2)])
                p.op("act", lambda e: e.activation(out=sg, in_=ga[i2], func=AF.Sigmoid, scale=1.702), reads=[("ga", i2)], writes=["sg"])
                p.op("dve", lambda e: e.tensor_scalar(out=la, in0=Lp[:, 0:CAP], scalar1=b1l[:, col:col + 1], scalar2=7.0, op0=ALU.add, op1=ALU.min),
                     reads=["Lp", "b1l"], writes=["la"])
                p.op("dve", lambda e: e.tensor_scalar(out=la, in0=la, scalar1=-7.0, scalar2=1.0, op0=ALU.max, op1=ALU.add), reads=["la"], writes=["la"])
                p.op("pool", lambda e: e.tensor_tensor(out=ga[i2], in0=ga[i2], in1=sg, op=ALU.mult), reads=[("ga", i2), "sg"], writes=[("ga", i2)])
                p.op("pool", lambda e: e.tensor_tensor(out=actT[:, fc, :], in0=ga[i2], in1=la, op=ALU.mult), reads=[("ga", i2), "la"], writes=[("actT", fc)])
            at_ = toks("actT", range(16))
            for dp in range(8):
                b2 = nb
                if dp + 1 < 8:
                    nb = fetch(dr["moe_w2"], r0, (dp + 1) * 256, False)
                wt = [("wbf", b2, 0), ("wbf", b2, 1)]
                dsl = slice(dp * 256, (dp + 1) * 256)
                bb = dp % 2
                p.dma(b2t[bb], dr["moe_b2"][L * NE + e_:L * NE + e_ + 1, dsl].partition_broadcast(128), writes=[("b2t", bb)])
                for st in range(2):
                    p.mm([(lambda e, c=c: e.matmul(Yp[:, 0:256], lhsT=actT[:, c, st * 128:(st + 1) * 128], rhs=wbf[b2][:, c, :], start=(c == 0), stop=(c == KC - 1)))
                          for c in range(KC)], reads=wt + at_, writes=["Yp"])
                    p.op("dve", lambda e, st=st: e.tensor_tensor(out=Yb[:, st, dsl], in0=Yp[:, 0:256], in1=b2t[bb], op=ALU.add), reads=["Yp", ("b2t", bb)],
                         writes=[("Yb", st, dp)])
            yt = [("Yb", st, dp) for st in range(2) for dp in range(8)]
            for t in range(8):
                for dq in range(4):
                    qs = slice(dq * 512, (dq + 1) * 512)
                    p.mm([(lambda e, st=st: e.matmul(SC[:, 0:512], lhsT=PGT[:, st, t, :], rhs=Yb[:, st, qs], start=(st == 0), stop=(st == 1))) for st in range(2)],
                         reads=yt + [("PGT", 0), ("PGT", 1)], writes=["SC"])
                    if e_ == 0:
                        p.op("dve", lambda e: e.tensor_copy(out=acc[:, t, qs], in_=SC[:, 0:512]), reads=["SC"], writes=[("acc", t, dq)])
                    else:
                        p.op("dve", lambda e: e.tensor_tensor(out=acc[:, t, qs], in0=SC[:, 0:512], in1=acc[:, t, qs], op=ALU.add), reads=["SC", ("acc", t, dq)],
                             writes=[("acc", t, dq)])
        gbc = wst[0].rearrange("p k c -> p (k c)")[:, 0:D]
        bbc = wst[1].rearrange("p k c -> p (k c)")[:, 0:D]
        gtok, btok = ("wst", 0), ("wst", 1)
        p.dma(gbc, g_row.partition_broadcast(128), writes=[gtok])
        p.dma(bbc, b_row.partition_broadcast(128), writes=[btok])
        for t in range(8):
            at = [("acc", t, dq) for dq in range(4)]
            p.dma(hld, hv[gi * 8 + t], writes=["hld"])
            p.op("dve", lambda e, t=t: e.scalar_tensor_tensor(out=acc[:, t, :], in0=hld, scalar=DN_ALPHA, in1=acc[:, t, :], op0=ALU.mult, op1=ALU.add),
                 reads=["hld"] + at, writes=at)
            ln_tile(cx, acc[:, t, :], ("acc", t, 0), hld, "hld", sm, "sm", gbc, gtok, bbc, btok)
            p.dma(ov[gi * 8 + t], acc[:, t, :], reads=at)
    S.close()


def stage_moe_sparse2(cx, L, h_in, g_row, b_row, h_out, NE=32):
    p, nc, dr = cx.p, cx.nc, cx.dram
    hv = h_in.rearrange("(t p) d -> t p d", p=128)
    ov = h_out.rearrange("(t p) d -> t p d", p=128)
    XG = dr["XG"].rearrange("(e p) x -> e p x", p=128)
    PGd = dr["PGTd"].rearrange("(e p) x -> e p x", p=128)
    YBd = dr["YBd"].rearrange("(e p) x -> e p x", p=128)
    S = Scope(p)
    identb = load_const(cx, S, "identb", "identb")
    trius = load_const(cx, S, "trius", "trius")
    ones128 = load_const(cx, S, "ones128", "ones128")
    iota = load_const(cx, S, "iota", "iota")
    GA = [S.ps("GA", [128, 512], F32) for _ in range(4)]
    PT = [S.ps("PT", [128, 1024], BF16) for _ in range(2)]
    RK = S.ps("RK", [128, 512], F32)
    hb = S.sb("hb", [128, 8, D], BF16)
    hld = [S.sb("hld", [128, D], F32) for _ in range(2)]
    gts = S.sb("gts", [128, 8, 32], F32)
    sel = S.sb("sel", [128, 8, 32], F32)
    rank = S.sb("rank", [128, 8, 32], F32)
    Pm = [S.sb("Pm", [128, 8, CAP], BF16) for _ in range(2)]
    PG = [S.sb("PG", [128, 8, CAP], BF16) for _ in range(2)]
    PGT = [S.sb("PGT", [128, 2, 8, 128], BF16) for _ in range(2)]
    xgT = [S.sb("xgT", [128, KC, CAP], BF16) for _ in range(2)]
    for gi in range(2):
        for t in range(8):
            hl = hld[t % 2]
            p.dma(hl, hv[gi * 8 + t], writes=[("hld", t % 2)])
            if t % 2:
                p.op("pool", lambda e, t=t, hl=hl: e.tensor_copy(out=hb[:, t, :], in_=hl), reads=[("hld", t % 2)], writes=[("hb", t)])
            else:
                p.op("act", lambda e, t=t, hl=hl: e.activation(out=hb[:, t, :], in_=hl, func=AF.Copy), reads=[("hld", t % 2)], writes=[("hb", t)])
        p.dma(gts, dr["gateo"][gi * 1024:(gi + 1) * 1024, :].rearrange("(t p) e -> p t e", p=128), writes=["gts"])
        p.op("dve", lambda e: e.tensor_scalar(out=sel, in0=gts, scalar1=0.0, scalar2=0.0, op0=ALU.is_gt, op1=ALU.add), reads=["gts"], writes=["sel"])
        for t in range(8):
            fns = [lambda e, t=t: e.matmul(RK[:, 0:32], lhsT=trius, rhs=sel[:, t, :], start=True, stop=(t == 0))]
            for t2 in range(t):
                fns.append(lambda e, t2=t2, t=t: e.matmul(RK[:, 0:32], lhsT=ones128, rhs=sel[:, t2, :], start=False, stop=(t2 == t - 1)))
            p.mm(fns, reads=["sel", "trius", "ones128"], writes=["RK"])
            p.op("dve", lambda e, t=t: e.tensor_copy(out=rank[:, t, :], in_=RK[:, 0:32]), reads=["RK"], writes=["rank"])
        hbt = toks("hb", range(8))
        k4 = 0
        for e_ in range(NE):
            pb = e_ % 2
            for t in range(8):
                p.op("dve", lambda e, t=t: e.tensor_scalar(out=Pm[pb][:, t, :], in0=iota, scalar1=rank[:, t, e_:e_ + 1], scalar2=sel[:, t, e_:e_ + 1],
                                                        op0=ALU.is_equal, op1=ALU.mult), reads=["iota", "rank", "sel"], writes=[("Pm", pb)])
                p.op("dve", lambda e, t=t: e.tensor_scalar(out=PG[pb][:, t, :], in0=iota, scalar1=rank[:, t, e_:e_ + 1], scalar2=gts[:, t, e_:e_ + 1],
                                                        op0=ALU.is_equal, op1=ALU.mult), reads=["iota", "rank", "gts"], writes=[("PG", pb)])
            for st in range(2):
                p.mm([(lambda e, t=t, st=st: e.transpose(out=PT[st][:, t * 128:(t + 1) * 128], in_=PG[pb][:, t, st * 128:(st + 1) * 128], identity=identb))
                      for t in range(8)], reads=[("PG", pb), "identb"], writes=[("PT", st)])
                p.op("act", lambda e, st=st: e.activation(out=PGT[pb][:, st, :, :], in_=PT[st].rearrange("p (a c) -> p a c", a=8), func=AF.Copy),
                     reads=[("PT", st)], writes=[("PGT", pb, st)])
            p.dma(PGd[e_][:, gi * 2048:(gi + 1) * 2048], PGT[pb].rearrange("p a b c -> p (a b c)"), reads=[("PGT", pb, 0), ("PGT", pb, 1)])
            for k2 in range(8):
                gb = k4 % 4
                k4 += 1
                fns = []
                for j in range(2):
                    kc = k2 * 2 + j
                    for t in range(8):
                        fns.append(lambda e, kc=kc, t=t, j=j, gb=gb: e.matmul(GA[gb][:, j * 256:(j + 1) * 256], lhsT=hb[:, t, kc * 128:(kc + 1) * 128], rhs=Pm[pb][:, t, :],
                                                                              start=(t == 0), stop=(t == 7)))
                p.mm(fns, reads=hbt + [("Pm", pb)], writes=[("GA", gb)])
                if k2 % 2:
                    p.op("act", lambda e, k2=k2, gb=gb: e.activation(out=xgT[pb][:, k2 * 2:k2 * 2 + 2, :], in_=GA[gb].rearrange("p (a c) -> p a c", a=2), func=AF.Copy),
                         reads=[("GA", gb)], writes=[("xgT", pb, k2)])
                else:
                    p.op("pool" if False else "dve", lambda e, k2=k2, gb=gb: e.tensor_copy(out=xgT[pb][:, k2 * 2:k2 * 2 + 2, :], in_=GA[gb].rearrange("p (a c) -> p a c", a=2)),
                         reads=[("GA", gb)], writes=[("xgT", pb, k2)])
            p.dma(XG[e_].rearrange("p (k c) -> p k c", c=512)[:, :, gi * 256:(gi + 1) * 256], xgT[pb], reads=[("xgT", pb, k2) for k2 in range(8)], writes=["XG"])
    S.close()
    S = Scope(p)
    ident = load_const(cx, S, "ident", "ident")
    Gp = [S.ps("Gp", [128, 512], F32) for _ in range(2)]
    Lp = [S.ps("Lp", [128, 512], F32) for _ in range(2)]
    Yp = [S.ps("Yp", [128, 512], F32) for _ in range(2)]
    BP = S.ps("BP", [128, 512], F32)
    nrow = NE * 16
    nch = (nrow + 127) // 128
    b1g = S.sb("b1g", [128, nrow], F32)
    b1l = S.sb("b1l", [128, nrow], F32)
    b1st = S.sb("b1st", [128, 256], F32)
    b1v = dr["moe_b1"][L * NE:(L + 1) * NE, :].rearrange("e (f c) -> (e f) c", c=256)
    for ch in range(nch):
        r = min(128, nrow - ch * 128)
        p.dma(b1st[0:r, :], b1v[ch * 128:ch * 128 + r, :], writes=["b1st"])
        p.mm([lambda e, r=r: e.transpose(out=BP[:, 0:r], in_=b1st[0:r, 0:256:2], identity=ident[0:r, 0:r]),
              lambda e, r=r: e.transpose(out=BP[:, 128:128 + r], in_=b1st[0:r, 1:256:2], identity=ident[0:r, 0:r])], reads=["b1st", "ident"], writes=["BP"])
        p.op("dve", lambda e, r=r, ch=ch: e.tensor_copy(out=b1g[:, ch * 128:ch * 128 + r], in_=BP[:, 0:r]), reads=["BP"], writes=["b1g"])
        p.op("dve", lambda e, r=r, ch=ch: e.tensor_copy(out=b1l[:, ch * 128:ch * 128 + r], in_=BP[:, 128:128 + r]), reads=["BP"], writes=["b1l"])
    NB = 4
    wst = [S.sb("wst", [128, KC, 256], F32) for _ in range(NB)]
    wbf = [S.sb("wbf", [128, KC, 256], BF16) for _ in range(NB)]
    xg = [S.sb("xg", [128, KC, 512], BF16) for _ in range(2)]
    actT = S.sb("actT", [128, KC, 512], BF16)
    Yb = [S.sb("Yb", [128, 4, D], BF16) for _ in range(2)]
    b2t = [S.sb("b2t", [128, 256], F32) for _ in range(2)]
    ga = [S.sb("ga", [128, 512], F32) for _ in range(2)]
    sg = [S.sb("sg", [128, 512], F32) for _ in range(2)]
    la = [S.sb("la", [128, 512], F32) for _ in range(2)]
    st_ = {"n": 0, "it": 0}
    plist = []
    for e_ in range(NE):
        r0 = (L * NE + e_) * D
        plist += [(e_, "w1", r0, fc * 256) for fc in range(16)] + [(e_, "w2", r0, dp * 256) for dp in range(8)]
    fetched = {}

    def fetch(i):
        if i >= len(plist) or i in fetched:
            return
        e_, kind, r0, c0 = plist[i]
        b = i % NB
        w2d = dr["moe_w1"] if kind == "w1" else dr["moe_w2"]
        src = w2d[r0:r0 + D, c0:c0 + 256].rearrange("(k p) n -> p k n", p=128)
        p.dma(wst[b], src, writes=[("wst", b)])
        if kind == "w1":
            sv = wst[b].rearrange("p k (i two) -> p k i two", two=2)
            p.op("act", lambda e, b=b, sv=sv: e.activation(out=wbf[b][:, :, 0:128], in_=sv[:, :, :, 0], func=AF.Copy), reads=[("wst", b)], writes=[("wbf", b, 0)])
            p.op("pool", lambda e, b=b, sv=sv: e.tensor_copy(out=wbf[b][:, :, 128:256], in_=sv[:, :, :, 1]), reads=[("wst", b)], writes=[("wbf", b, 1)])
        else:
            p.op("act", lambda e, b=b: e.activation(out=wbf[b][:, 0:8, :], in_=wst[b][:, 0:8, :], func=AF.Copy), reads=[("wst", b)], writes=[("wbf", b, 0)])
            p.op("pool", lambda e, b=b: e.tensor_copy(out=wbf[b][:, 8:16, :], in_=wst[b][:, 8:16, :]), reads=[("wst", b)], writes=[("wbf", b, 1)])
        fetched[i] = b
    for i in range(NB - 1):
        fetch(i)
    p.dma(xg[0].rearrange("p k c -> p (k c)"), XG[0], reads=["XG"], writes=[("xg", 0)])
    pi = 0
    for e_ in range(NE):
        xb = e_ % 2
        if e_ + 1 < NE:
            p.dma(xg[(e_ + 1) % 2].rearrange("p k c -> p (k c)"), XG[e_ + 1], reads=["XG"], writes=[("xg", (e_ + 1) % 2)])
        for fc in range(16):
            fetch(pi + NB - 1)
            b1 = fetched[pi]
            pi += 1
            wt = [("wbf", b1, 0), ("wbf", b1, 1)]
            i2 = st_["it"] % 2
            st_["it"] += 1
            p.mm([(lambda e, c=c: e.matmul(Gp[i2][:, 0:512], lhsT=wbf[b1][:, c, 0:128], rhs=xg[xb][:, c, :], start=(c == 0), stop=(c == KC - 1)))
                  for c in range(KC)], reads=wt + [("xg", xb)], writes=[("Gp", i2)])
            p.mm([(lambda e, c=c: e.matmul(Lp[i2][:, 0:512], lhsT=wbf[b1][:, c, 128:256], rhs=xg[xb][:, c, :], start=(c == 0), stop=(c == KC - 1)))
                  for c in range(KC)], reads=wt + [("xg", xb)], writes=[("Lp", i2)])
            col = e_ * 16 + fc
            p.op("dve", lambda e: e.tensor_scalar(out=ga[i2], in0=Gp[i2][:, 0:512], scalar1=b1g[:, col:col + 1], scalar2=7.0, op0=ALU.add, op1=ALU.min),
                 reads=[("Gp", i2), "b1g"], writes=[("ga", i2)])
            p.op("act", lambda e: e.activation(out=sg[i2], in_=ga[i2], func=AF.Sigmoid, scale=1.702), reads=[("ga", i2)], writes=[("sg", i2)])
            p.op("dve", lambda e: e.tensor_scalar(out=la[i2], in0=Lp[i2][:, 0:512], scalar1=b1l[:, col:col + 1], scalar2=7.0, op0=ALU.add, op1=ALU.min),
                 reads=[("Lp", i2), "b1l"], writes=[("la", i2)])
            p.op("dve", lambda e: e.tensor_scalar(out=la[i2], in0=la[i2], scalar1=-7.0, scalar2=1.0, op0=ALU.max, op1=ALU.add), reads=[("la", i2)], writes=[("la", i2)])
            p.op("pool", lambda e: e.tensor_tensor(out=ga[i2], in0=ga[i2], in1=sg[i2], op=ALU.mult), reads=[("ga", i2), ("sg", i2)], writes=[("ga", i2)])
            p.op("pool", lambda e: e.tensor_tensor(out=actT[:, fc, :], in0=ga[i2], in1=la[i2], op=ALU.mult), reads=[("ga", i2), ("la", i2)], writes=[("actT", fc)])
        at_ = toks("actT", range(16))
        yb_ = Yb[e_ % 2]
        for dp in range(8):
            fetch(pi + NB - 1)
            b2 = fetched[pi]
            pi += 1
            wt = [("wbf", b2, 0), ("wbf", b2, 1)]
            dsl = slice(dp * 256, (dp + 1) * 256)
            bb = dp % 2
            p.dma(b2t[bb], dr["moe_b2"][L * NE + e_:L * NE + e_ + 1, dsl].partition_broadcast(128), writes=[("b2t", bb)])
            for st in range(4):
                i2 = st_["it"] % 2
                st_["it"] += 1
                p.mm([(lambda e, c=c: e.matmul(Yp[i2][:, 0:256], lhsT=actT[:, c, st * 128:(st + 1) * 128], rhs=wbf[b2][:, c, :], start=(c == 0), stop=(c == KC - 1)))
                      for c in range(KC)], reads=wt + at_, writes=[("Yp", i2)])
                p.op("dve", lambda e, st=st: e.tensor_tensor(out=yb_[:, st, dsl], in0=Yp[i2][:, 0:256], in1=b2t[bb], op=ALU.add), reads=[("Yp", i2), ("b2t", bb)],
                     writes=[("Yb", e_ % 2, dp)])
        p.dma(YBd[e_], yb_.rearrange("p a d -> p (a d)"), reads=[("Yb", e_ % 2, dp) for dp in range(8)], writes=["YBd"])
    S.close()
    S = Scope(p)
    SC = [S.ps("SC", [128, 512], F32) for _ in range(4)]
    acc = S.sb("acc", [128, 8, D], F32)
    gbc, gtok = bcast_row(cx, S, g_row, D, "gbc")
    bbc, btok = bcast_row(cx, S, b_row, D, "bbc")
    pg = [S.sb("pgl", [128, 2, 8, 128], BF16) for _ in range(2)]
    yl = [S.sb("yl", [128, 2, D], BF16) for _ in range(2)]
    hl = S.sb("hlc", [128, D], F32)
    sm = S.sb("sm", [128, 8], F32)
    k4 = 0
    for gi in range(2):
        def ld(e_):
            b = e_ % 2
            p.dma(pg[b].rearrange("p a b c -> p (a b c)"), PGd[e_][:, gi * 2048:(gi + 1) * 2048], writes=[("pgl", b)])
            p.dma(yl[b].rearrange("p a d -> p (a d)"), YBd[e_][:, gi * 4096:(gi + 1) * 4096], writes=[("yl", b)])
        ld(0)
        for e_ in range(NE):
            if e_ + 1 < NE:
                ld(e_ + 1)
            b = e_ % 2
            for t in range(8):
                for dq in range(4):
                    qs = slice(dq * 512, (dq + 1) * 512)
                    sb_ = k4 % 4
                    k4 += 1
                    p.mm([(lambda e, st=st, sb_=sb_: e.matmul(SC[sb_][:, 0:512], lhsT=pg[b][:, st, t, :], rhs=yl[b][:, st, qs], start=(st == 0), stop=(st == 1)))
                          for st in range(2)], reads=[("pgl", b), ("yl", b)], writes=[("SC", sb_)])
                    if e_ == 0:
                        p.op("act", lambda e, sb_=sb_: e.activation(out=acc[:, t, qs], in_=SC[sb_][:, 0:512], func=AF.Copy), reads=[("SC", sb_)], writes=[("acc", t, dq)])
                    else:
                        p.op("dve", lambda e, sb_=sb_: e.tensor_tensor(out=acc[:, t, qs], in0=SC[sb_][:, 0:512], in1=acc[:, t, qs], op=ALU.add), reads=[("SC", sb_), ("acc", t, dq)],
                             writes=[("acc", t, dq)])
        for t in range(8):
            at = [("acc", t, dq) for dq in range(4)]
            p.dma(hl, hv[gi * 8 + t], writes=["hlc"])
            p.op("dve", lambda e, t=t: e.scalar_tensor_tensor(out=acc[:, t, :], in0=hl, scalar=DN_ALPHA, in1=acc[:, t, :], op0=ALU.mult, op1=ALU.add),
                 reads=["hlc"] + at, writes=at)
            ln_tile(cx, acc[:, t, :], ("acc", t, 0), hl, "hlc", sm, "sm", gbc, gtok, bbc, btok)
            p.dma(ov[gi * 8 + t], acc[:, t, :], reads=at)
    S.close()


def moe_layer(cx, L, h_in, h_out):
    dr, p = cx.dram, cx.p
    stage_router(cx, L, h_in)
    p.allreduce(dr["GH"], dr["GH2"], reads=["GH"], writes=["GH2"])
    p.allreduce(dr["GG"], dr["GG2"], reads=["GG"], writes=["GG2"])
    stage_precast(cx, L)
    p.barrier()
    stage_experts(cx, L, dr["GH2"], dr["GG2"])
    p.allreduce(dr["PART"], dr["PART2"], reads=["PART"], writes=["PART2"])
    p.barrier()
    stage_combine_ln(cx, dr["PART2"], h_in, dr["ln2_g"][L:L + 1, :], dr["ln2_b"][L:L + 1, :], h_out)


def full_forward(cx):
    dr = cx.dram
    stage_l0_proj(cx, dr["x"])
    stage_l0_ssd(cx)
    stage_l0_att(cx)
    stage_outproj_ln(cx, dr["yT"], 32, dr["hyb_w_out"], dr["x"], dr["ln1_g"][0:1, :], dr["ln1_b"][0:1, :], dr["h1"])
    moe_layer(cx, 0, dr["h1"], dr["h2"])
    stage_gla_proj(cx, dr["h2"])
    stage_gla_core(cx)
    stage_outproj_ln(cx, dr["yT"][0:2048, :], 16, dr["gla_w_out"], dr["h2"], dr["ln1_g"][1:2, :], dr["ln1_b"][1:2, :], dr["h3"])
    moe_layer(cx, 1, dr["h3"], dr["out"])


CONST_NAMES = list(CONST_SHAPES.keys())


def _run(nc, in_maps):
    res = run_bass_kernel_spmd(nc, in_maps, core_ids=list(range(NCORES)))
    return res.results


def kernel_unfused(**inputs):
    x = np.asarray(inputs["x"], dtype=np.float32)
    f = lambda k: np.ascontiguousarray(np.asarray(inputs[k], dtype=np.float32))
    consts = make_consts()
    ln = {"ln1_g": f("ln1_g"), "ln1_b": f("ln1_b"), "ln2_g": f("ln2_g"), "ln2_b": f("ln2_b")}
    rt = {"moe_w_router": f("moe_w_router").reshape(2 * D, 32), "moe_b_router": f("moe_b_router")}
    hyb = {"hyb_w_in": f("hyb_w_in")[0], "hyb_conv_w": f("hyb_conv_w")[0], "hyb_conv_b": f("hyb_conv_b").reshape(1, 3072),
           "hyb_dt_bias": f("hyb_dt_bias").reshape(1, 32), "hyb_a_log": f("hyb_a_log").reshape(1, 32), "hyb_d": f("hyb_d").reshape(1, 32),
           "hyb_norm": f("hyb_norm").reshape(1, 2048), "hyb_w_out": f("hyb_w_out")[0]}
    gla = {"gla_w_in": f("gla_w_in")[0], "gla_w_gate2": f("gla_w_gate2")[0], "gla_b_gate": f("gla_b_gate").reshape(1, 1024),
           "gla_norm": f("gla_norm").reshape(1, 512), "gla_w_out": f("gla_w_out")[0]}
    w1, b1, w2, b2 = inputs["moe_w1"], inputs["moe_b1"], inputs["moe_w2"], inputs["moe_b2"]
    ohs = []
    for c in range(NCORES):
        oh = np.zeros((128, 8), np.float32)
        oh[:, c] = 1.0
        ohs.append(oh)

    def stA(cx):
        dr = cx.dram
        stage_l0_proj(cx, dr["x"])
        stage_l0_ssd(cx)
        stage_l0_att(cx)
        stage_outproj_ln(cx, dr["yT"], 32, dr["hyb_w_out"], dr["x"], dr["ln1_g"][0:1, :], dr["ln1_b"][0:1, :], dr["h1"])
        stage_router(cx, 0, dr["h1"], masked=False)
    need = ["x"] + list(hyb) + ["ln1_g", "ln1_b", "moe_w_router", "moe_b_router", "oh"]
    ncA, _ = build([stA], dbg=("h1", "hTo", "gateo"), needed_inputs=need)
    maps = [dict(hyb, **consts, **rt, x=np.ascontiguousarray(x[c]), ln1_g=ln["ln1_g"], ln1_b=ln["ln1_b"], oh=ohs[c]) for c in range(NCORES)]
    rA = _run(ncA, maps)
    h1 = [np.asarray(rA[c]["h1"]) for c in range(NCORES)]

    def stB(cx):
        dr = cx.dram
        stage_precast(cx, 0)
        stage_experts(cx, 0, dr["GH2"], dr["GG2"])
    needB = ["moe_w1", "moe_b1", "moe_w2", "moe_b2", "oh"]
    ncB, _ = build([stB], dbg=("PART",), needed_inputs=needB, ext_in=("GH2", "GG2"), moe_layers=1)

    def experts(L, rprev):
        GH = np.concatenate([np.asarray(rprev[c]["hTo"]) for c in range(NCORES)], axis=0)
        GG = np.concatenate([np.asarray(rprev[c]["gateo"]) for c in range(NCORES)], axis=0)
        maps = []
        for c in range(NCORES):
            e0 = 4 * c
            maps.append(dict(consts, GH2=GH, GG2=GG, oh=ohs[c],
                             moe_w1=np.ascontiguousarray(np.asarray(w1[L, e0:e0 + 4], dtype=np.float32)).reshape(4 * D, 2 * D),
                             moe_b1=np.ascontiguousarray(np.asarray(b1[L, e0:e0 + 4], dtype=np.float32)).reshape(4, 2 * D),
                             moe_w2=np.ascontiguousarray(np.asarray(w2[L, e0:e0 + 4], dtype=np.float32)).reshape(4 * D, D),
                             moe_b2=np.ascontiguousarray(np.asarray(b2[L, e0:e0 + 4], dtype=np.float32)).reshape(4, D)))
        rB = _run(ncB, maps)
        return [np.concatenate([np.asarray(rB[j]["PART"])[c * T:(c + 1) * T] for j in range(NCORES)], axis=0) for c in range(NCORES)]
    parts = experts(0, rA)

    def stC(cx):
        dr = cx.dram
        stage_combine_ln(cx, dr["PART2"], dr["h1"], dr["ln2_g"][0:1, :], dr["ln2_b"][0:1, :], dr["h2"], use_oh=False)
        stage_gla_proj(cx, dr["h2"])
        stage_gla_core(cx)
        stage_outproj_ln(cx, dr["yT"][0:2048, :], 16, dr["gla_w_out"], dr["h2"], dr["ln1_g"][1:2, :], dr["ln1_b"][1:2, :], dr["h3"])
        stage_router(cx, 1, dr["h3"], masked=False)
    need = list(gla) + ["ln1_g", "ln1_b", "ln2_g", "ln2_b", "moe_w_router", "moe_b_router", "oh"]
    ncC, _ = build([stC], dbg=("h3", "hTo", "gateo"), needed_inputs=need, ext_in=("PART2", "h1"))
    maps = [dict(gla, **consts, **rt, **ln, oh=ohs[c], PART2=parts[c], h1=h1[c]) for c in range(NCORES)]
    rC = _run(ncC, maps)
    h3 = [np.asarray(rC[c]["h3"]) for c in range(NCORES)]
    parts = experts(1, rC)

    def stE(cx):
        dr = cx.dram
        stage_combine_ln(cx, dr["PART2"], dr["h3"], dr["ln2_g"][1:2, :], dr["ln2_b"][1:2, :], dr["out"], use_oh=False)
    ncE, _ = build([stE], dbg=("out",), needed_inputs=["ln2_g", "ln2_b", "oh"], ext_in=("PART2", "h3"))
    maps = [dict(consts, oh=ohs[c], ln2_g=ln["ln2_g"], ln2_b=ln["ln2_b"], PART2=parts[c], h3=h3[c]) for c in range(NCORES)]
    rE = _run(ncE, maps)
    return np.stack([np.asarray(rE[c]["out"], dtype=np.float32) for c in range(NCORES)], axis=0)


def fused_forward(cx, NE=32, moe=None):
    moe = moe or stage_moe_sparse2
    dr = cx.dram
    stage_l0_proj(cx, dr["x"])
    stage_l0_ssd(cx)
    stage_l0_att(cx)
    stage_outproj_ln(cx, dr["yT"], 32, dr["hyb_w_out"], dr["x"], dr["ln1_g"][0:1, :], dr["ln1_b"][0:1, :], dr["h1"])
    stage_router(cx, 0, dr["h1"], masked=False)
    moe(cx, 0, dr["h1"], dr["ln2_g"][0:1, :], dr["ln2_b"][0:1, :], dr["h2"], NE=NE)
    stage_gla_proj(cx, dr["h2"])
    stage_gla_core(cx)
    stage_outproj_ln(cx, dr["yT"][0:2048, :], 16, dr["gla_w_out"], dr["h2"], dr["ln1_g"][1:2, :], dr["ln1_b"][1:2, :], dr["h3"])
    stage_router(cx, 1, dr["h3"], masked=False)
    moe(cx, 1, dr["h3"], dr["ln2_g"][1:2, :], dr["ln2_b"][1:2, :], dr["out"], NE=NE)


def kernel(**inputs):
    x = np.asarray(inputs["x"], dtype=np.float32)
    f = lambda k: np.ascontiguousarray(np.asarray(inputs[k], dtype=np.float32))
    shared = {
        "hyb_w_in": f("hyb_w_in")[0], "hyb_conv_w": f("hyb_conv_w")[0], "hyb_conv_b": f("hyb_conv_b").reshape(1, 3072),
        "hyb_dt_bias": f("hyb_dt_bias").reshape(1, 32), "hyb_a_log": f("hyb_a_log").reshape(1, 32), "hyb_d": f("hyb_d").reshape(1, 32),
        "hyb_norm": f("hyb_norm").reshape(1, 2048), "hyb_w_out": f("hyb_w_out")[0],
        "gla_w_in": f("gla_w_in")[0], "gla_w_gate2": f("gla_w_gate2")[0], "gla_b_gate": f("gla_b_gate").reshape(1, 1024),
        "gla_norm": f("gla_norm").reshape(1, 512), "gla_w_out": f("gla_w_out")[0],
        "ln1_g": f("ln1_g"), "ln1_b": f("ln1_b"), "ln2_g": f("ln2_g"), "ln2_b": f("ln2_b"),
        "moe_w_router": f("moe_w_router").reshape(2 * D, 32), "moe_b_router": f("moe_b_router"),
        "moe_w1": f("moe_w1").reshape(2 * 32 * D, 2 * D), "moe_b1": f("moe_b1").reshape(2 * 32, 2 * D),
        "moe_w2": f("moe_w2").reshape(2 * 32 * D, D), "moe_b2": f("moe_b2").reshape(2 * 32, D),
        "oh": np.zeros((128, 8), np.float32),
    }
    shared.update(make_consts())
    in_maps = [dict(shared, x=np.ascontiguousarray(x[c])) for c in range(NCORES)]
    nc, cx = build([fused_forward], dbg=("out",), moe_experts=32)
    res = run_bass_kernel_spmd(nc, in_maps, core_ids=list(range(NCORES)))
    return np.stack([np.asarray(res.results[c]["out"], dtype=np.float32) for c in range(NCORES)], axis=0)
```

```python
import contextlib
import math
import numpy as np
import ml_dtypes
import concourse.bass as bass
import concourse.mybir as mybir
from concourse.bass_utils import run_bass_kernel_spmd

F32 = mybir.dt.float32
BF16 = mybir.dt.bfloat16
AF = mybir.ActivationFunctionType
ALU = mybir.AluOpType
AX = mybir.AxisListType

NCORES = 8
T = 2048
D = 2048
NT = T // 128
KC = D // 128
NEG = -1.0e30
DN_ALPHA = 4 ** 0.25
EPS = 1e-5
HYB_IN = 11296
GLA_IN = 6160


class Prog:
    RING = 8

    def __init__(self, nc):
        self.nc = nc
        self.E = {"pe": nc.tensor, "act": nc.scalar, "dve": nc.vector,
                  "pool": nc.gpsimd, "sp": nc.sync}
        self.sem = {}
        self.cnt = {}
        for k in self.E:
            self.sem[k] = nc.alloc_semaphore("s_" + k)
            self.cnt[k] = 0
        self.ring = {}
        self.ring_cnt = {}
        self.ring_next = {}
        for q in ("sp", "pool", "act"):
            self.ring[q] = [nc.alloc_semaphore(f"d_{q}{i}") for i in range(self.RING)]
            self.ring_cnt[q] = [0] * self.RING
            self.ring_next[q] = 0
        self.cc_sem = nc.alloc_semaphore("cc_sem")
        self.cc_cnt = 0
        self.seen = {k: {} for k in self.E}
        self.tok = {}
        self.nwaits = 0
        self.nops = 0

    def _semh(self, key):
        if key[0] == "e":
            return self.sem[key[1]]
        if key[0] == "c":
            return self.cc_sem
        return self.ring[key[1]][key[2]]

    def _wait(self, eng, ev):
        key, val = ev
        if key == ("e", "pe") and eng == "pe":
            return
        if self.seen[eng].get(key, 0) >= val:
            return
        self.E[eng].wait_ge(self._semh(key), val)
        self.seen[eng][key] = val
        self.nwaits += 1

    def _deps(self, reads, writes):
        deps = {}

        def add(k, v):
            if deps.get(k, 0) < v:
                deps[k] = v
        for t in reads:
            st = self.tok.get(t)
            if st and st["w"]:
                add(*st["w"])
        for t in writes:
            st = self.tok.get(t)
            if st:
                if st["w"]:
                    add(*st["w"])
                for k, v in st["r"].items():
                    add(k, v)
        return deps

    def _commit(self, ev, reads, writes):
        k, v = ev
        for t in reads:
            st = self.tok.setdefault(t, {"w": None, "r": {}})
            if st["r"].get(k, 0) < v:
                st["r"][k] = v
        for t in writes:
            self.tok[t] = {"w": ev, "r": {}}

    def op(self, eng, fn, reads=(), writes=()):
        for k, v in self._deps(reads, writes).items():
            self._wait(eng, (k, v))
        ins = fn(self.E[eng])
        self.cnt[eng] += 1
        ins.then_inc(self.sem[eng], 1)
        self._commit((("e", eng), self.cnt[eng]), reads, writes)
        self.nops += 1
        return ins

    def mm(self, fns, reads=(), writes=()):
        for k, v in self._deps(reads, writes).items():
            self._wait("pe", (k, v))
        ins = None
        for fn in fns:
            ins = fn(self.E["pe"])
        self.cnt["pe"] += 1
        ins.then_inc(self.sem["pe"], 1)
        self._commit((("e", "pe"), self.cnt["pe"]), reads, writes)
        self.nops += len(fns)

    def dma(self, out, in_, reads=(), writes=(), q="sp", **kw):
        i = self.ring_next[q]
        self.ring_next[q] = (i + 1) % self.RING
        key = ("d", q, i)
        if self.ring_cnt[q][i] > 0:
            self._wait(q, (key, 16 * self.ring_cnt[q][i]))
        for k, v in self._deps(reads, writes).items():
            self._wait(q, (k, v))
        ins = self.E[q].dma_start(out=out, in_=in_, **kw)
        self.ring_cnt[q][i] += 1
        ins.then_inc(self.ring[q][i], 16)
        self._commit((key, 16 * self.ring_cnt[q][i]), reads, writes)
        self.nops += 1
        return ins

    def allreduce(self, in_ap, out_ap, reads=(), writes=()):
        for k, v in self._deps(reads, writes).items():
            self._wait("pool", (k, v))
        ins = self.nc.gpsimd.collective_compute(
            "AllReduce", ALU.add, replica_groups=[list(range(NCORES))],
            ins=[in_ap.opt()], outs=[out_ap.opt()])
        self.cc_cnt += 1
        ins.then_inc(self.cc_sem)
        self._commit((("c",), self.cc_cnt), reads, writes)

    def barrier(self):
        evs = []
        for k in self.E:
            if self.cnt[k]:
                evs.append((("e", k), self.cnt[k]))
        for q in self.ring:
            for i in range(self.RING):
                if self.ring_cnt[q][i]:
                    evs.append((("d", q, i), 16 * self.ring_cnt[q][i]))
        if self.cc_cnt:
            evs.append((("c",), self.cc_cnt))
        for eng in self.E:
            for ev in evs:
                if ev[0] == ("e", eng):
                    continue
                self._wait(eng, ev)
        self.tok = {}


_UID = [0]


class Scope:
    def _name(self, name):
        _UID[0] += 1
        return f"{name}_{_UID[0]}"

    def __init__(self, p):
        self.p = p
        self.nc = p.nc
        self.es = contextlib.ExitStack()
        self.n = 0

    def sb(self, name, shape, dt=F32):
        self.n += 1
        h = self.es.enter_context(self.nc.sbuf_tensor(self._name(name), list(shape), dt))
        return h.ap()

    def ps(self, name, shape, dt=F32):
        self.n += 1
        h = self.es.enter_context(self.nc.psum_tensor(self._name(name), list(shape), dt))
        return h.ap()

    def close(self):
        self.p.barrier()
        self.es.close()


def make_consts():
    c = {}
    c["ident"] = np.eye(128, dtype=np.float32)
    c["identb"] = np.eye(128).astype(ml_dtypes.bfloat16)
    r = np.arange(128)
    c["triu"] = (r[:, None] <= r[None, :]).astype(np.float32)
    c["maskneg"] = np.where(r[None, :] < r[:, None], NEG, 0.0).astype(np.float32)
    sel = np.zeros((32, 32, 128), np.float32)
    for h in range(32):
        sel[h, h, :] = 1.0
    c["sel"] = sel.reshape(32, 32 * 128)
    rt = np.zeros((128, 128), np.float32)
    for m in range(64):
        rt[m + 64, m] = -1.0
    for m in range(64, 128):
        rt[m - 64, m] = 1.0
    c["rt"] = rt
    inv = np.exp(-math.log(10000.0) * np.arange(64, dtype=np.float32) / 64).astype(np.float32)
    ang = np.arange(T, dtype=np.float32)[None, :] * inv[:, None]
    c["cosT"] = np.concatenate([np.cos(ang), np.cos(ang)], 0).astype(np.float32)
    c["sinT"] = np.concatenate([np.sin(ang), np.sin(ang)], 0).astype(np.float32)
    c["causal"] = np.where(r[None, :] <= r[:, None], 0.0, NEG).astype(np.float32)
    same = (r[:, None] // 64) == (r[None, :] // 64)
    c["btri"] = (same & (r[:, None] <= r[None, :])).astype(np.float32)
    c["bgt"] = (same & (r[:, None] > r[None, :])).astype(np.float32)
    c["ones"] = np.ones((1, 128), np.float32)
    ls = np.zeros((128, 128), np.float32)
    ls[127, :] = 1.0
    c["lastsel"] = ls
    c["trius"] = (r[:, None] < r[None, :]).astype(np.float32)
    c["ones128"] = np.ones((128, 128), np.float32)
    c["iota"] = np.tile(np.arange(256, dtype=np.float32)[None, :], (128, 1))
    return c


CONST_SHAPES = {"ident": ([128, 128], F32), "identb": ([128, 128], BF16), "triu": ([128, 128], F32),
                "maskneg": ([128, 128], F32), "sel": ([32, 4096], F32), "rt": ([128, 128], F32),
                "cosT": ([128, T], F32), "sinT": ([128, T], F32), "causal": ([128, 128], F32),
                "btri": ([128, 128], F32), "bgt": ([128, 128], F32), "ones": ([1, 128], F32), "lastsel": ([128, 128], F32), "trius": ([128, 128], F32),
                "ones128": ([128, 128], F32), "iota": ([128, 256], F32)}


class Ctx:
    pass


def load_const(cx, S, name, tokname=None):
    shape, dt = CONST_SHAPES[name]
    t = S.sb("c_" + name, shape, dt)
    cx.p.dma(t, cx.dram[name], writes=[tokname or ("c", name, id(S))])
    return t


def bcast_row(cx, S, row_ap, n, name):
    t = S.sb(name, [128, n], F32)
    cx.p.dma(t, row_ap.partition_broadcast(128), writes=[("bc", name, id(S))])
    return t, ("bc", name, id(S))


def toks(base, rng):
    return [(base, i) for i in rng]


def transpose_in(cx, S, src_tm, dstT, dst_tok, ident, ident_tok, nt=NT, t0=0):
    p = cx.p
    ld = [S.sb("tin_ld", [128, D], F32) for _ in range(2)]
    ps = [S.ps("tin_ps", [128, 512], F32) for _ in range(2)]
    src = src_tm.rearrange("(t p) d -> t p d", p=128)
    k = 0
    for t in range(nt):
        b = ld[t % 2]
        p.dma(b, src[t0 + t], writes=[("tin_ld", t % 2)])
        for g in range(4):
            pp = ps[k % 2]
            ptok = ("tin_ps", k % 2)
            p.mm([(lambda e, pp=pp, b=b, g=g, j=j: e.transpose(out=pp[:, j * 128:(j + 1) * 128],
                                                              in_=b[:, (g * 4 + j) * 128:(g * 4 + j + 1) * 128],
                                                              identity=ident)) for j in range(4)],
                 reads=[("tin_ld", t % 2), ident_tok], writes=[ptok])
            out = dstT[:, g * 4:(g + 1) * 4, t * 128:(t + 1) * 128]
            src_ps = pp.rearrange("p (a b) -> p a b", a=4)
            if k % 2 == 0:
                p.op("dve", lambda e, out=out, s=src_ps: e.tensor_copy(out=out, in_=s), reads=[ptok], writes=[(dst_tok, t, g)])
            else:
                p.op("act", lambda e, out=out, s=src_ps: e.activation(out=out, in_=s, func=AF.Copy), reads=[ptok], writes=[(dst_tok, t, g)])
            k += 1


def xtoks(xtok, tiles):
    return [(xtok, t, g) for t in tiles for g in range(4)]


def load_cols(cx, S, vec_ap2d, r, name, ident, ident_tok, ps, pstok, ncol=128):
    p = cx.p
    st = S.sb(name + "_st", [r, ncol], F32)
    out = S.sb(name, [128, r], F32)
    p.dma(st, vec_ap2d, writes=[(name, "st")])
    p.mm([lambda e: e.transpose(out=ps[0:ncol, 0:r], in_=st, identity=ident[0:r, 0:r])],
         reads=[(name, "st"), ident_tok], writes=[pstok])
    p.op("dve", lambda e: e.tensor_copy(out=out[0:ncol, :], in_=ps[0:ncol, 0:r]), reads=[pstok], writes=[(name,)])
    return out, (name,)


class WStream:
    def __init__(self, cx, S, kc, pw, name):
        self.cx, self.kc, self.pw, self.name = cx, kc, pw, name
        self.st = [S.sb(name + "_st", [128, kc, pw], F32) for _ in range(2)]
        self.bf = [S.sb(name + "_bf", [128, kc, pw], BF16) for _ in range(2)]
        self.i = 0

    def fetch(self, w2d, c0, w):
        p = self.cx.p
        b = self.i % 2
        self.i += 1
        st, bf = self.st[b], self.bf[b]
        stok, btok = (self.name, "st", b), (self.name, "bf", b)
        src = w2d.rearrange("(k p) n -> p k n", p=128)[:, :, c0:c0 + w]
        p.dma(st[:, :, 0:w], src, writes=[stok])
        p.op("pool", lambda e: e.tensor_copy(out=bf[:, :, 0:w], in_=st[:, :, 0:w]), reads=[stok], writes=[btok])
        return bf, btok


def gemm_panels(cx, ws, w2d, panels, xT, xtok, mode, ps_list, epilogue, kc=KC, nt=NT):
    p = cx.p
    nxt = ws.fetch(w2d, panels[0][0], panels[0][1])
    k = 0
    for pi, (c0, w, tag) in enumerate(panels):
        bf, btok = nxt
        if pi + 1 < len(panels):
            nxt = ws.fetch(w2d, panels[pi + 1][0], panels[pi + 1][1])
        if mode == "tm":
            for t in range(nt):
                ps, ptok = ps_list[k % len(ps_list)]
                k += 1
                p.mm([(lambda e, c=c, ps=ps, t=t: e.matmul(ps[:, 0:w], lhsT=xT[:, c, t * 128:(t + 1) * 128], rhs=bf[:, c, 0:w],
                                                         start=(c == 0), stop=(c == kc - 1))) for c in range(kc)],
                     reads=[btok] + xtoks(xtok, [t]), writes=[ptok])
                epilogue(tag, c0, w, t, ps, ptok)
        else:
            nj = max(1, w // 128)
            m = 128
            for j in range(nj):
                for tb in range(nt // 4):
                    ps, ptok = ps_list[k % len(ps_list)]
                    k += 1
                    p.mm([(lambda e, c=c, ps=ps, tb=tb, j=j: e.matmul(ps[0:m, 0:512], lhsT=bf[:, c, j * 128:j * 128 + m],
                                                                     rhs=xT[:, c, tb * 512:(tb + 1) * 512],
                                                                     start=(c == 0), stop=(c == kc - 1))) for c in range(kc)],
                         reads=[btok] + xtoks(xtok, range(tb * 4, tb * 4 + 4)), writes=[ptok])
                    epilogue(tag, c0 + j * 128, m, tb, ps, ptok)


def stage_l0_proj(cx, h_tm):
    p, nc, dr = cx.p, cx.nc, cx.dram
    S = Scope(p)
    ident = load_const(cx, S, "ident", "ident")
    rt = load_const(cx, S, "rt", "rt")
    cosT = load_const(cx, S, "cosT", "cosT")
    sinT = load_const(cx, S, "sinT", "sinT")
    xT = S.sb("xT", [128, KC, T], BF16)
    S2 = Scope(p)
    transpose_in(cx, S2, h_tm, xT, "xT", ident, "ident")
    S2.close()
    w_in = dr["hyb_w_in"]
    ws = WStream(cx, S, KC, 256, "win")
    gps = [(S.ps("gps", [128, 512], F32), ("gps", i)) for i in range(3)]
    tps = [(S.ps("tps", [128, 512], F32), ("tps", i)) for i in range(2)]
    rps, rtok = S.ps("rps", [128, 512], F32), "rps"
    mps, mtok = S.ps("mps", [128, 512], F32), "mps"
    cwst = S.sb("cwst", [4, 3072], F32)
    p.dma(cwst, dr["hyb_conv_w"], writes=["cwst"])
    cw = S.sb("cw", [128, 24, 4], F32)
    p.mm([(lambda e, c=c: e.transpose(out=mps[:, c * 4:(c + 1) * 4], in_=cwst[0:4, c * 128:(c + 1) * 128], identity=ident[0:4, 0:4]))
          for c in range(24)], reads=["cwst", "ident"], writes=[mtok])
    p.op("dve", lambda e: e.tensor_copy(out=cw.rearrange("p a b -> p (a b)"), in_=mps[:, 0:96]), reads=[mtok], writes=["cw"])
    cb, cbtok = load_cols(cx, S, dr["hyb_conv_b"].rearrange("o (c p) -> (o c) p", p=128), 24, "cb", ident, "ident", mps, mtok)
    dtb, dtbtok = bcast_row(cx, S, dr["hyb_dt_bias"], 32, "dtb")
    abc, abctok = bcast_row(cx, S, dr["hyb_a_log"], 32, "abc")
    p.op("act", lambda e: e.activation(out=abc, in_=abc, func=AF.Exp), reads=[abctok], writes=[abctok])
    p.op("dve", lambda e: e.tensor_scalar(out=abc, in0=abc, scalar1=-1.0, scalar2=None, op0=ALU.mult), reads=[abctok], writes=[abctok])
    cin = S.sb("cin", [128, 3 + T], F32)
    p.op("pool", lambda e: e.memset(cin[:, 0:3], 0.0), writes=["cin_pad"])
    wa = S.sb("wa", [128, T], F32)
    wb = S.sb("wb", [128, T], F32)
    wc = S.sb("wc", [128, T], F32)
    obf = [S.sb("obf", [128, T], BF16) for _ in range(2)]
    tmst = S.sb("tmst", [128, NT, 128], F32)
    tmstb = S.sb("tmstb", [128, NT, 128], BF16)
    zst = [S.sb("zst", [128, 256], F32) for _ in range(2)]
    vst = [S.sb("vst", [128, 256], BF16) for _ in range(2)]
    dts = S.sb("dts", [128, NT, 32], F32)
    adts = S.sb("adts", [128, NT, 32], F32)
    sp1 = S.sb("sp1", [128, 32], F32)
    sp2 = S.sb("sp2", [128, 32], F32)
    kmean = S.sb("kmean", [128, 16, 8], F32)
    gst = S.sb("gst", [128, NT, 8], F32)
    cnt = {"z": 0, "v": 0, "o": 0, "t": 0}

    def transposes_to(dst3, dtok, src, stok):
        for g in range(4):
            ps, ptok = tps[cnt["t"] % 2]
            cnt["t"] += 1
            p.mm([(lambda e, j=j, ps=ps, g=g: e.transpose(out=ps[:, j * 128:(j + 1) * 128], in_=src[:, (g * 4 + j) * 128:(g * 4 + j + 1) * 128],
                                                        identity=ident)) for j in range(4)], reads=[stok, "ident"], writes=[ptok])
            p.op("act", lambda e, ps=ps, g=g: e.activation(out=dst3[:, g * 4:(g + 1) * 4, :], in_=ps.rearrange("p (a b) -> p a b", a=4), func=AF.Copy),
                 reads=[ptok], writes=[dtok])

    def ep(tag, c0, w, idx, ps, ptok):
        if tag == "z":
            b = cnt["z"] % 2
            cnt["z"] += 1
            p.op("act", lambda e: e.activation(out=zst[b][:, 0:w], in_=ps[:, 0:w], func=AF.Silu), reads=[ptok], writes=[("zst", b)])
            p.dma(dr["sz"][idx * 128:(idx + 1) * 128, c0:c0 + w], zst[b][:, 0:w], reads=[("zst", b)])
        elif tag == "v":
            b = cnt["v"] % 2
            cnt["v"] += 1
            p.op("dve", lambda e: e.tensor_copy(out=vst[b][:, 0:w], in_=ps[:, 0:w]), reads=[ptok], writes=[("vst", b)])
            p.dma(dr["v_tm"][idx * 128:(idx + 1) * 128, c0 - 9248:c0 - 9248 + w], vst[b][:, 0:w], reads=[("vst", b)])
        elif tag == "dt":
            t = idx
            p.op("dve", lambda e: e.tensor_tensor(out=sp1, in0=ps[:, 0:32], in1=dtb, op=ALU.add), reads=[ptok, dtbtok], writes=["sp1"])
            p.op("act", lambda e: e.activation(out=sp2, in_=sp1, func=AF.Abs), reads=["sp1"], writes=["sp2"])
            p.op("act", lambda e: e.activation(out=sp2, in_=sp2, func=AF.Exp, scale=-1.0), reads=["sp2"], writes=["sp2"])
            p.op("act", lambda e: e.activation(out=sp2, in_=sp2, func=AF.Ln, bias=1.0), reads=["sp2"], writes=["sp2"])
            p.op("dve", lambda e: e.scalar_tensor_tensor(out=dts[:, t, :], in0=sp1, scalar=0.0, in1=sp2, op0=ALU.max, op1=ALU.add),
                 reads=["sp1", "sp2"], writes=["dts"])
            p.op("dve", lambda e: e.tensor_tensor(out=adts[:, t, :], in0=dts[:, t, :], in1=abc, op=ALU.mult), reads=["dts", abctok], writes=["adts"])
            if t == NT - 1:
                p.dma(dr["dt_tm"].rearrange("(t p) h -> p t h", p=128), dts, reads=["dts"])
                p.dma(dr["adt_tm"].rearrange("(t p) h -> p t h", p=128), adts, reads=["adts"])
        elif tag == "xbc":
            tb = idx
            ci = (c0 - 2048) // 128
            p.op("act", lambda e: e.activation(out=cin[:, 3 + tb * 512:3 + (tb + 1) * 512], in_=ps[:, 0:512], func=AF.Copy),
                 reads=[ptok], writes=[("cin", tb)])
            if tb < 3:
                return
            ctoks = toks("cin", range(4)) + ["cin_pad"]
            p.op("dve", lambda e: e.tensor_scalar(out=wa, in0=cin[:, 0:T], scalar1=cw[:, ci, 0:1], scalar2=None, op0=ALU.mult),
                 reads=ctoks + ["cw"], writes=["wa"])
            for j in range(1, 4):
                p.op("dve", lambda e, j=j: e.scalar_tensor_tensor(out=wa, in0=cin[:, j:j + T], scalar=cw[:, ci, j:j + 1], in1=wa,
                                                                 op0=ALU.mult, op1=ALU.add), reads=ctoks + ["cw", "wa"], writes=["wa"])
            p.op("act", lambda e: e.activation(out=wb, in_=wa, func=AF.Silu, bias=cb[:, ci:ci + 1], scale=1.0), reads=["wa", cbtok], writes=["wb"])
            if ci < 16:
                transposes_to(tmst, "tmst", wb, "wb")
                p.dma(dr["xs_tm"].rearrange("(t p) c -> p t c", p=128)[:, :, ci * 128:(ci + 1) * 128], tmst, reads=["tmst"])
            else:
                b = cnt["o"] % 2
                cnt["o"] += 1
                p.op("pool", lambda e: e.tensor_copy(out=obf[b], in_=wb), reads=["wb"], writes=[("obf", b)])
                g = (ci - 16) % 4
                dst = dr["B_fm"] if ci < 20 else dr["C_fm"]
                p.dma(dst[g * 128:(g + 1) * 128, :], obf[b], reads=[("obf", b)])
                if ci < 20:
                    transposes_to(tmstb, "tmstb", wb, "wb")
                    p.dma(dr["B_tm"].rearrange("(t p) n -> p t n", p=128)[:, :, g * 128:(g + 1) * 128], tmstb, reads=["tmstb"])
        elif tag in ("q", "k"):
            tb = idx
            base = 5152 if tag == "q" else 7200
            h = (c0 - base) // 128
            p.op("act", lambda e: e.activation(out=wa[:, tb * 512:(tb + 1) * 512], in_=ps[:, 0:512], func=AF.Copy), reads=[ptok], writes=[("waq", tb)])
            if tb < 3:
                return
            for b4 in range(4):
                sl = slice(b4 * 512, (b4 + 1) * 512)
                p.mm([lambda e, sl=sl: e.matmul(rps[:, 0:512], lhsT=rt, rhs=wa[:, sl], start=True, stop=True)], reads=[("waq", b4), "rt"], writes=[rtok])
                p.op("dve", lambda e, sl=sl: e.tensor_tensor(out=wb[:, sl], in0=rps[:, 0:512], in1=sinT[:, sl], op=ALU.mult), reads=[rtok, "sinT"], writes=[("wbq", b4)])
                p.op("pool", lambda e, sl=sl: e.tensor_tensor(out=wc[:, sl], in0=wa[:, sl], in1=cosT[:, sl], op=ALU.mult), reads=[("waq", b4), "cosT"], writes=[("wcq", b4)])
                p.op("pool", lambda e, sl=sl: e.tensor_tensor(out=wc[:, sl], in0=wc[:, sl], in1=wb[:, sl], op=ALU.add), reads=[("wcq", b4), ("wbq", b4)], writes=[("wcq", b4)])
            b = cnt["o"] % 2
            cnt["o"] += 1
            wctoks = toks("wcq", range(4))
            if tag == "k":
                p.op("act", lambda e: e.activation(out=obf[b], in_=wc, func=AF.Copy), reads=wctoks, writes=[("obf", b)])
                p.dma(dr["k_fm"][h * 128:(h + 1) * 128, :], obf[b], reads=[("obf", b)])
                p.op("dve", lambda e: e.tensor_reduce(out=kmean[:, h, :], in_=wc.rearrange("p (a b) -> p a b", a=8), axis=AX.X, op=ALU.add),
                     reads=wctoks, writes=[("kmean", h)])
                p.op("dve", lambda e: e.tensor_scalar(out=kmean[:, h, :], in0=kmean[:, h, :], scalar1=1.0 / 256.0, scalar2=None, op0=ALU.mult),
                     reads=[("kmean", h)], writes=[("kmean", h)])
            else:
                p.op("act", lambda e: e.activation(out=obf[b], in_=wc, func=AF.Copy, scale=128.0 ** -0.5), reads=wctoks, writes=[("obf", b)])
                p.dma(dr["q_fm"][h * 128:(h + 1) * 128, :], obf[b], reads=[("obf", b)])
                p.mm([(lambda e, t=t: e.matmul(mps[:, t * 8:(t + 1) * 8], lhsT=wc[:, t * 128:(t + 1) * 128], rhs=kmean[:, h, :], start=True, stop=True))
                      for t in range(NT)], reads=wctoks + [("kmean", h)], writes=[mtok])
                p.op("dve", lambda e: e.tensor_copy(out=gst.rearrange("p a b -> p (a b)"), in_=mps[:, 0:128]), reads=[mtok], writes=["gst"])
                p.dma(dr["gate_d"][h].rearrange("(t p) e -> p t e", p=128), gst, reads=["gst"])

    panels = [(2048 + i * 256, 256, "xbc") for i in range(12)]
    gemm_panels(cx, ws, w_in, panels, xT, "xT", "fm", gps, ep)
    p.barrier()
    gemm_panels(cx, ws, w_in, [(5120, 32, "dt")], xT, "xT", "tm", gps, ep)
    p.barrier()
    panels = [(7200 + i * 256, 256, "k") for i in range(8)] + [(5152 + i * 256, 256, "q") for i in range(8)]
    gemm_panels(cx, ws, w_in, panels, xT, "xT", "fm", gps, ep)
    p.barrier()
    panels = [(i * 256, 256, "z") for i in range(8)] + [(9248 + i * 256, 256, "v") for i in range(8)]
    gemm_panels(cx, ws, w_in, panels, xT, "xT", "tm", gps, ep)
    S.close()


INPUT_SHAPES = {
    "x": ([T, D], F32),
    "hyb_w_in": ([D, HYB_IN], F32), "hyb_conv_w": ([4, 3072], F32), "hyb_conv_b": ([1, 3072], F32),
    "hyb_dt_bias": ([1, 32], F32), "hyb_a_log": ([1, 32], F32), "hyb_d": ([1, 32], F32),
    "hyb_norm": ([1, 2048], F32), "hyb_w_out": ([4096, D], F32),
    "gla_w_in": ([D, GLA_IN], F32), "gla_w_gate2": ([16, 1024], F32), "gla_b_gate": ([1, 1024], F32),
    "gla_norm": ([1, 512], F32), "gla_w_out": ([D, D], F32),
    "ln1_g": ([2, D], F32), "ln1_b": ([2, D], F32), "ln2_g": ([2, D], F32), "ln2_b": ([2, D], F32),
    "moe_w_router": ([2 * D, 32], F32), "moe_b_router": ([2, 32], F32),
    "moe_w1": ([2 * 4 * D, 2 * D], F32), "moe_b1": ([2 * 4, 2 * D], F32),
    "moe_w2": ([2 * 4 * D, D], F32), "moe_b2": ([2 * 4, D], F32),
    "oh": ([128, 8], F32),
}

SCRATCH = {
    "sz": ([T, 2048], F32), "v_tm": ([T, 2048], BF16), "dt_tm": ([T, 32], F32), "adt_tm": ([T, 32], F32),
    "xs_tm": ([T, 2048], F32), "B_fm": ([512, T], BF16), "C_fm": ([512, T], BF16), "B_tm": ([T, 512], BF16),
    "q_fm": ([2048, T], BF16), "k_fm": ([2048, T], BF16), "gate_d": ([16, T, 8], F32),
    "yT": ([4096, T], BF16), "h1": ([T, D], F32), "h2": ([T, D], F32), "h3": ([T, D], F32),
    "GH": ([8 * D, T], BF16), "GH2": ([8 * D, T], BF16), "GG": ([8 * T, 32], F32), "GG2": ([8 * T, 32], F32),
    "w1b": ([4 * 16 * 128, 4096], BF16), "w2b": ([4 * 4 * 128, 8192], BF16),
    "PART": ([8 * T, D], F32), "PART2": ([8 * T, D], F32),
    "gq_fm": ([1024, T], F32), "gk_fm": ([1024, T], F32), "gk_tm": ([T, 1024], F32), "gv_tm": ([T, 2048], BF16),
    "gsg": ([T, 2048], F32), "gla_d": ([T, 1024], F32), "out": ([T, D], F32),
    "hTo": ([D, T], BF16), "gateo": ([T, 32], F32),
    "XG": ([32 * 128, 16 * 512], BF16), "PGTd": ([32 * 128, 4096], BF16), "YBd": ([32 * 128, 4 * 2048], BF16),
}


def build(stages, dbg=(), needed_inputs=None, ext_in=(), moe_layers=2, moe_experts=4):
    nc = bass.Bass("TRN2", target_bir_lowering=False)
    cx = Ctx()
    cx.nc = nc
    cx.dram = {}
    for k, (shape, dt) in INPUT_SHAPES.items():
        if needed_inputs is not None and k not in needed_inputs:
            continue
        if k in ("moe_w1", "moe_b1", "moe_w2", "moe_b2"):
            shape = [shape[0] // 4 * moe_experts, shape[1]]
            if moe_layers == 1:
                shape = [shape[0] // 2, shape[1]]
        cx.dram[k] = nc.dram_tensor(k, shape, dt, kind="ExternalInput").ap()
    for k, (shape, dt) in CONST_SHAPES.items():
        cx.dram[k] = nc.dram_tensor(k, shape, dt, kind="ExternalInput").ap()
    for k, (shape, dt) in SCRATCH.items():
        if k in ext_in:
            cx.dram[k] = nc.dram_tensor(k, shape, dt, kind="ExternalInput").ap()
        elif k in dbg:
            cx.dram[k] = nc.dram_tensor(k, shape, dt, kind="ExternalOutput").ap()
        else:
            cx.dram[k] = nc.dram_tensor(k, shape, dt).ap()
    cx.p = Prog(nc)
    for st in stages:
        st(cx)
    cx.p.barrier()
    return nc, cx


def stage_l0_ssd(cx):
    LV = 9
    p, nc, dr = cx.p, cx.nc, cx.dram
    S = Scope(p)
    ident = load_const(cx, S, "ident", "ident")
    identb = load_const(cx, S, "identb", "identb")
    triu = load_const(cx, S, "triu", "triu")
    maskneg = load_const(cx, S, "maskneg", "maskneg")
    sel = load_const(cx, S, "sel", "sel")
    lastsel = load_const(cx, S, "lastsel", "lastsel")
    if LV == -1:
        S.close()
        return
    Bf = S.sb("Bf", [128, 4, T], BF16)
    Cf = S.sb("Cf", [128, 4, T], BF16)
    Bt = S.sb("Bt", [128, NT, 512], BF16)
    dts = S.sb("dts", [128, NT, 32], F32)
    adts = S.sb("adts", [128, NT, 32], F32)
    p.dma(Bf, dr["B_fm"].rearrange("(g p) t -> p g t", p=128), writes=["Bf"])
    p.dma(Cf, dr["C_fm"].rearrange("(g p) t -> p g t", p=128), writes=["Cf"])
    p.dma(Bt, dr["B_tm"].rearrange("(t p) n -> p t n", p=128), writes=["Bt"])
    p.dma(dts, dr["dt_tm"].rearrange("(t p) h -> p t h", p=128), writes=["dts"])
    p.dma(adts, dr["adt_tm"].rearrange("(t p) h -> p t h", p=128), writes=["adts"])
    if LV == -2:
        S.close()
        return
    dbc, dbctok = bcast_row(cx, S, dr["hyb_d"], 32, "dbc")
    nw, nwtok = bcast_row(cx, S, dr["hyb_norm"], 2048, "nw")
    xs = [S.sb("xs", [128, 2048], F32) for _ in range(2)]
    szb = [S.sb("szb", [128, 2048], F32) for _ in range(2)]
    prev = S.sb("prev", [128, 4, 512], F32)
    prevb = S.sb("prevb", [128, 4, 512], BF16)
    p.op("pool", lambda e: e.memset(prev, 0.0), writes=["prev"])
    p.op("pool", lambda e: e.memset(prevb, 0.0), writes=["prevb"])
    if LV == -3:
        S.close()
        return
    adp = S.sb("adp", [128, 128], F32)
    p.op("pool", lambda e: e.memset(adp, 0.0), writes=["adp"])
    acum = S.sb("acum", [128, 32], F32)
    nacum = S.sb("nacum", [128, 32], F32)
    eac = S.sb("eac", [128, 32], F32)
    acf = S.sb("acf", [32, 128], F32)
    Dm = S.sb("Dm", [128, 8, 128], F32)
    cbt = S.sb("cbt", [128, 128], F32)
    M = S.sb("M", [128, 8, 128], BF16)
    xdt = S.sb("xdt", [128, 8, 64], BF16)
    xdte = S.sb("xdte", [128, 8, 64], BF16)
    t1 = S.sb("t1", [128, 512], F32)
    t2 = S.sb("t2", [128, 512], F32)
    ysb = S.sb("ysb", [128, 512], F32)
    junk = S.sb("junk", [128, 512], F32)
    ybf = S.sb("ybf", [128, 512], BF16)
    sm = S.sb("sm", [128, 16], F32)
    dte = S.sb("dte", [128, 32], F32)
    wde = S.sb("wde", [128, 32], F32)
    cd = S.sb("cd", [128, 32], F32)
    ptmp = S.sb("ptmp", [128, 512], F32)
    yT = S.sb("yTs", [128, 16, T], BF16)
    E = S.ps("E", [128, 1024], F32)
    aps = S.ps("aps", [128, 512], F32)
    cps = S.ps("cps", [128, 512], F32)
    Yd = S.ps("Yd", [128, 512], F32)
    Yo = S.ps("Yo", [128, 512], F32)
    Sp = S.ps("Sp", [128, 512], F32)
    tp = S.ps("tp", [128, 512], BF16)
    xsv = dr["xs_tm"].rearrange("(t p) c -> t p c", p=128)
    szv = dr["sz"].rearrange("(t p) c -> t p c", p=128)

    def load(c):
        p.dma(xs[c % 2], xsv[c], writes=[("xs", c % 2)])
        p.dma(szb[c % 2], szv[c], writes=[("szb", c % 2)])
    load(0)
    for c in range(NT if LV > 0 else 0):
        if c + 1 < NT:
            load(c + 1)
        X, Z = xs[c % 2], szb[c % 2]
        xtok, ztok = ("xs", c % 2), ("szb", c % 2)
        cs = slice(c * 128, (c + 1) * 128)
        p.mm([lambda e: e.matmul(aps[:, 0:32], lhsT=triu, rhs=adts[:, c, :], start=True, stop=True)], reads=["triu", "adts"], writes=["aps"])
        p.op("act", lambda e: e.activation(out=acum, in_=aps[:, 0:32], func=AF.Copy), reads=["aps"], writes=["acum"])
        p.op("dve", lambda e: e.tensor_scalar(out=nacum, in0=aps[:, 0:32], scalar1=-1.0, scalar2=None, op0=ALU.mult), reads=["aps"], writes=["nacum"])
        p.op("act", lambda e: e.activation(out=eac, in_=aps[:, 0:32], func=AF.Exp), reads=["aps"], writes=["eac"])
        p.op("dve", lambda e: e.tensor_copy(out=adp[:, 0:32], in_=adts[:, c, :]), reads=["adts"], writes=["adp"])
        p.mm([lambda e: e.matmul(aps[:, 128:256], lhsT=adp, rhs=triu, start=True, stop=True)], reads=["triu", "adp", "aps"], writes=["aps"])
        p.op("dve", lambda e: e.tensor_copy(out=acf, in_=aps[0:32, 128:256]), reads=["aps"], writes=["acf"])
        p.mm([lambda e: e.matmul(aps[:, 256:288], lhsT=lastsel, rhs=acum, start=True, stop=True)], reads=["lastsel", "acum", "aps"], writes=["aps"])
        p.op("dve", lambda e: e.tensor_tensor(out=dte, in0=aps[:, 256:288], in1=nacum, op=ALU.add), reads=["aps", "nacum"], writes=["dte"])
        p.op("act", lambda e: e.activation(out=dte, in_=dte, func=AF.Exp), reads=["dte"], writes=["dte"])
        p.op("act", lambda e: e.activation(out=cd, in_=aps[:, 256:288], func=AF.Exp), reads=["aps"], writes=["cd"])
        p.op("dve", lambda e: e.tensor_tensor(out=wde, in0=dte, in1=dts[:, c, :], op=ALU.mult), reads=["dte", "dts"], writes=["wde"])
        for g in range(4 if LV >= 2 else 0):
            hs = slice(g * 8, (g + 1) * 8)
            gs = slice(g * 512, (g + 1) * 512)
            fns = []
            for j in range(8):
                h = g * 8 + j
                fns.append(lambda e, j=j, h=h: e.matmul(E[:, j * 128:(j + 1) * 128], lhsT=sel[:, h * 128:(h + 1) * 128], rhs=acf, start=True, stop=False))
                fns.append(lambda e, j=j: e.matmul(E[:, j * 128:(j + 1) * 128], lhsT=ident, rhs=maskneg, start=False, stop=True))
            p.mm(fns, reads=["sel", "acf", "ident", "maskneg"], writes=["E"])
            for j in range(8):
                h = g * 8 + j
                p.op("act", lambda e, j=j, h=h: e.activation(out=Dm[:, j, :], in_=E[:, j * 128:(j + 1) * 128], func=AF.Exp, bias=nacum[:, h:h + 1], scale=1.0),
                     reads=["E", "nacum"], writes=[("Dm", j)])
            if LV < 3:
                continue
            p.mm([lambda e: e.matmul(cps[:, 0:128], lhsT=Bf[:, g, cs], rhs=Cf[:, g, cs], start=True, stop=True)], reads=["Bf", "Cf"], writes=["cps"])
            p.op("act", lambda e: e.activation(out=cbt, in_=cps[:, 0:128], func=AF.Copy), reads=["cps"], writes=["cbt"])
            p.op("dve", lambda e: e.tensor_tensor(out=M, in0=Dm, in1=cbt.unsqueeze(1).to_broadcast([128, 8, 128]), op=ALU.mult),
                 reads=toks("Dm", range(8)) + ["cbt"], writes=["M"])
            Xg = X[:, gs].rearrange("p (a b) -> p a b", a=8)
            p.op("pool", lambda e: e.tensor_tensor(out=xdt, in0=Xg, in1=dts[:, c, hs].unsqueeze(2).to_broadcast([128, 8, 64]), op=ALU.mult),
                 reads=[xtok, "dts"], writes=["xdt"])
            p.mm([(lambda e, j=j: e.matmul(Yd[:, j * 64:(j + 1) * 64], lhsT=M[:, j, :], rhs=xdt[:, j, :], start=True, stop=True)) for j in range(8)],
                 reads=["M", "xdt"], writes=["Yd"])
            p.mm([lambda e: e.matmul(Yo[:, 0:512], lhsT=Cf[:, g, cs], rhs=prevb[:, g, :], start=True, stop=True)], reads=["Cf", ("prevb", g)], writes=["Yo"])
            if LV < 4:
                continue
            p.op("dve", lambda e: e.tensor_tensor(out=t1.rearrange("p (a b) -> p a b", a=8), in0=Yo.rearrange("p (a b) -> p a b", a=8),
                                                  in1=eac[:, hs].unsqueeze(2).to_broadcast([128, 8, 64]), op=ALU.mult), reads=["Yo", "eac"], writes=["t1"])
            p.op("pool", lambda e: e.tensor_tensor(out=t2.rearrange("p (a b) -> p a b", a=8), in0=Xg,
                                                   in1=dbc[:, hs].unsqueeze(2).to_broadcast([128, 8, 64]), op=ALU.mult), reads=[xtok, dbctok], writes=["t2"])
            p.op("pool", lambda e: e.tensor_tensor(out=t2, in0=t2, in1=t1, op=ALU.add), reads=["t1", "t2"], writes=["t2"])
            p.op("dve", lambda e: e.tensor_tensor(out=ysb, in0=Yd[:, 0:512], in1=t2, op=ALU.add), reads=["Yd", "t2"], writes=["ysb"])
            p.op("pool", lambda e: e.tensor_tensor(out=ysb, in0=ysb, in1=Z[:, gs], op=ALU.mult), reads=["ysb", ztok], writes=["ysb"])
            p.op("act", lambda e: e.activation(out=junk, in_=ysb, func=AF.Square, accum_out=sm[:, 0:1]), reads=["ysb"], writes=["junk", "sm"])
            p.op("dve", lambda e: e.tensor_scalar(out=sm[:, 1:2], in0=sm[:, 0:1], scalar1=1.0 / 512.0, scalar2=EPS, op0=ALU.mult, op1=ALU.add), reads=["sm"], writes=["sm"])
            p.op("act", lambda e: e.activation(out=sm[:, 2:3], in_=sm[:, 1:2], func=AF.Sqrt), reads=["sm"], writes=["sm"])
            p.op("dve", lambda e: e.reciprocal(out=sm[:, 3:4], in_=sm[:, 2:3]), reads=["sm"], writes=["sm"])
            p.op("dve", lambda e: e.scalar_tensor_tensor(out=ybf, in0=ysb, scalar=sm[:, 3:4], in1=nw[:, gs], op0=ALU.mult, op1=ALU.mult),
                 reads=["ysb", "sm", nwtok], writes=["ybf"])
            if LV < 5:
                continue
            p.mm([(lambda e, j=j: e.transpose(out=tp[:, j * 128:(j + 1) * 128], in_=ybf[:, j * 128:(j + 1) * 128], identity=identb)) for j in range(4)],
                 reads=["ybf", "identb"], writes=["tp"])
            p.op("act", lambda e: e.activation(out=yT[:, g * 4:(g + 1) * 4, cs], in_=tp[:, 0:512].rearrange("p (a b) -> p a b", a=4), func=AF.Copy),
                 reads=["tp"], writes=[("yT", c, g)])
            if LV < 6:
                continue
            p.op("dve", lambda e: e.tensor_tensor(out=xdte, in0=Xg, in1=wde[:, hs].unsqueeze(2).to_broadcast([128, 8, 64]), op=ALU.mult),
                 reads=[xtok, "wde"], writes=["xdte"])
            p.mm([lambda e: e.matmul(Sp[:, 0:512], lhsT=Bt[:, c, g * 128:(g + 1) * 128], rhs=xdte.rearrange("p a b -> p (a b)"), start=True, stop=True)],
                 reads=["Bt", "xdte"], writes=["Sp"])
            if LV < 7:
                continue
            p.op("pool", lambda e: e.tensor_tensor(out=ptmp.rearrange("p (a b) -> p a b", a=8), in0=prev[:, g, :].rearrange("p (a b) -> p a b", a=8),
                                                   in1=cd[:, hs].unsqueeze(2).to_broadcast([128, 8, 64]), op=ALU.mult), reads=[("prev", g), "cd"], writes=["ptmp"])
            if LV < 8:
                continue
            p.op("dve", lambda e: e.tensor_tensor(out=prev[:, g, :], in0=Sp[:, 0:512], in1=ptmp, op=ALU.add), reads=["Sp", "ptmp"], writes=[("prev", g)])
            p.op("act", lambda e: e.activation(out=prevb[:, g, :], in_=prev[:, g, :], func=AF.Copy), reads=[("prev", g)], writes=[("prevb", g)])
    for k in range(16):
        p.dma(dr["yT"][k * 128:(k + 1) * 128, :], yT[:, k, :], reads=[("yT", c, k // 4) for c in range(NT)])
    S.close()


def stage_l0_att(cx):
    p, nc, dr = cx.p, cx.nc, cx.dram
    S = Scope(p)
    identb = load_const(cx, S, "identb", "identb")
    causal = load_const(cx, S, "causal", "causal")
    qT = [S.sb("qT", [128, T], BF16) for _ in range(2)]
    kT = [S.sb("kT", [128, T], BF16) for _ in range(2)]
    vt = [S.sb("vt", [128, NT, 128], BF16) for _ in range(2)]
    gt = [S.sb("gt", [128, NT, 8], F32) for _ in range(2)]
    yatt = [S.sb("yatt", [128, T], BF16) for _ in range(2)]
    Ssb = [S.sb("Ssb", [128, T], F32) for _ in range(2)]
    Pb = [S.sb("Pb", [128, T], BF16) for _ in range(2)]
    PTs = [S.sb("PTs", [128, NT, 128], BF16) for _ in range(2)]
    gsb = [S.sb("gsb", [128, 8], F32) for _ in range(2)]
    m8 = [S.sb("m8", [128, 8], F32) for _ in range(2)]
    bs = [S.sb("bs", [128, 8], F32) for _ in range(2)]
    st = [S.sb("st", [128, 4], F32) for _ in range(2)]
    Sps = [S.ps("Sps", [128, 512], F32) for _ in range(4)]
    PT = [S.ps("PT", [128, 1024], BF16) for _ in range(2)]
    OT = S.ps("OT", [128, 512], F32)

    def load(h):
        b = h % 2
        p.dma(qT[b], dr["q_fm"][h * 128:(h + 1) * 128, :], writes=[("qT", b)])
        p.dma(kT[b], dr["k_fm"][h * 128:(h + 1) * 128, :], writes=[("kT", b)])
        p.dma(vt[b], dr["v_tm"].rearrange("(t p) d -> p t d", p=128)[:, :, h * 128:(h + 1) * 128], writes=[("vt", b)])
        p.dma(gt[b], dr["gate_d"][h].rearrange("(t p) e -> p t e", p=128), writes=[("gt", b)])
    load(0)
    it = 0
    for h in range(16):
        if h + 1 < 16:
            load(h + 1)
        hb = h % 2
        for qi in range(NT):
            b = it % 2
            it += 1
            qb, half = qi // 2, qi % 2
            npast = qb * 256
            nk = npast + (half + 1) * 128
            nseg = (nk + 511) // 512
            for sg in range(nseg):
                w = min(512, nk - sg * 512)
                p.mm([lambda e, sg=sg, w=w: e.matmul(Sps[sg][:, 0:w], lhsT=qT[hb][:, qi * 128:(qi + 1) * 128], rhs=kT[hb][:, sg * 512:sg * 512 + w],
                                                     start=True, stop=True)], reads=[("qT", hb), ("kT", hb)], writes=[("Sps", sg)])
            sel = qb >= 3
            if sel:
                p.op("dve", lambda e: e.tensor_copy(out=gsb[b], in_=gt[hb][:, qi, :]), reads=[("gt", hb)], writes=[("gsb", b)])
                p.op("pool", lambda e: e.memset(gsb[b][:, qb:8], NEG), reads=[("gsb", b)], writes=[("gsb", b)])
                p.op("dve", lambda e: e.max(out=m8[b], in_=gsb[b]), reads=[("gsb", b)], writes=[("m8", b)])
                p.op("dve", lambda e: e.tensor_scalar(out=bs[b], in0=gsb[b], scalar1=m8[b][:, 2:3], scalar2=-1.0, op0=ALU.is_ge, op1=ALU.add),
                     reads=[("gsb", b), ("m8", b)], writes=[("bs", b)])
                p.op("dve", lambda e: e.tensor_scalar(out=bs[b], in0=bs[b], scalar1=1.0e30, scalar2=None, op0=ALU.mult), reads=[("bs", b)], writes=[("bs", b)])
            stok = ("Ssb", b)
            for sg in range((npast + 511) // 512):
                wp = min(512, npast - sg * 512)
                nb = wp // 256
                if sel:
                    p.op("dve", lambda e, sg=sg, wp=wp, nb=nb: e.tensor_tensor(
                        out=Ssb[b][:, sg * 512:sg * 512 + wp].rearrange("p (a c) -> p a c", a=nb),
                        in0=Sps[sg][:, 0:wp].rearrange("p (a c) -> p a c", a=nb),
                        in1=bs[b][:, sg * 2:sg * 2 + nb].unsqueeze(2).to_broadcast([128, nb, 256]), op=ALU.add),
                        reads=[("Sps", sg), ("bs", b)], writes=[stok])
                else:
                    p.op("act", lambda e, sg=sg, wp=wp: e.activation(out=Ssb[b][:, sg * 512:sg * 512 + wp], in_=Sps[sg][:, 0:wp], func=AF.Copy),
                         reads=[("Sps", sg)], writes=[stok])
            so, off = npast // 512, npast % 512
            if half == 1:
                p.op("act", lambda e: e.activation(out=Ssb[b][:, npast:npast + 128], in_=Sps[so][:, off:off + 128], func=AF.Copy),
                     reads=[("Sps", so)], writes=[stok])
            o2 = off + half * 128
            p.op("dve", lambda e: e.tensor_tensor(out=Ssb[b][:, nk - 128:nk], in0=Sps[so][:, o2:o2 + 128], in1=causal, op=ALU.add),
                 reads=[("Sps", so), "causal"], writes=[stok])
            p.op("dve", lambda e: e.reduce_max(out=st[b][:, 0:1], in_=Ssb[b][:, 0:nk], axis=AX.X, negate=True), reads=[stok], writes=[("st", b)])
            p.op("act", lambda e: e.activation(out=Ssb[b][:, 0:nk], in_=Ssb[b][:, 0:nk], func=AF.Exp, bias=st[b][:, 0:1], scale=1.0, accum_out=st[b][:, 1:2]),
                 reads=[stok, ("st", b)], writes=[stok, ("st", b)])
            p.op("dve", lambda e: e.reciprocal(out=st[b][:, 2:3], in_=st[b][:, 1:2]), reads=[("st", b)], writes=[("st", b)])
            p.op("dve", lambda e: e.tensor_scalar(out=Pb[b][:, 0:nk], in0=Ssb[b][:, 0:nk], scalar1=st[b][:, 2:3], scalar2=None, op0=ALU.mult),
                 reads=[stok, ("st", b)], writes=[("Pb", b)])
            nkb = nk // 128
            for bank in range((nkb + 7) // 8):
                n8 = min(8, nkb - bank * 8)
                p.mm([(lambda e, kb=kb, bank=bank: e.transpose(out=PT[bank][:, (kb % 8) * 128:(kb % 8 + 1) * 128], in_=Pb[b][:, kb * 128:(kb + 1) * 128],
                                                            identity=identb)) for kb in range(bank * 8, bank * 8 + n8)],
                     reads=[("Pb", b), "identb"], writes=[("PT", bank)])
                eng = "act" if bank == 0 else "dve"
                outap = PTs[b][:, bank * 8:bank * 8 + n8, :]
                inap = PT[bank][:, 0:n8 * 128].rearrange("p (a c) -> p a c", a=n8)
                if eng == "act":
                    p.op("act", lambda e, outap=outap, inap=inap: e.activation(out=outap, in_=inap, func=AF.Copy), reads=[("PT", bank)], writes=[("PTs", b, bank)])
                else:
                    p.op("dve", lambda e, outap=outap, inap=inap: e.tensor_copy(out=outap, in_=inap), reads=[("PT", bank)], writes=[("PTs", b, bank)])
            p.mm([(lambda e, kb=kb: e.matmul(OT[:, 0:128], lhsT=vt[hb][:, kb, :], rhs=PTs[b][:, kb, :], start=(kb == 0), stop=(kb == nkb - 1)))
                  for kb in range(nkb)], reads=[("vt", hb), ("PTs", b, 0), ("PTs", b, 1)], writes=["OT"])
            p.op("act", lambda e: e.activation(out=yatt[hb][:, qi * 128:(qi + 1) * 128], in_=OT[:, 0:128], func=AF.Copy), reads=["OT"], writes=[("yatt", hb)])
        p.dma(dr["yT"][2048 + h * 128:2048 + (h + 1) * 128, :], yatt[hb], reads=[("yatt", hb)])
    S.close()


def ln_tile(cx, r, rtok, junk, jtok, sm, smtok, gbc, gtok, bbc, btok):
    p = cx.p
    p.op("act", lambda e: e.activation(out=junk, in_=r, func=AF.Copy, accum_out=sm[:, 0:1]), reads=[rtok], writes=[jtok, smtok])
    p.op("act", lambda e: e.activation(out=junk, in_=r, func=AF.Square, accum_out=sm[:, 1:2]), reads=[rtok, jtok], writes=[jtok, smtok])
    p.op("dve", lambda e: e.tensor_scalar(out=sm[:, 2:3], in0=sm[:, 0:1], scalar1=1.0 / D, scalar2=None, op0=ALU.mult), reads=[smtok], writes=[smtok])
    p.op("dve", lambda e: e.scalar_tensor_tensor(out=sm[:, 3:4], in0=sm[:, 2:3], scalar=-1.0, in1=sm[:, 2:3], op0=ALU.mult, op1=ALU.mult),
         reads=[smtok], writes=[smtok])
    p.op("dve", lambda e: e.scalar_tensor_tensor(out=sm[:, 4:5], in0=sm[:, 1:2], scalar=1.0 / D, in1=sm[:, 3:4], op0=ALU.mult, op1=ALU.add),
         reads=[smtok], writes=[smtok])
    p.op("dve", lambda e: e.tensor_scalar(out=sm[:, 4:5], in0=sm[:, 4:5], scalar1=EPS, scalar2=None, op0=ALU.add), reads=[smtok], writes=[smtok])
    p.op("act", lambda e: e.activation(out=sm[:, 5:6], in_=sm[:, 4:5], func=AF.Sqrt), reads=[smtok], writes=[smtok])
    p.op("dve", lambda e: e.reciprocal(out=sm[:, 6:7], in_=sm[:, 5:6]), reads=[smtok], writes=[smtok])
    p.op("dve", lambda e: e.scalar_tensor_tensor(out=sm[:, 7:8], in0=sm[:, 2:3], scalar=-1.0, in1=sm[:, 6:7], op0=ALU.mult, op1=ALU.mult),
         reads=[smtok], writes=[smtok])
    p.op("act", lambda e: e.activation(out=r, in_=r, func=AF.Identity, scale=sm[:, 6:7], bias=sm[:, 7:8]), reads=[rtok, smtok], writes=[rtok])
    p.op("pool", lambda e: e.tensor_tensor(out=r, in0=r, in1=gbc, op=ALU.mult), reads=[rtok, gtok], writes=[rtok])
    p.op("pool", lambda e: e.tensor_tensor(out=r, in0=r, in1=bbc, op=ALU.add), reads=[rtok, btok], writes=[rtok])


def stage_outproj_ln(cx, yT_d, kc, W_d, h_in, g_row, b_row, h_out):
    p, nc, dr = cx.p, cx.nc, cx.dram
    S = Scope(p)
    pw = 128 if kc == 32 else 256
    ws = WStream(cx, S, kc, pw, "wout")
    gbc, gtok = bcast_row(cx, S, g_row, D, "gbc")
    bbc, btok = bcast_row(cx, S, b_row, D, "bbc")
    racc = S.sb("racc", [128, 4, D], F32)
    yTq = S.sb("yTq", [128, kc, 512], BF16)
    junk = S.sb("junk", [128, D], F32)
    sm = S.sb("sm", [128, 8], F32)
    gps = [(S.ps("ops", [128, 512], F32), ("ops", i)) for i in range(4)]
    yv = yT_d.rearrange("(k p) t -> p k t", p=128)
    hv = h_in.rearrange("(t p) d -> t p d", p=128)
    ov = h_out.rearrange("(t p) d -> t p d", p=128)
    panels = [(i * pw, pw) for i in range(D // pw)]
    k = 0
    for qtr in range(4):
        p.dma(yTq, yv[:, :, qtr * 512:(qtr + 1) * 512], writes=["yTq"])
        for j in range(4):
            p.dma(racc[:, j, :], hv[qtr * 4 + j], writes=[("racc", j)])
            p.op("pool", lambda e, j=j: e.tensor_scalar(out=racc[:, j, :], in0=racc[:, j, :], scalar1=DN_ALPHA, scalar2=None, op0=ALU.mult),
                 reads=[("racc", j)], writes=[("racc", j)])
        nxt = ws.fetch(W_d, panels[0][0], pw)
        for pi, (c0, w) in enumerate(panels):
            bf, btk = nxt
            if pi + 1 < len(panels):
                nxt = ws.fetch(W_d, panels[pi + 1][0], pw)
            for j in range(4):
                ps, ptok = gps[k % 4]
                k += 1
                p.mm([(lambda e, c=c, ps=ps, j=j: e.matmul(ps[:, 0:w], lhsT=yTq[:, c, j * 128:(j + 1) * 128], rhs=bf[:, c, 0:w],
                                                         start=(c == 0), stop=(c == kc - 1))) for c in range(kc)], reads=[btk, "yTq"], writes=[ptok])
                p.op("dve", lambda e, ps=ps, j=j, c0=c0: e.tensor_tensor(out=racc[:, j, c0:c0 + w], in0=ps[:, 0:w], in1=racc[:, j, c0:c0 + w], op=ALU.add),
                     reads=[ptok, ("racc", j)], writes=[("racc", j)])
        for j in range(4):
            ln_tile(cx, racc[:, j, :], ("racc", j), junk, "junk", sm, "sm", gbc, gtok, bbc, btok)
            p.dma(ov[qtr * 4 + j], racc[:, j, :], reads=[("racc", j)])
    S.close()


def stage_router(cx, L, h_tm, masked=True):
    p, nc, dr = cx.p, cx.nc, cx.dram
    S = Scope(p)
    ident = load_const(cx, S, "ident", "ident")
    oh = S.sb("oh", [128, 8], F32)
    p.dma(oh, dr["oh"], writes=["oh"])
    wr = S.sb("wr", [128, KC, 32], F32)
    p.dma(wr, dr["moe_w_router"][L * D:(L + 1) * D, :].rearrange("(k p) e -> p k e", p=128), writes=["wr"])
    brbc, brtok = bcast_row(cx, S, dr["moe_b_router"][L:L + 1, :], 32, "brbc")
    hT = S.sb("hTr", [128, KC, T], BF16)
    hTf = S.sb("hTf", [128, KC, 128], F32)
    gl = S.sb("gl", [128, NT, 32], F32)
    ld = [S.sb("rld", [128, D], F32) for _ in range(2)]
    lg = S.sb("lg", [128, 32], F32)
    ex = S.sb("ex", [128, 32], F32)
    selm = S.sb("selm", [128, 32], F32)
    m8 = S.sb("m8", [128, 8], F32)
    sm = S.sb("rsm", [128, 4], F32)
    tps = [S.ps("rtp", [128, 512], F32) for _ in range(2)]
    lps = S.ps("lps", [128, 512], F32)
    src = h_tm.rearrange("(t p) d -> t p d", p=128)
    k = 0
    for t in range(NT):
        b = ld[t % 2]
        p.dma(b, src[t], writes=[("rld", t % 2)])
        for g in range(4):
            pp, ptok = tps[k % 2], ("rtp", k % 2)
            k += 1
            p.mm([(lambda e, pp=pp, b=b, g=g, j=j: e.transpose(out=pp[:, j * 128:(j + 1) * 128], in_=b[:, (g * 4 + j) * 128:(g * 4 + j + 1) * 128],
                                                              identity=ident)) for j in range(4)], reads=[("rld", t % 2), "ident"], writes=[ptok])
            src_ps = pp.rearrange("p (a b) -> p a b", a=4)
            p.op("act", lambda e, g=g, s=src_ps: e.activation(out=hTf[:, g * 4:(g + 1) * 4, :], in_=s, func=AF.Copy), reads=[ptok], writes=[("hTf", g)])
            p.op("dve", lambda e, g=g, t=t: e.tensor_copy(out=hT[:, g * 4:(g + 1) * 4, t * 128:(t + 1) * 128], in_=hTf[:, g * 4:(g + 1) * 4, :]),
                 reads=[("hTf", g)], writes=[("hTr", t, g)])
        p.mm([(lambda e, c=c: e.matmul(lps[:, 0:32], lhsT=hTf[:, c, :], rhs=wr[:, c, :], start=(c == 0), stop=(c == KC - 1))) for c in range(KC)],
             reads=toks("hTf", range(4)) + ["wr"], writes=["lps"])
        p.op("dve", lambda e: e.tensor_tensor(out=lg, in0=lps[:, 0:32], in1=brbc, op=ALU.add), reads=["lps", brtok], writes=["lg"])
        p.op("dve", lambda e: e.max(out=m8, in_=lg), reads=["lg"], writes=["m8"])
        p.op("dve", lambda e: e.tensor_scalar(out=sm[:, 0:1], in0=m8[:, 0:1], scalar1=-1.0, scalar2=None, op0=ALU.mult), reads=["m8"], writes=["rsm"])
        p.op("act", lambda e: e.activation(out=ex, in_=lg, func=AF.Exp, bias=sm[:, 0:1], scale=1.0), reads=["lg", "rsm"], writes=["ex"])
        p.op("dve", lambda e: e.tensor_scalar(out=selm, in0=lg, scalar1=m8[:, 3:4], scalar2=0.0, op0=ALU.is_ge, op1=ALU.add), reads=["lg", "m8"], writes=["selm"])
        p.op("dve", lambda e: e.tensor_tensor(out=ex, in0=ex, in1=selm, op=ALU.mult), reads=["ex", "selm"], writes=["ex"])
        p.op("dve", lambda e: e.tensor_reduce(out=sm[:, 1:2], in_=ex, axis=AX.X, op=ALU.add), reads=["ex"], writes=["rsm"])
        p.op("dve", lambda e: e.reciprocal(out=sm[:, 2:3], in_=sm[:, 1:2]), reads=["rsm"], writes=["rsm"])
        p.op("dve", lambda e, t=t: e.tensor_scalar(out=gl[:, t, :], in0=ex, scalar1=sm[:, 2:3], scalar2=None, op0=ALU.mult), reads=["ex", "rsm"], writes=["gl"])
    if not masked:
        for k in range(KC):
            p.dma(dr["hTo"][k * 128:(k + 1) * 128, :], hT[:, k, :], reads=[("hTr", t, k // 4) for t in range(NT)])
        p.dma(dr["gateo"].rearrange("(t p) e -> p t e", p=128), gl, reads=["gl"])
        S.close()
        return
    scb = [S.sb("scb", [128, 4, T], BF16) for _ in range(2)]
    gsc = [S.sb("gsc", [128, NT, 32], F32) for _ in range(2)]
    alltok = [("hTr", t, g) for t in range(NT) for g in range(4)]
    n = 0
    for j in range(8):
        for kq in range(4):
            b = n % 2
            eng = "dve" if n % 2 == 0 else "pool"
            n += 1
            p.op(eng, lambda e, b=b, kq=kq, j=j: e.tensor_scalar(out=scb[b], in0=hT[:, kq * 4:(kq + 1) * 4, :], scalar1=oh[:, j:j + 1], scalar2=None, op0=ALU.mult),
                 reads=[("hTr", t, kq) for t in range(NT)] + ["oh"], writes=[("scb", b)])
            p.dma(dr["GH"][j * D:(j + 1) * D, :].rearrange("(k p) t -> p k t", p=128)[:, kq * 4:(kq + 1) * 4, :], scb[b], reads=[("scb", b)], writes=["GH"])
        b = j % 2
        p.op("dve", lambda e, b=b, j=j: e.tensor_scalar(out=gsc[b], in0=gl, scalar1=oh[:, j:j + 1], scalar2=None, op0=ALU.mult), reads=["gl", "oh"], writes=[("gsc", b)])
        p.dma(dr["GG"][j * T:(j + 1) * T, :].rearrange("(t p) e -> p t e", p=128), gsc[b], reads=[("gsc", b)], writes=["GG"])
    S.close()


def stage_precast(cx, L):
    p, nc, dr = cx.p, cx.nc, cx.dram
    S = Scope(p)
    st1 = [S.sb("pc1", [128, 4096], F32) for _ in range(2)]
    o1 = [S.sb("po1", [128, 16, 256], BF16) for _ in range(2)]
    st2 = [S.sb("pc2", [128, 2048], F32) for _ in range(2)]
    o2 = [S.sb("po2", [128, 2048], BF16) for _ in range(2)]
    n = 0
    for e_ in range(4):
        r0 = (L * 4 + e_) * D
        for kc in range(KC):
            b = n % 2
            n += 1
            p.dma(st1[b], dr["moe_w1"][r0 + kc * 128:r0 + (kc + 1) * 128, :], writes=[("pc1", b)])
            sv = st1[b].rearrange("p (f i two) -> p f i two", f=16, two=2)
            p.op("dve", lambda e, b=b, sv=sv: e.tensor_copy(out=o1[b][:, :, 0:128], in_=sv[:, :, :, 0]), reads=[("pc1", b)], writes=[("po1", b)])
            p.op("pool", lambda e, b=b, sv=sv: e.tensor_copy(out=o1[b][:, :, 128:256], in_=sv[:, :, :, 1]), reads=[("pc1", b)], writes=[("po1", b)])
            p.dma(dr["w1b"][e_ * 16 * 128:(e_ + 1) * 16 * 128, :].rearrange("(f p) (k c) -> p f k c", p=128, c=256)[:, :, kc, :], o1[b],
                  reads=[("po1", b)], writes=["w1b"])
            p.dma(st2[b], dr["moe_w2"][r0 + kc * 128:r0 + (kc + 1) * 128, :], writes=[("pc2", b)])
            p.op("act", lambda e, b=b: e.activation(out=o2[b], in_=st2[b], func=AF.Copy), reads=[("pc2", b)], writes=[("po2", b)])
            p.dma(dr["w2b"][e_ * 4 * 128:(e_ + 1) * 4 * 128, :].rearrange("(d p) (f c) -> p d f c", p=128, c=512)[:, :, kc, :],
                  o2[b].rearrange("p (d c) -> p d c", d=4), reads=[("po2", b)], writes=["w2b"])
    S.close()


def stage_experts(cx, L, GH2, GG2, ngroups=16):
    p, nc, dr = cx.p, cx.nc, cx.dram
    S = Scope(p)
    ident = load_const(cx, S, "ident", "ident")
    oh = S.sb("oh", [128, 8], F32)
    p.dma(oh, dr["oh"], writes=["oh"])
    mps = S.ps("emps", [128, 512], F32)
    b1st = S.sb("b1st", [64, 256], F32)
    p.dma(b1st, dr["moe_b1"][L * 4:(L + 1) * 4, :].rearrange("e (f c) -> (e f) c", c=256), writes=["b1st"])
    b1g = S.sb("b1g", [128, 64], F32)
    b1l = S.sb("b1l", [128, 64], F32)
    p.mm([lambda e: e.transpose(out=mps[:, 0:64], in_=b1st[:, 0:256:2], identity=ident[0:64, 0:64]),
          lambda e: e.transpose(out=mps[:, 64:128], in_=b1st[:, 1:256:2], identity=ident[0:64, 0:64])], reads=["b1st", "ident"], writes=["emps"])
    p.op("dve", lambda e: e.tensor_copy(out=b1g, in_=mps[:, 0:64]), reads=["emps"], writes=["b1g"])
    p.op("dve", lambda e: e.tensor_copy(out=b1l, in_=mps[:, 64:128]), reads=["emps"], writes=["b1l"])
    b2t = [S.sb("b2t", [128, 512], F32) for _ in range(2)]
    hTg = S.sb("hTg", [128, KC, 1024], BF16)
    actT = S.sb("actT", [128, KC, 1024], BF16)
    acc = S.sb("acc", [128, 8, D], F32)
    gts = S.sb("gts", [128, 8, 32], F32)
    gsel = S.sb("gsel", [128, 8, 4], F32)
    gtmp = S.sb("gtmp", [128, 8, 4], F32)
    w1p = [S.sb("w1p", [128, KC, 256], BF16) for _ in range(2)]
    w2p = [S.sb("w2p", [128, KC, 512], BF16) for _ in range(2)]
    ga = [S.sb("ga", [128, 512], F32) for _ in range(2)]
    sg = [S.sb("sg", [128, 512], F32) for _ in range(2)]
    la = [S.sb("la", [128, 512], F32) for _ in range(2)]
    yb = [S.sb("yb", [128, 512], F32) for _ in range(2)]
    Gp = [S.ps("Gp", [128, 512], F32) for _ in range(2)]
    Lp = [S.ps("Lp", [128, 512], F32) for _ in range(2)]
    Yp = [S.ps("Yp", [128, 512], F32) for _ in range(2)]
    w1v = dr["w1b"].rearrange("(ef p) x -> ef p x", p=128)
    w2v = dr["w2b"].rearrange("(ed p) x -> ed p x", p=128)
    n1 = n2 = it = 0

    def fetch1(e_, fc):
        nonlocal n1
        b = n1 % 2
        n1 += 1
        p.dma(w1p[b].rearrange("p k c -> p (k c)"), w1v[e_ * 16 + fc], reads=["w1b"], writes=[("w1p", b)])
        return b

    def fetch2(e_, dp):
        nonlocal n2
        b = n2 % 2
        n2 += 1
        p.dma(w2p[b].rearrange("p k c -> p (k c)"), w2v[e_ * 4 + dp], reads=["w2b"], writes=[("w2p", b)])
        return b
    for gi in range(ngroups):
        jb, hf = gi // 2, gi % 2
        p.dma(hTg, GH2[jb * D:(jb + 1) * D, :].rearrange("(k p) t -> p k t", p=128)[:, :, hf * 1024:(hf + 1) * 1024], reads=["GH2"], writes=["hTg"])
        p.dma(gts, GG2[jb * T + hf * 1024:jb * T + (hf + 1) * 1024, :].rearrange("(t p) e -> p t e", p=128), reads=["GG2"], writes=["gts"])
        gv = gts.rearrange("p t (j e) -> p t j e", e=4)
        p.op("dve", lambda e: e.tensor_scalar(out=gsel, in0=gv[:, :, 0, :], scalar1=oh[:, 0:1], scalar2=None, op0=ALU.mult), reads=["gts", "oh"], writes=["gsel"])
        for j in range(1, 8):
            p.op("dve", lambda e, j=j: e.scalar_tensor_tensor(out=gsel, in0=gv[:, :, j, :], scalar=oh[:, j:j + 1], in1=gsel, op0=ALU.mult, op1=ALU.add),
                 reads=["gts", "oh", "gsel"], writes=["gsel"])
        for e_ in range(4):
            nb = fetch1(e_, 0)
            for fc in range(16):
                b1 = nb
                if fc + 1 < 16:
                    nb = fetch1(e_, fc + 1)
                for tb in range(2):
                    i2 = it % 2
                    it += 1
                    tsl = slice(tb * 512, (tb + 1) * 512)
                    p.mm([(lambda e, c=c: e.matmul(Gp[i2][:, 0:512], lhsT=w1p[b1][:, c, 0:128], rhs=hTg[:, c, tsl], start=(c == 0), stop=(c == KC - 1)))
                          for c in range(KC)], reads=[("w1p", b1), "hTg"], writes=[("Gp", i2)])
                    p.mm([(lambda e, c=c: e.matmul(Lp[i2][:, 0:512], lhsT=w1p[b1][:, c, 128:256], rhs=hTg[:, c, tsl], start=(c == 0), stop=(c == KC - 1)))
                          for c in range(KC)], reads=[("w1p", b1), "hTg"], writes=[("Lp", i2)])
                    col = e_ * 16 + fc
                    p.op("dve", lambda e: e.tensor_scalar(out=ga[i2], in0=Gp[i2][:, 0:512], scalar1=b1g[:, col:col + 1], scalar2=7.0, op0=ALU.add, op1=ALU.min),
                         reads=[("Gp", i2), "b1g"], writes=[("ga", i2)])
                    p.op("act", lambda e: e.activation(out=sg[i2], in_=ga[i2], func=AF.Sigmoid, scale=1.702), reads=[("ga", i2)], writes=[("sg", i2)])
                    p.op("dve", lambda e: e.tensor_scalar(out=la[i2], in0=Lp[i2][:, 0:512], scalar1=b1l[:, col:col + 1], scalar2=7.0, op0=ALU.add, op1=ALU.min),
                         reads=[("Lp", i2), "b1l"], writes=[("la", i2)])
                    p.op("pool", lambda e: e.tensor_scalar(out=la[i2], in0=la[i2], scalar1=-7.0, scalar2=1.0, op0=ALU.max, op1=ALU.add),
                         reads=[("la", i2)], writes=[("la", i2)])
                    p.op("pool", lambda e: e.tensor_tensor(out=ga[i2], in0=ga[i2], in1=sg[i2], op=ALU.mult), reads=[("ga", i2), ("sg", i2)], writes=[("ga", i2)])
                    p.op("pool", lambda e: e.tensor_tensor(out=actT[:, fc, tsl], in0=ga[i2], in1=la[i2], op=ALU.mult), reads=[("ga", i2), ("la", i2)],
                         writes=[("actT", fc, tb)])
            nb = fetch2(e_, 0)
            for dp in range(4):
                b2 = nb
                if dp + 1 < 4:
                    nb = fetch2(e_, dp + 1)
                dsl = slice(dp * 512, (dp + 1) * 512)
                bb = n2 % 2
                p.dma(b2t[bb], dr["moe_b2"][L * 4 + e_:L * 4 + e_ + 1, dsl].partition_broadcast(128), writes=[("b2t", bb)])
                for t in range(8):
                    i2 = it % 2
                    it += 1
                    p.mm([(lambda e, c=c: e.matmul(Yp[i2][:, 0:512], lhsT=actT[:, c, t * 128:(t + 1) * 128], rhs=w2p[b2][:, c, :], start=(c == 0), stop=(c == KC - 1)))
                          for c in range(KC)], reads=[("w2p", b2)] + [("actT", c, t // 4) for c in range(KC)], writes=[("Yp", i2)])
                    p.op("dve", lambda e: e.tensor_tensor(out=yb[i2], in0=Yp[i2][:, 0:512], in1=b2t[bb], op=ALU.add), reads=[("Yp", i2), ("b2t", bb)],
                         writes=[("yb", i2)])
                    if e_ == 0:
                        p.op("pool", lambda e: e.tensor_scalar(out=acc[:, t, dsl], in0=yb[i2], scalar1=gsel[:, t, e_:e_ + 1], scalar2=None, op0=ALU.mult),
                             reads=[("yb", i2), "gsel"], writes=[("acc", t, dp)])
                    else:
                        p.op("pool", lambda e: e.tensor_scalar(out=yb[i2], in0=yb[i2], scalar1=gsel[:, t, e_:e_ + 1], scalar2=None, op0=ALU.mult),
                             reads=[("yb", i2), "gsel"], writes=[("yb", i2)])
                        p.op("pool", lambda e: e.tensor_tensor(out=acc[:, t, dsl], in0=acc[:, t, dsl], in1=yb[i2], op=ALU.add),
                             reads=[("yb", i2), ("acc", t, dp)], writes=[("acc", t, dp)])
        p.dma(dr["PART"][gi * 1024:(gi + 1) * 1024, :].rearrange("(t p) d -> p t d", p=128), acc,
              reads=[("acc", t, dp) for t in range(8) for dp in range(4)], writes=["PART"])
    S.close()


def stage_combine_ln(cx, PART2, h_in, g_row, b_row, h_out, nblk=8, use_oh=True):
    p, nc, dr = cx.p, cx.nc, cx.dram
    S = Scope(p)
    oh = S.sb("oh", [128, 8], F32)
    p.dma(oh, dr["oh"], writes=["oh"])
    gbc, gtok = bcast_row(cx, S, g_row, D, "gbc")
    bbc, btok = bcast_row(cx, S, b_row, D, "bbc")
    r = [S.sb("cr", [128, D], F32) for _ in range(2)]
    pl = [S.sb("cpl", [128, D], F32) for _ in range(3)]
    junk = S.sb("junk", [128, D], F32)
    sm = S.sb("sm", [128, 8], F32)
    hv = h_in.rearrange("(t p) d -> t p d", p=128)
    ov = h_out.rearrange("(t p) d -> t p d", p=128)
    n = 0
    for t in range(NT):
        b = t % 2
        rt = ("cr", b)
        p.dma(r[b], hv[t], writes=[rt])
        p.op("pool", lambda e, b=b: e.tensor_scalar(out=r[b], in0=r[b], scalar1=DN_ALPHA, scalar2=None, op0=ALU.mult), reads=[rt], writes=[rt])
        for j in range(nblk):
            c = n % 3
            n += 1
            p.dma(pl[c], PART2[j * T + t * 128:j * T + (t + 1) * 128, :], reads=["PART2"], writes=[("cpl", c)])
            if use_oh:
                p.op("dve", lambda e, b=b, c=c, j=j: e.scalar_tensor_tensor(out=r[b], in0=pl[c], scalar=oh[:, j:j + 1], in1=r[b], op0=ALU.mult, op1=ALU.add),
                     reads=[("cpl", c), "oh", rt], writes=[rt])
            else:
                p.op("dve" if j % 2 == 0 else "pool", lambda e, b=b, c=c: e.tensor_tensor(out=r[b], in0=r[b], in1=pl[c], op=ALU.add),
                     reads=[("cpl", c), rt], writes=[rt])
        ln_tile(cx, r[b], rt, junk, "junk", sm, "sm", gbc, gtok, bbc, btok)
        p.dma(ov[t], r[b], reads=[rt])
    S.close()


def stage_gla_proj(cx, h_tm):
    p, nc, dr = cx.p, cx.nc, cx.dram
    S = Scope(p)
    ident = load_const(cx, S, "ident", "ident")
    xT = S.sb("xT", [128, KC, T], BF16)
    S2 = Scope(p)
    transpose_in(cx, S2, h_tm, xT, "xT", ident, "ident")
    S2.close()
    w_in = dr["gla_w_in"]
    ws = WStream(cx, S, KC, 256, "gwin")
    gps = [(S.ps("gps", [128, 512], F32), ("gps", i)) for i in range(3)]
    lps = [S.ps("glps", [128, 512], F32) for _ in range(2)]
    fst = [S.sb("fst", [128, T], F32) for _ in range(2)]
    tst = [S.sb("tst", [128, 256], F32) for _ in range(2)]
    vst = [S.sb("gvst", [128, 256], BF16) for _ in range(2)]
    glT = S.sb("glT", [16, T], F32)
    cnt = {"f": 0, "t": 0, "v": 0}

    def ep(tag, c0, w, idx, ps, ptok):
        if tag in ("q", "k"):
            tb = idx
            b = cnt["f"] % 2
            sc = (1.0 / 16.0) if tag == "q" else 1.0
            p.op("act", lambda e: e.activation(out=fst[b][:, tb * 512:(tb + 1) * 512], in_=ps[:, 0:512], func=AF.Copy, scale=sc), reads=[ptok], writes=[("fst", b, tb)])
            if tb == 3:
                cnt["f"] += 1
                dst = dr["gq_fm"] if tag == "q" else dr["gk_fm"]
                r0 = c0 if tag == "q" else c0 - 1024
                p.dma(dst[r0:r0 + 128, :], fst[b], reads=[("fst", b, i) for i in range(4)])
        elif tag == "gl":
            tb = idx
            p.op("act", lambda e: e.activation(out=glT[:, tb * 512:(tb + 1) * 512], in_=ps[0:16, 0:512], func=AF.Copy), reads=[ptok], writes=[("glT", tb)])
        elif tag in ("ktm", "g"):
            b = cnt["t"] % 2
            cnt["t"] += 1
            if tag == "g":
                p.op("act", lambda e: e.activation(out=tst[b][:, 0:w], in_=ps[:, 0:w], func=AF.Silu), reads=[ptok], writes=[("tst", b)])
                p.dma(dr["gsg"][idx * 128:(idx + 1) * 128, c0 - 4096:c0 - 4096 + w], tst[b][:, 0:w], reads=[("tst", b)])
            else:
                p.op("dve", lambda e: e.tensor_copy(out=tst[b][:, 0:w], in_=ps[:, 0:w]), reads=[ptok], writes=[("tst", b)])
                p.dma(dr["gk_tm"][idx * 128:(idx + 1) * 128, c0 - 1024:c0 - 1024 + w], tst[b][:, 0:w], reads=[("tst", b)])
        elif tag == "v":
            b = cnt["v"] % 2
            cnt["v"] += 1
            p.op("dve", lambda e: e.tensor_copy(out=vst[b][:, 0:w], in_=ps[:, 0:w]), reads=[ptok], writes=[("gvst", b)])
            p.dma(dr["gv_tm"][idx * 128:(idx + 1) * 128, c0 - 2048:c0 - 2048 + w], vst[b][:, 0:w], reads=[("gvst", b)])

    panels = [(i * 256, 256, "q") for i in range(4)] + [(1024 + i * 256, 256, "k") for i in range(4)] + [(6144, 16, "gl")]
    gemm_panels(cx, ws, w_in, panels, xT, "xT", "fm", gps, ep)
    p.barrier()
    panels = [(1024 + i * 256, 256, "ktm") for i in range(4)] + [(2048 + i * 256, 256, "v") for i in range(8)] + [(4096 + i * 256, 256, "g") for i in range(8)]
    gemm_panels(cx, ws, w_in, panels, xT, "xT", "tm", gps, ep)
    p.barrier()
    wg2 = S.sb("wg2", [16, 1024], F32)
    p.dma(wg2, dr["gla_w_gate2"], writes=["wg2"])
    bg, bgtok = bcast_row(cx, S, dr["gla_b_gate"], 1024, "bgate")
    xg = [S.sb("xg", [128, 1024], F32) for _ in range(2)]
    lg = [S.sb("lgl", [128, 1024], F32) for _ in range(2)]
    for t in range(NT):
        b = t % 2
        for hf in range(2):
            p.mm([lambda e, hf=hf: e.matmul(lps[hf][:, 0:512], lhsT=glT[:, t * 128:(t + 1) * 128], rhs=wg2[:, hf * 512:(hf + 1) * 512], start=True, stop=True)],
                 reads=toks("glT", range(4)) + ["wg2"], writes=[("glps", hf)])
            p.op("dve", lambda e, hf=hf: e.tensor_tensor(out=xg[b][:, hf * 512:(hf + 1) * 512], in0=lps[hf][:, 0:512], in1=bg[:, hf * 512:(hf + 1) * 512], op=ALU.add),
                 reads=[("glps", hf), bgtok], writes=[("xg", b, hf)])
        xt2 = [("xg", b, 0), ("xg", b, 1)]
        p.op("act", lambda e: e.activation(out=lg[b], in_=xg[b], func=AF.Abs), reads=xt2, writes=[("lgl", b)])
        p.op("act", lambda e: e.activation(out=lg[b], in_=lg[b], func=AF.Exp, scale=-1.0), reads=[("lgl", b)], writes=[("lgl", b)])
        p.op("act", lambda e: e.activation(out=lg[b], in_=lg[b], func=AF.Ln, bias=1.0), reads=[("lgl", b)], writes=[("lgl", b)])
        p.op("dve", lambda e: e.tensor_scalar(out=xg[b], in0=xg[b], scalar1=0.0, scalar2=1.0 / 16.0, op0=ALU.min, op1=ALU.mult), reads=xt2, writes=xt2)
        p.op("dve", lambda e: e.scalar_tensor_tensor(out=lg[b], in0=lg[b], scalar=-1.0 / 16.0, in1=xg[b], op0=ALU.mult, op1=ALU.add),
             reads=xt2 + [("lgl", b)], writes=[("lgl", b)])
        p.dma(dr["gla_d"][t * 128:(t + 1) * 128, :], lg[b], reads=[("lgl", b)])
    S.close()


def stage_gla_core(cx):
    p, nc, dr = cx.p, cx.nc, cx.dram
    S = Scope(p)
    identb = load_const(cx, S, "identb", "identb")
    btri = load_const(cx, S, "btri", "btri")
    bgt = load_const(cx, S, "bgt", "bgt")
    gnb, gntok = bcast_row(cx, S, dr["gla_norm"], 512, "gnb")
    yT = S.sb("gyT", [128, 16, T], BF16)
    St = S.sb("St", [128, 8, 512], F32)
    Sb = S.sb("Sb", [128, 8, 512], BF16)
    S1 = S.sb("S1", [128, 2, 512], F32)
    S1b = S.sb("S1b", [128, 2, 512], BF16)
    p.op("pool", lambda e: e.memset(St, 0.0), writes=toks("St", range(8)))
    p.op("pool", lambda e: e.memset(Sb, 0.0), writes=toks("Sb", range(8)))
    la = [S.sb("la", [128, 1024], F32) for _ in range(2)]
    ktm = [S.sb("ktm", [128, 1024], F32) for _ in range(2)]
    vt = [S.sb("gvt", [128, 2048], BF16) for _ in range(2)]
    sg = [S.sb("gsg", [128, 2048], F32) for _ in range(2)]
    qf = [S.sb("gqf", [128, 8, 128], F32) for _ in range(2)]
    kf = [S.sb("gkf", [128, 8, 128], F32) for _ in range(2)]
    eg = S.sb("eg", [128, 8, 128], F32)
    eng = S.sb("eng", [128, 8, 128], F32)
    egl = S.sb("egl", [128, 1024], F32)
    qin = S.sb("qin", [128, 8, 128], BF16)
    kin = S.sb("kin", [128, 8, 128], BF16)
    qA = S.sb("qA", [128, 8, 128], BF16)
    qB = S.sb("qB", [128, 8, 128], BF16)
    p.op("pool", lambda e: e.memset(qA, 0.0), writes=["qA"])
    p.op("pool", lambda e: e.memset(qB, 0.0), writes=["qB"])
    ke = S.sb("ke", [128, 1024], BF16)
    atm = S.sb("atm", [128, 128], BF16)
    junk = S.sb("gjunk", [128, 512], F32)
    on = S.sb("on", [128, 512], F32)
    ybf = S.sb("gybf", [128, 512], BF16)
    sm = S.sb("gsm", [128, 4], F32)
    GC = [S.ps("GC", [128, 512], F32) for _ in range(2)]
    GL = [S.ps("GL", [128, 512], F32) for _ in range(2)]
    ATp = S.ps("ATp", [128, 512], F32)
    Op = S.ps("Op", [128, 512], F32)
    Dp = S.ps("Dp", [128, 512], F32)
    tp = S.ps("gtp", [128, 512], BF16)
    qv = dr["gq_fm"].rearrange("(c p) t -> p c t", p=128)
    kv = dr["gk_fm"].rearrange("(c p) t -> p c t", p=128)

    def load(t):
        b = t % 2
        rs = slice(t * 128, (t + 1) * 128)
        p.dma(la[b], dr["gla_d"][rs, :], writes=[("la", b)])
        p.dma(ktm[b], dr["gk_tm"][rs, :], writes=[("ktm", b)])
        p.dma(vt[b], dr["gv_tm"][rs, :], writes=[("gvt", b)])
        p.dma(sg[b], dr["gsg"][rs, :], writes=[("gsg", b)])
        p.dma(qf[b], qv[:, :, rs], writes=[("gqf", b)])
        p.dma(kf[b], kv[:, :, rs], writes=[("gkf", b)])
    load(0)
    for t in range(NT):
        if t + 1 < NT:
            load(t + 1)
        b = t % 2
        cs = slice(t * 128, (t + 1) * 128)
        for hf in range(2):
            p.mm([(lambda e, j=j, hf=hf: e.matmul(GC[hf][:, j * 128:(j + 1) * 128], lhsT=la[b][:, (hf * 4 + j) * 128:(hf * 4 + j + 1) * 128], rhs=btri,
                                                  start=True, stop=True)) for j in range(4)], reads=[("la", b), "btri"], writes=[("GC", hf)])
            p.op("act", lambda e, hf=hf: e.activation(out=eg[:, hf * 4:(hf + 1) * 4, :], in_=GC[hf].rearrange("p (a c) -> p a c", a=4), func=AF.Exp),
                 reads=[("GC", hf)], writes=[("eg", hf)])
            p.op("act", lambda e, hf=hf: e.activation(out=eng[:, hf * 4:(hf + 1) * 4, :], in_=GC[hf].rearrange("p (a c) -> p a c", a=4), func=AF.Exp, scale=-1.0),
                 reads=[("GC", hf)], writes=[("eng", hf)])
            p.mm([lambda e, hf=hf: e.matmul(GL[hf][:, 0:512], lhsT=bgt, rhs=la[b][:, hf * 512:(hf + 1) * 512], start=True, stop=True)],
                 reads=[("la", b), "bgt"], writes=[("GL", hf)])
            p.op("act", lambda e, hf=hf: e.activation(out=egl[:, hf * 512:(hf + 1) * 512], in_=GL[hf][:, 0:512], func=AF.Exp), reads=[("GL", hf)], writes=[("egl", hf)])
        egt = [("eg", 0), ("eg", 1)]
        p.op("dve", lambda e: e.tensor_tensor(out=qin, in0=qf[b], in1=eg, op=ALU.mult), reads=[("gqf", b)] + egt, writes=["qin"])
        p.op("pool", lambda e: e.tensor_tensor(out=kin, in0=kf[b], in1=eng, op=ALU.mult), reads=[("gkf", b), ("eng", 0), ("eng", 1)], writes=["kin"])
        p.op("pool", lambda e: e.tensor_copy(out=qA[:, :, 0:64], in_=qin[:, :, 0:64]), reads=["qin"], writes=["qA"])
        p.op("pool", lambda e: e.tensor_copy(out=qB[:, :, 64:128], in_=qin[:, :, 64:128]), reads=["qin"], writes=["qB"])
        p.op("dve", lambda e: e.tensor_tensor(out=ke, in0=ktm[b], in1=egl, op=ALU.mult), reads=[("ktm", b), ("egl", 0), ("egl", 1)], writes=["ke"])
        for hd in range(4):
            vs = slice(hd * 512, (hd + 1) * 512)
            p.mm([(lambda e, dcl=dcl: e.matmul(ATp[:, 0:128], lhsT=kin[:, 2 * hd + dcl, :], rhs=qin[:, 2 * hd + dcl, :], start=(dcl == 0), stop=(dcl == 1)))
                  for dcl in range(2)], reads=["kin", "qin"], writes=["ATp"])
            p.op("dve", lambda e: e.tensor_tensor(out=atm, in0=ATp[:, 0:128], in1=btri, op=ALU.mult), reads=["ATp", "btri"], writes=["atm"])
            for dcl in range(2):
                dc = 2 * hd + dcl
                p.mm([lambda e, dc=dc: e.matmul(Dp[:, 0:512], lhsT=ke[0:64, dc * 128:(dc + 1) * 128], rhs=vt[b][0:64, vs], start=True, stop=True)],
                     reads=["ke", ("gvt", b)], writes=["Dp"])
                p.op("dve", lambda e, dc=dc, dcl=dcl: e.scalar_tensor_tensor(out=S1[:, dcl, :], in0=St[:, dc, :], scalar=eg[:, dc, 63:64], in1=Dp[:, 0:512],
                                                                            op0=ALU.mult, op1=ALU.add), reads=[("St", dc), "Dp"] + egt, writes=[("S1", dcl)])
                p.op("act", lambda e, dcl=dcl: e.activation(out=S1b[:, dcl, :], in_=S1[:, dcl, :], func=AF.Copy), reads=[("S1", dcl)], writes=[("S1b", dcl)])
            fns = [lambda e: e.matmul(Op[:, 0:512], lhsT=atm, rhs=vt[b][:, vs], start=True, stop=False)]
            for dcl in range(2):
                dc = 2 * hd + dcl
                fns.append(lambda e, dc=dc: e.matmul(Op[:, 0:512], lhsT=qA[:, dc, :], rhs=Sb[:, dc, :], start=False, stop=False))
                fns.append(lambda e, dc=dc, dcl=dcl: e.matmul(Op[:, 0:512], lhsT=qB[:, dc, :], rhs=S1b[:, dcl, :], start=False, stop=(dcl == 1)))
            p.mm(fns, reads=["atm", ("gvt", b), "qA", "qB", ("Sb", 2 * hd), ("Sb", 2 * hd + 1), ("S1b", 0), ("S1b", 1)], writes=["Op"])
            for dcl in range(2):
                dc = 2 * hd + dcl
                p.mm([lambda e, dc=dc: e.matmul(Dp[:, 0:512], lhsT=ke[64:128, dc * 128:(dc + 1) * 128], rhs=vt[b][64:128, vs], start=True, stop=True)],
                     reads=["ke", ("gvt", b)], writes=["Dp"])
                p.op("dve", lambda e, dc=dc, dcl=dcl: e.scalar_tensor_tensor(out=St[:, dc, :], in0=S1[:, dcl, :], scalar=eg[:, dc, 127:128], in1=Dp[:, 0:512],
                                                                            op0=ALU.mult, op1=ALU.add), reads=[("S1", dcl), "Dp"] + egt, writes=[("St", dc)])
                p.op("act", lambda e, dc=dc: e.activation(out=Sb[:, dc, :], in_=St[:, dc, :], func=AF.Copy), reads=[("St", dc)], writes=[("Sb", dc)])
            p.op("act", lambda e: e.activation(out=junk, in_=Op[:, 0:512], func=AF.Square, accum_out=sm[:, 0:1]), reads=["Op"], writes=["gjunk", "gsm"])
            p.op("dve", lambda e: e.tensor_scalar(out=sm[:, 1:2], in0=sm[:, 0:1], scalar1=1.0 / 512.0, scalar2=EPS, op0=ALU.mult, op1=ALU.add), reads=["gsm"], writes=["gsm"])
            p.op("act", lambda e: e.activation(out=sm[:, 2:3], in_=sm[:, 1:2], func=AF.Sqrt), reads=["gsm"], writes=["gsm"])
            p.op("dve", lambda e: e.reciprocal(out=sm[:, 3:4], in_=sm[:, 2:3]), reads=["gsm"], writes=["gsm"])
            p.op("dve", lambda e: e.scalar_tensor_tensor(out=on, in0=Op[:, 0:512], scalar=sm[:, 3:4], in1=gnb, op0=ALU.mult, op1=ALU.mult),
                 reads=["Op", "gsm", gntok], writes=["on"])
            p.op("pool", lambda e: e.tensor_tensor(out=ybf, in0=on, in1=sg[b][:, vs], op=ALU.mult), reads=["on", ("gsg", b)], writes=["gybf"])
            p.mm([(lambda e, j=j: e.transpose(out=tp[:, j * 128:(j + 1) * 128], in_=ybf[:, j * 128:(j + 1) * 128], identity=identb)) for j in range(4)],
                 reads=["gybf", "identb"], writes=["gtp"])
            p.op("act", lambda e: e.activation(out=yT[:, hd * 4:(hd + 1) * 4, cs], in_=tp[:, 0:512].rearrange("p (a c) -> p a c", a=4), func=AF.Copy),
                 reads=["gtp"], writes=[("gyT", t, hd)])
    for k in range(16):
        p.dma(dr["yT"][k * 128:(k + 1) * 128, :], yT[:, k, :], reads=[("gyT", t, k // 4) for t in range(NT)])
    S.close()


def stage_moe_local(cx, L, h_in, g_row, b_row, h_out, NE=32):
    p, nc, dr = cx.p, cx.nc, cx.dram
    S = Scope(p)
    ident = load_const(cx, S, "ident", "ident")
    mps = S.ps("emps", [128, 512], F32)
    nrow = NE * 16
    nch = (nrow + 127) // 128
    b1g = S.sb("b1g", [128, nrow], F32)
    b1l = S.sb("b1l", [128, nrow], F32)
    b1st = S.sb("b1st", [128, 256], F32)
    b1v = dr["moe_b1"][L * NE:(L + 1) * NE, :].rearrange("e (f c) -> (e f) c", c=256)
    for ch in range(nch):
        r = min(128, nrow - ch * 128)
        p.dma(b1st[0:r, :], b1v[ch * 128:ch * 128 + r, :], writes=["b1st"])
        p.mm([lambda e, r=r: e.transpose(out=mps[:, 0:r], in_=b1st[0:r, 0:256:2], identity=ident[0:r, 0:r]),
              lambda e, r=r: e.transpose(out=mps[:, 128:128 + r], in_=b1st[0:r, 1:256:2], identity=ident[0:r, 0:r])], reads=["b1st", "ident"], writes=["emps"])
        p.op("dve", lambda e, r=r, ch=ch: e.tensor_copy(out=b1g[:, ch * 128:ch * 128 + r], in_=mps[:, 0:r]), reads=["emps"], writes=["b1g"])
        p.op("dve", lambda e, r=r, ch=ch: e.tensor_copy(out=b1l[:, ch * 128:ch * 128 + r], in_=mps[:, 128:128 + r]), reads=["emps"], writes=["b1l"])
    hTg = S.sb("hTg", [128, KC, 1024], BF16)
    actT = S.sb("actT", [128, KC, 1024], BF16)
    acc = S.sb("acc", [128, 8, D], F32)
    gts = S.sb("gts", [128, 8, 32], F32)
    wst = [S.sb("wst", [128, KC, 256], F32) for _ in range(2)]
    wbf = [S.sb("wbf", [128, KC, 256], BF16) for _ in range(2)]
    b2t = [S.sb("b2t", [128, 256], F32) for _ in range(2)]
    ga = [S.sb("ga", [128, 512], F32) for _ in range(2)]
    sg = [S.sb("sg", [128, 512], F32)] * 2
    la = [S.sb("la", [128, 512], F32)] * 2
    yb = [S.sb("yb", [128, 256], F32) for _ in range(2)]
    hld = S.sb("hld", [128, D], F32)
    sm = S.sb("sm", [128, 8], F32)
    Gp = [S.ps("Gp", [128, 512], F32) for _ in range(2)]
    Lp = [S.ps("Lp", [128, 512], F32) for _ in range(2)]
    Yp = [S.ps("Yp", [128, 512], F32) for _ in range(2)]
    st_ = {"n": 0, "it": 0}

    def fetch(w2d, r0, c0, deint):
        b = st_["n"] % 2
        st_["n"] += 1
        src = w2d[r0:r0 + D, c0:c0 + 256].rearrange("(k p) n -> p k n", p=128)
        p.dma(wst[b], src, writes=[("wst", b)])
        if deint:
            sv = wst[b].rearrange("p k (i two) -> p k i two", two=2)
            p.op("act", lambda e, b=b, sv=sv: e.activation(out=wbf[b][:, :, 0:128], in_=sv[:, :, :, 0], func=AF.Copy), reads=[("wst", b)], writes=[("wbf", b, 0)])
            p.op("pool", lambda e, b=b, sv=sv: e.tensor_copy(out=wbf[b][:, :, 128:256], in_=sv[:, :, :, 1]), reads=[("wst", b)], writes=[("wbf", b, 1)])
        else:
            p.op("act", lambda e, b=b: e.activation(out=wbf[b][:, 0:8, :], in_=wst[b][:, 0:8, :], func=AF.Copy), reads=[("wst", b)], writes=[("wbf", b, 0)])
            p.op("pool", lambda e, b=b: e.tensor_copy(out=wbf[b][:, 8:16, :], in_=wst[b][:, 8:16, :]), reads=[("wst", b)], writes=[("wbf", b, 1)])
        return b
    hv = h_in.rearrange("(t p) d -> t p d", p=128)
    ov = h_out.rearrange("(t p) d -> t p d", p=128)
    for gi in range(2):
        p.dma(hTg, dr["hTo"].rearrange("(k p) t -> p k t", p=128)[:, :, gi * 1024:(gi + 1) * 1024], writes=["hTg"])
        p.dma(gts, dr["gateo"][gi * 1024:(gi + 1) * 1024, :].rearrange("(t p) e -> p t e", p=128), writes=["gts"])
        for e_ in range(NE):
            r0 = (L * NE + e_) * D
            nb = fetch(dr["moe_w1"], r0, 0, True)
            for fc in range(16):
                b1 = nb
                if fc + 1 < 16:
                    nb = fetch(dr["moe_w1"], r0, (fc + 1) * 256, True)
                else:
                    nb = fetch(dr["moe_w2"], r0, 0, False)
                wt = [("wbf", b1, 0), ("wbf", b1, 1)]
                for tb in range(2):
                    i2 = st_["it"] % 2
                    st_["it"] += 1
                    tsl = slice(tb * 512, (tb + 1) * 512)
                    p.mm([(lambda e, c=c: e.matmul(Gp[i2][:, 0:512], lhsT=wbf[b1][:, c, 0:128], rhs=hTg[:, c, tsl], start=(c == 0), stop=(c == KC - 1)))
                          for c in range(KC)], reads=wt + ["hTg"], writes=[("Gp", i2)])
                    p.mm([(lambda e, c=c: e.matmul(Lp[i2][:, 0:512], lhsT=wbf[b1][:, c, 128:256], rhs=hTg[:, c, tsl], start=(c == 0), stop=(c == KC - 1)))
                          for c in range(KC)], reads=wt + ["hTg"], writes=[("Lp", i2)])
                    col = e_ * 16 + fc
                    p.op("dve", lambda e: e.tensor_scalar(out=ga[i2], in0=Gp[i2][:, 0:512], scalar1=b1g[:, col:col + 1], scalar2=7.0, op0=ALU.add, op1=ALU.min),
                         reads=[("Gp", i2), "b1g"], writes=[("ga", i2)])
                    p.op("act", lambda e: e.activation(out=sg[i2], in_=ga[i2], func=AF.Sigmoid, scale=1.702), reads=[("ga", i2)], writes=["sg1"])
                    p.op("dve", lambda e: e.tensor_scalar(out=la[i2], in0=Lp[i2][:, 0:512], scalar1=b1l[:, col:col + 1], scalar2=7.0, op0=ALU.add, op1=ALU.min),
                         reads=[("Lp", i2), "b1l"], writes=["la1"])
                    p.op("dve", lambda e: e.tensor_scalar(out=la[i2], in0=la[i2], scalar1=-7.0, scalar2=1.0, op0=ALU.max, op1=ALU.add),
                         reads=["la1"], writes=["la1"])
                    p.op("pool", lambda e: e.tensor_tensor(out=ga[i2], in0=ga[i2], in1=sg[i2], op=ALU.mult), reads=[("ga", i2), "sg1"], writes=[("ga", i2)])
                    p.op("pool", lambda e: e.tensor_tensor(out=actT[:, fc, tsl], in0=ga[i2], in1=la[i2], op=ALU.mult), reads=[("ga", i2), "la1"],
                         writes=[("actT", fc, tb)])
            for dp in range(8):
                b2 = nb
                if dp + 1 < 8:
                    nb = fetch(dr["moe_w2"], r0, (dp + 1) * 256, False)
                wt = [("wbf", b2, 0), ("wbf", b2, 1)]
                dsl = slice(dp * 256, (dp + 1) * 256)
                bb = dp % 2
                p.dma(b2t[bb], dr["moe_b2"][L * NE + e_:L * NE + e_ + 1, dsl].partition_broadcast(128), writes=[("b2t", bb)])
                for t in range(8):
                    i2 = st_["it"] % 2
                    st_["it"] += 1
                    p.mm([(lambda e, c=c: e.matmul(Yp[i2][:, 0:256], lhsT=actT[:, c, t * 128:(t + 1) * 128], rhs=wbf[b2][:, c, :], start=(c == 0), stop=(c == KC - 1)))
                          for c in range(KC)], reads=wt + [("actT", c, t // 4) for c in range(KC)], writes=[("Yp", i2)])
                    p.op("dve", lambda e: e.tensor_tensor(out=yb[i2], in0=Yp[i2][:, 0:256], in1=b2t[bb], op=ALU.add), reads=[("Yp", i2), ("b2t", bb)],
                         writes=[("yb", i2)])
                    if e_ == 0:
                        p.op("dve", lambda e: e.tensor_scalar(out=acc[:, t, dsl], in0=yb[i2], scalar1=gts[:, t, e_:e_ + 1], scalar2=None, op0=ALU.mult),
                             reads=[("yb", i2), "gts"], writes=[("acc", t, dp)])
                    else:
                        p.op("dve", lambda e: e.scalar_tensor_tensor(out=acc[:, t, dsl], in0=yb[i2], scalar=gts[:, t, e_:e_ + 1], in1=acc[:, t, dsl],
                                                                    op0=ALU.mult, op1=ALU.add), reads=[("yb", i2), "gts", ("acc", t, dp)], writes=[("acc", t, dp)])
        gbc = wst[0].rearrange("p k c -> p (k c)")[:, 0:D]
        bbc = wst[1].rearrange("p k c -> p (k c)")[:, 0:D]
        gtok, btok = ("wst", 0), ("wst", 1)
        p.dma(gbc, g_row.partition_broadcast(128), writes=[gtok])
        p.dma(bbc, b_row.partition_broadcast(128), writes=[btok])
        for t in range(8):
            at = [("acc", t, dp) for dp in range(8)]
            p.dma(hld, hv[gi * 8 + t], writes=["hld"])
            p.op("dve", lambda e, t=t: e.scalar_tensor_tensor(out=acc[:, t, :], in0=hld, scalar=DN_ALPHA, in1=acc[:, t, :], op0=ALU.mult, op1=ALU.add),
                 reads=["hld"] + at, writes=at)
            ln_tile(cx, acc[:, t, :], ("acc", t, 0), hld, "hld", sm, "sm", gbc, gtok, bbc, btok)
            p.dma(ov[gi * 8 + t], acc[:, t, :], reads=at)
    S.close()


CAP = 256


def stage_moe_sparse(cx, L, h_in, g_row, b_row, h_out, NE=32):
    p, nc, dr = cx.p, cx.nc, cx.dram
    S = Scope(p)
    ident = load_const(cx, S, "ident", "ident")
    identb = load_const(cx, S, "identb", "identb")
    trius = load_const(cx, S, "trius", "trius")
    ones128 = load_const(cx, S, "ones128", "ones128")
    iota = load_const(cx, S, "iota", "iota")
    GA = [S.ps("GA", [128, 512], F32) for _ in range(2)]
    Gp = S.ps("Gp", [128, 512], F32)
    Lp = S.ps("Lp", [128, 512], F32)
    Yp = S.ps("Yp", [128, 512], F32)
    SC = S.ps("SC", [128, 512], F32)
    PT = [S.ps("PT", [128, 1024], BF16) for _ in range(2)]
    nrow = NE * 16
    nch = (nrow + 127) // 128
    b1g = S.sb("b1g", [128, nrow], F32)
    b1l = S.sb("b1l", [128, nrow], F32)
    b1st = S.sb("b1st", [128, 256], F32)
    b1v = dr["moe_b1"][L * NE:(L + 1) * NE, :].rearrange("e (f c) -> (e f) c", c=256)
    for ch in range(nch):
        r = min(128, nrow - ch * 128)
        p.dma(b1st[0:r, :], b1v[ch * 128:ch * 128 + r, :], writes=["b1st"])
        p.mm([lambda e, r=r: e.transpose(out=SC[:, 0:r], in_=b1st[0:r, 0:256:2], identity=ident[0:r, 0:r]),
              lambda e, r=r: e.transpose(out=SC[:, 128:128 + r], in_=b1st[0:r, 1:256:2], identity=ident[0:r, 0:r])], reads=["b1st", "ident"], writes=["SC"])
        p.op("dve", lambda e, r=r, ch=ch: e.tensor_copy(out=b1g[:, ch * 128:ch * 128 + r], in_=SC[:, 0:r]), reads=["SC"], writes=["b1g"])
        p.op("dve", lambda e, r=r, ch=ch: e.tensor_copy(out=b1l[:, ch * 128:ch * 128 + r], in_=SC[:, 128:128 + r]), reads=["SC"], writes=["b1l"])
    hb = S.sb("hb", [128, 8, D], BF16)
    acc = S.sb("acc", [128, 8, D], F32)
    gts = S.sb("gts", [128, 8, 32], F32)
    sel = S.sb("sel", [128, 8, 32], F32)
    rank = S.sb("rank", [128, 8, 32], F32)
    Pm = [S.sb("Pm", [128, 8, CAP], BF16)] * 2
    PG = S.sb("PG", [128, 8, CAP], BF16)
    PGT = S.sb("PGT", [128, 2, 8, 128], BF16)
    xgT = S.sb("xgT", [128, KC, CAP], BF16)
    actT = S.sb("actT", [128, KC, CAP], BF16)
    Yb = S.sb("Yb", [128, 2, D], BF16)
    wst = [S.sb("wst", [128, KC, 256], F32) for _ in range(2)]
    wbf = [S.sb("wbf", [128, KC, 256], BF16) for _ in range(2)]
    b2t = [S.sb("b2t", [128, 256], F32) for _ in range(2)]
    ga = [S.sb("ga", [128, CAP], F32) for _ in range(2)]
    sg = S.sb("sg", [128, CAP], F32)
    la = S.sb("la", [128, CAP], F32)
    hld = S.sb("hld", [128, D], F32)
    sm = S.sb("sm", [128, 8], F32)
    st_ = {"n": 0, "it": 0}

    def fetch(w2d, r0, c0, deint):
        b = st_["n"] % 2
        st_["n"] += 1
        src = w2d[r0:r0 + D, c0:c0 + 256].rearrange("(k p) n -> p k n", p=128)
        p.dma(wst[b], src, writes=[("wst", b)])
        if deint:
            sv = wst[b].rearrange("p k (i two) -> p k i two", two=2)
            p.op("act", lambda e, b=b, sv=sv: e.activation(out=wbf[b][:, :, 0:128], in_=sv[:, :, :, 0], func=AF.Copy), reads=[("wst", b)], writes=[("wbf", b, 0)])
            p.op("pool", lambda e, b=b, sv=sv: e.tensor_copy(out=wbf[b][:, :, 128:256], in_=sv[:, :, :, 1]), reads=[("wst", b)], writes=[("wbf", b, 1)])
        else:
            p.op("act", lambda e, b=b: e.activation(out=wbf[b][:, 0:8, :], in_=wst[b][:, 0:8, :], func=AF.Copy), reads=[("wst", b)], writes=[("wbf", b, 0)])
            p.op("pool", lambda e, b=b: e.tensor_copy(out=wbf[b][:, 8:16, :], in_=wst[b][:, 8:16, :]), reads=[("wst", b)], writes=[("wbf", b, 1)])
        return b
    hv = h_in.rearrange("(t p) d -> t p d", p=128)
    ov = h_out.rearrange("(t p) d -> t p d", p=128)
    for gi in range(2):
        for t in range(8):
            p.dma(hld, hv[gi * 8 + t], writes=["hld"])
            p.op("pool" if t % 2 else "act", (lambda e, t=t: e.tensor_copy(out=hb[:, t, :], in_=hld)) if t % 2 else
                 (lambda e, t=t: e.activation(out=hb[:, t, :], in_=hld, func=AF.Copy)), reads=["hld"], writes=[("hb", t)])
        p.dma(gts, dr["gateo"][gi * 1024:(gi + 1) * 1024, :].rearrange("(t p) e -> p t e", p=128), writes=["gts"])
        p.op("dve", lambda e: e.tensor_scalar(out=sel, in0=gts, scalar1=0.0, scalar2=0.0, op0=ALU.is_gt, op1=ALU.add), reads=["gts"], writes=["sel"])
        for t in range(8):
            fns = [lambda e, t=t: e.matmul(GA[0][:, 0:32], lhsT=trius, rhs=sel[:, t, :], start=True, stop=(t == 0))]
            for t2 in range(t):
                fns.append(lambda e, t2=t2, t=t: e.matmul(GA[0][:, 0:32], lhsT=ones128, rhs=sel[:, t2, :], start=False, stop=(t2 == t - 1)))
            p.mm(fns, reads=["sel", "trius", "ones128"], writes=[("GA", 0)])
            p.op("dve", lambda e, t=t: e.tensor_copy(out=rank[:, t, :], in_=GA[0][:, 0:32]), reads=[("GA", 0)], writes=["rank"])
        hbt = toks("hb", range(8))
        for e_ in range(NE):
            r0 = (L * NE + e_) * D
            nb = fetch(dr["moe_w1"], r0, 0, True)
            pb = 0
            for t in range(8):
                p.op("dve", lambda e, t=t: e.tensor_scalar(out=Pm[pb][:, t, :], in0=iota, scalar1=rank[:, t, e_:e_ + 1], scalar2=sel[:, t, e_:e_ + 1],
                                                        op0=ALU.is_equal, op1=ALU.mult), reads=["iota", "rank", "sel"], writes=[("Pm", pb)])
                p.op("dve", lambda e, t=t: e.tensor_scalar(out=PG[:, t, :], in0=iota, scalar1=rank[:, t, e_:e_ + 1], scalar2=gts[:, t, e_:e_ + 1],
                                                        op0=ALU.is_equal, op1=ALU.mult), reads=["iota", "rank", "gts"], writes=["PG"])
            for st in range(2):
                p.mm([(lambda e, t=t, st=st: e.transpose(out=PT[st][:, t * 128:(t + 1) * 128], in_=PG[:, t, st * 128:(st + 1) * 128], identity=identb))
                      for t in range(8)], reads=["PG", "identb"], writes=[("PT", st)])
                p.op("act", lambda e, st=st: e.activation(out=PGT[:, st, :, :], in_=PT[st].rearrange("p (a c) -> p a c", a=8), func=AF.Copy),
                     reads=[("PT", st)], writes=[("PGT", st)])
            for k2 in range(8):
                gb = k2 % 2
                fns = []
                for j in range(2):
                    kc = k2 * 2 + j
                    for t in range(8):
                        fns.append(lambda e, kc=kc, t=t, j=j: e.matmul(GA[gb][:, j * 256:(j + 1) * 256], lhsT=hb[:, t, kc * 128:(kc + 1) * 128], rhs=Pm[pb][:, t, :],
                                                                       start=(t == 0), stop=(t == 7)))
                p.mm(fns, reads=hbt + [("Pm", pb)], writes=[("GA", gb)])
                p.op("act" if k2 % 2 else "dve",
                     (lambda e, k2=k2, gb=gb: e.activation(out=xgT[:, k2 * 2:k2 * 2 + 2, :], in_=GA[gb].rearrange("p (a c) -> p a c", a=2), func=AF.Copy)) if k2 % 2 else
                     (lambda e, k2=k2, gb=gb: e.tensor_copy(out=xgT[:, k2 * 2:k2 * 2 + 2, :], in_=GA[gb].rearrange("p (a c) -> p a c", a=2))),
                     reads=[("GA", gb)], writes=[("xgT", k2)])
            xt = toks("xgT", range(8))
            for fc in range(16):
                b1 = nb
                if fc + 1 < 16:
                    nb = fetch(dr["moe_w1"], r0, (fc + 1) * 256, True)
                else:
                    nb = fetch(dr["moe_w2"], r0, 0, False)
                wt = [("wbf", b1, 0), ("wbf", b1, 1)]
                i2 = st_["it"] % 2
                st_["it"] += 1
                p.mm([(lambda e, c=c: e.matmul(Gp[:, 0:CAP], lhsT=wbf[b1][:, c, 0:128], rhs=xgT[:, c, :], start=(c == 0), stop=(c == KC - 1)))
                      for c in range(KC)], reads=wt + xt, writes=["Gp"])
                p.mm([(lambda e, c=c: e.matmul(Lp[:, 0:CAP], lhsT=wbf[b1][:, c, 128:256], rhs=xgT[:, c, :], start=(c == 0), stop=(c == KC - 1)))
                      for c in range(KC)], reads=wt + xt, writes=["Lp"])
                col = e_ * 16 + fc
                p.op("dve", lambda e: e.tensor_scalar(out=ga[i2], in0=Gp[:, 0:CAP], scalar1=b1g[:, col:col + 1], scalar2=7.0, op0=ALU.add, op1=ALU.min),
                     reads=["Gp", "b1g"], writes=[("ga", i2)])
                p.op("act", lambda e: e.activation(out=sg, in_=ga[i2], func=AF.Sigmoid, scale=1.702), reads=[("ga", i2)], writes=["sg"])
                p.op("dve", lambda e: e.tensor_scalar(out=la, in0=Lp[:, 0:CAP], scalar1=b1l[:, col:col + 1], scalar2=7.0, op0=ALU.add, op1=ALU.min),
                     reads=["Lp", "b1l"], writes=["la"])
                p.op("dve", lambda e: e.tensor_scalar(out=la, in0=la, scalar1=-7.0, scalar2=1.0, op0=ALU.max, op1=ALU.add), reads=["la"], writes=["la"])
                p.op("pool", lambda e: e.tensor_tensor(out=ga[i2], in0=ga[i2], in1=sg, op=ALU.mult), reads=[("ga", i2), "sg"], writes=[("ga", i2)])
                p.op("pool", lambda e: e.tensor_tensor(out=actT[:, fc, :], in0=ga[i2], in1=la, op=ALU.mult), reads=[("ga", i2), "la"], writes=[("actT", fc)])
            at_ = toks("actT", range(16))
            for dp in range(8):
                b2 = nb
                if dp + 1 < 8:
                    nb = fetch(dr["moe_w2"], r0, (dp + 1) * 256, False)
                wt = [("wbf", b2, 0), ("wbf", b2, 1)]
                dsl = slice(dp * 256, (dp + 1) * 256)
                bb = dp % 2
                p.dma(b2t[bb], dr["moe_b2"][L * NE + e_:L * NE + e_ + 1, dsl].partition_broadcast(128), writes=[("b2t", bb)])
                for st in range(2):
                    p.mm([(lambda e, c=c: e.matmul(Yp[:, 0:256], lhsT=actT[:, c, st * 128:(st + 1) * 128], rhs=wbf[b2][:, c, :], start=(c == 0), stop=(c == KC - 1)))
                          for c in range(KC)], reads=wt + at_, writes=["Yp"])
                    p.op("dve", lambda e, st=st: e.tensor_tensor(out=Yb[:, st, dsl], in0=Yp[:, 0:256], in1=b2t[bb], op=ALU.add), reads=["Yp", ("b2t", bb)],
                         writes=[("Yb", st, dp)])
            yt = [("Yb", st, dp) for st in range(2) for dp in range(8)]
            for t in range(8):
                for dq in range(4):
                    qs = slice(dq * 512, (dq + 1) * 512)
                    p.mm([(lambda e, st=st: e.matmul(SC[:, 0:512], lhsT=PGT[:, st, t, :], rhs=Yb[:, st, qs], start=(st == 0), stop=(st == 1))) for st in range(2)],
                         reads=yt + [("PGT", 0), ("PGT", 1)], writes=["SC"])
                    if e_ == 0:
                        p.op("dve", lambda e: e.tensor_copy(out=acc[:, t, qs], in_=SC[:, 0:512]), reads=["SC"], writes=[("acc", t, dq)])
                    else:
                        p.op("dve", lambda e: e.tensor_tensor(out=acc[:, t, qs], in0=SC[:, 0:512], in1=acc[:, t, qs], op=ALU.add), reads=["SC", ("acc", t, dq)],
                             writes=[("acc", t, dq)])
        gbc = wst[0].rearrange("p k c -> p (k c)")[:, 0:D]
        bbc = wst[1].rearrange("p k c -> p (k c)")[:, 0:D]
        gtok, btok = ("wst", 0), ("wst", 1)
        p.dma(gbc, g_row.partition_broadcast(128), writes=[gtok])
        p.dma(bbc, b_row.partition_broadcast(128), writes=[btok])
        for t in range(8):
            at = [("acc", t, dq) for dq in range(4)]
            p.dma(hld, hv[gi * 8 + t], writes=["hld"])
            p.op("dve", lambda e, t=t: e.scalar_tensor_tensor(out=acc[:, t, :], in0=hld, scalar=DN_ALPHA, in1=acc[:, t, :], op0=ALU.mult, op1=ALU.add),
                 reads=["hld"] + at, writes=at)
            ln_tile(cx, acc[:, t, :], ("acc", t, 0), hld, "hld", sm, "sm", gbc, gtok, bbc, btok)
            p.dma(ov[gi * 8 + t], acc[:, t, :], reads=at)
    S.close()


def stage_moe_sparse2(cx, L, h_in, g_row, b_row, h_out, NE=32):
    p, nc, dr = cx.p, cx.nc, cx.dram
    hv = h_in.rearrange("(t p) d -> t p d", p=128)
    ov = h_out.rearrange("(t p) d -> t p d", p=128)
    XG = dr["XG"].rearrange("(e p) x -> e p x", p=128)
    PGd = dr["PGTd"].rearrange("(e p) x -> e p x", p=128)
    YBd = dr["YBd"].rearrange("(e p) x -> e p x", p=128)
    S = Scope(p)
    identb = load_const(cx, S, "identb", "identb")
    trius = load_const(cx, S, "trius", "trius")
    ones128 = load_const(cx, S, "ones128", "ones128")
    iota = load_const(cx, S, "iota", "iota")
    GA = [S.ps("GA", [128, 512], F32) for _ in range(4)]
    PT = [S.ps("PT", [128, 1024], BF16) for _ in range(2)]
    RK = S.ps("RK", [128, 512], F32)
    hb = S.sb("hb", [128, 8, D], BF16)
    hld = [S.sb("hld", [128, D], F32) for _ in range(2)]
    gts = S.sb("gts", [128, 8, 32], F32)
    sel = S.sb("sel", [128, 8, 32], F32)
    rank = S.sb("rank", [128, 8, 32], F32)
    Pm = [S.sb("Pm", [128, 8, CAP], BF16) for _ in range(2)]
    PG = [S.sb("PG", [128, 8, CAP], BF16) for _ in range(2)]
    PGT = [S.sb("PGT", [128, 2, 8, 128], BF16) for _ in range(2)]
    xgT = [S.sb("xgT", [128, KC, CAP], BF16) for _ in range(2)]
    for gi in range(2):
        for t in range(8):
            hl = hld[t % 2]
            p.dma(hl, hv[gi * 8 + t], writes=[("hld", t % 2)])
            if t % 2:
                p.op("pool", lambda e, t=t, hl=hl: e.tensor_copy(out=hb[:, t, :], in_=hl), reads=[("hld", t % 2)], writes=[("hb", t)])
            else:
                p.op("act", lambda e, t=t, hl=hl: e.activation(out=hb[:, t, :], in_=hl, func=AF.Copy), reads=[("hld", t % 2)], writes=[("hb", t)])
        p.dma(gts, dr["gateo"][gi * 1024:(gi + 1) * 1024, :].rearrange("(t p) e -> p t e", p=128), writes=["gts"])
        p.op("dve", lambda e: e.tensor_scalar(out=sel, in0=gts, scalar1=0.0, scalar2=0.0, op0=ALU.is_gt, op1=ALU.add), reads=["gts"], writes=["sel"])
        for t in range(8):
            fns = [lambda e, t=t: e.matmul(RK[:, 0:32], lhsT=trius, rhs=sel[:, t, :], start=True, stop=(t == 0))]
            for t2 in range(t):
                fns.append(lambda e, t2=t2, t=t: e.matmul(RK[:, 0:32], lhsT=ones128, rhs=sel[:, t2, :], start=False, stop=(t2 == t - 1)))
            p.mm(fns, reads=["sel", "trius", "ones128"], writes=["RK"])
            p.op("dve", lambda e, t=t: e.tensor_copy(out=rank[:, t, :], in_=RK[:, 0:32]), reads=["RK"], writes=["rank"])
        hbt = toks("hb", range(8))
        k4 = 0
        for e_ in range(NE):
            pb = e_ % 2
            for t in range(8):
                p.op("dve", lambda e, t=t: e.tensor_scalar(out=Pm[pb][:, t, :], in0=iota, scalar1=rank[:, t, e_:e_ + 1], scalar2=sel[:, t, e_:e_ + 1],
                                                        op0=ALU.is_equal, op1=ALU.mult), reads=["iota", "rank", "sel"], writes=[("Pm", pb)])
                p.op("dve", lambda e, t=t: e.tensor_scalar(out=PG[pb][:, t, :], in0=iota, scalar1=rank[:, t, e_:e_ + 1], scalar2=gts[:, t, e_:e_ + 1],
                                                        op0=ALU.is_equal, op1=ALU.mult), reads=["iota", "rank", "gts"], writes=[("PG", pb)])
            for st in range(2):
                p.mm([(lambda e, t=t, st=st: e.transpose(out=PT[st][:, t * 128:(t + 1) * 128], in_=PG[pb][:, t, st * 128:(st + 1) * 128], identity=identb))
                      for t in range(8)], reads=[("PG", pb), "identb"], writes=[("PT", st)])
                p.op("act", lambda e, st=st: e.activation(out=PGT[pb][:, st, :, :], in_=PT[st].rearrange("p (a c) -> p a c", a=8), func=AF.Copy),
                     reads=[("PT", st)], writes=[("PGT", pb, st)])
            p.dma(PGd[e_][:, gi * 2048:(gi + 1) * 2048], PGT[pb].rearrange("p a b c -> p (a b c)"), reads=[("PGT", pb, 0), ("PGT", pb, 1)])
            for k2 in range(8):
                gb = k4 % 4
                k4 += 1
                fns = []
                for j in range(2):
                    kc = k2 * 2 + j
                    for t in range(8):
                        fns.append(lambda e, kc=kc, t=t, j=j, gb=gb: e.matmul(GA[gb][:, j * 256:(j + 1) * 256], lhsT=hb[:, t, kc * 128:(kc + 1) * 128], rhs=Pm[pb][:, t, :],
                                                                              start=(t == 0), stop=(t == 7)))
                p.mm(fns, reads=hbt + [("Pm", pb)], writes=[("GA", gb)])
                if k2 % 2:
                    p.op("act", lambda e, k2=k2, gb=gb: e.activation(out=xgT[pb][:, k2 * 2:k2 * 2 + 2, :], in_=GA[gb].rearrange("p (a c) -> p a c", a=2), func=AF.Copy),
                         reads=[("GA", gb)], writes=[("xgT", pb, k2)])
                else:
                    p.op("pool" if False else "dve", lambda e, k2=k2, gb=gb: e.tensor_copy(out=xgT[pb][:, k2 * 2:k2 * 2 + 2, :], in_=GA[gb].rearrange("p (a c) -> p a c", a=2)),
                         reads=[("GA", gb)], writes=[("xgT", pb, k2)])
            p.dma(XG[e_].rearrange("p (k c) -> p k c", c=512)[:, :, gi * 256:(gi + 1) * 256], xgT[pb], reads=[("xgT", pb, k2) for k2 in range(8)], writes=["XG"])
    S.close()
    S = Scope(p)
    ident = load_const(cx, S, "ident", "ident")
    Gp = [S.ps("Gp", [128, 512], F32) for _ in range(2)]
    Lp = [S.ps("Lp", [128, 512], F32) for _ in range(2)]
    Yp = [S.ps("Yp", [128, 512], F32) for _ in range(2)]
    BP = S.ps("BP", [128, 512], F32)
    nrow = NE * 16
    nch = (nrow + 127) // 128
    b1g = S.sb("b1g", [128, nrow], F32)
    b1l = S.sb("b1l", [128, nrow], F32)
    b1st = S.sb("b1st", [128, 256], F32)
    b1v = dr["moe_b1"][L * NE:(L + 1) * NE, :].rearrange("e (f c) -> (e f) c", c=256)
    for ch in range(nch):
        r = min(128, nrow - ch * 128)
        p.dma(b1st[0:r, :], b1v[ch * 128:ch * 128 + r, :], writes=["b1st"])
        p.mm([lambda e, r=r: e.transpose(out=BP[:, 0:r], in_=b1st[0:r, 0:256:2], identity=ident[0:r, 0:r]),
              lambda e, r=r: e.transpose(out=BP[:, 128:128 + r], in_=b1st[0:r, 1:256:2], identity=ident[0:r, 0:r])], reads=["b1st", "ident"], writes=["BP"])
        p.op("dve", lambda e, r=r, ch=ch: e.tensor_copy(out=b1g[:, ch * 128:ch * 128 + r], in_=BP[:, 0:r]), reads=["BP"], writes=["b1g"])
        p.op("dve", lambda e, r=r, ch=ch: e.tensor_copy(out=b1l[:, ch * 128:ch * 128 + r], in_=BP[:, 128:128 + r]), reads=["BP"], writes=["b1l"])
    NB = 4
    wst = [S.sb("wst", [128, 8, 512], F32) for _ in range(NB)]
    wbf = [S.sb("wbf", [128, 8, 512], BF16) for _ in range(NB)]
    xg = [S.sb("xg", [128, KC, 512], BF16) for _ in range(2)]
    actT = S.sb("actT", [128, KC, 512], BF16)
    Yb = [S.sb("Yb", [128, 4, D], BF16) for _ in range(2)]
    b2t = [S.sb("b2t", [128, 512], F32) for _ in range(2)]
    ga = [S.sb("ga", [128, 512], F32) for _ in range(2)]
    sg = [S.sb("sg", [128, 512], F32) for _ in range(2)]
    la = [S.sb("la", [128, 512], F32) for _ in range(2)]
    st_ = {"it": 0}
    plist = []
    for e_ in range(NE):
        r0 = (L * NE + e_) * D
        for fp in range(8):
            plist += [("w1", r0, fp * 512, 0), ("w1", r0, fp * 512, 1)]
        for dq in range(4):
            plist += [("w2", r0, dq * 512, 0), ("w2", r0, dq * 512, 1)]
    fetched = {}

    def fetch(i):
        if i >= len(plist) or i in fetched:
            return
        kind, r0, c0, h = plist[i]
        b = i % NB
        w2d = dr["moe_w1"] if kind == "w1" else dr["moe_w2"]
        src = w2d[r0 + h * 1024:r0 + (h + 1) * 1024, c0:c0 + 512].rearrange("(k p) n -> p k n", p=128)
        p.dma(wst[b], src, writes=[("wst", b)])
        if kind == "w1":
            sv = wst[b].rearrange("p k (f i two) -> p k f i two", f=2, two=2)
            dv = wbf[b].rearrange("p k (f two i) -> p k f two i", f=2, two=2)
            for fl in range(2):
                p.op("act", lambda e, sv=sv, dv=dv, fl=fl: e.activation(out=dv[:, :, fl, 0, :], in_=sv[:, :, fl, :, 0], func=AF.Copy),
                     reads=[("wst", b)], writes=[("wbf", b, 0, fl)])
                p.op("pool", lambda e, sv=sv, dv=dv, fl=fl: e.tensor_copy(out=dv[:, :, fl, 1, :], in_=sv[:, :, fl, :, 1]),
                     reads=[("wst", b)], writes=[("wbf", b, 1, fl)])
        else:
            p.op("act", lambda e, b=b: e.activation(out=wbf[b][:, 0:4, :], in_=wst[b][:, 0:4, :], func=AF.Copy), reads=[("wst", b)], writes=[("wbf", b, 0, 0), ("wbf", b, 0, 1)])
            p.op("pool", lambda e, b=b: e.tensor_copy(out=wbf[b][:, 4:8, :], in_=wst[b][:, 4:8, :]), reads=[("wst", b)], writes=[("wbf", b, 1, 0), ("wbf", b, 1, 1)])
        fetched[i] = b

    def wtoks(b):
        return [("wbf", b, x, y) for x in range(2) for y in range(2)]
    for i in range(NB):
        fetch(i)
    p.dma(xg[0].rearrange("p k c -> p (k c)"), XG[0], reads=["XG"], writes=[("xg", 0)])
    pi = 0
    for e_ in range(NE):
        xb = e_ % 2
        if e_ + 1 < NE:
            p.dma(xg[(e_ + 1) % 2].rearrange("p k c -> p (k c)"), XG[e_ + 1], reads=["XG"], writes=[("xg", (e_ + 1) % 2)])
        for fp in range(8):
            bA, bB = fetched[pi], fetched[pi + 1]
            vA = wbf[bA].rearrange("p k (f two i) -> p k f two i", f=2, two=2)
            vB = wbf[bB].rearrange("p k (f two i) -> p k f two i", f=2, two=2)
            wt = wtoks(bA) + wtoks(bB)
            for fl in range(2):
                fc = fp * 2 + fl
                i2 = st_["it"] % 2
                st_["it"] += 1
                p.mm([(lambda e, c=c, fl=fl: e.matmul(Gp[i2][:, 0:512], lhsT=(vA if c < 8 else vB)[:, c % 8, fl, 0, :], rhs=xg[xb][:, c, :],
                                                      start=(c == 0), stop=(c == KC - 1))) for c in range(KC)], reads=wt + [("xg", xb)], writes=[("Gp", i2)])
                p.mm([(lambda e, c=c, fl=fl: e.matmul(Lp[i2][:, 0:512], lhsT=(vA if c < 8 else vB)[:, c % 8, fl, 1, :], rhs=xg[xb][:, c, :],
                                                      start=(c == 0), stop=(c == KC - 1))) for c in range(KC)], reads=wt + [("xg", xb)], writes=[("Lp", i2)])
                col = e_ * 16 + fc
                p.op("dve", lambda e, col=col: e.tensor_scalar(out=ga[i2], in0=Gp[i2][:, 0:512], scalar1=b1g[:, col:col + 1], scalar2=7.0, op0=ALU.add, op1=ALU.min),
                     reads=[("Gp", i2), "b1g"], writes=[("ga", i2)])
                p.op("act", lambda e: e.activation(out=sg[i2], in_=ga[i2], func=AF.Sigmoid, scale=1.702), reads=[("ga", i2)], writes=[("sg", i2)])
                p.op("dve", lambda e, col=col: e.tensor_scalar(out=la[i2], in0=Lp[i2][:, 0:512], scalar1=b1l[:, col:col + 1], scalar2=7.0, op0=ALU.add, op1=ALU.min),
                     reads=[("Lp", i2), "b1l"], writes=[("la", i2)])
                p.op("dve", lambda e: e.tensor_scalar(out=la[i2], in0=la[i2], scalar1=-7.0, scalar2=1.0, op0=ALU.max, op1=ALU.add), reads=[("la", i2)], writes=[("la", i2)])
                p.op("pool", lambda e: e.tensor_tensor(out=ga[i2], in0=ga[i2], in1=sg[i2], op=ALU.mult), reads=[("ga", i2), ("sg", i2)], writes=[("ga", i2)])
                p.op("pool", lambda e, fc=fc: e.tensor_tensor(out=actT[:, fc, :], in0=ga[i2], in1=la[i2], op=ALU.mult), reads=[("ga", i2), ("la", i2)], writes=[("actT", fc)])
            pi += 2
            fetch(pi + 2)
            fetch(pi + 3)
        at_ = toks("actT", range(16))
        yb_ = Yb[e_ % 2]
        for dq in range(4):
            bA, bB = fetched[pi], fetched[pi + 1]
            wt = wtoks(bA) + wtoks(bB)
            dsl = slice(dq * 512, (dq + 1) * 512)
            bb = dq % 2
            p.dma(b2t[bb], dr["moe_b2"][L * NE + e_:L * NE + e_ + 1, dsl].partition_broadcast(128), writes=[("b2t", bb)])
            for st in range(4):
                i2 = st_["it"] % 2
                st_["it"] += 1
                p.mm([(lambda e, c=c, st=st: e.matmul(Yp[i2][:, 0:512], lhsT=actT[:, c, st * 128:(st + 1) * 128], rhs=wbf[bA if c < 8 else bB][:, c % 8, :],
                                                      start=(c == 0), stop=(c == KC - 1))) for c in range(KC)], reads=wt + at_, writes=[("Yp", i2)])
                p.op("dve", lambda e, st=st: e.tensor_tensor(out=yb_[:, st, dsl], in0=Yp[i2][:, 0:512], in1=b2t[bb], op=ALU.add), reads=[("Yp", i2), ("b2t", bb)],
                     writes=[("Yb", e_ % 2, dq)])
            pi += 2
            fetch(pi + 2)
            fetch(pi + 3)
        p.dma(YBd[e_], yb_.rearrange("p a d -> p (a d)"), reads=[("Yb", e_ % 2, dq) for dq in range(4)], writes=["YBd"])
    S.close()
    S = Scope(p)
    SC = [S.ps("SC", [128, 512], F32) for _ in range(4)]
    acc = S.sb("acc", [128, 8, D], F32)
    gbc, gtok = bcast_row(cx, S, g_row, D, "gbc")
    bbc, btok = bcast_row(cx, S, b_row, D, "bbc")
    pg = [S.sb("pgl", [128, 2, 8, 128], BF16) for _ in range(2)]
    yl = [S.sb("yl", [128, 2, D], BF16) for _ in range(2)]
    hl = S.sb("hlc", [128, D], F32)
    sm = S.sb("sm", [128, 8], F32)
    k4 = 0
    for gi in range(2):
        def ld(e_):
            b = e_ % 2
            p.dma(pg[b].rearrange("p a b c -> p (a b c)"), PGd[e_][:, gi * 2048:(gi + 1) * 2048], writes=[("pgl", b)])
            p.dma(yl[b].rearrange("p a d -> p (a d)"), YBd[e_][:, gi * 4096:(gi + 1) * 4096], writes=[("yl", b)])
        ld(0)
        for e_ in range(NE):
            if e_ + 1 < NE:
                ld(e_ + 1)
            b = e_ % 2
            for t in range(8):
                for dq in range(4):
                    qs = slice(dq * 512, (dq + 1) * 512)
                    sb_ = k4 % 4
                    k4 += 1
                    p.mm([(lambda e, st=st, sb_=sb_: e.matmul(SC[sb_][:, 0:512], lhsT=pg[b][:, st, t, :], rhs=yl[b][:, st, qs], start=(st == 0), stop=(st == 1)))
                          for st in range(2)], reads=[("pgl", b), ("yl", b)], writes=[("SC", sb_)])
                    if e_ == 0:
                        p.op("act", lambda e, sb_=sb_: e.activation(out=acc[:, t, qs], in_=SC[sb_][:, 0:512], func=AF.Copy), reads=[("SC", sb_)], writes=[("acc", t, dq)])
                    else:
                        p.op("dve", lambda e, sb_=sb_: e.tensor_tensor(out=acc[:, t, qs], in0=SC[sb_][:, 0:512], in1=acc[:, t, qs], op=ALU.add), reads=[("SC", sb_), ("acc", t, dq)],
                             writes=[("acc", t, dq)])
        for t in range(8):
            at = [("acc", t, dq) for dq in range(4)]
            p.dma(hl, hv[gi * 8 + t], writes=["hlc"])
            p.op("dve", lambda e, t=t: e.scalar_tensor_tensor(out=acc[:, t, :], in0=hl, scalar=DN_ALPHA, in1=acc[:, t, :], op0=ALU.mult, op1=ALU.add),
                 reads=["hlc"] + at, writes=at)
            ln_tile(cx, acc[:, t, :], ("acc", t, 0), hl, "hlc", sm, "sm", gbc, gtok, bbc, btok)
            p.dma(ov[gi * 8 + t], acc[:, t, :], reads=at)
    S.close()


def moe_layer(cx, L, h_in, h_out):
    dr, p = cx.dram, cx.p
    stage_router(cx, L, h_in)
    p.allreduce(dr["GH"], dr["GH2"], reads=["GH"], writes=["GH2"])
    p.allreduce(dr["GG"], dr["GG2"], reads=["GG"], writes=["GG2"])
    stage_precast(cx, L)
    p.barrier()
    stage_experts(cx, L, dr["GH2"], dr["GG2"])
    p.allreduce(dr["PART"], dr["PART2"], reads=["PART"], writes=["PART2"])
    p.barrier()
    stage_combine_ln(cx, dr["PART2"], h_in, dr["ln2_g"][L:L + 1, :], dr["ln2_b"][L:L + 1, :], h_out)


def full_forward(cx):
    dr = cx.dram
    stage_l0_proj(cx, dr["x"])
    stage_l0_ssd(cx)
    stage_l0_att(cx)
    stage_outproj_ln(cx, dr["yT"], 32, dr["hyb_w_out"], dr["x"], dr["ln1_g"][0:1, :], dr["ln1_b"][0:1, :], dr["h1"])
    moe_layer(cx, 0, dr["h1"], dr["h2"])
    stage_gla_proj(cx, dr["h2"])
    stage_gla_core(cx)
    stage_outproj_ln(cx, dr["yT"][0:2048, :], 16, dr["gla_w_out"], dr["h2"], dr["ln1_g"][1:2, :], dr["ln1_b"][1:2, :], dr["h3"])
    moe_layer(cx, 1, dr["h3"], dr["out"])


CONST_NAMES = list(CONST_SHAPES.keys())


def _run(nc, in_maps):
    res = run_bass_kernel_spmd(nc, in_maps, core_ids=list(range(NCORES)))
    return res.results


def kernel_unfused(**inputs):
    x = np.asarray(inputs["x"], dtype=np.float32)
    f = lambda k: np.ascontiguousarray(np.asarray(inputs[k], dtype=np.float32))
    consts = make_consts()
    ln = {"ln1_g": f("ln1_g"), "ln1_b": f("ln1_b"), "ln2_g": f("ln2_g"), "ln2_b": f("ln2_b")}
    rt = {"moe_w_router": f("moe_w_router").reshape(2 * D, 32), "moe_b_router": f("moe_b_router")}
    hyb = {"hyb_w_in": f("hyb_w_in")[0], "hyb_conv_w": f("hyb_conv_w")[0], "hyb_conv_b": f("hyb_conv_b").reshape(1, 3072),
           "hyb_dt_bias": f("hyb_dt_bias").reshape(1, 32), "hyb_a_log": f("hyb_a_log").reshape(1, 32), "hyb_d": f("hyb_d").reshape(1, 32),
           "hyb_norm": f("hyb_norm").reshape(1, 2048), "hyb_w_out": f("hyb_w_out")[0]}
    gla = {"gla_w_in": f("gla_w_in")[0], "gla_w_gate2": f("gla_w_gate2")[0], "gla_b_gate": f("gla_b_gate").reshape(1, 1024),
           "gla_norm": f("gla_norm").reshape(1, 512), "gla_w_out": f("gla_w_out")[0]}
    w1, b1, w2, b2 = inputs["moe_w1"], inputs["moe_b1"], inputs["moe_w2"], inputs["moe_b2"]
    ohs = []
    for c in range(NCORES):
        oh = np.zeros((128, 8), np.float32)
        oh[:, c] = 1.0
        ohs.append(oh)

    def stA(cx):
        dr = cx.dram
        stage_l0_proj(cx, dr["x"])
        stage_l0_ssd(cx)
        stage_l0_att(cx)
        stage_outproj_ln(cx, dr["yT"], 32, dr["hyb_w_out"], dr["x"], dr["ln1_g"][0:1, :], dr["ln1_b"][0:1, :], dr["h1"])
        stage_router(cx, 0, dr["h1"], masked=False)
    need = ["x"] + list(hyb) + ["ln1_g", "ln1_b", "moe_w_router", "moe_b_router", "oh"]
    ncA, _ = build([stA], dbg=("h1", "hTo", "gateo"), needed_inputs=need)
    maps = [dict(hyb, **consts, **rt, x=np.ascontiguousarray(x[c]), ln1_g=ln["ln1_g"], ln1_b=ln["ln1_b"], oh=ohs[c]) for c in range(NCORES)]
    rA = _run(ncA, maps)
    h1 = [np.asarray(rA[c]["h1"]) for c in range(NCORES)]

    def stB(cx):
        dr = cx.dram
        stage_precast(cx, 0)
        stage_experts(cx, 0, dr["GH2"], dr["GG2"])
    needB = ["moe_w1", "moe_b1", "moe_w2", "moe_b2", "oh"]
    ncB, _ = build([stB], dbg=("PART",), needed_inputs=needB, ext_in=("GH2", "GG2"), moe_layers=1)

    def experts(L, rprev):
        GH = np.concatenate([np.asarray(rprev[c]["hTo"]) for c in range(NCORES)], axis=0)
        GG = np.concatenate([np.asarray(rprev[c]["gateo"]) for c in range(NCORES)], axis=0)
        maps = []
        for c in range(NCORES):
            e0 = 4 * c
            maps.append(dict(consts, GH2=GH, GG2=GG, oh=ohs[c],
                             moe_w1=np.ascontiguousarray(np.asarray(w1[L, e0:e0 + 4], dtype=np.float32)).reshape(4 * D, 2 * D),
                             moe_b1=np.ascontiguousarray(np.asarray(b1[L, e0:e0 + 4], dtype=np.float32)).reshape(4, 2 * D),
                             moe_w2=np.ascontiguousarray(np.asarray(w2[L, e0:e0 + 4], dtype=np.float32)).reshape(4 * D, D),
                             moe_b2=np.ascontiguousarray(np.asarray(b2[L, e0:e0 + 4], dtype=np.float32)).reshape(4, D)))
        rB = _run(ncB, maps)
        return [np.concatenate([np.asarray(rB[j]["PART"])[c * T:(c + 1) * T] for j in range(NCORES)], axis=0) for c in range(NCORES)]
    parts = experts(0, rA)

    def stC(cx):
        dr = cx.dram
        stage_combine_ln(cx, dr["PART2"], dr["h1"], dr["ln2_g"][0:1, :], dr["ln2_b"][0:1, :], dr["h2"], use_oh=False)
        stage_gla_proj(cx, dr["h2"])
        stage_gla_core(cx)
        stage_outproj_ln(cx, dr["yT"][0:2048, :], 16, dr["gla_w_out"], dr["h2"], dr["ln1_g"][1:2, :], dr["ln1_b"][1:2, :], dr["h3"])
        stage_router(cx, 1, dr["h3"], masked=False)
    need = list(gla) + ["ln1_g", "ln1_b", "ln2_g", "ln2_b", "moe_w_router", "moe_b_router", "oh"]
    ncC, _ = build([stC], dbg=("h3", "hTo", "gateo"), needed_inputs=need, ext_in=("PART2", "h1"))
    maps = [dict(gla, **consts, **rt, **ln, oh=ohs[c], PART2=parts[c], h1=h1[c]) for c in range(NCORES)]
    rC = _run(ncC, maps)
    h3 = [np.asarray(rC[c]["h3"]) for c in range(NCORES)]
    parts = experts(1, rC)

    def stE(cx):
        dr = cx.dram
        stage_combine_ln(cx, dr["PART2"], dr["h3"], dr["ln2_g"][1:2, :], dr["ln2_b"][1:2, :], dr["out"], use_oh=False)
    ncE, _ = build([stE], dbg=("out",), needed_inputs=["ln2_g", "ln2_b", "oh"], ext_in=("PART2", "h3"))
    maps = [dict(consts, oh=ohs[c], ln2_g=ln["ln2_g"], ln2_b=ln["ln2_b"], PART2=parts[c], h3=h3[c]) for c in range(NCORES)]
    rE = _run(ncE, maps)
    return np.stack([np.asarray(rE[c]["out"], dtype=np.float32) for c in range(NCORES)], axis=0)


def fused_forward(cx, NE=32, moe=None):
    moe = moe or stage_moe_sparse2
    dr = cx.dram
    stage_l0_proj(cx, dr["x"])
    stage_l0_ssd(cx)
    stage_l0_att(cx)
    stage_outproj_ln(cx, dr["yT"], 32, dr["hyb_w_out"], dr["x"], dr["ln1_g"][0:1, :], dr["ln1_b"][0:1, :], dr["h1"])
    stage_router(cx, 0, dr["h1"], masked=False)
    moe(cx, 0, dr["h1"], dr["ln2_g"][0:1, :], dr["ln2_b"][0:1, :], dr["h2"], NE=NE)
    stage_gla_proj(cx, dr["h2"])
    stage_gla_core(cx)
    stage_outproj_ln(cx, dr["yT"][0:2048, :], 16, dr["gla_w_out"], dr["h2"], dr["ln1_g"][1:2, :], dr["ln1_b"][1:2, :], dr["h3"])
    stage_router(cx, 1, dr["h3"], masked=False)
    moe(cx, 1, dr["h3"], dr["ln2_g"][1:2, :], dr["ln2_b"][1:2, :], dr["out"], NE=NE)


def kernel(**inputs):
    x = np.asarray(inputs["x"], dtype=np.float32)
    f = lambda k: np.ascontiguousarray(np.asarray(inputs[k], dtype=np.float32))
    shared = {
        "hyb_w_in": f("hyb_w_in")[0], "hyb_conv_w": f("hyb_conv_w")[0], "hyb_conv_b": f("hyb_conv_b").reshape(1, 3072),
        "hyb_dt_bias": f("hyb_dt_bias").reshape(1, 32), "hyb_a_log": f("hyb_a_log").reshape(1, 32), "hyb_d": f("hyb_d").reshape(1, 32),
        "hyb_norm": f("hyb_norm").reshape(1, 2048), "hyb_w_out": f("hyb_w_out")[0],
        "gla_w_in": f("gla_w_in")[0], "gla_w_gate2": f("gla_w_gate2")[0], "gla_b_gate": f("gla_b_gate").reshape(1, 1024),
        "gla_norm": f("gla_norm").reshape(1, 512), "gla_w_out": f("gla_w_out")[0],
        "ln1_g": f("ln1_g"), "ln1_b": f("ln1_b"), "ln2_g": f("ln2_g"), "ln2_b": f("ln2_b"),
        "moe_w_router": f("moe_w_router").reshape(2 * D, 32), "moe_b_router": f("moe_b_router"),
        "moe_w1": f("moe_w1").reshape(2 * 32 * D, 2 * D), "moe_b1": f("moe_b1").reshape(2 * 32, 2 * D),
        "moe_w2": f("moe_w2").reshape(2 * 32 * D, D), "moe_b2": f("moe_b2").reshape(2 * 32, D),
        "oh": np.zeros((128, 8), np.float32),
    }
    shared.update(make_consts())
    in_maps = [dict(shared, x=np.ascontiguousarray(x[c])) for c in range(NCORES)]
    nc, cx = build([fused_forward], dbg=("out",), moe_experts=32)
    res = run_bass_kernel_spmd(nc, in_maps, core_ids=list(range(NCORES)))
    return np.stack([np.asarray(res.results[c]["out"], dtype=np.float32) for c in range(NCORES)], axis=0)
```

```python
import contextlib
import math
import numpy as np
import ml_dtypes
import concourse.bass as bass
import concourse.mybir as mybir
from concourse.bass_utils import run_bass_kernel_spmd

F32 = mybir.dt.float32
BF16 = mybir.dt.bfloat16
AF = mybir.ActivationFunctionType
ALU = mybir.AluOpType
AX = mybir.AxisListType

NCORES = 8
T = 2048
D = 2048
NT = T // 128
KC = D // 128
NEG = -1.0e30
DN_ALPHA = 4 ** 0.25
EPS = 1e-5
HYB_IN = 11296
GLA_IN = 6160


class Prog:
    RING = 8

    def __init__(self, nc):
        self.nc = nc
        self.E = {"pe": nc.tensor, "act": nc.scalar, "dve": nc.vector,
                  "pool": nc.gpsimd, "sp": nc.sync}
        self.sem = {}
        self.cnt = {}
        for k in self.E:
            self.sem[k] = nc.alloc_semaphore("s_" + k)
            self.cnt[k] = 0
        self.ring = {}
        self.ring_cnt = {}
        self.ring_next = {}
        for q in ("sp", "pool", "act"):
            self.ring[q] = [nc.alloc_semaphore(f"d_{q}{i}") for i in range(self.RING)]
            self.ring_cnt[q] = [0] * self.RING
            self.ring_next[q] = 0
        self.cc_sem = nc.alloc_semaphore("cc_sem")
        self.cc_cnt = 0
        self.seen = {k: {} for k in self.E}
        self.tok = {}
        self.nwaits = 0
        self.nops = 0

    def _semh(self, key):
        if key[0] == "e":
            return self.sem[key[1]]
        if key[0] == "c":
            return self.cc_sem
        return self.ring[key[1]][key[2]]

    def _wait(self, eng, ev):
        key, val = ev
        if key == ("e", "pe") and eng == "pe":
            return
        if self.seen[eng].get(key, 0) >= val:
            return
        self.E[eng].wait_ge(self._semh(key), val)
        self.seen[eng][key] = val
        self.nwaits += 1

    def _deps(self, reads, writes):
        deps = {}

        def add(k, v):
            if deps.get(k, 0) < v:
                deps[k] = v
        for t in reads:
            st = self.tok.get(t)
            if st and st["w"]:
                add(*st["w"])
        for t in writes:
            st = self.tok.get(t)
            if st:
                if st["w"]:
                    add(*st["w"])
                for k, v in st["r"].items():
                    add(k, v)
        return deps

    def _commit(self, ev, reads, writes):
        k, v = ev
        for t in reads:
            st = self.tok.setdefault(t, {"w": None, "r": {}})
            if st["r"].get(k, 0) < v:
                st["r"][k] = v
        for t in writes:
            self.tok[t] = {"w": ev, "r": {}}

    def op(self, eng, fn, reads=(), writes=()):
        for k, v in self._deps(reads, writes).items():
            self._wait(eng, (k, v))
        ins = fn(self.E[eng])
        self.cnt[eng] += 1
        ins.then_inc(self.sem[eng], 1)
        self._commit((("e", eng), self.cnt[eng]), reads, writes)
        self.nops += 1
        return ins

    def mm(self, fns, reads=(), writes=()):
        for k, v in self._deps(reads, writes).items():
            self._wait("pe", (k, v))
        ins = None
        for fn in fns:
            ins = fn(self.E["pe"])
        self.cnt["pe"] += 1
        ins.then_inc(self.sem["pe"], 1)
        self._commit((("e", "pe"), self.cnt["pe"]), reads, writes)
        self.nops += len(fns)

    def dma(self, out, in_, reads=(), writes=(), q="sp", **kw):
        i = self.ring_next[q]
        self.ring_next[q] = (i + 1) % self.RING
        key = ("d", q, i)
        if self.ring_cnt[q][i] > 0:
            self._wait(q, (key, 16 * self.ring_cnt[q][i]))
        for k, v in self._deps(reads, writes).items():
            self._wait(q, (k, v))
        ins = self.E[q].dma_start(out=out, in_=in_, **kw)
        self.ring_cnt[q][i] += 1
        ins.then_inc(self.ring[q][i], 16)
        self._commit((key, 16 * self.ring_cnt[q][i]), reads, writes)
        self.nops += 1
        return ins

    def allreduce(self, in_ap, out_ap, reads=(), writes=()):
        for k, v in self._deps(reads, writes).items():
            self._wait("pool", (k, v))
        ins = self.nc.gpsimd.collective_compute(
            "AllReduce", ALU.add, replica_groups=[list(range(NCORES))],
            ins=[in_ap.opt()], outs=[out_ap.opt()])
        self.cc_cnt += 1
        ins.then_inc(self.cc_sem)
        self._commit((("c",), self.cc_cnt), reads, writes)

    def barrier(self):
        evs = []
        for k in self.E:
            if self.cnt[k]:
                evs.append((("e", k), self.cnt[k]))
        for q in self.ring:
            for i in range(self.RING):
                if self.ring_cnt[q][i]:
                    evs.append((("d", q, i), 16 * self.ring_cnt[q][i]))
        if self.cc_cnt:
            evs.append((("c",), self.cc_cnt))
        for eng in self.E:
            for ev in evs:
                if ev[0] == ("e", eng):
                    continue
                self._wait(eng, ev)
        self.tok = {}


_UID = [0]


class Scope:
    def _name(self, name):
        _UID[0] += 1
        return f"{name}_{_UID[0]}"

    def __init__(self, p):
        self.p = p
        self.nc = p.nc
        self.es = contextlib.ExitStack()
        self.n = 0

    def sb(self, name, shape, dt=F32):
        self.n += 1
        h = self.es.enter_context(self.nc.sbuf_tensor(self._name(name), list(shape), dt))
        return h.ap()

    def ps(self, name, shape, dt=F32):
        self.n += 1
        h = self.es.enter_context(self.nc.psum_tensor(self._name(name), list(shape), dt))
        return h.ap()

    def close(self):
        self.p.barrier()
        self.es.close()


def make_consts():
    c = {}
    c["ident"] = np.eye(128, dtype=np.float32)
    c["identb"] = np.eye(128).astype(ml_dtypes.bfloat16)
    r = np.arange(128)
    c["triu"] = (r[:, None] <= r[None, :]).astype(np.float32)
    c["maskneg"] = np.where(r[None, :] < r[:, None], NEG, 0.0).astype(np.float32)
    sel = np.zeros((32, 32, 128), np.float32)
    for h in range(32):
        sel[h, h, :] = 1.0
    c["sel"] = sel.reshape(32, 32 * 128)
    rt = np.zeros((128, 128), np.float32)
    for m in range(64):
        rt[m + 64, m] = -1.0
    for m in range(64, 128):
        rt[m - 64, m] = 1.0
    c["rt"] = rt
    inv = np.exp(-math.log(10000.0) * np.arange(64, dtype=np.float32) / 64).astype(np.float32)
    ang = np.arange(T, dtype=np.float32)[None, :] * inv[:, None]
    c["cosT"] = np.concatenate([np.cos(ang), np.cos(ang)], 0).astype(np.float32)
    c["sinT"] = np.concatenate([np.sin(ang), np.sin(ang)], 0).astype(np.float32)
    c["causal"] = np.where(r[None, :] <= r[:, None], 0.0, NEG).astype(np.float32)
    same = (r[:, None] // 64) == (r[None, :] // 64)
    c["btri"] = (same & (r[:, None] <= r[None, :])).astype(np.float32)
    c["bgt"] = (same & (r[:, None] > r[None, :])).astype(np.float32)
    c["ones"] = np.ones((1, 128), np.float32)
    ls = np.zeros((128, 128), np.float32)
    ls[127, :] = 1.0
    c["lastsel"] = ls
    c["trius"] = (r[:, None] < r[None, :]).astype(np.float32)
    c["ones128"] = np.ones((128, 128), np.float32)
    c["iota"] = np.tile(np.arange(256, dtype=np.float32)[None, :], (128, 1))
    return c


CONST_SHAPES = {"ident": ([128, 128], F32), "identb": ([128, 128], BF16), "triu": ([128, 128], F32),
                "maskneg": ([128, 128], F32), "sel": ([32, 4096], F32), "rt": ([128, 128], F32),
                "cosT": ([128, T], F32), "sinT": ([128, T], F32), "causal": ([128, 128], F32),
                "btri": ([128, 128], F32), "bgt": ([128, 128], F32), "ones": ([1, 128], F32), "lastsel": ([128, 128], F32), "trius": ([128, 128], F32),
                "ones128": ([128, 128], F32), "iota": ([128, 256], F32)}


class Ctx:
    pass


def load_const(cx, S, name, tokname=None):
    shape, dt = CONST_SHAPES[name]
    t = S.sb("c_" + name, shape, dt)
    cx.p.dma(t, cx.dram[name], writes=[tokname or ("c", name, id(S))])
    return t


def bcast_row(cx, S, row_ap, n, name):
    t = S.sb(name, [128, n], F32)
    cx.p.dma(t, row_ap.partition_broadcast(128), writes=[("bc", name, id(S))])
    return t, ("bc", name, id(S))


def toks(base, rng):
    return [(base, i) for i in rng]


def transpose_in(cx, S, src_tm, dstT, dst_tok, ident, ident_tok, nt=NT, t0=0):
    p = cx.p
    ld = [S.sb("tin_ld", [128, D], F32) for _ in range(2)]
    ps = [S.ps("tin_ps", [128, 512], F32) for _ in range(2)]
    src = src_tm.rearrange("(t p) d -> t p d", p=128)
    k = 0
    for t in range(nt):
        b = ld[t % 2]
        p.dma(b, src[t0 + t], writes=[("tin_ld", t % 2)])
        for g in range(4):
            pp = ps[k % 2]
            ptok = ("tin_ps", k % 2)
            p.mm([(lambda e, pp=pp, b=b, g=g, j=j: e.transpose(out=pp[:, j * 128:(j + 1) * 128],
                                                              in_=b[:, (g * 4 + j) * 128:(g * 4 + j + 1) * 128],
                                                              identity=ident)) for j in range(4)],
                 reads=[("tin_ld", t % 2), ident_tok], writes=[ptok])
            out = dstT[:, g * 4:(g + 1) * 4, t * 128:(t + 1) * 128]
            src_ps = pp.rearrange("p (a b) -> p a b", a=4)
            if k % 2 == 0:
                p.op("dve", lambda e, out=out, s=src_ps: e.tensor_copy(out=out, in_=s), reads=[ptok], writes=[(dst_tok, t, g)])
            else:
                p.op("act", lambda e, out=out, s=src_ps: e.activation(out=out, in_=s, func=AF.Copy), reads=[ptok], writes=[(dst_tok, t, g)])
            k += 1


def xtoks(xtok, tiles):
    return [(xtok, t, g) for t in tiles for g in range(4)]


def load_cols(cx, S, vec_ap2d, r, name, ident, ident_tok, ps, pstok, ncol=128):
    p = cx.p
    st = S.sb(name + "_st", [r, ncol], F32)
    out = S.sb(name, [128, r], F32)
    p.dma(st, vec_ap2d, writes=[(name, "st")])
    p.mm([lambda e: e.transpose(out=ps[0:ncol, 0:r], in_=st, identity=ident[0:r, 0:r])],
         reads=[(name, "st"), ident_tok], writes=[pstok])
    p.op("dve", lambda e: e.tensor_copy(out=out[0:ncol, :], in_=ps[0:ncol, 0:r]), reads=[pstok], writes=[(name,)])
    return out, (name,)


class WStream:
    def __init__(self, cx, S, kc, pw, name):
        self.cx, self.kc, self.pw, self.name = cx, kc, pw, name
        self.st = [S.sb(name + "_st", [128, kc, pw], F32) for _ in range(2)]
        self.bf = [S.sb(name + "_bf", [128, kc, pw], BF16) for _ in range(2)]
        self.i = 0

    def fetch(self, w2d, c0, w):
        p = self.cx.p
        b = self.i % 2
        self.i += 1
        st, bf = self.st[b], self.bf[b]
        stok, btok = (self.name, "st", b), (self.name, "bf", b)
        src = w2d.rearrange("(k p) n -> p k n", p=128)[:, :, c0:c0 + w]
        p.dma(st[:, :, 0:w], src, writes=[stok])
        p.op("pool", lambda e: e.tensor_copy(out=bf[:, :, 0:w], in_=st[:, :, 0:w]), reads=[stok], writes=[btok])
        return bf, btok


def gemm_panels(cx, ws, w2d, panels, xT, xtok, mode, ps_list, epilogue, kc=KC, nt=NT):
    p = cx.p
    nxt = ws.fetch(w2d, panels[0][0], panels[0][1])
    k = 0
    for pi, (c0, w, tag) in enumerate(panels):
        bf, btok = nxt
        if pi + 1 < len(panels):
            nxt = ws.fetch(w2d, panels[pi + 1][0], panels[pi + 1][1])
        if mode == "tm":
            for t in range(nt):
                ps, ptok = ps_list[k % len(ps_list)]
                k += 1
                p.mm([(lambda e, c=c, ps=ps, t=t: e.matmul(ps[:, 0:w], lhsT=xT[:, c, t * 128:(t + 1) * 128], rhs=bf[:, c, 0:w],
                                                         start=(c == 0), stop=(c == kc - 1))) for c in range(kc)],
                     reads=[btok] + xtoks(xtok, [t]), writes=[ptok])
                epilogue(tag, c0, w, t, ps, ptok)
        else:
            nj = max(1, w // 128)
            m = 128
            for j in range(nj):
                for tb in range(nt // 4):
                    ps, ptok = ps_list[k % len(ps_list)]
                    k += 1
                    p.mm([(lambda e, c=c, ps=ps, tb=tb, j=j: e.matmul(ps[0:m, 0:512], lhsT=bf[:, c, j * 128:j * 128 + m],
                                                                     rhs=xT[:, c, tb * 512:(tb + 1) * 512],
                                                                     start=(c == 0), stop=(c == kc - 1))) for c in range(kc)],
                         reads=[btok] + xtoks(xtok, range(tb * 4, tb * 4 + 4)), writes=[ptok])
                    epilogue(tag, c0 + j * 128, m, tb, ps, ptok)


def stage_l0_proj(cx, h_tm):
    p, nc, dr = cx.p, cx.nc, cx.dram
    S = Scope(p)
    ident = load_const(cx, S, "ident", "ident")
    rt = load_const(cx, S, "rt", "rt")
    cosT = load_const(cx, S, "cosT", "cosT")
    sinT = load_const(cx, S, "sinT", "sinT")
    xT = S.sb("xT", [128, KC, T], BF16)
    S2 = Scope(p)
    transpose_in(cx, S2, h_tm, xT, "xT", ident, "ident")
    S2.close()
    w_in = dr["hyb_w_in"]
    ws = WStream(cx, S, KC, 256, "win")
    gps = [(S.ps("gps", [128, 512], F32), ("gps", i)) for i in range(3)]
    tps = [(S.ps("tps", [128, 512], F32), ("tps", i)) for i in range(2)]
    rps, rtok = S.ps("rps", [128, 512], F32), "rps"
    mps, mtok = S.ps("mps", [128, 512], F32), "mps"
    cwst = S.sb("cwst", [4, 3072], F32)
    p.dma(cwst, dr["hyb_conv_w"], writes=["cwst"])
    cw = S.sb("cw", [128, 24, 4], F32)
    p.mm([(lambda e, c=c: e.transpose(out=mps[:, c * 4:(c + 1) * 4], in_=cwst[0:4, c * 128:(c + 1) * 128], identity=ident[0:4, 0:4]))
          for c in range(24)], reads=["cwst", "ident"], writes=[mtok])
    p.op("dve", lambda e: e.tensor_copy(out=cw.rearrange("p a b -> p (a b)"), in_=mps[:, 0:96]), reads=[mtok], writes=["cw"])
    cb, cbtok = load_cols(cx, S, dr["hyb_conv_b"].rearrange("o (c p) -> (o c) p", p=128), 24, "cb", ident, "ident", mps, mtok)
    dtb, dtbtok = bcast_row(cx, S, dr["hyb_dt_bias"], 32, "dtb")
    abc, abctok = bcast_row(cx, S, dr["hyb_a_log"], 32, "abc")
    p.op("act", lambda e: e.activation(out=abc, in_=abc, func=AF.Exp), reads=[abctok], writes=[abctok])
    p.op("dve", lambda e: e.tensor_scalar(out=abc, in0=abc, scalar1=-1.0, scalar2=None, op0=ALU.mult), reads=[abctok], writes=[abctok])
    cin = S.sb("cin", [128, 3 + T], F32)
    p.op("pool", lambda e: e.memset(cin[:, 0:3], 0.0), writes=["cin_pad"])
    wa = S.sb("wa", [128, T], F32)
    wb = S.sb("wb", [128, T], F32)
    wc = S.sb("wc", [128, T], F32)
    obf = [S.sb("obf", [128, T], BF16) for _ in range(2)]
    tmst = S.sb("tmst", [128, NT, 128], F32)
    tmstb = S.sb("tmstb", [128, NT, 128], BF16)
    zst = [S.sb("zst", [128, 256], F32) for _ in range(2)]
    vst = [S.sb("vst", [128, 256], BF16) for _ in range(2)]
    dts = S.sb("dts", [128, NT, 32], F32)
    adts = S.sb("adts", [128, NT, 32], F32)
    sp1 = S.sb("sp1", [128, 32], F32)
    sp2 = S.sb("sp2", [128, 32], F32)
    kmean = S.sb("kmean", [128, 16, 8], F32)
    gst = S.sb("gst", [128, NT, 8], F32)
    cnt = {"z": 0, "v": 0, "o": 0, "t": 0}

    def transposes_to(dst3, dtok, src, stok):
        for g in range(4):
            ps, ptok = tps[cnt["t"] % 2]
            cnt["t"] += 1
            p.mm([(lambda e, j=j, ps=ps, g=g: e.transpose(out=ps[:, j * 128:(j + 1) * 128], in_=src[:, (g * 4 + j) * 128:(g * 4 + j + 1) * 128],
                                                        identity=ident)) for j in range(4)], reads=[stok, "ident"], writes=[ptok])
            p.op("act", lambda e, ps=ps, g=g: e.activation(out=dst3[:, g * 4:(g + 1) * 4, :], in_=ps.rearrange("p (a b) -> p a b", a=4), func=AF.Copy),
                 reads=[ptok], writes=[dtok])

    def ep(tag, c0, w, idx, ps, ptok):
        if tag == "z":
            b = cnt["z"] % 2
            cnt["z"] += 1
            p.op("act", lambda e: e.activation(out=zst[b][:, 0:w], in_=ps[:, 0:w], func=AF.Silu), reads=[ptok], writes=[("zst", b)])
            p.dma(dr["sz"][idx * 128:(idx + 1) * 128, c0:c0 + w], zst[b][:, 0:w], reads=[("zst", b)])
        elif tag == "v":
            b = cnt["v"] % 2
            cnt["v"] += 1
            p.op("dve", lambda e: e.tensor_copy(out=vst[b][:, 0:w], in_=ps[:, 0:w]), reads=[ptok], writes=[("vst", b)])
            p.dma(dr["v_tm"][idx * 128:(idx + 1) * 128, c0 - 9248:c0 - 9248 + w], vst[b][:, 0:w], reads=[("vst", b)])
        elif tag == "dt":
            t = idx
            p.op("dve", lambda e: e.tensor_tensor(out=sp1, in0=ps[:, 0:32], in1=dtb, op=ALU.add), reads=[ptok, dtbtok], writes=["sp1"])
            p.op("act", lambda e: e.activation(out=sp2, in_=sp1, func=AF.Abs), reads=["sp1"], writes=["sp2"])
            p.op("act", lambda e: e.activation(out=sp2, in_=sp2, func=AF.Exp, scale=-1.0), reads=["sp2"], writes=["sp2"])
            p.op("act", lambda e: e.activation(out=sp2, in_=sp2, func=AF.Ln, bias=1.0), reads=["sp2"], writes=["sp2"])
            p.op("dve", lambda e: e.scalar_tensor_tensor(out=dts[:, t, :], in0=sp1, scalar=0.0, in1=sp2, op0=ALU.max, op1=ALU.add),
                 reads=["sp1", "sp2"], writes=["dts"])
            p.op("dve", lambda e: e.tensor_tensor(out=adts[:, t, :], in0=dts[:, t, :], in1=abc, op=ALU.mult), reads=["dts", abctok], writes=["adts"])
            if t == NT - 1:
                p.dma(dr["dt_tm"].rearrange("(t p) h -> p t h", p=128), dts, reads=["dts"])
                p.dma(dr["adt_tm"].rearrange("(t p) h -> p t h", p=128), adts, reads=["adts"])
        elif tag == "xbc":
            tb = idx
            ci = (c0 - 2048) // 128
            p.op("act", lambda e: e.activation(out=cin[:, 3 + tb * 512:3 + (tb + 1) * 512], in_=ps[:, 0:512], func=AF.Copy),
                 reads=[ptok], writes=[("cin", tb)])
            if tb < 3:
                return
            ctoks = toks("cin", range(4)) + ["cin_pad"]
            p.op("dve", lambda e: e.tensor_scalar(out=wa, in0=cin[:, 0:T], scalar1=cw[:, ci, 0:1], scalar2=None, op0=ALU.mult),
                 reads=ctoks + ["cw"], writes=["wa"])
            for j in range(1, 4):
                p.op("dve", lambda e, j=j: e.scalar_tensor_tensor(out=wa, in0=cin[:, j:j + T], scalar=cw[:, ci, j:j + 1], in1=wa,
                                                                 op0=ALU.mult, op1=ALU.add), reads=ctoks + ["cw", "wa"], writes=["wa"])
            p.op("act", lambda e: e.activation(out=wb, in_=wa, func=AF.Silu, bias=cb[:, ci:ci + 1], scale=1.0), reads=["wa", cbtok], writes=["wb"])
            if ci < 16:
                transposes_to(tmst, "tmst", wb, "wb")
                p.dma(dr["xs_tm"].rearrange("(t p) c -> p t c", p=128)[:, :, ci * 128:(ci + 1) * 128], tmst, reads=["tmst"])
            else:
                b = cnt["o"] % 2
                cnt["o"] += 1
                p.op("pool", lambda e: e.tensor_copy(out=obf[b], in_=wb), reads=["wb"], writes=[("obf", b)])
                g = (ci - 16) % 4
                dst = dr["B_fm"] if ci < 20 else dr["C_fm"]
                p.dma(dst[g * 128:(g + 1) * 128, :], obf[b], reads=[("obf", b)])
                if ci < 20:
                    transposes_to(tmstb, "tmstb", wb, "wb")
                    p.dma(dr["B_tm"].rearrange("(t p) n -> p t n", p=128)[:, :, g * 128:(g + 1) * 128], tmstb, reads=["tmstb"])
        elif tag in ("q", "k"):
            tb = idx
            base = 5152 if tag == "q" else 7200
            h = (c0 - base) // 128
            p.op("act", lambda e: e.activation(out=wa[:, tb * 512:(tb + 1) * 512], in_=ps[:, 0:512], func=AF.Copy), reads=[ptok], writes=[("waq", tb)])
            if tb < 3:
                return
            for b4 in range(4):
                sl = slice(b4 * 512, (b4 + 1) * 512)
                p.mm([lambda e, sl=sl: e.matmul(rps[:, 0:512], lhsT=rt, rhs=wa[:, sl], start=True, stop=True)], reads=[("waq", b4), "rt"], writes=[rtok])
                p.op("dve", lambda e, sl=sl: e.tensor_tensor(out=wb[:, sl], in0=rps[:, 0:512], in1=sinT[:, sl], op=ALU.mult), reads=[rtok, "sinT"], writes=[("wbq", b4)])
                p.op("pool", lambda e, sl=sl: e.tensor_tensor(out=wc[:, sl], in0=wa[:, sl], in1=cosT[:, sl], op=ALU.mult), reads=[("waq", b4), "cosT"], writes=[("wcq", b4)])
                p.op("pool", lambda e, sl=sl: e.tensor_tensor(out=wc[:, sl], in0=wc[:, sl], in1=wb[:, sl], op=ALU.add), reads=[("wcq", b4), ("wbq", b4)], writes=[("wcq", b4)])
            b = cnt["o"] % 2
            cnt["o"] += 1
            wctoks = toks("wcq", range(4))
            if tag == "k":
                p.op("act", lambda e: e.activation(out=obf[b], in_=wc, func=AF.Copy), reads=wctoks, writes=[("obf", b)])
                p.dma(dr["k_fm"][h * 128:(h + 1) * 128, :], obf[b], reads=[("obf", b)])
                p.op("dve", lambda e: e.tensor_reduce(out=kmean[:, h, :], in_=wc.rearrange("p (a b) -> p a b", a=8), axis=AX.X, op=ALU.add),
                     reads=wctoks, writes=[("kmean", h)])
                p.op("dve", lambda e: e.tensor_scalar(out=kmean[:, h, :], in0=kmean[:, h, :], scalar1=1.0 / 256.0, scalar2=None, op0=ALU.mult),
                     reads=[("kmean", h)], writes=[("kmean", h)])
            else:
                p.op("act", lambda e: e.activation(out=obf[b], in_=wc, func=AF.Copy, scale=128.0 ** -0.5), reads=wctoks, writes=[("obf", b)])
                p.dma(dr["q_fm"][h * 128:(h + 1) * 128, :], obf[b], reads=[("obf", b)])
                p.mm([(lambda e, t=t: e.matmul(mps[:, t * 8:(t + 1) * 8], lhsT=wc[:, t * 128:(t + 1) * 128], rhs=kmean[:, h, :], start=True, stop=True))
                      for t in range(NT)], reads=wctoks + [("kmean", h)], writes=[mtok])
                p.op("dve", lambda e: e.tensor_copy(out=gst.rearrange("p a b -> p (a b)"), in_=mps[:, 0:128]), reads=[mtok], writes=["gst"])
                p.dma(dr["gate_d"][h].rearrange("(t p) e -> p t e", p=128), gst, reads=["gst"])

    panels = [(2048 + i * 256, 256, "xbc") for i in range(12)]
    gemm_panels(cx, ws, w_in, panels, xT, "xT", "fm", gps, ep)
    p.barrier()
    gemm_panels(cx, ws, w_in, [(5120, 32, "dt")], xT, "xT", "tm", gps, ep)
    p.barrier()
    panels = [(7200 + i * 256, 256, "k") for i in range(8)] + [(5152 + i * 256, 256, "q") for i in range(8)]
    gemm_panels(cx, ws, w_in, panels, xT, "xT", "fm", gps, ep)
    p.barrier()
    panels = [(i * 256, 256, "z") for i in range(8)] + [(9248 + i * 256, 256, "v") for i in range(8)]
    gemm_panels(cx, ws, w_in, panels, xT, "xT", "tm", gps, ep)
    S.close()


INPUT_SHAPES = {
    "x": ([T, D], F32),
    "hyb_w_in": ([D, HYB_IN], F32), "hyb_conv_w": ([4, 3072], F32), "hyb_conv_b": ([1, 3072], F32),
    "hyb_dt_bias": ([1, 32], F32), "hyb_a_log": ([1, 32], F32), "hyb_d": ([1, 32], F32),
    "hyb_norm": ([1, 2048], F32), "hyb_w_out": ([4096, D], F32),
    "gla_w_in": ([D, GLA_IN], F32), "gla_w_gate2": ([16, 1024], F32), "gla_b_gate": ([1, 1024], F32),
    "gla_norm": ([1, 512], F32), "gla_w_out": ([D, D], F32),
    "ln1_g": ([2, D], F32), "ln1_b": ([2, D], F32), "ln2_g": ([2, D], F32), "ln2_b": ([2, D], F32),
    "moe_w_router": ([2 * D, 32], F32), "moe_b_router": ([2, 32], F32),
    "moe_w1": ([2 * 4 * D, 2 * D], F32), "moe_b1": ([2 * 4, 2 * D], F32),
    "moe_w2": ([2 * 4 * D, D], F32), "moe_b2": ([2 * 4, D], F32),
    "oh": ([128, 8], F32),
}

SCRATCH = {
    "sz": ([T, 2048], F32), "v_tm": ([T, 2048], BF16), "dt_tm": ([T, 32], F32), "adt_tm": ([T, 32], F32),
    "xs_tm": ([T, 2048], F32), "B_fm": ([512, T], BF16), "C_fm": ([512, T], BF16), "B_tm": ([T, 512], BF16),
    "q_fm": ([2048, T], BF16), "k_fm": ([2048, T], BF16), "gate_d": ([16, T, 8], F32),
    "yT": ([4096, T], BF16), "h1": ([T, D], F32), "h2": ([T, D], F32), "h3": ([T, D], F32),
    "GH": ([8 * D, T], BF16), "GH2": ([8 * D, T], BF16), "GG": ([8 * T, 32], F32), "GG2": ([8 * T, 32], F32),
    "w1b": ([4 * 16 * 128, 4096], BF16), "w2b": ([4 * 4 * 128, 8192], BF16),
    "PART": ([8 * T, D], F32), "PART2": ([8 * T, D], F32),
    "gq_fm": ([1024, T], F32), "gk_fm": ([1024, T], F32), "gk_tm": ([T, 1024], F32), "gv_tm": ([T, 2048], BF16),
    "gsg": ([T, 2048], F32), "gla_d": ([T, 1024], F32), "out": ([T, D], F32),
    "hTo": ([D, T], BF16), "gateo": ([T, 32], F32),
    "XG": ([32 * 128, 16 * 512], BF16), "PGTd": ([32 * 128, 4096], BF16), "YBd": ([32 * 128, 4 * 2048], BF16),
}


def build(stages, dbg=(), needed_inputs=None, ext_in=(), moe_layers=2, moe_experts=4):
    nc = bass.Bass("TRN2", target_bir_lowering=False)
    cx = Ctx()
    cx.nc = nc
    cx.dram = {}
    for k, (shape, dt) in INPUT_SHAPES.items():
        if needed_inputs is not None and k not in needed_inputs:
            continue
        if k in ("moe_w1", "moe_b1", "moe_w2", "moe_b2"):
            shape = [shape[0] // 4 * moe_experts, shape[1]]
            if moe_layers == 1:
                shape = [shape[0] // 2, shape[1]]
        cx.dram[k] = nc.dram_tensor(k, shape, dt, kind="ExternalInput").ap()
    for k, (shape, dt) in CONST_SHAPES.items():
        cx.dram[k] = nc.dram_tensor(k, shape, dt, kind="ExternalInput").ap()
    for k, (shape, dt) in SCRATCH.items():
        if k in ext_in:
            cx.dram[k] = nc.dram_tensor(k, shape, dt, kind="ExternalInput").ap()
        elif k in dbg:
            cx.dram[k] = nc.dram_tensor(k, shape, dt, kind="ExternalOutput").ap()
        else:
            cx.dram[k] = nc.dram_tensor(k, shape, dt).ap()
    cx.p = Prog(nc)
    for st in stages:
        st(cx)
    cx.p.barrier()
    return nc, cx


def stage_l0_ssd(cx):
    LV = 9
    p, nc, dr = cx.p, cx.nc, cx.dram
    S = Scope(p)
    ident = load_const(cx, S, "ident", "ident")
    identb = load_const(cx, S, "identb", "identb")
    triu = load_const(cx, S, "triu", "triu")
    maskneg = load_const(cx, S, "maskneg", "maskneg")
    sel = load_const(cx, S, "sel", "sel")
    lastsel = load_const(cx, S, "lastsel", "lastsel")
    if LV == -1:
        S.close()
        return
    Bf = S.sb("Bf", [128, 4, T], BF16)
    Cf = S.sb("Cf", [128, 4, T], BF16)
    Bt = S.sb("Bt", [128, NT, 512], BF16)
    dts = S.sb("dts", [128, NT, 32], F32)
    adts = S.sb("adts", [128, NT, 32], F32)
    p.dma(Bf, dr["B_fm"].rearrange("(g p) t -> p g t", p=128), writes=["Bf"])
    p.dma(Cf, dr["C_fm"].rearrange("(g p) t -> p g t", p=128), writes=["Cf"])
    p.dma(Bt, dr["B_tm"].rearrange("(t p) n -> p t n", p=128), writes=["Bt"])
    p.dma(dts, dr["dt_tm"].rearrange("(t p) h -> p t h", p=128), writes=["dts"])
    p.dma(adts, dr["adt_tm"].rearrange("(t p) h -> p t h", p=128), writes=["adts"])
    if LV == -2:
        S.close()
        return
    dbc, dbctok = bcast_row(cx, S, dr["hyb_d"], 32, "dbc")
    nw, nwtok = bcast_row(cx, S, dr["hyb_norm"], 2048, "nw")
    xs = [S.sb("xs", [128, 2048], F32) for _ in range(2)]
    szb = [S.sb("szb", [128, 2048], F32) for _ in range(2)]
    prev = S.sb("prev", [128, 4, 512], F32)
    prevb = S.sb("prevb", [128, 4, 512], BF16)
    p.op("pool", lambda e: e.memset(prev, 0.0), writes=["prev"])
    p.op("pool", lambda e: e.memset(prevb, 0.0), writes=["prevb"])
    if LV == -3:
        S.close()
        return
    adp = S.sb("adp", [128, 128], F32)
    p.op("pool", lambda e: e.memset(adp, 0.0), writes=["adp"])
    acum = S.sb("acum", [128, 32], F32)
    nacum = S.sb("nacum", [128, 32], F32)
    eac = S.sb("eac", [128, 32], F32)
    acf = S.sb("acf", [32, 128], F32)
    Dm = S.sb("Dm", [128, 8, 128], F32)
    cbt = S.sb("cbt", [128, 128], F32)
    M = S.sb("M", [128, 8, 128], BF16)
    xdt = S.sb("xdt", [128, 8, 64], BF16)
    xdte = S.sb("xdte", [128, 8, 64], BF16)
    t1 = S.sb("t1", [128, 512], F32)
    t2 = S.sb("t2", [128, 512], F32)
    ysb = S.sb("ysb", [128, 512], F32)
    junk = S.sb("junk", [128, 512], F32)
    ybf = S.sb("ybf", [128, 512], BF16)
    sm = S.sb("sm", [128, 16], F32)
    dte = S.sb("dte", [128, 32], F32)
    wde = S.sb("wde", [128, 32], F32)
    cd = S.sb("cd", [128, 32], F32)
    ptmp = S.sb("ptmp", [128, 512], F32)
    yT = S.sb("yTs", [128, 16, T], BF16)
    E = S.ps("E", [128, 1024], F32)
    aps = S.ps("aps", [128, 512], F32)
    cps = S.ps("cps", [128, 512], F32)
    Yd = S.ps("Yd", [128, 512], F32)
    Yo = S.ps("Yo", [128, 512], F32)
    Sp = S.ps("Sp", [128, 512], F32)
    tp = S.ps("tp", [128, 512], BF16)
    xsv = dr["xs_tm"].rearrange("(t p) c -> t p c", p=128)
    szv = dr["sz"].rearrange("(t p) c -> t p c", p=128)

    def load(c):
        p.dma(xs[c % 2], xsv[c], writes=[("xs", c % 2)])
        p.dma(szb[c % 2], szv[c], writes=[("szb", c % 2)])
    load(0)
    for c in range(NT if LV > 0 else 0):
        if c + 1 < NT:
            load(c + 1)
        X, Z = xs[c % 2], szb[c % 2]
        xtok, ztok = ("xs", c % 2), ("szb", c % 2)
        cs = slice(c * 128, (c + 1) * 128)
        p.mm([lambda e: e.matmul(aps[:, 0:32], lhsT=triu, rhs=adts[:, c, :], start=True, stop=True)], reads=["triu", "adts"], writes=["aps"])
        p.op("act", lambda e: e.activation(out=acum, in_=aps[:, 0:32], func=AF.Copy), reads=["aps"], writes=["acum"])
        p.op("dve", lambda e: e.tensor_scalar(out=nacum, in0=aps[:, 0:32], scalar1=-1.0, scalar2=None, op0=ALU.mult), reads=["aps"], writes=["nacum"])
        p.op("act", lambda e: e.activation(out=eac, in_=aps[:, 0:32], func=AF.Exp), reads=["aps"], writes=["eac"])
        p.op("dve", lambda e: e.tensor_copy(out=adp[:, 0:32], in_=adts[:, c, :]), reads=["adts"], writes=["adp"])
        p.mm([lambda e: e.matmul(aps[:, 128:256], lhsT=adp, rhs=triu, start=True, stop=True)], reads=["triu", "adp", "aps"], writes=["aps"])
        p.op("dve", lambda e: e.tensor_copy(out=acf, in_=aps[0:32, 128:256]), reads=["aps"], writes=["acf"])
        p.mm([lambda e: e.matmul(aps[:, 256:288], lhsT=lastsel, rhs=acum, start=True, stop=True)], reads=["lastsel", "acum", "aps"], writes=["aps"])
        p.op("dve", lambda e: e.tensor_tensor(out=dte, in0=aps[:, 256:288], in1=nacum, op=ALU.add), reads=["aps", "nacum"], writes=["dte"])
        p.op("act", lambda e: e.activation(out=dte, in_=dte, func=AF.Exp), reads=["dte"], writes=["dte"])
        p.op("act", lambda e: e.activation(out=cd, in_=aps[:, 256:288], func=AF.Exp), reads=["aps"], writes=["cd"])
        p.op("dve", lambda e: e.tensor_tensor(out=wde, in0=dte, in1=dts[:, c, :], op=ALU.mult), reads=["dte", "dts"], writes=["wde"])
        for g in range(4 if LV >= 2 else 0):
            hs = slice(g * 8, (g + 1) * 8)
            gs = slice(g * 512, (g + 1) * 512)
            fns = []
            for j in range(8):
                h = g * 8 + j
                fns.append(lambda e, j=j, h=h: e.matmul(E[:, j * 128:(j + 1) * 128], lhsT=sel[:, h * 128:(h + 1) * 128], rhs=acf, start=True, stop=False))
                fns.append(lambda e, j=j: e.matmul(E[:, j * 128:(j + 1) * 128], lhsT=ident, rhs=maskneg, start=False, stop=True))
            p.mm(fns, reads=["sel", "acf", "ident", "maskneg"], writes=["E"])
            for j in range(8):
                h = g * 8 + j
                p.op("act", lambda e, j=j, h=h: e.activation(out=Dm[:, j, :], in_=E[:, j * 128:(j + 1) * 128], func=AF.Exp, bias=nacum[:, h:h + 1], scale=1.0),
                     reads=["E", "nacum"], writes=[("Dm", j)])
            if LV < 3:
                continue
            p.mm([lambda e: e.matmul(cps[:, 0:128], lhsT=Bf[:, g, cs], rhs=Cf[:, g, cs], start=True, stop=True)], reads=["Bf", "Cf"], writes=["cps"])
            p.op("act", lambda e: e.activation(out=cbt, in_=cps[:, 0:128], func=AF.Copy), reads=["cps"], writes=["cbt"])
            p.op("dve", lambda e: e.tensor_tensor(out=M, in0=Dm, in1=cbt.unsqueeze(1).to_broadcast([128, 8, 128]), op=ALU.mult),
                 reads=toks("Dm", range(8)) + ["cbt"], writes=["M"])
            Xg = X[:, gs].rearrange("p (a b) -> p a b", a=8)
            p.op("pool", lambda e: e.tensor_tensor(out=xdt, in0=Xg, in1=dts[:, c, hs].unsqueeze(2).to_broadcast([128, 8, 64]), op=ALU.mult),
                 reads=[xtok, "dts"], writes=["xdt"])
            p.mm([(lambda e, j=j: e.matmul(Yd[:, j * 64:(j + 1) * 64], lhsT=M[:, j, :], rhs=xdt[:, j, :], start=True, stop=True)) for j in range(8)],
                 reads=["M", "xdt"], writes=["Yd"])
            p.mm([lambda e: e.matmul(Yo[:, 0:512], lhsT=Cf[:, g, cs], rhs=prevb[:, g, :], start=True, stop=True)], reads=["Cf", ("prevb", g)], writes=["Yo"])
            if LV < 4:
                continue
            p.op("dve", lambda e: e.tensor_tensor(out=t1.rearrange("p (a b) -> p a b", a=8), in0=Yo.rearrange("p (a b) -> p a b", a=8),
                                                  in1=eac[:, hs].unsqueeze(2).to_broadcast([128, 8, 64]), op=ALU.mult), reads=["Yo", "eac"], writes=["t1"])
            p.op("pool", lambda e: e.tensor_tensor(out=t2.rearrange("p (a b) -> p a b", a=8), in0=Xg,
                                                   in1=dbc[:, hs].unsqueeze(2).to_broadcast([128, 8, 64]), op=ALU.mult), reads=[xtok, dbctok], writes=["t2"])
            p.op("pool", lambda e: e.tensor_tensor(out=t2, in0=t2, in1=t1, op=ALU.add), reads=["t1", "t2"], writes=["t2"])
            p.op("dve", lambda e: e.tensor_tensor(out=ysb, in0=Yd[:, 0:512], in1=t2, op=ALU.add), reads=["Yd", "t2"], writes=["ysb"])
            p.op("pool", lambda e: e.tensor_tensor(out=ysb, in0=ysb, in1=Z[:, gs], op=ALU.mult), reads=["ysb", ztok], writes=["ysb"])
            p.op("act", lambda e: e.activation(out=junk, in_=ysb, func=AF.Square, accum_out=sm[:, 0:1]), reads=["ysb"], writes=["junk", "sm"])
            p.op("dve", lambda e: e.tensor_scalar(out=sm[:, 1:2], in0=sm[:, 0:1], scalar1=1.0 / 512.0, scalar2=EPS, op0=ALU.mult, op1=ALU.add), reads=["sm"], writes=["sm"])
            p.op("act", lambda e: e.activation(out=sm[:, 2:3], in_=sm[:, 1:2], func=AF.Sqrt), reads=["sm"], writes=["sm"])
            p.op("dve", lambda e: e.reciprocal(out=sm[:, 3:4], in_=sm[:, 2:3]), reads=["sm"], writes=["sm"])
            p.op("dve", lambda e: e.scalar_tensor_tensor(out=ybf, in0=ysb, scalar=sm[:, 3:4], in1=nw[:, gs], op0=ALU.mult, op1=ALU.mult),
                 reads=["ysb", "sm", nwtok], writes=["ybf"])
            if LV < 5:
                continue
            p.mm([(lambda e, j=j: e.transpose(out=tp[:, j * 128:(j + 1) * 128], in_=ybf[:, j * 128:(j + 1) * 128], identity=identb)) for j in range(4)],
                 reads=["ybf", "identb"], writes=["tp"])
            p.op("act", lambda e: e.activation(out=yT[:, g * 4:(g + 1) * 4, cs], in_=tp[:, 0:512].rearrange("p (a b) -> p a b", a=4), func=AF.Copy),
                 reads=["tp"], writes=[("yT", c, g)])
            if LV < 6:
                continue
            p.op("dve", lambda e: e.tensor_tensor(out=xdte, in0=Xg, in1=wde[:, hs].unsqueeze(2).to_broadcast([128, 8, 64]), op=ALU.mult),
                 reads=[xtok, "wde"], writes=["xdte"])
            p.mm([lambda e: e.matmul(Sp[:, 0:512], lhsT=Bt[:, c, g * 128:(g + 1) * 128], rhs=xdte.rearrange("p a b -> p (a b)"), start=True, stop=True)],
                 reads=["Bt", "xdte"], writes=["Sp"])
            if LV < 7:
                continue
            p.op("pool", lambda e: e.tensor_tensor(out=ptmp.rearrange("p (a b) -> p a b", a=8), in0=prev[:, g, :].rearrange("p (a b) -> p a b", a=8),
                                                   in1=cd[:, hs].unsqueeze(2).to_broadcast([128, 8, 64]), op=ALU.mult), reads=[("prev", g), "cd"], writes=["ptmp"])
            if LV < 8:
                continue
            p.op("dve", lambda e: e.tensor_tensor(out=prev[:, g, :], in0=Sp[:, 0:512], in1=ptmp, op=ALU.add), reads=["Sp", "ptmp"], writes=[("prev", g)])
            p.op("act", lambda e: e.activation(out=prevb[:, g, :], in_=prev[:, g, :], func=AF.Copy), reads=[("prev", g)], writes=[("prevb", g)])
    for k in range(16):
        p.dma(dr["yT"][k * 128:(k + 1) * 128, :], yT[:, k, :], reads=[("yT", c, k // 4) for c in range(NT)])
    S.close()


def stage_l0_att(cx):
    p, nc, dr = cx.p, cx.nc, cx.dram
    S = Scope(p)
    identb = load_const(cx, S, "identb", "identb")
    causal = load_const(cx, S, "causal", "causal")
    qT = [S.sb("qT", [128, T], BF16) for _ in range(2)]
    kT = [S.sb("kT", [128, T], BF16) for _ in range(2)]
    vt = [S.sb("vt", [128, NT, 128], BF16) for _ in range(2)]
    gt = [S.sb("gt", [128, NT, 8], F32) for _ in range(2)]
    yatt = [S.sb("yatt", [128, T], BF16) for _ in range(2)]
    Ssb = [S.sb("Ssb", [128, T], F32) for _ in range(2)]
    Pb = [S.sb("Pb", [128, T], BF16) for _ in range(2)]
    PTs = [S.sb("PTs", [128, NT, 128], BF16) for _ in range(2)]
    gsb = [S.sb("gsb", [128, 8], F32) for _ in range(2)]
    m8 = [S.sb("m8", [128, 8], F32) for _ in range(2)]
    bs = [S.sb("bs", [128, 8], F32) for _ in range(2)]
    st = [S.sb("st", [128, 4], F32) for _ in range(2)]
    Sps = [S.ps("Sps", [128, 512], F32) for _ in range(4)]
    PT = [S.ps("PT", [128, 1024], BF16) for _ in range(2)]
    OT = S.ps("OT", [128, 512], F32)

    def load(h):
        b = h % 2
        p.dma(qT[b], dr["q_fm"][h * 128:(h + 1) * 128, :], writes=[("qT", b)])
        p.dma(kT[b], dr["k_fm"][h * 128:(h + 1) * 128, :], writes=[("kT", b)])
        p.dma(vt[b], dr["v_tm"].rearrange("(t p) d -> p t d", p=128)[:, :, h * 128:(h + 1) * 128], writes=[("vt", b)])
        p.dma(gt[b], dr["gate_d"][h].rearrange("(t p) e -> p t e", p=128), writes=[("gt", b)])
    load(0)
    it = 0
    for h in range(16):
        if h + 1 < 16:
            load(h + 1)
        hb = h % 2
        for qi in range(NT):
            b = it % 2
            it += 1
            qb, half = qi // 2, qi % 2
            npast = qb * 256
            nk = npast + (half + 1) * 128
            nseg = (nk + 511) // 512
            for sg in range(nseg):
                w = min(512, nk - sg * 512)
                p.mm([lambda e, sg=sg, w=w: e.matmul(Sps[sg][:, 0:w], lhsT=qT[hb][:, qi * 128:(qi + 1) * 128], rhs=kT[hb][:, sg * 512:sg * 512 + w],
                                                     start=True, stop=True)], reads=[("qT", hb), ("kT", hb)], writes=[("Sps", sg)])
            sel = qb >= 3
            if sel:
                p.op("dve", lambda e: e.tensor_copy(out=gsb[b], in_=gt[hb][:, qi, :]), reads=[("gt", hb)], writes=[("gsb", b)])
                p.op("pool", lambda e: e.memset(gsb[b][:, qb:8], NEG), reads=[("gsb", b)], writes=[("gsb", b)])
                p.op("dve", lambda e: e.max(out=m8[b], in_=gsb[b]), reads=[("gsb", b)], writes=[("m8", b)])
                p.op("dve", lambda e: e.tensor_scalar(out=bs[b], in0=gsb[b], scalar1=m8[b][:, 2:3], scalar2=-1.0, op0=ALU.is_ge, op1=ALU.add),
                     reads=[("gsb", b), ("m8", b)], writes=[("bs", b)])
                p.op("dve", lambda e: e.tensor_scalar(out=bs[b], in0=bs[b], scalar1=1.0e30, scalar2=None, op0=ALU.mult), reads=[("bs", b)], writes=[("bs", b)])
            stok = ("Ssb", b)
            for sg in range((npast + 511) // 512):
                wp = min(512, npast - sg * 512)
                nb = wp // 256
                if sel:
                    p.op("dve", lambda e, sg=sg, wp=wp, nb=nb: e.tensor_tensor(
                        out=Ssb[b][:, sg * 512:sg * 512 + wp].rearrange("p (a c) -> p a c", a=nb),
                        in0=Sps[sg][:, 0:wp].rearrange("p (a c) -> p a c", a=nb),
                        in1=bs[b][:, sg * 2:sg * 2 + nb].unsqueeze(2).to_broadcast([128, nb, 256]), op=ALU.add),
                        reads=[("Sps", sg), ("bs", b)], writes=[stok])
                else:
                    p.op("act", lambda e, sg=sg, wp=wp: e.activation(out=Ssb[b][:, sg * 512:sg * 512 + wp], in_=Sps[sg][:, 0:wp], func=AF.Copy),
                         reads=[("Sps", sg)], writes=[stok])
            so, off = npast // 512, npast % 512
            if half == 1:
                p.op("act", lambda e: e.activation(out=Ssb[b][:, npast:npast + 128], in_=Sps[so][:, off:off + 128], func=AF.Copy),
                     reads=[("Sps", so)], writes=[stok])
            o2 = off + half * 128
            p.op("dve", lambda e: e.tensor_tensor(out=Ssb[b][:, nk - 128:nk], in0=Sps[so][:, o2:o2 + 128], in1=causal, op=ALU.add),
                 reads=[("Sps", so), "causal"], writes=[stok])
            p.op("dve", lambda e: e.reduce_max(out=st[b][:, 0:1], in_=Ssb[b][:, 0:nk], axis=AX.X, negate=True), reads=[stok], writes=[("st", b)])
            p.op("act", lambda e: e.activation(out=Ssb[b][:, 0:nk], in_=Ssb[b][:, 0:nk], func=AF.Exp, bias=st[b][:, 0:1], scale=1.0, accum_out=st[b][:, 1:2]),
                 reads=[stok, ("st", b)], writes=[stok, ("st", b)])
            p.op("dve", lambda e: e.reciprocal(out=st[b][:, 2:3], in_=st[b][:, 1:2]), reads=[("st", b)], writes=[("st", b)])
            p.op("dve", lambda e: e.tensor_scalar(out=Pb[b][:, 0:nk], in0=Ssb[b][:, 0:nk], scalar1=st[b][:, 2:3], scalar2=None, op0=ALU.mult),
                 reads=[stok, ("st", b)], writes=[("Pb", b)])
            nkb = nk // 128
            for bank in range((nkb + 7) // 8):
                n8 = min(8, nkb - bank * 8)
                p.mm([(lambda e, kb=kb, bank=bank: e.transpose(out=PT[bank][:, (kb % 8) * 128:(kb % 8 + 1) * 128], in_=Pb[b][:, kb * 128:(kb + 1) * 128],
                                                            identity=identb)) for kb in range(bank * 8, bank * 8 + n8)],
                     reads=[("Pb", b), "identb"], writes=[("PT", bank)])
                eng = "act" if bank == 0 else "dve"
                outap = PTs[b][:, bank * 8:bank * 8 + n8, :]
                inap = PT[bank][:, 0:n8 * 128].rearrange("p (a c) -> p a c", a=n8)
                if eng == "act":
                    p.op("act", lambda e, outap=outap, inap=inap: e.activation(out=outap, in_=inap, func=AF.Copy), reads=[("PT", bank)], writes=[("PTs", b, bank)])
                else:
                    p.op("dve", lambda e, outap=outap, inap=inap: e.tensor_copy(out=outap, in_=inap), reads=[("PT", bank)], writes=[("PTs", b, bank)])
            p.mm([(lambda e, kb=kb: e.matmul(OT[:, 0:128], lhsT=vt[hb][:, kb, :], rhs=PTs[b][:, kb, :], start=(kb == 0), stop=(kb == nkb - 1)))
                  for kb in range(nkb)], reads=[("vt", hb), ("PTs", b, 0), ("PTs", b, 1)], writes=["OT"])
            p.op("act", lambda e: e.activation(out=yatt[hb][:, qi * 128:(qi + 1) * 128], in_=OT[:, 0:128], func=AF.Copy), reads=["OT"], writes=[("yatt", hb)])
        p.dma(dr["yT"][2048 + h * 128:2048 + (h + 1) * 128, :], yatt[hb], reads=[("yatt", hb)])
    S.close()


def ln_tile(cx, r, rtok, junk, jtok, sm, smtok, gbc, gtok, bbc, btok):
    p = cx.p
    p.op("act", lambda e: e.activation(out=junk, in_=r, func=AF.Copy, accum_out=sm[:, 0:1]), reads=[rtok], writes=[jtok, smtok])
    p.op("act", lambda e: e.activation(out=junk, in_=r, func=AF.Square, accum_out=sm[:, 1:2]), reads=[rtok, jtok], writes=[jtok, smtok])
    p.op("dve", lambda e: e.tensor_scalar(out=sm[:, 2:3], in0=sm[:, 0:1], scalar1=1.0 / D, scalar2=None, op0=ALU.mult), reads=[smtok], writes=[smtok])
    p.op("dve", lambda e: e.scalar_tensor_tensor(out=sm[:, 3:4], in0=sm[:, 2:3], scalar=-1.0, in1=sm[:, 2:3], op0=ALU.mult, op1=ALU.mult),
         reads=[smtok], writes=[smtok])
    p.op("dve", lambda e: e.scalar_tensor_tensor(out=sm[:, 4:5], in0=sm[:, 1:2], scalar=1.0 / D, in1=sm[:, 3:4], op0=ALU.mult, op1=ALU.add),
         reads=[smtok], writes=[smtok])
    p.op("dve", lambda e: e.tensor_scalar(out=sm[:, 4:5], in0=sm[:, 4:5], scalar1=EPS, scalar2=None, op0=ALU.add), reads=[smtok], writes=[smtok])
    p.op("act", lambda e: e.activation(out=sm[:, 5:6], in_=sm[:, 4:5], func=AF.Sqrt), reads=[smtok], writes=[smtok])
    p.op("dve", lambda e: e.reciprocal(out=sm[:, 6:7], in_=sm[:, 5:6]), reads=[smtok], writes=[smtok])
    p.op("dve", lambda e: e.scalar_tensor_tensor(out=sm[:, 7:8], in0=sm[:, 2:3], scalar=-1.0, in1=sm[:, 6:7], op0=ALU.mult, op1=ALU.mult),
         reads=[smtok], writes=[smtok])
    p.op("act", lambda e: e.activation(out=r, in_=r, func=AF.Identity, scale=sm[:, 6:7], bias=sm[:, 7:8]), reads=[rtok, smtok], writes=[rtok])
    p.op("pool", lambda e: e.tensor_tensor(out=r, in0=r, in1=gbc, op=ALU.mult), reads=[rtok, gtok], writes=[rtok])
    p.op("pool", lambda e: e.tensor_tensor(out=r, in0=r, in1=bbc, op=ALU.add), reads=[rtok, btok], writes=[rtok])


def stage_outproj_ln(cx, yT_d, kc, W_d, h_in, g_row, b_row, h_out):
    p, nc, dr = cx.p, cx.nc, cx.dram
    S = Scope(p)
    pw = 256
    ws = WStream(cx, S, kc, pw, "wout")
    gbc, gtok = bcast_row(cx, S, g_row, D, "gbc")
    bbc, btok = bcast_row(cx, S, b_row, D, "bbc")
    racc = S.sb("racc", [128, 4, D], F32)
    yTq = S.sb("yTq", [128, kc, 512], BF16)
    junk = S.sb("junk", [128, D], F32)
    sm = S.sb("sm", [128, 8], F32)
    gps = [(S.ps("ops", [128, 512], F32), ("ops", i)) for i in range(4)]
    yv = yT_d.rearrange("(k p) t -> p k t", p=128)
    hv = h_in.rearrange("(t p) d -> t p d", p=128)
    ov = h_out.rearrange("(t p) d -> t p d", p=128)
    panels = [(i * pw, pw) for i in range(D // pw)]
    k = 0
    for qtr in range(4):
        p.dma(yTq, yv[:, :, qtr * 512:(qtr + 1) * 512], writes=["yTq"])
        for j in range(4):
            p.dma(racc[:, j, :], hv[qtr * 4 + j], writes=[("racc", j)])
            p.op("pool", lambda e, j=j: e.tensor_scalar(out=racc[:, j, :], in0=racc[:, j, :], scalar1=DN_ALPHA, scalar2=None, op0=ALU.mult),
                 reads=[("racc", j)], writes=[("racc", j)])
        nxt = ws.fetch(W_d, panels[0][0], pw)
        for pi, (c0, w) in enumerate(panels):
            bf, btk = nxt
            if pi + 1 < len(panels):
                nxt = ws.fetch(W_d, panels[pi + 1][0], pw)
            for j in range(4):
                ps, ptok = gps[k % 4]
                k += 1
                p.mm([(lambda e, c=c, ps=ps, j=j: e.matmul(ps[:, 0:w], lhsT=yTq[:, c, j * 128:(j + 1) * 128], rhs=bf[:, c, 0:w],
                                                         start=(c == 0), stop=(c == kc - 1))) for c in range(kc)], reads=[btk, "yTq"], writes=[ptok])
                p.op("dve", lambda e, ps=ps, j=j, c0=c0: e.tensor_tensor(out=racc[:, j, c0:c0 + w], in0=ps[:, 0:w], in1=racc[:, j, c0:c0 + w], op=ALU.add),
                     reads=[ptok, ("racc", j)], writes=[("racc", j)])
        for j in range(4):
            ln_tile(cx, racc[:, j, :], ("racc", j), junk, "junk", sm, "sm", gbc, gtok, bbc, btok)
            p.dma(ov[qtr * 4 + j], racc[:, j, :], reads=[("racc", j)])
    S.close()


def stage_router(cx, L, h_tm, masked=True):
    p, nc, dr = cx.p, cx.nc, cx.dram
    S = Scope(p)
    ident = load_const(cx, S, "ident", "ident")
    oh = S.sb("oh", [128, 8], F32)
    p.dma(oh, dr["oh"], writes=["oh"])
    wr = S.sb("wr", [128, KC, 32], F32)
    p.dma(wr, dr["moe_w_router"][L * D:(L + 1) * D, :].rearrange("(k p) e -> p k e", p=128), writes=["wr"])
    brbc, brtok = bcast_row(cx, S, dr["moe_b_router"][L:L + 1, :], 32, "brbc")
    hT = S.sb("hTr", [128, KC, T], BF16)
    hTf = S.sb("hTf", [128, KC, 128], F32)
    gl = S.sb("gl", [128, NT, 32], F32)
    ld = [S.sb("rld", [128, D], F32) for _ in range(2)]
    lg = S.sb("lg", [128, 32], F32)
    ex = S.sb("ex", [128, 32], F32)
    selm = S.sb("selm", [128, 32], F32)
    m8 = S.sb("m8", [128, 8], F32)
    sm = S.sb("rsm", [128, 4], F32)
    tps = [S.ps("rtp", [128, 512], F32) for _ in range(2)]
    lps = S.ps("lps", [128, 512], F32)
    src = h_tm.rearrange("(t p) d -> t p d", p=128)
    k = 0
    for t in range(NT):
        b = ld[t % 2]
        p.dma(b, src[t], writes=[("rld", t % 2)])
        for g in range(4):
            pp, ptok = tps[k % 2], ("rtp", k % 2)
            k += 1
            p.mm([(lambda e, pp=pp, b=b, g=g, j=j: e.transpose(out=pp[:, j * 128:(j + 1) * 128], in_=b[:, (g * 4 + j) * 128:(g * 4 + j + 1) * 128],
                                                              identity=ident)) for j in range(4)], reads=[("rld", t % 2), "ident"], writes=[ptok])
            src_ps = pp.rearrange("p (a b) -> p a b", a=4)
            p.op("act", lambda e, g=g, s=src_ps: e.activation(out=hTf[:, g * 4:(g + 1) * 4, :], in_=s, func=AF.Copy), reads=[ptok], writes=[("hTf", g)])
            p.op("dve", lambda e, g=g, t=t: e.tensor_copy(out=hT[:, g * 4:(g + 1) * 4, t * 128:(t + 1) * 128], in_=hTf[:, g * 4:(g + 1) * 4, :]),
                 reads=[("hTf", g)], writes=[("hTr", t, g)])
        p.mm([(lambda e, c=c: e.matmul(lps[:, 0:32], lhsT=hTf[:, c, :], rhs=wr[:, c, :], start=(c == 0), stop=(c == KC - 1))) for c in range(KC)],
             reads=toks("hTf", range(4)) + ["wr"], writes=["lps"])
        p.op("dve", lambda e: e.tensor_tensor(out=lg, in0=lps[:, 0:32], in1=brbc, op=ALU.add), reads=["lps", brtok], writes=["lg"])
        p.op("dve", lambda e: e.max(out=m8, in_=lg), reads=["lg"], writes=["m8"])
        p.op("dve", lambda e: e.tensor_scalar(out=sm[:, 0:1], in0=m8[:, 0:1], scalar1=-1.0, scalar2=None, op0=ALU.mult), reads=["m8"], writes=["rsm"])
        p.op("act", lambda e: e.activation(out=ex, in_=lg, func=AF.Exp, bias=sm[:, 0:1], scale=1.0), reads=["lg", "rsm"], writes=["ex"])
        p.op("dve", lambda e: e.tensor_scalar(out=selm, in0=lg, scalar1=m8[:, 3:4], scalar2=0.0, op0=ALU.is_ge, op1=ALU.add), reads=["lg", "m8"], writes=["selm"])
        p.op("dve", lambda e: e.tensor_tensor(out=ex, in0=ex, in1=selm, op=ALU.mult), reads=["ex", "selm"], writes=["ex"])
        p.op("dve", lambda e: e.tensor_reduce(out=sm[:, 1:2], in_=ex, axis=AX.X, op=ALU.add), reads=["ex"], writes=["rsm"])
        p.op("dve", lambda e: e.reciprocal(out=sm[:, 2:3], in_=sm[:, 1:2]), reads=["rsm"], writes=["rsm"])
        p.op("dve", lambda e, t=t: e.tensor_scalar(out=gl[:, t, :], in0=ex, scalar1=sm[:, 2:3], scalar2=None, op0=ALU.mult), reads=["ex", "rsm"], writes=["gl"])
    if not masked:
        for k in range(KC):
            p.dma(dr["hTo"][k * 128:(k + 1) * 128, :], hT[:, k, :], reads=[("hTr", t, k // 4) for t in range(NT)])
        p.dma(dr["gateo"].rearrange("(t p) e -> p t e", p=128), gl, reads=["gl"])
        S.close()
        return
    scb = [S.sb("scb", [128, 4, T], BF16) for _ in range(2)]
    gsc = [S.sb("gsc", [128, NT, 32], F32) for _ in range(2)]
    alltok = [("hTr", t, g) for t in range(NT) for g in range(4)]
    n = 0
    for j in range(8):
        for kq in range(4):
            b = n % 2
            eng = "dve" if n % 2 == 0 else "pool"
            n += 1
            p.op(eng, lambda e, b=b, kq=kq, j=j: e.tensor_scalar(out=scb[b], in0=hT[:, kq * 4:(kq + 1) * 4, :], scalar1=oh[:, j:j + 1], scalar2=None, op0=ALU.mult),
                 reads=[("hTr", t, kq) for t in range(NT)] + ["oh"], writes=[("scb", b)])
            p.dma(dr["GH"][j * D:(j + 1) * D, :].rearrange("(k p) t -> p k t", p=128)[:, kq * 4:(kq + 1) * 4, :], scb[b], reads=[("scb", b)], writes=["GH"])
        b = j % 2
        p.op("dve", lambda e, b=b, j=j: e.tensor_scalar(out=gsc[b], in0=gl, scalar1=oh[:, j:j + 1], scalar2=None, op0=ALU.mult), reads=["gl", "oh"], writes=[("gsc", b)])
        p.dma(dr["GG"][j * T:(j + 1) * T, :].rearrange("(t p) e -> p t e", p=128), gsc[b], reads=[("gsc", b)], writes=["GG"])
    S.close()


def stage_precast(cx, L):
    p, nc, dr = cx.p, cx.nc, cx.dram
    S = Scope(p)
    st1 = [S.sb("pc1", [128, 4096], F32) for _ in range(2)]
    o1 = [S.sb("po1", [128, 16, 256], BF16) for _ in range(2)]
    st2 = [S.sb("pc2", [128, 2048], F32) for _ in range(2)]
    o2 = [S.sb("po2", [128, 2048], BF16) for _ in range(2)]
    n = 0
    for e_ in range(4):
        r0 = (L * 4 + e_) * D
        for kc in range(KC):
            b = n % 2
            n += 1
            p.dma(st1[b], dr["moe_w1"][r0 + kc * 128:r0 + (kc + 1) * 128, :], writes=[("pc1", b)])
            sv = st1[b].rearrange("p (f i two) -> p f i two", f=16, two=2)
            p.op("dve", lambda e, b=b, sv=sv: e.tensor_copy(out=o1[b][:, :, 0:128], in_=sv[:, :, :, 0]), reads=[("pc1", b)], writes=[("po1", b)])
            p.op("pool", lambda e, b=b, sv=sv: e.tensor_copy(out=o1[b][:, :, 128:256], in_=sv[:, :, :, 1]), reads=[("pc1", b)], writes=[("po1", b)])
            p.dma(dr["w1b"][e_ * 16 * 128:(e_ + 1) * 16 * 128, :].rearrange("(f p) (k c) -> p f k c", p=128, c=256)[:, :, kc, :], o1[b],
                  reads=[("po1", b)], writes=["w1b"])
            p.dma(st2[b], dr["moe_w2"][r0 + kc * 128:r0 + (kc + 1) * 128, :], writes=[("pc2", b)])
            p.op("act", lambda e, b=b: e.activation(out=o2[b], in_=st2[b], func=AF.Copy), reads=[("pc2", b)], writes=[("po2", b)])
            p.dma(dr["w2b"][e_ * 4 * 128:(e_ + 1) * 4 * 128, :].rearrange("(d p) (f c) -> p d f c", p=128, c=512)[:, :, kc, :],
                  o2[b].rearrange("p (d c) -> p d c", d=4), reads=[("po2", b)], writes=["w2b"])
    S.close()


def stage_experts(cx, L, GH2, GG2, ngroups=16):
    p, nc, dr = cx.p, cx.nc, cx.dram
    S = Scope(p)
    ident = load_const(cx, S, "ident", "ident")
    oh = S.sb("oh", [128, 8], F32)
    p.dma(oh, dr["oh"], writes=["oh"])
    mps = S.ps("emps", [128, 512], F32)
    b1st = S.sb("b1st", [64, 256], F32)
    p.dma(b1st, dr["moe_b1"][L * 4:(L + 1) * 4, :].rearrange("e (f c) -> (e f) c", c=256), writes=["b1st"])
    b1g = S.sb("b1g", [128, 64], F32)
    b1l = S.sb("b1l", [128, 64], F32)
    p.mm([lambda e: e.transpose(out=mps[:, 0:64], in_=b1st[:, 0:256:2], identity=ident[0:64, 0:64]),
          lambda e: e.transpose(out=mps[:, 64:128], in_=b1st[:, 1:256:2], identity=ident[0:64, 0:64])], reads=["b1st", "ident"], writes=["emps"])
    p.op("dve", lambda e: e.tensor_copy(out=b1g, in_=mps[:, 0:64]), reads=["emps"], writes=["b1g"])
    p.op("dve", lambda e: e.tensor_copy(out=b1l, in_=mps[:, 64:128]), reads=["emps"], writes=["b1l"])
    b2t = [S.sb("b2t", [128, 512], F32) for _ in range(2)]
    hTg = S.sb("hTg", [128, KC, 1024], BF16)
    actT = S.sb("actT", [128, KC, 1024], BF16)
    acc = S.sb("acc", [128, 8, D], F32)
    gts = S.sb("gts", [128, 8, 32], F32)
    gsel = S.sb("gsel", [128, 8, 4], F32)
    gtmp = S.sb("gtmp", [128, 8, 4], F32)
    w1p = [S.sb("w1p", [128, KC, 256], BF16) for _ in range(2)]
    w2p = [S.sb("w2p", [128, KC, 512], BF16) for _ in range(2)]
    ga = [S.sb("ga", [128, 512], F32) for _ in range(2)]
    sg = [S.sb("sg", [128, 512], F32) for _ in range(2)]
    la = [S.sb("la", [128, 512], F32) for _ in range(2)]
    yb = [S.sb("yb", [128, 512], F32) for _ in range(2)]
    Gp = [S.ps("Gp", [128, 512], F32) for _ in range(2)]
    Lp = [S.ps("Lp", [128, 512], F32) for _ in range(2)]
    Yp = [S.ps("Yp", [128, 512], F32) for _ in range(2)]
    w1v = dr["w1b"].rearrange("(ef p) x -> ef p x", p=128)
    w2v = dr["w2b"].rearrange("(ed p) x -> ed p x", p=128)
    n1 = n2 = it = 0

    def fetch1(e_, fc):
        nonlocal n1
        b = n1 % 2
        n1 += 1
        p.dma(w1p[b].rearrange("p k c -> p (k c)"), w1v[e_ * 16 + fc], reads=["w1b"], writes=[("w1p", b)])
        return b

    def fetch2(e_, dp):
        nonlocal n2
        b = n2 % 2
        n2 += 1
        p.dma(w2p[b].rearrange("p k c -> p (k c)"), w2v[e_ * 4 + dp], reads=["w2b"], writes=[("w2p", b)])
        return b
    for gi in range(ngroups):
        jb, hf = gi // 2, gi % 2
        p.dma(hTg, GH2[jb * D:(jb + 1) * D, :].rearrange("(k p) t -> p k t", p=128)[:, :, hf * 1024:(hf + 1) * 1024], reads=["GH2"], writes=["hTg"])
        p.dma(gts, GG2[jb * T + hf * 1024:jb * T + (hf + 1) * 1024, :].rearrange("(t p) e -> p t e", p=128), reads=["GG2"], writes=["gts"])
        gv = gts.rearrange("p t (j e) -> p t j e", e=4)
        p.op("dve", lambda e: e.tensor_scalar(out=gsel, in0=gv[:, :, 0, :], scalar1=oh[:, 0:1], scalar2=None, op0=ALU.mult), reads=["gts", "oh"], writes=["gsel"])
        for j in range(1, 8):
            p.op("dve", lambda e, j=j: e.scalar_tensor_tensor(out=gsel, in0=gv[:, :, j, :], scalar=oh[:, j:j + 1], in1=gsel, op0=ALU.mult, op1=ALU.add),
                 reads=["gts", "oh", "gsel"], writes=["gsel"])
        for e_ in range(4):
            nb = fetch1(e_, 0)
            for fc in range(16):
                b1 = nb
                if fc + 1 < 16:
                    nb = fetch1(e_, fc + 1)
                for tb in range(2):
                    i2 = it % 2
                    it += 1
                    tsl = slice(tb * 512, (tb + 1) * 512)
                    p.mm([(lambda e, c=c: e.matmul(Gp[i2][:, 0:512], lhsT=w1p[b1][:, c, 0:128], rhs=hTg[:, c, tsl], start=(c == 0), stop=(c == KC - 1)))
                          for c in range(KC)], reads=[("w1p", b1), "hTg"], writes=[("Gp", i2)])
                    p.mm([(lambda e, c=c: e.matmul(Lp[i2][:, 0:512], lhsT=w1p[b1][:, c, 128:256], rhs=hTg[:, c, tsl], start=(c == 0), stop=(c == KC - 1)))
                          for c in range(KC)], reads=[("w1p", b1), "hTg"], writes=[("Lp", i2)])
                    col = e_ * 16 + fc
                    p.op("dve", lambda e: e.tensor_scalar(out=ga[i2], in0=Gp[i2][:, 0:512], scalar1=b1g[:, col:col + 1], scalar2=7.0, op0=ALU.add, op1=ALU.min),
                         reads=[("Gp", i2), "b1g"], writes=[("ga", i2)])
                    p.op("act", lambda e: e.activation(out=sg[i2], in_=ga[i2], func=AF.Sigmoid, scale=1.702), reads=[("ga", i2)], writes=[("sg", i2)])
                    p.op("dve", lambda e: e.tensor_scalar(out=la[i2], in0=Lp[i2][:, 0:512], scalar1=b1l[:, col:col + 1], scalar2=7.0, op0=ALU.add, op1=ALU.min),
                         reads=[("Lp", i2), "b1l"], writes=[("la", i2)])
                    p.op("pool", lambda e: e.tensor_scalar(out=la[i2], in0=la[i2], scalar1=-7.0, scalar2=1.0, op0=ALU.max, op1=ALU.add),
                         reads=[("la", i2)], writes=[("la", i2)])
                    p.op("pool", lambda e: e.tensor_tensor(out=ga[i2], in0=ga[i2], in1=sg[i2], op=ALU.mult), reads=[("ga", i2), ("sg", i2)], writes=[("ga", i2)])
                    p.op("pool", lambda e: e.tensor_tensor(out=actT[:, fc, tsl], in0=ga[i2], in1=la[i2], op=ALU.mult), reads=[("ga", i2), ("la", i2)],
                         writes=[("actT", fc, tb)])
            nb = fetch2(e_, 0)
            for dp in range(4):
                b2 = nb
                if dp + 1 < 4:
                    nb = fetch2(e_, dp + 1)
                dsl = slice(dp * 512, (dp + 1) * 512)
                bb = n2 % 2
                p.dma(b2t[bb], dr["moe_b2"][L * 4 + e_:L * 4 + e_ + 1, dsl].partition_broadcast(128), writes=[("b2t", bb)])
                for t in range(8):
                    i2 = it % 2
                    it += 1
                    p.mm([(lambda e, c=c: e.matmul(Yp[i2][:, 0:512], lhsT=actT[:, c, t * 128:(t + 1) * 128], rhs=w2p[b2][:, c, :], start=(c == 0), stop=(c == KC - 1)))
                          for c in range(KC)], reads=[("w2p", b2)] + [("actT", c, t // 4) for c in range(KC)], writes=[("Yp", i2)])
                    p.op("dve", lambda e: e.tensor_tensor(out=yb[i2], in0=Yp[i2][:, 0:512], in1=b2t[bb], op=ALU.add), reads=[("Yp", i2), ("b2t", bb)],
                         writes=[("yb", i2)])
                    if e_ == 0:
                        p.op("pool", lambda e: e.tensor_scalar(out=acc[:, t, dsl], in0=yb[i2], scalar1=gsel[:, t, e_:e_ + 1], scalar2=None, op0=ALU.mult),
                             reads=[("yb", i2), "gsel"], writes=[("acc", t, dp)])
                    else:
                        p.op("pool", lambda e: e.tensor_scalar(out=yb[i2], in0=yb[i2], scalar1=gsel[:, t, e_:e_ + 1], scalar2=None, op0=ALU.mult),
                             reads=[("yb", i2), "gsel"], writes=[("yb", i2)])
                        p.op("pool", lambda e: e.tensor_tensor(out=acc[:, t, dsl], in0=acc[:, t, dsl], in1=yb[i2], op=ALU.add),
                             reads=[("yb", i2), ("acc", t, dp)], writes=[("acc", t, dp)])
        p.dma(dr["PART"][gi * 1024:(gi + 1) * 1024, :].rearrange("(t p) d -> p t d", p=128), acc,
              reads=[("acc", t, dp) for t in range(8) for dp in range(4)], writes=["PART"])
    S.close()


def stage_combine_ln(cx, PART2, h_in, g_row, b_row, h_out, nblk=8, use_oh=True):
    p, nc, dr = cx.p, cx.nc, cx.dram
    S = Scope(p)
    oh = S.sb("oh", [128, 8], F32)
    p.dma(oh, dr["oh"], writes=["oh"])
    gbc, gtok = bcast_row(cx, S, g_row, D, "gbc")
    bbc, btok = bcast_row(cx, S, b_row, D, "bbc")
    r = [S.sb("cr", [128, D], F32) for _ in range(2)]
    pl = [S.sb("cpl", [128, D], F32) for _ in range(3)]
    junk = S.sb("junk", [128, D], F32)
    sm = S.sb("sm", [128, 8], F32)
    hv = h_in.rearrange("(t p) d -> t p d", p=128)
    ov = h_out.rearrange("(t p) d -> t p d", p=128)
    n = 0
    for t in range(NT):
        b = t % 2
        rt = ("cr", b)
        p.dma(r[b], hv[t], writes=[rt])
        p.op("pool", lambda e, b=b: e.tensor_scalar(out=r[b], in0=r[b], scalar1=DN_ALPHA, scalar2=None, op0=ALU.mult), reads=[rt], writes=[rt])
        for j in range(nblk):
            c = n % 3
            n += 1
            p.dma(pl[c], PART2[j * T + t * 128:j * T + (t + 1) * 128, :], reads=["PART2"], writes=[("cpl", c)])
            if use_oh:
                p.op("dve", lambda e, b=b, c=c, j=j: e.scalar_tensor_tensor(out=r[b], in0=pl[c], scalar=oh[:, j:j + 1], in1=r[b], op0=ALU.mult, op1=ALU.add),
                     reads=[("cpl", c), "oh", rt], writes=[rt])
            else:
                p.op("dve" if j % 2 == 0 else "pool", lambda e, b=b, c=c: e.tensor_tensor(out=r[b], in0=r[b], in1=pl[c], op=ALU.add),
                     reads=[("cpl", c), rt], writes=[rt])
        ln_tile(cx, r[b], rt, junk, "junk", sm, "sm", gbc, gtok, bbc, btok)
        p.dma(ov[t], r[b], reads=[rt])
    S.close()


def stage_gla_proj(cx, h_tm):
    p, nc, dr = cx.p, cx.nc, cx.dram
    S = Scope(p)
    ident = load_const(cx, S, "ident", "ident")
    xT = S.sb("xT", [128, KC, T], BF16)
    S2 = Scope(p)
    transpose_in(cx, S2, h_tm, xT, "xT", ident, "ident")
    S2.close()
    w_in = dr["gla_w_in"]
    ws = WStream(cx, S, KC, 256, "gwin")
    gps = [(S.ps("gps", [128, 512], F32), ("gps", i)) for i in range(3)]
    lps = [S.ps("glps", [128, 512], F32) for _ in range(2)]
    fst = [S.sb("fst", [128, T], F32) for _ in range(2)]
    tst = [S.sb("tst", [128, 256], F32) for _ in range(2)]
    vst = [S.sb("gvst", [128, 256], BF16) for _ in range(2)]
    glT = S.sb("glT", [16, T], F32)
    cnt = {"f": 0, "t": 0, "v": 0}

    def ep(tag, c0, w, idx, ps, ptok):
        if tag in ("q", "k"):
            tb = idx
            b = cnt["f"] % 2
            sc = (1.0 / 16.0) if tag == "q" else 1.0
            p.op("act", lambda e: e.activation(out=fst[b][:, tb * 512:(tb + 1) * 512], in_=ps[:, 0:512], func=AF.Copy, scale=sc), reads=[ptok], writes=[("fst", b, tb)])
            if tb == 3:
                cnt["f"] += 1
                dst = dr["gq_fm"] if tag == "q" else dr["gk_fm"]
                r0 = c0 if tag == "q" else c0 - 1024
                p.dma(dst[r0:r0 + 128, :], fst[b], reads=[("fst", b, i) for i in range(4)])
        elif tag == "gl":
            tb = idx
            p.op("act", lambda e: e.activation(out=glT[:, tb * 512:(tb + 1) * 512], in_=ps[0:16, 0:512], func=AF.Copy), reads=[ptok], writes=[("glT", tb)])
        elif tag in ("ktm", "g"):
            b = cnt["t"] % 2
            cnt["t"] += 1
            if tag == "g":
                p.op("act", lambda e: e.activation(out=tst[b][:, 0:w], in_=ps[:, 0:w], func=AF.Silu), reads=[ptok], writes=[("tst", b)])
                p.dma(dr["gsg"][idx * 128:(idx + 1) * 128, c0 - 4096:c0 - 4096 + w], tst[b][:, 0:w], reads=[("tst", b)])
            else:
                p.op("dve", lambda e: e.tensor_copy(out=tst[b][:, 0:w], in_=ps[:, 0:w]), reads=[ptok], writes=[("tst", b)])
                p.dma(dr["gk_tm"][idx * 128:(idx + 1) * 128, c0 - 1024:c0 - 1024 + w], tst[b][:, 0:w], reads=[("tst", b)])
        elif tag == "v":
            b = cnt["v"] % 2
            cnt["v"] += 1
            p.op("dve", lambda e: e.tensor_copy(out=vst[b][:, 0:w], in_=ps[:, 0:w]), reads=[ptok], writes=[("gvst", b)])
            p.dma(dr["gv_tm"][idx * 128:(idx + 1) * 128, c0 - 2048:c0 - 2048 + w], vst[b][:, 0:w], reads=[("gvst", b)])

    panels = [(i * 256, 256, "q") for i in range(4)] + [(1024 + i * 256, 256, "k") for i in range(4)] + [(6144, 16, "gl")]
    gemm_panels(cx, ws, w_in, panels, xT, "xT", "fm", gps, ep)
    p.barrier()
    panels = [(1024 + i * 256, 256, "ktm") for i in range(4)] + [(2048 + i * 256, 256, "v") for i in range(8)] + [(4096 + i * 256, 256, "g") for i in range(8)]
    gemm_panels(cx, ws, w_in, panels, xT, "xT", "tm", gps, ep)
    p.barrier()
    wg2 = S.sb("wg2", [16, 1024], F32)
    p.dma(wg2, dr["gla_w_gate2"], writes=["wg2"])
    bg, bgtok = bcast_row(cx, S, dr["gla_b_gate"], 1024, "bgate")
    xg = [S.sb("xg", [128, 1024], F32) for _ in range(2)]
    lg = [S.sb("lgl", [128, 1024], F32) for _ in range(2)]
    for t in range(NT):
        b = t % 2
        for hf in range(2):
            p.mm([lambda e, hf=hf: e.matmul(lps[hf][:, 0:512], lhsT=glT[:, t * 128:(t + 1) * 128], rhs=wg2[:, hf * 512:(hf + 1) * 512], start=True, stop=True)],
                 reads=toks("glT", range(4)) + ["wg2"], writes=[("glps", hf)])
            p.op("dve", lambda e, hf=hf: e.tensor_tensor(out=xg[b][:, hf * 512:(hf + 1) * 512], in0=lps[hf][:, 0:512], in1=bg[:, hf * 512:(hf + 1) * 512], op=ALU.add),
                 reads=[("glps", hf), bgtok], writes=[("xg", b, hf)])
        xt2 = [("xg", b, 0), ("xg", b, 1)]
        p.op("act", lambda e: e.activation(out=lg[b], in_=xg[b], func=AF.Abs), reads=xt2, writes=[("lgl", b)])
        p.op("act", lambda e: e.activation(out=lg[b], in_=lg[b], func=AF.Exp, scale=-1.0), reads=[("lgl", b)], writes=[("lgl", b)])
        p.op("act", lambda e: e.activation(out=lg[b], in_=lg[b], func=AF.Ln, bias=1.0), reads=[("lgl", b)], writes=[("lgl", b)])
        p.op("dve", lambda e: e.tensor_scalar(out=xg[b], in0=xg[b], scalar1=0.0, scalar2=1.0 / 16.0, op0=ALU.min, op1=ALU.mult), reads=xt2, writes=xt2)
        p.op("dve", lambda e: e.scalar_tensor_tensor(out=lg[b], in0=lg[b], scalar=-1.0 / 16.0, in1=xg[b], op0=ALU.mult, op1=ALU.add),
             reads=xt2 + [("lgl", b)], writes=[("lgl", b)])
        p.dma(dr["gla_d"][t * 128:(t + 1) * 128, :], lg[b], reads=[("lgl", b)])
    S.close()


def stage_gla_core(cx):
    p, nc, dr = cx.p, cx.nc, cx.dram
    S = Scope(p)
    identb = load_const(cx, S, "identb", "identb")
    btri = load_const(cx, S, "btri", "btri")
    bgt = load_const(cx, S, "bgt", "bgt")
    gnb, gntok = bcast_row(cx, S, dr["gla_norm"], 512, "gnb")
    yT = S.sb("gyT", [128, 16, T], BF16)
    St = S.sb("St", [128, 8, 512], F32)
    Sb = S.sb("Sb", [128, 8, 512], BF16)
    S1 = S.sb("S1", [128, 2, 512], F32)
    S1b = S.sb("S1b", [128, 2, 512], BF16)
    p.op("pool", lambda e: e.memset(St, 0.0), writes=toks("St", range(8)))
    p.op("pool", lambda e: e.memset(Sb, 0.0), writes=toks("Sb", range(8)))
    la = [S.sb("la", [128, 1024], F32) for _ in range(2)]
    ktm = [S.sb("ktm", [128, 1024], F32) for _ in range(2)]
    vt = [S.sb("gvt", [128, 2048], BF16) for _ in range(2)]
    sg = [S.sb("gsg", [128, 2048], F32) for _ in range(2)]
    qf = [S.sb("gqf", [128, 8, 128], F32) for _ in range(2)]
    kf = [S.sb("gkf", [128, 8, 128], F32) for _ in range(2)]
    eg = S.sb("eg", [128, 8, 128], F32)
    eng = S.sb("eng", [128, 8, 128], F32)
    egl = S.sb("egl", [128, 1024], F32)
    qin = S.sb("qin", [128, 8, 128], BF16)
    kin = S.sb("kin", [128, 8, 128], BF16)
    qA = S.sb("qA", [128, 8, 128], BF16)
    qB = S.sb("qB", [128, 8, 128], BF16)
    p.op("pool", lambda e: e.memset(qA, 0.0), writes=["qA"])
    p.op("pool", lambda e: e.memset(qB, 0.0), writes=["qB"])
    ke = S.sb("ke", [128, 1024], BF16)
    atm = S.sb("atm", [128, 128], BF16)
    junk = S.sb("gjunk", [128, 512], F32)
    on = S.sb("on", [128, 512], F32)
    ybf = S.sb("gybf", [128, 512], BF16)
    sm = S.sb("gsm", [128, 4], F32)
    GC = [S.ps("GC", [128, 512], F32) for _ in range(2)]
    GL = [S.ps("GL", [128, 512], F32) for _ in range(2)]
    ATp = S.ps("ATp", [128, 512], F32)
    Op = S.ps("Op", [128, 512], F32)
    Dp = S.ps("Dp", [128, 512], F32)
    tp = S.ps("gtp", [128, 512], BF16)
    qv = dr["gq_fm"].rearrange("(c p) t -> p c t", p=128)
    kv = dr["gk_fm"].rearrange("(c p) t -> p c t", p=128)

    def load(t):
        b = t % 2
        rs = slice(t * 128, (t + 1) * 128)
        p.dma(la[b], dr["gla_d"][rs, :], writes=[("la", b)])
        p.dma(ktm[b], dr["gk_tm"][rs, :], writes=[("ktm", b)])
        p.dma(vt[b], dr["gv_tm"][rs, :], writes=[("gvt", b)])
        p.dma(sg[b], dr["gsg"][rs, :], writes=[("gsg", b)])
        p.dma(qf[b], qv[:, :, rs], writes=[("gqf", b)])
        p.dma(kf[b], kv[:, :, rs], writes=[("gkf", b)])
    load(0)
    for t in range(NT):
        if t + 1 < NT:
            load(t + 1)
        b = t % 2
        cs = slice(t * 128, (t + 1) * 128)
        for hf in range(2):
            p.mm([(lambda e, j=j, hf=hf: e.matmul(GC[hf][:, j * 128:(j + 1) * 128], lhsT=la[b][:, (hf * 4 + j) * 128:(hf * 4 + j + 1) * 128], rhs=btri,
                                                  start=True, stop=True)) for j in range(4)], reads=[("la", b), "btri"], writes=[("GC", hf)])
            p.op("act", lambda e, hf=hf: e.activation(out=eg[:, hf * 4:(hf + 1) * 4, :], in_=GC[hf].rearrange("p (a c) -> p a c", a=4), func=AF.Exp),
                 reads=[("GC", hf)], writes=[("eg", hf)])
            p.op("act", lambda e, hf=hf: e.activation(out=eng[:, hf * 4:(hf + 1) * 4, :], in_=GC[hf].rearrange("p (a c) -> p a c", a=4), func=AF.Exp, scale=-1.0),
                 reads=[("GC", hf)], writes=[("eng", hf)])
            p.mm([lambda e, hf=hf: e.matmul(GL[hf][:, 0:512], lhsT=bgt, rhs=la[b][:, hf * 512:(hf + 1) * 512], start=True, stop=True)],
                 reads=[("la", b), "bgt"], writes=[("GL", hf)])
            p.op("act", lambda e, hf=hf: e.activation(out=egl[:, hf * 512:(hf + 1) * 512], in_=GL[hf][:, 0:512], func=AF.Exp), reads=[("GL", hf)], writes=[("egl", hf)])
        egt = [("eg", 0), ("eg", 1)]
        p.op("dve", lambda e: e.tensor_tensor(out=qin, in0=qf[b], in1=eg, op=ALU.mult), reads=[("gqf", b)] + egt, writes=["qin"])
        p.op("pool", lambda e: e.tensor_tensor(out=kin, in0=kf[b], in1=eng, op=ALU.mult), reads=[("gkf", b), ("eng", 0), ("eng", 1)], writes=["kin"])
        p.op("pool", lambda e: e.tensor_copy(out=qA[:, :, 0:64], in_=qin[:, :, 0:64]), reads=["qin"], writes=["qA"])
        p.op("pool", lambda e: e.tensor_copy(out=qB[:, :, 64:128], in_=qin[:, :, 64:128]), reads=["qin"], writes=["qB"])
        p.op("dve", lambda e: e.tensor_tensor(out=ke, in0=ktm[b], in1=egl, op=ALU.mult), reads=[("ktm", b), ("egl", 0), ("egl", 1)], writes=["ke"])
        for hd in range(4):
            vs = slice(hd * 512, (hd + 1) * 512)
            p.mm([(lambda e, dcl=dcl: e.matmul(ATp[:, 0:128], lhsT=kin[:, 2 * hd + dcl, :], rhs=qin[:, 2 * hd + dcl, :], start=(dcl == 0), stop=(dcl == 1)))
                  for dcl in range(2)], reads=["kin", "qin"], writes=["ATp"])
            p.op("dve", lambda e: e.tensor_tensor(out=atm, in0=ATp[:, 0:128], in1=btri, op=ALU.mult), reads=["ATp", "btri"], writes=["atm"])
            for dcl in range(2):
                dc = 2 * hd + dcl
                p.mm([lambda e, dc=dc: e.matmul(Dp[:, 0:512], lhsT=ke[0:64, dc * 128:(dc + 1) * 128], rhs=vt[b][0:64, vs], start=True, stop=True)],
                     reads=["ke", ("gvt", b)], writes=["Dp"])
                p.op("dve", lambda e, dc=dc, dcl=dcl: e.scalar_tensor_tensor(out=S1[:, dcl, :], in0=St[:, dc, :], scalar=eg[:, dc, 63:64], in1=Dp[:, 0:512],
                                                                            op0=ALU.mult, op1=ALU.add), reads=[("St", dc), "Dp"] + egt, writes=[("S1", dcl)])
                p.op("act", lambda e, dcl=dcl: e.activation(out=S1b[:, dcl, :], in_=S1[:, dcl, :], func=AF.Copy), reads=[("S1", dcl)], writes=[("S1b", dcl)])
            fns = [lambda e: e.matmul(Op[:, 0:512], lhsT=atm, rhs=vt[b][:, vs], start=True, stop=False)]
            for dcl in range(2):
                dc = 2 * hd + dcl
                fns.append(lambda e, dc=dc: e.matmul(Op[:, 0:512], lhsT=qA[:, dc, :], rhs=Sb[:, dc, :], start=False, stop=False))
                fns.append(lambda e, dc=dc, dcl=dcl: e.matmul(Op[:, 0:512], lhsT=qB[:, dc, :], rhs=S1b[:, dcl, :], start=False, stop=(dcl == 1)))
            p.mm(fns, reads=["atm", ("gvt", b), "qA", "qB", ("Sb", 2 * hd), ("Sb", 2 * hd + 1), ("S1b", 0), ("S1b", 1)], writes=["Op"])
            for dcl in range(2):
                dc = 2 * hd + dcl
                p.mm([lambda e, dc=dc: e.matmul(Dp[:, 0:512], lhsT=ke[64:128, dc * 128:(dc + 1) * 128], rhs=vt[b][64:128, vs], start=True, stop=True)],
                     reads=["ke", ("gvt", b)], writes=["Dp"])
                p.op("dve", lambda e, dc=dc, dcl=dcl: e.scalar_tensor_tensor(out=St[:, dc, :], in0=S1[:, dcl, :], scalar=eg[:, dc, 127:128], in1=Dp[:, 0:512],
                                                                            op0=ALU.mult, op1=ALU.add), reads=[("S1", dcl), "Dp"] + egt, writes=[("St", dc)])
                p.op("act", lambda e, dc=dc: e.activation(out=Sb[:, dc, :], in_=St[:, dc, :], func=AF.Copy), reads=[("St", dc)], writes=[("Sb", dc)])
            p.op("act", lambda e: e.activation(out=junk, in_=Op[:, 0:512], func=AF.Square, accum_out=sm[:, 0:1]), reads=["Op"], writes=["gjunk", "gsm"])
            p.op("dve", lambda e: e.tensor_scalar(out=sm[:, 1:2], in0=sm[:, 0:1], scalar1=1.0 / 512.0, scalar2=EPS, op0=ALU.mult, op1=ALU.add), reads=["gsm"], writes=["gsm"])
            p.op("act", lambda e: e.activation(out=sm[:, 2:3], in_=sm[:, 1:2], func=AF.Sqrt), reads=["gsm"], writes=["gsm"])
            p.op("dve", lambda e: e.reciprocal(out=sm[:, 3:4], in_=sm[:, 2:3]), reads=["gsm"], writes=["gsm"])
            p.op("dve", lambda e: e.scalar_tensor_tensor(out=on, in0=Op[:, 0:512], scalar=sm[:, 3:4], in1=gnb, op0=ALU.mult, op1=ALU.mult),
                 reads=["Op", "gsm", gntok], writes=["on"])
            p.op("pool", lambda e: e.tensor_tensor(out=ybf, in0=on, in1=sg[b][:, vs], op=ALU.mult), reads=["on", ("gsg", b)], writes=["gybf"])
            p.mm([(lambda e, j=j: e.transpose(out=tp[:, j * 128:(j + 1) * 128], in_=ybf[:, j * 128:(j + 1) * 128], identity=identb)) for j in range(4)],
                 reads=["gybf", "identb"], writes=["gtp"])
            p.op("act", lambda e: e.activation(out=yT[:, hd * 4:(hd + 1) * 4, cs], in_=tp[:, 0:512].rearrange("p (a c) -> p a c", a=4), func=AF.Copy),
                 reads=["gtp"], writes=[("gyT", t, hd)])
    for k in range(16):
        p.dma(dr["yT"][k * 128:(k + 1) * 128, :], yT[:, k, :], reads=[("gyT", t, k // 4) for t in range(NT)])
    S.close()


def stage_moe_local(cx, L, h_in, g_row, b_row, h_out, NE=32):
    p, nc, dr = cx.p, cx.nc, cx.dram
    S = Scope(p)
    ident = load_const(cx, S, "ident", "ident")
    mps = S.ps("emps", [128, 512], F32)
    nrow = NE * 16
    nch = (nrow + 127) // 128
    b1g = S.sb("b1g", [128, nrow], F32)
    b1l = S.sb("b1l", [128, nrow], F32)
    b1st = S.sb("b1st", [128, 256], F32)
    b1v = dr["moe_b1"][L * NE:(L + 1) * NE, :].rearrange("e (f c) -> (e f) c", c=256)
    for ch in range(nch):
        r = min(128, nrow - ch * 128)
        p.dma(b1st[0:r, :], b1v[ch * 128:ch * 128 + r, :], writes=["b1st"])
        p.mm([lambda e, r=r: e.transpose(out=mps[:, 0:r], in_=b1st[0:r, 0:256:2], identity=ident[0:r, 0:r]),
              lambda e, r=r: e.transpose(out=mps[:, 128:128 + r], in_=b1st[0:r, 1:256:2], identity=ident[0:r, 0:r])], reads=["b1st", "ident"], writes=["emps"])
        p.op("dve", lambda e, r=r, ch=ch: e.tensor_copy(out=b1g[:, ch * 128:ch * 128 + r], in_=mps[:, 0:r]), reads=["emps"], writes=["b1g"])
        p.op("dve", lambda e, r=r, ch=ch: e.tensor_copy(out=b1l[:, ch * 128:ch * 128 + r], in_=mps[:, 128:128 + r]), reads=["emps"], writes=["b1l"])
    hTg = S.sb("hTg", [128, KC, 1024], BF16)
    actT = S.sb("actT", [128, KC, 1024], BF16)
    acc = S.sb("acc", [128, 8, D], F32)
    gts = S.sb("gts", [128, 8, 32], F32)
    wst = [S.sb("wst", [128, KC, 256], F32) for _ in range(2)]
    wbf = [S.sb("wbf", [128, KC, 256], BF16) for _ in range(2)]
    b2t = [S.sb("b2t", [128, 256], F32) for _ in range(2)]
    ga = [S.sb("ga", [128, 512], F32) for _ in range(2)]
    sg = [S.sb("sg", [128, 512], F32)] * 2
    la = [S.sb("la", [128, 512], F32)] * 2
    yb = [S.sb("yb", [128, 256], F32) for _ in range(2)]
    hld = S.sb("hld", [128, D], F32)
    sm = S.sb("sm", [128, 8], F32)
    Gp = [S.ps("Gp", [128, 512], F32) for _ in range(2)]
    Lp = [S.ps("Lp", [128, 512], F32) for _ in range(2)]
    Yp = [S.ps("Yp", [128, 512], F32) for _ in range(2)]
    st_ = {"n": 0, "it": 0}

    def fetch(w2d, r0, c0, deint):
        b = st_["n"] % 2
        st_["n"] += 1
        src = w2d[r0:r0 + D, c0:c0 + 256].rearrange("(k p) n -> p k n", p=128)
        p.dma(wst[b], src, writes=[("wst", b)])
        if deint:
            sv = wst[b].rearrange("p k (i two) -> p k i two", two=2)
            p.op("act", lambda e, b=b, sv=sv: e.activation(out=wbf[b][:, :, 0:128], in_=sv[:, :, :, 0], func=AF.Copy), reads=[("wst", b)], writes=[("wbf", b, 0)])
            p.op("pool", lambda e, b=b, sv=sv: e.tensor_copy(out=wbf[b][:, :, 128:256], in_=sv[:, :, :, 1]), reads=[("wst", b)], writes=[("wbf", b, 1)])
        else:
            p.op("act", lambda e, b=b: e.activation(out=wbf[b][:, 0:8, :], in_=wst[b][:, 0:8, :], func=AF.Copy), reads=[("wst", b)], writes=[("wbf", b, 0)])
            p.op("pool", lambda e, b=b: e.tensor_copy(out=wbf[b][:, 8:16, :], in_=wst[b][:, 8:16, :]), reads=[("wst", b)], writes=[("wbf", b, 1)])
        return b
    hv = h_in.rearrange("(t p) d -> t p d", p=128)
    ov = h_out.rearrange("(t p) d -> t p d", p=128)
    for gi in range(2):
        p.dma(hTg, dr["hTo"].rearrange("(k p) t -> p k t", p=128)[:, :, gi * 1024:(gi + 1) * 1024], writes=["hTg"])
        p.dma(gts, dr["gateo"][gi * 1024:(gi + 1) * 1024, :].rearrange("(t p) e -> p t e", p=128), writes=["gts"])
        for e_ in range(NE):
            r0 = (L * NE + e_) * D
            nb = fetch(dr["moe_w1"], r0, 0, True)
            for fc in range(16):
                b1 = nb
                if fc + 1 < 16:
                    nb = fetch(dr["moe_w1"], r0, (fc + 1) * 256, True)
                else:
                    nb = fetch(dr["moe_w2"], r0, 0, False)
                wt = [("wbf", b1, 0), ("wbf", b1, 1)]
                for tb in range(2):
                    i2 = st_["it"] % 2
                    st_["it"] += 1
                    tsl = slice(tb * 512, (tb + 1) * 512)
                    p.mm([(lambda e, c=c: e.matmul(Gp[i2][:, 0:512], lhsT=wbf[b1][:, c, 0:128], rhs=hTg[:, c, tsl], start=(c == 0), stop=(c == KC - 1)))
                          for c in range(KC)], reads=wt + ["hTg"], writes=[("Gp", i2)])
                    p.mm([(lambda e, c=c: e.matmul(Lp[i2][:, 0:512], lhsT=wbf[b1][:, c, 128:256], rhs=hTg[:, c, tsl], start=(c == 0), stop=(c == KC - 1)))
                          for c in range(KC)], reads=wt + ["hTg"], writes=[("Lp", i2)])
                    col = e_ * 16 + fc
                    p.op("dve", lambda e: e.tensor_scalar(out=ga[i2], in0=Gp[i2][:, 0:512], scalar1=b1g[:, col:col + 1], scalar2=7.0, op0=ALU.add, op1=ALU.min),
                         reads=[("Gp", i2), "b1g"], writes=[("ga", i2)])
                    p.op("act", lambda e: e.activation(out=sg[i2], in_=ga[i2], func=AF.Sigmoid, scale=1.702), reads=[("ga", i2)], writes=["sg1"])
                    p.op("dve", lambda e: e.tensor_scalar(out=la[i2], in0=Lp[i2][:, 0:512], scalar1=b1l[:, col:col + 1], scalar2=7.0, op0=ALU.add, op1=ALU.min),
                         reads=[("Lp", i2), "b1l"], writes=["la1"])
                    p.op("dve", lambda e: e.tensor_scalar(out=la[i2], in0=la[i2], scalar1=-7.0, scalar2=1.0, op0=ALU.max, op1=ALU.add),
                         reads=["la1"], writes=["la1"])
                    p.op("pool", lambda e: e.tensor_tensor(out=ga[i2], in0=ga[i2], in1=sg[i2], op=ALU.mult), reads=[("ga", i2), "sg1"], writes=[("ga", i2)])
                    p.op("pool", lambda e: e.tensor_tensor(out=actT[:, fc, tsl], in0=ga[i2], in1=la[i2], op=ALU.mult), reads=[("ga", i2), "la1"],
                         writes=[("actT", fc, tb)])
            for dp in range(8):
                b2 = nb
                if dp + 1 < 8:
                    nb = fetch(dr["moe_w2"], r0, (dp + 1) * 256, False)
                wt = [("wbf", b2, 0), ("wbf", b2, 1)]
                dsl = slice(dp * 256, (dp + 1) * 256)
                bb = dp % 2
                p.dma(b2t[bb], dr["moe_b2"][L * NE + e_:L * NE + e_ + 1, dsl].partition_broadcast(128), writes=[("b2t", bb)])
                for t in range(8):
                    i2 = st_["it"] % 2
                    st_["it"] += 1
                    p.mm([(lambda e, c=c: e.matmul(Yp[i2][:, 0:256], lhsT=actT[:, c, t * 128:(t + 1) * 128], rhs=wbf[b2][:, c, :], start=(c == 0), stop=(c == KC - 1)))
                          for c in range(KC)], reads=wt + [("actT", c, t // 4) for c in range(KC)], writes=[("Yp", i2)])
                    p.op("dve", lambda e: e.tensor_tensor(out=yb[i2], in0=Yp[i2][:, 0:256], in1=b2t[bb], op=ALU.add), reads=[("Yp", i2), ("b2t", bb)],
                         writes=[("yb", i2)])
                    if e_ == 0:
                        p.op("dve", lambda e: e.tensor_scalar(out=acc[:, t, dsl], in0=yb[i2], scalar1=gts[:, t, e_:e_ + 1], scalar2=None, op0=ALU.mult),
                             reads=[("yb", i2), "gts"], writes=[("acc", t, dp)])
                    else:
                        p.op("dve", lambda e: e.scalar_tensor_tensor(out=acc[:, t, dsl], in0=yb[i2], scalar=gts[:, t, e_:e_ + 1], in1=acc[:, t, dsl],
                                                                    op0=ALU.mult, op1=ALU.add), reads=[("yb", i2), "gts", ("acc", t, dp)], writes=[("acc", t, dp)])
        gbc = wst[0].rearrange("p k c -> p (k c)")[:, 0:D]
        bbc = wst[1].rearrange("p k c -> p (k c)")[:, 0:D]
        gtok, btok = ("wst", 0), ("wst", 1)
        p.dma(gbc, g_row.partition_broadcast(128), writes=[gtok])
        p.dma(bbc, b_row.partition_broadcast(128), writes=[btok])
        for t in range(8):
            at = [("acc", t, dp) for dp in range(8)]
            p.dma(hld, hv[gi * 8 + t], writes=["hld"])
            p.op("dve", lambda e, t=t: e.scalar_tensor_tensor(out=acc[:, t, :], in0=hld, scalar=DN_ALPHA, in1=acc[:, t, :], op0=ALU.mult, op1=ALU.add),
                 reads=["hld"] + at, writes=at)
            ln_tile(cx, acc[:, t, :], ("acc", t, 0), hld, "hld", sm, "sm", gbc, gtok, bbc, btok)
            p.dma(ov[gi * 8 + t], acc[:, t, :], reads=at)
    S.close()


CAP = 256


def stage_moe_sparse(cx, L, h_in, g_row, b_row, h_out, NE=32):
    p, nc, dr = cx.p, cx.nc, cx.dram
    S = Scope(p)
    ident = load_const(cx, S, "ident", "ident")
    identb = load_const(cx, S, "identb", "identb")
    trius = load_const(cx, S, "trius", "trius")
    ones128 = load_const(cx, S, "ones128", "ones128")
    iota = load_const(cx, S, "iota", "iota")
    GA = [S.ps("GA", [128, 512], F32) for _ in range(2)]
    Gp = S.ps("Gp", [128, 512], F32)
    Lp = S.ps("Lp", [128, 512], F32)
    Yp = S.ps("Yp", [128, 512], F32)
    SC = S.ps("SC", [128, 512], F32)
    PT = [S.ps("PT", [128, 1024], BF16) for _ in range(2)]
    nrow = NE * 16
    nch = (nrow + 127) // 128
    b1g = S.sb("b1g", [128, nrow], F32)
    b1l = S.sb("b1l", [128, nrow], F32)
    b1st = S.sb("b1st", [128, 256], F32)
    b1v = dr["moe_b1"][L * NE:(L + 1) * NE, :].rearrange("e (f c) -> (e f) c", c=256)
    for ch in range(nch):
        r = min(128, nrow - ch * 128)
        p.dma(b1st[0:r, :], b1v[ch * 128:ch * 128 + r, :], writes=["b1st"])
        p.mm([lambda e, r=r: e.transpose(out=SC[:, 0:r], in_=b1st[0:r, 0:256:2], identity=ident[0:r, 0:r]),
              lambda e, r=r: e.transpose(out=SC[:, 128:128 + r], in_=b1st[0:r, 1:256:2], identity=ident[0:r, 0:r])], reads=["b1st", "ident"], writes=["SC"])
        p.op("dve", lambda e, r=r, ch=ch: e.tensor_copy(out=b1g[:, ch * 128:ch * 128 + r], in_=SC[:, 0:r]), reads=["SC"], writes=["b1g"])
        p.op("dve", lambda e, r=r, ch=ch: e.tensor_copy(out=b1l[:, ch * 128:ch * 128 + r], in_=SC[:, 128:128 + r]), reads=["SC"], writes=["b1l"])
    hb = S.sb("hb", [128, 8, D], BF16)
    acc = S.sb("acc", [128, 8, D], F32)
    gts = S.sb("gts", [128, 8, 32], F32)
    sel = S.sb("sel", [128, 8, 32], F32)
    rank = S.sb("rank", [128, 8, 32], F32)
    Pm = [S.sb("Pm", [128, 8, CAP], BF16)] * 2
    PG = S.sb("PG", [128, 8, CAP], BF16)
    PGT = S.sb("PGT", [128, 2, 8, 128], BF16)
    xgT = S.sb("xgT", [128, KC, CAP], BF16)
    actT = S.sb("actT", [128, KC, CAP], BF16)
    Yb = S.sb("Yb", [128, 2, D], BF16)
    wst = [S.sb("wst", [128, KC, 256], F32) for _ in range(2)]
    wbf = [S.sb("wbf", [128, KC, 256], BF16) for _ in range(2)]
    b2t = [S.sb("b2t", [128, 256], F32) for _ in range(2)]
    ga = [S.sb("ga", [128, CAP], F32) for _ in range(2)]
    sg = S.sb("sg", [128, CAP], F32)
    la = S.sb("la", [128, CAP], F32)
    hld = S.sb("hld", [128, D], F32)
    sm = S.sb("sm", [128, 8], F32)
    st_ = {"n": 0, "it": 0}

    def fetch(w2d, r0, c0, deint):
        b = st_["n"] % 2
        st_["n"] += 1
        src = w2d[r0:r0 + D, c0:c0 + 256].rearrange("(k p) n -> p k n", p=128)
        p.dma(wst[b], src, writes=[("wst", b)])
        if deint:
            sv = wst[b].rearrange("p k (i two) -> p k i two", two=2)
            p.op("act", lambda e, b=b, sv=sv: e.activation(out=wbf[b][:, :, 0:128], in_=sv[:, :, :, 0], func=AF.Copy), reads=[("wst", b)], writes=[("wbf", b, 0)])
            p.op("pool", lambda e, b=b, sv=sv: e.tensor_copy(out=wbf[b][:, :, 128:256], in_=sv[:, :, :, 1]), reads=[("wst", b)], writes=[("wbf", b, 1)])
        else:
            p.op("act", lambda e, b=b: e.activation(out=wbf[b][:, 0:8, :], in_=wst[b][:, 0:8, :], func=AF.Copy), reads=[("wst", b)], writes=[("wbf", b, 0)])
            p.op("pool", lambda e, b=b: e.tensor_copy(out=wbf[b][:, 8:16, :], in_=wst[b][:, 8:16, :]), reads=[("wst", b)], writes=[("wbf", b, 1)])
        return b
    hv = h_in.rearrange("(t p) d -> t p d", p=128)
    ov = h_out.rearrange("(t p) d -> t p d", p=128)
    for gi in range(2):
        for t in range(8):
            p.dma(hld, hv[gi * 8 + t], writes=["hld"])
            p.op("pool" if t % 2 else "act", (lambda e, t=t: e.tensor_copy(out=hb[:, t, :], in_=hld)) if t % 2 else
                 (lambda e, t=t: e.activation(out=hb[:, t, :], in_=hld, func=AF.Copy)), reads=["hld"], writes=[("hb", t)])
        p.dma(gts, dr["gateo"][gi * 1024:(gi + 1) * 1024, :].rearrange("(t p) e -> p t e", p=128), writes=["gts"])
        p.op("dve", lambda e: e.tensor_scalar(out=sel, in0=gts, scalar1=0.0, scalar2=0.0, op0=ALU.is_gt, op1=ALU.add), reads=["gts"], writes=["sel"])
        for t in range(8):
            fns = [lambda e, t=t: e.matmul(GA[0][:, 0:32], lhsT=trius, rhs=sel[:, t, :], start=True, stop=(t == 0))]
            for t2 in range(t):
                fns.append(lambda e, t2=t2, t=t: e.matmul(GA[0][:, 0:32], lhsT=ones128, rhs=sel[:, t2, :], start=False, stop=(t2 == t - 1)))
            p.mm(fns, reads=["sel", "trius", "ones128"], writes=[("GA", 0)])
            p.op("dve", lambda e, t=t: e.tensor_copy(out=rank[:, t, :], in_=GA[0][:, 0:32]), reads=[("GA", 0)], writes=["rank"])
        hbt = toks("hb", range(8))
        for e_ in range(NE):
            r0 = (L * NE + e_) * D
            nb = fetch(dr["moe_w1"], r0, 0, True)
            pb = 0
            for t in range(8):
                p.op("dve", lambda e, t=t: e.tensor_scalar(out=Pm[pb][:, t, :], in0=iota, scalar1=rank[:, t, e_:e_ + 1], scalar2=sel[:, t, e_:e_ + 1],
                                                        op0=ALU.is_equal, op1=ALU.mult), reads=["iota", "rank", "sel"], writes=[("Pm", pb)])
                p.op("dve", lambda e, t=t: e.tensor_scalar(out=PG[:, t, :], in0=iota, scalar1=rank[:, t, e_:e_ + 1], scalar2=gts[:, t, e_:e_ + 1],
                                                        op0=ALU.is_equal, op1=ALU.mult), reads=["iota", "rank", "gts"], writes=["PG"])
            for st in range(2):
                p.mm([(lambda e, t=t, st=st: e.transpose(out=PT[st][:, t * 128:(t + 1) * 128], in_=PG[:, t, st * 128:(st + 1) * 128], identity=identb))
                      for t in range(8)], reads=["PG", "identb"], writes=[("PT", st)])
                p.op("act", lambda e, st=st: e.activation(out=PGT[:, st, :, :], in_=PT[st].rearrange("p (a c) -> p a c", a=8), func=AF.Copy),
                     reads=[("PT", st)], writes=[("PGT", st)])
            for k2 in range(8):
                gb = k2 % 2
                fns = []
                for j in range(2):
                    kc = k2 * 2 + j
                    for t in range(8):
                        fns.append(lambda e, kc=kc, t=t, j=j: e.matmul(GA[gb][:, j * 256:(j + 1) * 256], lhsT=hb[:, t, kc * 128:(kc + 1) * 128], rhs=Pm[pb][:, t, :],
                                                                       start=(t == 0), stop=(t == 7)))
                p.mm(fns, reads=hbt + [("Pm", pb)], writes=[("GA", gb)])
                p.op("act" if k2 % 2 else "dve",
                     (lambda e, k2=k2, gb=gb: e.activation(out=xgT[:, k2 * 2:k2 * 2 + 2, :], in_=GA[gb].rearrange("p (a c) -> p a c", a=2), func=AF.Copy)) if k2 % 2 else
                     (lambda e, k2=k2, gb=gb: e.tensor_copy(out=xgT[:, k2 * 2:k2 * 2 + 2, :], in_=GA[gb].rearrange("p (a c) -> p a c", a=2))),
                     reads=[("GA", gb)], writes=[("xgT", k2)])
            xt = toks("xgT", range(8))
            for fc in range(16):
                b1 = nb
                if fc + 1 < 16:
                    nb = fetch(dr["moe_w1"], r0, (fc + 1) * 256, True)
                else:
                    nb = fetch(dr["moe_w2"], r0, 0, False)
                wt = [("wbf", b1, 0), ("wbf", b1, 1)]
                i2 = st_["it"] % 2
                st_["it"] += 1
                p.mm([(lambda e, c=c: e.matmul(Gp[:, 0:CAP], lhsT=wbf[b1][:, c, 0:128], rhs=xgT[:, c, :], start=(c == 0), stop=(c == KC - 1)))
                      for c in range(KC)], reads=wt + xt, writes=["Gp"])
                p.mm([(lambda e, c=c: e.matmul(Lp[:, 0:CAP], lhsT=wbf[b1][:, c, 128:256], rhs=xgT[:, c, :], start=(c == 0), stop=(c == KC - 1)))
                      for c in range(KC)], reads=wt + xt, writes=["Lp"])
                col = e_ * 16 + fc
                p.op("dve", lambda e: e.tensor_scalar(out=ga[i2], in0=Gp[:, 0:CAP], scalar1=b1g[:, col:col + 1], scalar2=7.0, op0=ALU.add, op1=ALU.min),
                     reads=["Gp", "b1g"], writes=[("ga", i2)])
                p.op("act", lambda e: e.activation(out=sg, in_=ga[i2], func=AF.Sigmoid, scale=1.702), reads=[("ga", i2)], writes=["sg"])
                p.op("dve", lambda e: e.tensor_scalar(out=la, in0=Lp[:, 0:CAP], scalar1=b1l[:, col:col + 1], scalar2=7.0, op0=ALU.add, op1=ALU.min),
                     reads=["Lp", "b1l"], writes=["la"])
                p.op("dve", lambda e: e.tensor_scalar(out=la, in0=la, scalar1=-7.0, scalar2=1.0, op0=ALU.max, op1=ALU.add), reads=["la"], writes=["la"])
                p.op("pool", lambda e: e.tensor_tensor(out=ga[i2], in0=ga[i2], in1=sg, op=ALU.mult), reads=[("ga", i2), "sg"], writes=[("ga", i2)])
                p.op("pool", lambda e: e.tensor_tensor(out=actT[:, fc, :], in0=ga[i2], in1=la, op=ALU.mult), reads=[("ga", i2), "la"], writes=[("actT", fc)])
            at_ = toks("actT", range(16))
            for dp in range(8):
                b2 = nb
                if dp + 1 < 8:
                    nb = fetch(dr["moe_w2"], r0, (dp + 1) * 256, False)
                wt = [("wbf", b2, 0), ("wbf", b2, 1)]
                dsl = slice(dp * 256, (dp + 1) * 256)
                bb = dp % 2
                p.dma(b2t[bb], dr["moe_b2"][L * NE + e_:L * NE + e_ + 1, dsl].partition_broadcast(128), writes=[("b2t", bb)])
                for st in range(2):
                    p.mm([(lambda e, c=c: e.matmul(Yp[:, 0:256], lhsT=actT[:, c, st * 128:(st + 1) * 128], rhs=wbf[b2][:, c, :], start=(c == 0), stop=(c == KC - 1)))
                          for c in range(KC)], reads=wt + at_, writes=["Yp"])
                    p.op("dve", lambda e, st=st: e.tensor_tensor(out=Yb[:, st, dsl], in0=Yp[:, 0:256], in1=b2t[bb], op=ALU.add), reads=["Yp", ("b2t", bb)],
                         writes=[("Yb", st, dp)])
            yt = [("Yb", st, dp) for st in range(2) for dp in range(8)]
            for t in range(8):
                for dq in range(4):
                    qs = slice(dq * 512, (dq + 1) * 512)
                    p.mm([(lambda e, st=st: e.matmul(SC[:, 0:512], lhsT=PGT[:, st, t, :], rhs=Yb[:, st, qs], start=(st == 0), stop=(st == 1))) for st in range(2)],
                         reads=yt + [("PGT", 0), ("PGT", 1)], writes=["SC"])
                    if e_ == 0:
                        p.op("dve", lambda e: e.tensor_copy(out=acc[:, t, qs], in_=SC[:, 0:512]), reads=["SC"], writes=[("acc", t, dq)])
                    else:
                        p.op("dve", lambda e: e.tensor_tensor(out=acc[:, t, qs], in0=SC[:, 0:512], in1=acc[:, t, qs], op=ALU.add), reads=["SC", ("acc", t, dq)],
                             writes=[("acc", t, dq)])
        gbc = wst[0].rearrange("p k c -> p (k c)")[:, 0:D]
        bbc = wst[1].rearrange("p k c -> p (k c)")[:, 0:D]
        gtok, btok = ("wst", 0), ("wst", 1)
        p.dma(gbc, g_row.partition_broadcast(128), writes=[gtok])
        p.dma(bbc, b_row.partition_broadcast(128), writes=[btok])
        for t in range(8):
            at = [("acc", t, dq) for dq in range(4)]
            p.dma(hld, hv[gi * 8 + t], writes=["hld"])
            p.op("dve", lambda e, t=t: e.scalar_tensor_tensor(out=acc[:, t, :], in0=hld, scalar=DN_ALPHA, in1=acc[:, t, :], op0=ALU.mult, op1=ALU.add),
                 reads=["hld"] + at, writes=at)
            ln_tile(cx, acc[:, t, :], ("acc", t, 0), hld, "hld", sm, "sm", gbc, gtok, bbc, btok)
            p.dma(ov[gi * 8 + t], acc[:, t, :], reads=at)
    S.close()


def stage_moe_sparse2(cx, L, h_in, g_row, b_row, h_out, NE=32):
    p, nc, dr = cx.p, cx.nc, cx.dram
    hv = h_in.rearrange("(t p) d -> t p d", p=128)
    ov = h_out.rearrange("(t p) d -> t p d", p=128)
    XG = dr["XG"].rearrange("(e p) x -> e p x", p=128)
    PGd = dr["PGTd"].rearrange("(e p) x -> e p x", p=128)
    YBd = dr["YBd"].rearrange("(e p) x -> e p x", p=128)
    S = Scope(p)
    identb = load_const(cx, S, "identb", "identb")
    trius = load_const(cx, S, "trius", "trius")
    ones128 = load_const(cx, S, "ones128", "ones128")
    iota = load_const(cx, S, "iota", "iota")
    GA = [S.ps("GA", [128, 512], F32) for _ in range(4)]
    PT = [S.ps("PT", [128, 1024], BF16) for _ in range(2)]
    RK = S.ps("RK", [128, 512], F32)
    hb = S.sb("hb", [128, 8, D], BF16)
    hld = [S.sb("hld", [128, D], F32) for _ in range(2)]
    gts = S.sb("gts", [128, 8, 32], F32)
    sel = S.sb("sel", [128, 8, 32], F32)
    rank = S.sb("rank", [128, 8, 32], F32)
    Pm = [S.sb("Pm", [128, 8, CAP], BF16) for _ in range(2)]
    PG = [S.sb("PG", [128, 8, CAP], BF16) for _ in range(2)]
    PGT = [S.sb("PGT", [128, 2, 8, 128], BF16) for _ in range(2)]
    xgT = [S.sb("xgT", [128, KC, CAP], BF16) for _ in range(2)]
    for gi in range(2):
        for t in range(8):
            hl = hld[t % 2]
            p.dma(hl, hv[gi * 8 + t], writes=[("hld", t % 2)])
            if t % 2:
                p.op("pool", lambda e, t=t, hl=hl: e.tensor_copy(out=hb[:, t, :], in_=hl), reads=[("hld", t % 2)], writes=[("hb", t)])
            else:
                p.op("act", lambda e, t=t, hl=hl: e.activation(out=hb[:, t, :], in_=hl, func=AF.Copy), reads=[("hld", t % 2)], writes=[("hb", t)])
        p.dma(gts, dr["gateo"][gi * 1024:(gi + 1) * 1024, :].rearrange("(t p) e -> p t e", p=128), writes=["gts"])
        p.op("dve", lambda e: e.tensor_scalar(out=sel, in0=gts, scalar1=0.0, scalar2=0.0, op0=ALU.is_gt, op1=ALU.add), reads=["gts"], writes=["sel"])
        for t in range(8):
            fns = [lambda e, t=t: e.matmul(RK[:, 0:32], lhsT=trius, rhs=sel[:, t, :], start=True, stop=(t == 0))]
            for t2 in range(t):
                fns.append(lambda e, t2=t2, t=t: e.matmul(RK[:, 0:32], lhsT=ones128, rhs=sel[:, t2, :], start=False, stop=(t2 == t - 1)))
            p.mm(fns, reads=["sel", "trius", "ones128"], writes=["RK"])
            p.op("dve", lambda e, t=t: e.tensor_copy(out=rank[:, t, :], in_=RK[:, 0:32]), reads=["RK"], writes=["rank"])
        hbt = toks("hb", range(8))
        k4 = 0
        for e_ in range(NE):
            pb = e_ % 2
            for t in range(8):
                p.op("dve", lambda e, t=t: e.tensor_scalar(out=Pm[pb][:, t, :], in0=iota, scalar1=rank[:, t, e_:e_ + 1], scalar2=sel[:, t, e_:e_ + 1],
                                                        op0=ALU.is_equal, op1=ALU.mult), reads=["iota", "rank", "sel"], writes=[("Pm", pb)])
                p.op("dve", lambda e, t=t: e.tensor_scalar(out=PG[pb][:, t, :], in0=iota, scalar1=rank[:, t, e_:e_ + 1], scalar2=gts[:, t, e_:e_ + 1],
                                                        op0=ALU.is_equal, op1=ALU.mult), reads=["iota", "rank", "gts"], writes=[("PG", pb)])
            for st in range(2):
                p.mm([(lambda e, t=t, st=st: e.transpose(out=PT[st][:, t * 128:(t + 1) * 128], in_=PG[pb][:, t, st * 128:(st + 1) * 128], identity=identb))
                      for t in range(8)], reads=[("PG", pb), "identb"], writes=[("PT", st)])
                p.op("act", lambda e, st=st: e.activation(out=PGT[pb][:, st, :, :], in_=PT[st].rearrange("p (a c) -> p a c", a=8), func=AF.Copy),
                     reads=[("PT", st)], writes=[("PGT", pb, st)])
            p.dma(PGd[e_][:, gi * 2048:(gi + 1) * 2048], PGT[pb].rearrange("p a b c -> p (a b c)"), reads=[("PGT", pb, 0), ("PGT", pb, 1)])
            for k2 in range(8):
                gb = k4 % 4
                k4 += 1
                fns = []
                for j in range(2):
                    kc = k2 * 2 + j
                    for t in range(8):
                        fns.append(lambda e, kc=kc, t=t, j=j, gb=gb: e.matmul(GA[gb][:, j * 256:(j + 1) * 256], lhsT=hb[:, t, kc * 128:(kc + 1) * 128], rhs=Pm[pb][:, t, :],
                                                                              start=(t == 0), stop=(t == 7)))
                p.mm(fns, reads=hbt + [("Pm", pb)], writes=[("GA", gb)])
                if k2 % 2:
                    p.op("act", lambda e, k2=k2, gb=gb: e.activation(out=xgT[pb][:, k2 * 2:k2 * 2 + 2, :], in_=GA[gb].rearrange("p (a c) -> p a c", a=2), func=AF.Copy),
                         reads=[("GA", gb)], writes=[("xgT", pb, k2)])
                else:
                    p.op("pool" if False else "dve", lambda e, k2=k2, gb=gb: e.tensor_copy(out=xgT[pb][:, k2 * 2:k2 * 2 + 2, :], in_=GA[gb].rearrange("p (a c) -> p a c", a=2)),
                         reads=[("GA", gb)], writes=[("xgT", pb, k2)])
            p.dma(XG[e_].rearrange("p (k c) -> p k c", c=512)[:, :, gi * 256:(gi + 1) * 256], xgT[pb], reads=[("xgT", pb, k2) for k2 in range(8)], writes=["XG"])
    S.close()
    S = Scope(p)
    ident = load_const(cx, S, "ident", "ident")
    Gp = [S.ps("Gp", [128, 512], F32) for _ in range(2)]
    Lp = [S.ps("Lp", [128, 512], F32) for _ in range(2)]
    Yp = [S.ps("Yp", [128, 512], F32) for _ in range(2)]
    BP = S.ps("BP", [128, 512], F32)
    nrow = NE * 16
    nch = (nrow + 127) // 128
    b1g = S.sb("b1g", [128, nrow], F32)
    b1l = S.sb("b1l", [128, nrow], F32)
    b1st = S.sb("b1st", [128, 256], F32)
    b1v = dr["moe_b1"][L * NE:(L + 1) * NE, :].rearrange("e (f c) -> (e f) c", c=256)
    for ch in range(nch):
        r = min(128, nrow - ch * 128)
        p.dma(b1st[0:r, :], b1v[ch * 128:ch * 128 + r, :], writes=["b1st"])
        p.mm([lambda e, r=r: e.transpose(out=BP[:, 0:r], in_=b1st[0:r, 0:256:2], identity=ident[0:r, 0:r]),
              lambda e, r=r: e.transpose(out=BP[:, 128:128 + r], in_=b1st[0:r, 1:256:2], identity=ident[0:r, 0:r])], reads=["b1st", "ident"], writes=["BP"])
        p.op("dve", lambda e, r=r, ch=ch: e.tensor_copy(out=b1g[:, ch * 128:ch * 128 + r], in_=BP[:, 0:r]), reads=["BP"], writes=["b1g"])
        p.op("dve", lambda e, r=r, ch=ch: e.tensor_copy(out=b1l[:, ch * 128:ch * 128 + r], in_=BP[:, 128:128 + r]), reads=["BP"], writes=["b1l"])
    NB = 4
    wst = [S.sb("wst", [128, 8, 512], F32) for _ in range(NB)]
    wbf = [S.sb("wbf", [128, 8, 512], BF16) for _ in range(NB)]
    xg = [S.sb("xg", [128, KC, 512], BF16) for _ in range(2)]
    actT = S.sb("actT", [128, KC, 512], BF16)
    Yb = [S.sb("Yb", [128, 4, D], BF16) for _ in range(2)]
    b2t = [S.sb("b2t", [128, 512], F32) for _ in range(2)]
    ga = [S.sb("ga", [128, 512], F32) for _ in range(2)]
    sg = [S.sb("sg", [128, 512], F32) for _ in range(2)]
    la = [S.sb("la", [128, 512], F32) for _ in range(2)]
    st_ = {"it": 0}
    plist = []
    for e_ in range(NE):
        r0 = (L * NE + e_) * D
        for fp in range(8):
            plist += [("w1", r0, fp * 512, 0), ("w1", r0, fp * 512, 1)]
        for dq in range(4):
            plist += [("w2", r0, dq * 512, 0), ("w2", r0, dq * 512, 1)]
    fetched = {}

    def fetch(i):
        if i >= len(plist) or i in fetched:
            return
        kind, r0, c0, h = plist[i]
        b = i % NB
        w2d = dr["moe_w1"] if kind == "w1" else dr["moe_w2"]
        src = w2d[r0 + h * 1024:r0 + (h + 1) * 1024, c0:c0 + 512].rearrange("(k p) n -> p k n", p=128)
        p.dma(wst[b], src, writes=[("wst", b)])
        if kind == "w1":
            sv = wst[b].rearrange("p k (f i two) -> p k f i two", f=2, two=2)
            dv = wbf[b].rearrange("p k (f two i) -> p k f two i", f=2, two=2)
            for fl in range(2):
                p.op("act", lambda e, sv=sv, dv=dv, fl=fl: e.activation(out=dv[:, :, fl, 0, :], in_=sv[:, :, fl, :, 0], func=AF.Copy),
                     reads=[("wst", b)], writes=[("wbf", b, 0, fl)])
                p.op("pool", lambda e, sv=sv, dv=dv, fl=fl: e.tensor_copy(out=dv[:, :, fl, 1, :], in_=sv[:, :, fl, :, 1]),
                     reads=[("wst", b)], writes=[("wbf", b, 1, fl)])
        else:
            p.op("act", lambda e, b=b: e.activation(out=wbf[b][:, 0:4, :], in_=wst[b][:, 0:4, :], func=AF.Copy), reads=[("wst", b)], writes=[("wbf", b, 0, 0), ("wbf", b, 0, 1)])
            p.op("pool", lambda e, b=b: e.tensor_copy(out=wbf[b][:, 4:8, :], in_=wst[b][:, 4:8, :]), reads=[("wst", b)], writes=[("wbf", b, 1, 0), ("wbf", b, 1, 1)])
        fetched[i] = b

    def wtoks(b):
        return [("wbf", b, x, y) for x in range(2) for y in range(2)]
    for i in range(NB):
        fetch(i)
    p.dma(xg[0].rearrange("p k c -> p (k c)"), XG[0], reads=["XG"], writes=[("xg", 0)])
    pi = 0
    for e_ in range(NE):
        xb = e_ % 2
        if e_ + 1 < NE:
            p.dma(xg[(e_ + 1) % 2].rearrange("p k c -> p (k c)"), XG[e_ + 1], reads=["XG"], writes=[("xg", (e_ + 1) % 2)])
        for fp in range(8):
            bA, bB = fetched[pi], fetched[pi + 1]
            vA = wbf[bA].rearrange("p k (f two i) -> p k f two i", f=2, two=2)
            vB = wbf[bB].rearrange("p k (f two i) -> p k f two i", f=2, two=2)
            wt = wtoks(bA) + wtoks(bB)
            for fl in range(2):
                fc = fp * 2 + fl
                i2 = st_["it"] % 2
                st_["it"] += 1
                p.mm([(lambda e, c=c, fl=fl: e.matmul(Gp[i2][:, 0:512], lhsT=(vA if c < 8 else vB)[:, c % 8, fl, 0, :], rhs=xg[xb][:, c, :],
                                                      start=(c == 0), stop=(c == KC - 1))) for c in range(KC)], reads=wt + [("xg", xb)], writes=[("Gp", i2)])
                p.mm([(lambda e, c=c, fl=fl: e.matmul(Lp[i2][:, 0:512], lhsT=(vA if c < 8 else vB)[:, c % 8, fl, 1, :], rhs=xg[xb][:, c, :],
                                                      start=(c == 0), stop=(c == KC - 1))) for c in range(KC)], reads=wt + [("xg", xb)], writes=[("Lp", i2)])
                col = e_ * 16 + fc
                p.op("dve", lambda e, col=col: e.tensor_scalar(out=ga[i2], in0=Gp[i2][:, 0:512], scalar1=b1g[:, col:col + 1], scalar2=7.0, op0=ALU.add, op1=ALU.min),
                     reads=[("Gp", i2), "b1g"], writes=[("ga", i2)])
                p.op("act", lambda e: e.activation(out=sg[i2], in_=ga[i2], func=AF.Sigmoid, scale=1.702), reads=[("ga", i2)], writes=[("sg", i2)])
                p.op("dve", lambda e, col=col: e.tensor_scalar(out=la[i2], in0=Lp[i2][:, 0:512], scalar1=b1l[:, col:col + 1], scalar2=7.0, op0=ALU.add, op1=ALU.min),
                     reads=[("Lp", i2), "b1l"], writes=[("la", i2)])
                p.op("dve", lambda e: e.tensor_scalar(out=la[i2], in0=la[i2], scalar1=-7.0, scalar2=1.0, op0=ALU.max, op1=ALU.add), reads=[("la", i2)], writes=[("la", i2)])
                p.op("pool", lambda e: e.tensor_tensor(out=ga[i2], in0=ga[i2], in1=sg[i2], op=ALU.mult), reads=[("ga", i2), ("sg", i2)], writes=[("ga", i2)])
                p.op("pool", lambda e, fc=fc: e.tensor_tensor(out=actT[:, fc, :], in0=ga[i2], in1=la[i2], op=ALU.mult), reads=[("ga", i2), ("la", i2)], writes=[("actT", fc)])
            pi += 2
            fetch(pi + 2)
            fetch(pi + 3)
        at_ = toks("actT", range(16))
        yb_ = Yb[e_ % 2]
        for dq in range(4):
            bA, bB = fetched[pi], fetched[pi + 1]
            wt = wtoks(bA) + wtoks(bB)
            dsl = slice(dq * 512, (dq + 1) * 512)
            bb = dq % 2
            p.dma(b2t[bb], dr["moe_b2"][L * NE + e_:L * NE + e_ + 1, dsl].partition_broadcast(128), writes=[("b2t", bb)])
            for st in range(4):
                i2 = st_["it"] % 2
                st_["it"] += 1
                p.mm([(lambda e, c=c, st=st: e.matmul(Yp[i2][:, 0:512], lhsT=actT[:, c, st * 128:(st + 1) * 128], rhs=wbf[bA if c < 8 else bB][:, c % 8, :],
                                                      start=(c == 0), stop=(c == KC - 1))) for c in range(KC)], reads=wt + at_, writes=[("Yp", i2)])
                p.op("dve", lambda e, st=st: e.tensor_tensor(out=yb_[:, st, dsl], in0=Yp[i2][:, 0:512], in1=b2t[bb], op=ALU.add), reads=[("Yp", i2), ("b2t", bb)],
                     writes=[("Yb", e_ % 2, dq)])
            pi += 2
            fetch(pi + 2)
            fetch(pi + 3)
        p.dma(YBd[e_], yb_.rearrange("p a d -> p (a d)"), reads=[("Yb", e_ % 2, dq) for dq in range(4)], writes=["YBd"])
    S.close()
    S = Scope(p)
    SC = [S.ps("SC", [128, 512], F32) for _ in range(4)]
    acc = S.sb("acc", [128, 8, D], F32)
    gbc, gtok = bcast_row(cx, S, g_row, D, "gbc")
    bbc, btok = bcast_row(cx, S, b_row, D, "bbc")
    pg = [S.sb("pgl", [128, 2, 8, 128], BF16) for _ in range(2)]
    yl = [S.sb("yl", [128, 2, D], BF16) for _ in range(2)]
    hl = S.sb("hlc", [128, D], F32)
    sm = S.sb("sm", [128, 8], F32)
    k4 = 0
    for gi in range(2):
        def ld(e_):
            b = e_ % 2
            p.dma(pg[b].rearrange("p a b c -> p (a b c)"), PGd[e_][:, gi * 2048:(gi + 1) * 2048], writes=[("pgl", b)])
            p.dma(yl[b].rearrange("p a d -> p (a d)"), YBd[e_][:, gi * 4096:(gi + 1) * 4096], writes=[("yl", b)])
        ld(0)
        for e_ in range(NE):
            if e_ + 1 < NE:
                ld(e_ + 1)
            b = e_ % 2
            for t in range(8):
                for dq in range(4):
                    qs = slice(dq * 512, (dq + 1) * 512)
                    sb_ = k4 % 4
                    k4 += 1
                    p.mm([(lambda e, st=st, sb_=sb_: e.matmul(SC[sb_][:, 0:512], lhsT=pg[b][:, st, t, :], rhs=yl[b][:, st, qs], start=(st == 0), stop=(st == 1)))
                          for st in range(2)], reads=[("pgl", b), ("yl", b)], writes=[("SC", sb_)])
                    if e_ == 0:
                        p.op("act", lambda e, sb_=sb_: e.activation(out=acc[:, t, qs], in_=SC[sb_][:, 0:512], func=AF.Copy), reads=[("SC", sb_)], writes=[("acc", t, dq)])
                    else:
                        p.op("dve", lambda e, sb_=sb_: e.tensor_tensor(out=acc[:, t, qs], in0=SC[sb_][:, 0:512], in1=acc[:, t, qs], op=ALU.add), reads=[("SC", sb_), ("acc", t, dq)],
                             writes=[("acc", t, dq)])
        for t in range(8):
            at = [("acc", t, dq) for dq in range(4)]
            p.dma(hl, hv[gi * 8 + t], writes=["hlc"])
            p.op("dve", lambda e, t=t: e.scalar_tensor_tensor(out=acc[:, t, :], in0=hl, scalar=DN_ALPHA, in1=acc[:, t, :], op0=ALU.mult, op1=ALU.add),
                 reads=["hlc"] + at, writes=at)
            ln_tile(cx, acc[:, t, :], ("acc", t, 0), hl, "hlc", sm, "sm", gbc, gtok, bbc, btok)
            p.dma(ov[gi * 8 + t], acc[:, t, :], reads=at)
    S.close()


def moe_layer(cx, L, h_in, h_out):
    dr, p = cx.dram, cx.p
    stage_router(cx, L, h_in)
    p.allreduce(dr["GH"], dr["GH2"], reads=["GH"], writes=["GH2"])
    p.allreduce(dr["GG"], dr["GG2"], reads=["GG"], writes=["GG2"])
    stage_precast(cx, L)
    p.barrier()
    stage_experts(cx, L, dr["GH2"], dr["GG2"])
    p.allreduce(dr["PART"], dr["PART2"], reads=["PART"], writes=["PART2"])
    p.barrier()
    stage_combine_ln(cx, dr["PART2"], h_in, dr["ln2_g"][L:L + 1, :], dr["ln2_b"][L:L + 1, :], h_out)


def full_forward(cx):
    dr = cx.dram
    stage_l0_proj(cx, dr["x"])
    stage_l0_ssd(cx)
    stage_l0_att(cx)
    stage_outproj_ln(cx, dr["yT"], 32, dr["hyb_w_out"], dr["x"], dr["ln1_g"][0:1, :], dr["ln1_b"][0:1, :], dr["h1"])
    moe_layer(cx, 0, dr["h1"], dr["h2"])
    stage_gla_proj(cx, dr["h2"])
    stage_gla_core(cx)
    stage_outproj_ln(cx, dr["yT"][0:2048, :], 16, dr["gla_w_out"], dr["h2"], dr["ln1_g"][1:2, :], dr["ln1_b"][1:2, :], dr["h3"])
    moe_layer(cx, 1, dr["h3"], dr["out"])


CONST_NAMES = list(CONST_SHAPES.keys())


def _run(nc, in_maps):
    res = run_bass_kernel_spmd(nc, in_maps, core_ids=list(range(NCORES)))
    return res.results


def kernel_unfused(**inputs):
    x = np.asarray(inputs["x"], dtype=np.float32)
    f = lambda k: np.ascontiguousarray(np.asarray(inputs[k], dtype=np.float32))
    consts = make_consts()
    ln = {"ln1_g": f("ln1_g"), "ln1_b": f("ln1_b"), "ln2_g": f("ln2_g"), "ln2_b": f("ln2_b")}
    rt = {"moe_w_router": f("moe_w_router").reshape(2 * D, 32), "moe_b_router": f("moe_b_router")}
    hyb = {"hyb_w_in": f("hyb_w_in")[0], "hyb_conv_w": f("hyb_conv_w")[0], "hyb_conv_b": f("hyb_conv_b").reshape(1, 3072),
           "hyb_dt_bias": f("hyb_dt_bias").reshape(1, 32), "hyb_a_log": f("hyb_a_log").reshape(1, 32), "hyb_d": f("hyb_d").reshape(1, 32),
           "hyb_norm": f("hyb_norm").reshape(1, 2048), "hyb_w_out": f("hyb_w_out")[0]}
    gla = {"gla_w_in": f("gla_w_in")[0], "gla_w_gate2": f("gla_w_gate2")[0], "gla_b_gate": f("gla_b_gate").reshape(1, 1024),
           "gla_norm": f("gla_norm").reshape(1, 512), "gla_w_out": f("gla_w_out")[0]}
    w1, b1, w2, b2 = inputs["moe_w1"], inputs["moe_b1"], inputs["moe_w2"], inputs["moe_b2"]
    ohs = []
    for c in range(NCORES):
        oh = np.zeros((128, 8), np.float32)
        oh[:, c] = 1.0
        ohs.append(oh)

    def stA(cx):
        dr = cx.dram
        stage_l0_proj(cx, dr["x"])
        stage_l0_ssd(cx)
        stage_l0_att(cx)
        stage_outproj_ln(cx, dr["yT"], 32, dr["hyb_w_out"], dr["x"], dr["ln1_g"][0:1, :], dr["ln1_b"][0:1, :], dr["h1"])
        stage_router(cx, 0, dr["h1"], masked=False)
    need = ["x"] + list(hyb) + ["ln1_g", "ln1_b", "moe_w_router", "moe_b_router", "oh"]
    ncA, _ = build([stA], dbg=("h1", "hTo", "gateo"), needed_inputs=need)
    maps = [dict(hyb, **consts, **rt, x=np.ascontiguousarray(x[c]), ln1_g=ln["ln1_g"], ln1_b=ln["ln1_b"], oh=ohs[c]) for c in range(NCORES)]
    rA = _run(ncA, maps)
    h1 = [np.asarray(rA[c]["h1"]) for c in range(NCORES)]

    def stB(cx):
        dr = cx.dram
        stage_precast(cx, 0)
        stage_experts(cx, 0, dr["GH2"], dr["GG2"])
    needB = ["moe_w1", "moe_b1", "moe_w2", "moe_b2", "oh"]
    ncB, _ = build([stB], dbg=("PART",), needed_inputs=needB, ext_in=("GH2", "GG2"), moe_layers=1)

    def experts(L, rprev):
        GH = np.concatenate([np.asarray(rprev[c]["hTo"]) for c in range(NCORES)], axis=0)
        GG = np.concatenate([np.asarray(rprev[c]["gateo"]) for c in range(NCORES)], axis=0)
        maps = []
        for c in range(NCORES):
            e0 = 4 * c
            maps.append(dict(consts, GH2=GH, GG2=GG, oh=ohs[c],
                             moe_w1=np.ascontiguousarray(np.asarray(w1[L, e0:e0 + 4], dtype=np.float32)).reshape(4 * D, 2 * D),
                             moe_b1=np.ascontiguousarray(np.asarray(b1[L, e0:e0 + 4], dtype=np.float32)).reshape(4, 2 * D),
                             moe_w2=np.ascontiguousarray(np.asarray(w2[L, e0:e0 + 4], dtype=np.float32)).reshape(4 * D, D),
                             moe_b2=np.ascontiguousarray(np.asarray(b2[L, e0:e0 + 4], dtype=np.float32)).reshape(4, D)))
        rB = _run(ncB, maps)
        return [np.concatenate([np.asarray(rB[j]["PART"])[c * T:(c + 1) * T] for j in range(NCORES)], axis=0) for c in range(NCORES)]
    parts = experts(0, rA)

    def stC(cx):
        dr = cx.dram
        stage_combine_ln(cx, dr["PART2"], dr["h1"], dr["ln2_g"][0:1, :], dr["ln2_b"][0:1, :], dr["h2"], use_oh=False)
        stage_gla_proj(cx, dr["h2"])
        stage_gla_core(cx)
        stage_outproj_ln(cx, dr["yT"][0:2048, :], 16, dr["gla_w_out"], dr["h2"], dr["ln1_g"][1:2, :], dr["ln1_b"][1:2, :], dr["h3"])
        stage_router(cx, 1, dr["h3"], masked=False)
    need = list(gla) + ["ln1_g", "ln1_b", "ln2_g", "ln2_b", "moe_w_router", "moe_b_router", "oh"]
    ncC, _ = build([stC], dbg=("h3", "hTo", "gateo"), needed_inputs=need, ext_in=("PART2", "h1"))
    maps = [dict(gla, **consts, **rt, **ln, oh=ohs[c], PART2=parts[c], h1=h1[c]) for c in range(NCORES)]
    rC = _run(ncC, maps)
    h3 = [np.asarray(rC[c]["h3"]) for c in range(NCORES)]
    parts = experts(1, rC)

    def stE(cx):
        dr = cx.dram
        stage_combine_ln(cx, dr["PART2"], dr["h3"], dr["ln2_g"][1:2, :], dr["ln2_b"][1:2, :], dr["out"], use_oh=False)
    ncE, _ = build([stE], dbg=("out",), needed_inputs=["ln2_g", "ln2_b", "oh"], ext_in=("PART2", "h3"))
    maps = [dict(consts, oh=ohs[c], ln2_g=ln["ln2_g"], ln2_b=ln["ln2_b"], PART2=parts[c], h3=h3[c]) for c in range(NCORES)]
    rE = _run(ncE, maps)
    return np.stack([np.asarray(rE[c]["out"], dtype=np.float32) for c in range(NCORES)], axis=0)


def fused_forward(cx, NE=32, moe=None):
    moe = moe or stage_moe_sparse2
    dr = cx.dram
    stage_l0_proj(cx, dr["x"])
    stage_l0_ssd(cx)
    stage_l0_att(cx)
    stage_outproj_ln(cx, dr["yT"], 32, dr["hyb_w_out"], dr["x"], dr["ln1_g"][0:1, :], dr["ln1_b"][0:1, :], dr["h1"])
    stage_router(cx, 0, dr["h1"], masked=False)
    moe(cx, 0, dr["h1"], dr["ln2_g"][0:1, :], dr["ln2_b"][0:1, :], dr["h2"], NE=NE)
    stage_gla_proj(cx, dr["h2"])
    stage_gla_core(cx)
    stage_outproj_ln(cx, dr["yT"][0:2048, :], 16, dr["gla_w_out"], dr["h2"], dr["ln1_g"][1:2, :], dr["ln1_b"][1:2, :], dr["h3"])
    stage_router(cx, 1, dr["h3"], masked=False)
    moe(cx, 1, dr["h3"], dr["ln2_g"][1:2, :], dr["ln2_b"][1:2, :], dr["out"], NE=NE)


def kernel(**inputs):
    x = np.asarray(inputs["x"], dtype=np.float32)
    f = lambda k: np.ascontiguousarray(np.asarray(inputs[k], dtype=np.float32))
    shared = {
        "hyb_w_in": f("hyb_w_in")[0], "hyb_conv_w": f("hyb_conv_w")[0], "hyb_conv_b": f("hyb_conv_b").reshape(1, 3072),
        "hyb_dt_bias": f("hyb_dt_bias").reshape(1, 32), "hyb_a_log": f("hyb_a_log").reshape(1, 32), "hyb_d": f("hyb_d").reshape(1, 32),
        "hyb_norm": f("hyb_norm").reshape(1, 2048), "hyb_w_out": f("hyb_w_out")[0],
        "gla_w_in": f("gla_w_in")[0], "gla_w_gate2": f("gla_w_gate2")[0], "gla_b_gate": f("gla_b_gate").reshape(1, 1024),
        "gla_norm": f("gla_norm").reshape(1, 512), "gla_w_out": f("gla_w_out")[0],
        "ln1_g": f("ln1_g"), "ln1_b": f("ln1_b"), "ln2_g": f("ln2_g"), "ln2_b": f("ln2_b"),
        "moe_w_router": f("moe_w_router").reshape(2 * D, 32), "moe_b_router": f("moe_b_router"),
        "moe_w1": f("moe_w1").reshape(2 * 32 * D, 2 * D), "moe_b1": f("moe_b1").reshape(2 * 32, 2 * D),
        "moe_w2": f("moe_w2").reshape(2 * 32 * D, D), "moe_b2": f("moe_b2").reshape(2 * 32, D),
        "oh": np.zeros((128, 8), np.float32),
    }
    shared.update(make_consts())
    in_maps = [dict(shared, x=np.ascontiguousarray(x[c])) for c in range(NCORES)]
    nc, cx = build([fused_forward], dbg=("out",), moe_experts=32)
    res = run_bass_kernel_spmd(nc, in_maps, core_ids=list(range(NCORES)))
    return np.stack([np.asarray(res.results[c]["out"], dtype=np.float32) for c in range(NCORES)], axis=0)
```
